# Optimizing a Trainium2 kernel written in Bass

```python
import jax, jax.numpy as jnp
from jax import lax
import numpy as np

D_MODEL = 2048
BATCH = 1
SEQ = 16384
DEPTH = 2
DEC_BATCH = 4
DEC_SEQ = 8192
PAST_LEN = 128

A_WIDTH = D_MODEL // 4
N_HEADS = 8
HEAD_DIM = D_MODEL // 16
N_KV_HEADS = 2
ATTN_WIDTH = N_HEADS * HEAD_DIM
KV_WIDTH = N_KV_HEADS * HEAD_DIM
C_WIDTH = D_MODEL // 4
D_MIX = A_WIDTH + ATTN_WIDTH + C_WIDTH
IN_SPLITS = (A_WIDTH, A_WIDTH, ATTN_WIDTH, KV_WIDTH, KV_WIDTH, C_WIDTH, C_WIDTH, C_WIDTH)
D_IN = sum(IN_SPLITS)
CONV_A_WIDTH = 31
CONV_C_WIDTH = 3
WINDOW = 128
BLOCK = 128
N_GROUPS = 4
EXPERTS_PER_GROUP = 4
N_EXPERTS = N_GROUPS * EXPERTS_PER_GROUP
TOP_K = 2
D_EXPERT = D_MODEL // 4
EPS = 1e-6
NEG_INF = -1e30

kernel_name = 'hymba_style_hybrid_encoder_hier_moe'


def rmsnorm(x, g):
    xf = x.astype(jnp.float32)
    y = xf * lax.rsqrt(jnp.mean(xf * xf, axis=-1, keepdims=True) + EPS) * g.astype(jnp.float32)
    return y.astype(x.dtype)


def layernorm(x, g, b):
    xf = x.astype(jnp.float32)
    mu = jnp.mean(xf, axis=-1, keepdims=True)
    xc = xf - mu
    var = jnp.mean(xc * xc, axis=-1, keepdims=True)
    y = xc * lax.rsqrt(var + EPS) * g.astype(jnp.float32) + b.astype(jnp.float32)
    return y.astype(x.dtype)


def depthwise_conv(x, w):
    k = w.shape[0]
    pad = (k - 1) // 2
    return lax.conv_general_dilated(x, w[:, None, :].astype(x.dtype), window_strides=(1,),
                                    padding=[(pad, pad)], dimension_numbers=('NWC', 'WIO', 'NWC'),
                                    feature_group_count=x.shape[-1])


def alibi_slopes():
    return 2.0 ** (-8.0 * jnp.arange(1, N_HEADS + 1, dtype=jnp.float32) / N_HEADS)


def windowed_gqa(q, k, v, sink):
    bn, s, _, hd = q.shape
    nb = s // BLOCK
    grp = N_HEADS // N_KV_HEADS
    qb = q.reshape(bn, nb, BLOCK, N_KV_HEADS, grp, hd)
    kp = jnp.pad(k, ((0, 0), (BLOCK, BLOCK), (0, 0), (0, 0)))
    vp = jnp.pad(v, ((0, 0), (BLOCK, BLOCK), (0, 0), (0, 0)))

    def bands(t):
        return jnp.concatenate([t[:, i * BLOCK:i * BLOCK + s].reshape(bn, nb, BLOCK, N_KV_HEADS, hd)
                                for i in range(3)], axis=2)

    kb, vb = bands(kp), bands(vp)
    scores = jnp.einsum('bnqkgd,bnskd->bnkgqs', qb, kb,
                        preferred_element_type=jnp.float32) * (hd ** -0.5)
    q_idx = jnp.arange(BLOCK)[:, None]
    s_idx = jnp.arange(3 * BLOCK)[None, :]
    dist = jnp.abs(s_idx - BLOCK - q_idx)
    kpos = jnp.arange(nb)[:, None] * BLOCK + jnp.arange(3 * BLOCK)[None, :] - BLOCK
    valid = (dist <= WINDOW)[None] & ((kpos >= 0) & (kpos < s))[:, None, :]
    slopes = alibi_slopes().reshape(N_KV_HEADS, grp)
    bias = -slopes[:, :, None, None] * dist.astype(jnp.float32)[None, None]
    logits = jnp.where(valid[None, :, None, None], scores + bias, NEG_INF)
    sink_l = sink.astype(jnp.float32).reshape(N_KV_HEADS, grp)[:, :, None, None]
    m = jnp.maximum(jnp.max(logits, axis=-1, keepdims=True), sink_l)
    p = jnp.exp(logits - m)
    denom = jnp.sum(p, axis=-1, keepdims=True) + jnp.exp(sink_l - m)
    o = jnp.einsum('bnkgqs,bnskd->bnqkgd', p / denom, vb.astype(jnp.float32))
    return o.reshape(bn, s, N_HEADS * hd).astype(q.dtype)


def token_mixer(h, w_in, w_out, conv_a_w, conv_a_b, ln_a_g, ln_a_b, attn_sink, conv_c_w):
    bn, s, _ = h.shape
    z = h @ w_in
    a_val, a_gate, q, k, v, c_x, c_b, c_c = jnp.split(z, np.cumsum(IN_SPLITS)[:-1].tolist(), axis=-1)
    a = a_val * jax.nn.sigmoid(a_gate)
    a = depthwise_conv(a, conv_a_w) + conv_a_b
    a = jax.nn.silu(layernorm(a, ln_a_g, ln_a_b))
    o_b = windowed_gqa(q.reshape(bn, s, N_HEADS, HEAD_DIM),
                       k.reshape(bn, s, N_KV_HEADS, HEAD_DIM),
                       v.reshape(bn, s, N_KV_HEADS, HEAD_DIM), attn_sink)
    o_c = c_b * depthwise_conv(c_c * c_x, conv_c_w)
    return jnp.concatenate([a, o_b, o_c], axis=-1) @ w_out


def hier_moe(h, w_rg, b_rg, w_re, b_re, w_gate, w_up, w_down):
    bn, s, d = h.shape
    t = h.reshape(bn * s, d)
    lg = (t @ w_rg).astype(jnp.float32) + b_rg.astype(jnp.float32)
    pg = jax.nn.softmax(lg, axis=-1)
    g_star = jnp.argmax(lg, axis=-1)
    gp = jnp.take_along_axis(pg, g_star[:, None], axis=-1)
    le = ((t @ w_re).astype(jnp.float32) + b_re.astype(jnp.float32)).reshape(-1, N_GROUPS, EXPERTS_PER_GROUP)
    sel = jnp.take_along_axis(le, g_star[:, None, None], axis=1)[:, 0]
    topv, topi = lax.top_k(jax.nn.softmax(sel, axis=-1), TOP_K)
    topv = topv / jnp.sum(topv, axis=-1, keepdims=True)
    eidx = g_star[:, None] * EXPERTS_PER_GROUP + topi
    combine = gp * jnp.sum(topv[..., None] * jax.nn.one_hot(eidx, N_EXPERTS, dtype=jnp.float32), axis=1)
    y = jnp.zeros((bn * s, d), jnp.float32)
    for e in range(N_EXPERTS):
        he = jax.nn.silu(t @ w_gate[e]) * (t @ w_up[e])
        y = y + combine[:, e:e + 1] * (he @ w_down[e]).astype(jnp.float32)
    return y.astype(h.dtype).reshape(bn, s, d)


def encoder(x, c, norm_mix_g, norm_ffn_g, w_ada, b_ada, w_in, w_out, conv_a_w, conv_a_b, ln_a_g, ln_a_b,
            attn_sink, conv_c_w, w_router_group, b_router_group, w_router_expert, b_router_expert,
            w_gate, w_up, w_down, final_norm_g):
    for l in range(DEPTH):
        mod = jax.nn.silu(c) @ w_ada[l] + b_ada[l]
        sh_m, sc_m, g_m, sh_f, sc_f, g_f = [m[:, None, :] for m in jnp.split(mod, 6, axis=-1)]
        h = rmsnorm(x, norm_mix_g[l]) * (1 + sc_m) + sh_m
        x = x + g_m * token_mixer(h, w_in[l], w_out[l], conv_a_w[l], conv_a_b[l], ln_a_g[l], ln_a_b[l],
                                  attn_sink[l], conv_c_w[l])
        h = rmsnorm(x, norm_ffn_g[l]) * (1 + sc_f) + sh_f
        x = x + g_f * hier_moe(h, w_router_group[l], b_router_group[l], w_router_expert[l],
                               b_router_expert[l], w_gate[l], w_up[l], w_down[l])
    return rmsnorm(x, final_norm_g)


def setup_inputs(seed: int = 0) -> dict:
    key = jax.random.key(seed)
    ks = jax.random.split(key, 26)

    def nrm(k, shape, scale):
        return jax.random.normal(k, shape, jnp.float32) * scale

    D = D_MODEL
    return {
        'x_prompt': nrm(ks[0], (BATCH, SEQ, D), 1.0),
        'x_sample': nrm(ks[1], (DEC_BATCH, DEC_SEQ, D), 1.0),
        'c_prompt': nrm(ks[2], (BATCH, D), 1.0),
        'c_sample': nrm(ks[3], (DEC_BATCH, D), 1.0),
        'norm_mix_g': 1.0 + nrm(ks[4], (DEPTH, D), 0.02),
        'norm_ffn_g': 1.0 + nrm(ks[5], (DEPTH, D), 0.02),
        'w_ada': nrm(ks[6], (DEPTH, D, 6 * D), 0.5 * D ** -0.5),
        'b_ada': nrm(ks[7], (DEPTH, 6 * D), 0.02),
        'w_in': nrm(ks[8], (DEPTH, D, D_IN), D ** -0.5),
        'w_out': nrm(ks[9], (DEPTH, D_MIX, D), D_MIX ** -0.5),
        'conv_a_w': nrm(ks[10], (DEPTH, CONV_A_WIDTH, A_WIDTH), CONV_A_WIDTH ** -0.5),
        'conv_a_b': nrm(ks[11], (DEPTH, A_WIDTH), 0.02),
        'ln_a_g': 1.0 + nrm(ks[12], (DEPTH, A_WIDTH), 0.02),
        'ln_a_b': nrm(ks[13], (DEPTH, A_WIDTH), 0.02),
        'attn_sink': nrm(ks[14], (DEPTH, N_HEADS), 0.5),
        'conv_c_w': nrm(ks[15], (DEPTH, CONV_C_WIDTH, C_WIDTH), CONV_C_WIDTH ** -0.5),
        'w_router_group': nrm(ks[16], (DEPTH, D, N_GROUPS), D ** -0.5),
        'b_router_group': nrm(ks[17], (DEPTH, N_GROUPS), 0.01),
        'w_router_expert': nrm(ks[18], (DEPTH, D, N_EXPERTS), D ** -0.5),
        'b_router_expert': nrm(ks[19], (DEPTH, N_EXPERTS), 0.01),
        'w_gate': nrm(ks[20], (DEPTH, N_EXPERTS, D, D_EXPERT), D ** -0.5),
        'w_up': nrm(ks[21], (DEPTH, N_EXPERTS, D, D_EXPERT), D ** -0.5),
        'w_down': nrm(ks[22], (DEPTH, N_EXPERTS, D_EXPERT, D), D_EXPERT ** -0.5),
        'final_norm_g': 1.0 + nrm(ks[23], (D,), 0.02),
    }


def reference(x_prompt, x_sample, c_prompt, c_sample, norm_mix_g, norm_ffn_g, w_ada, b_ada, w_in, w_out,
              conv_a_w, conv_a_b, ln_a_g, ln_a_b, attn_sink, conv_c_w, w_router_group, b_router_group,
              w_router_expert, b_router_expert, w_gate, w_up, w_down, final_norm_g):
    y_prompt = encoder(x_prompt, c_prompt, norm_mix_g, norm_ffn_g, w_ada, b_ada, w_in, w_out, conv_a_w,
                       conv_a_b, ln_a_g, ln_a_b, attn_sink, conv_c_w, w_router_group, b_router_group,
                       w_router_expert, b_router_expert, w_gate, w_up, w_down, final_norm_g)
    y_sample = encoder(x_sample, c_sample, norm_mix_g, norm_ffn_g, w_ada, b_ada, w_in, w_out, conv_a_w,
                       conv_a_b, ln_a_g, ln_a_b, attn_sink, conv_c_w, w_router_group, b_router_group,
                       w_router_expert, b_router_expert, w_gate, w_up, w_down, final_norm_g)
    return (y_prompt, y_sample)
```

```python
import numpy as np
import ml_dtypes
from contextlib import ExitStack
import concourse.bass as bass
import concourse.mybir as mybir
from concourse.bass_utils import run_bass_kernel_spmd

F32 = mybir.dt.float32
BF16 = mybir.dt.bfloat16
AF = mybir.ActivationFunctionType
ALU = mybir.AluOpType
AX = mybir.AxisListType

D = 2048
NCORES = 8
SEGS = [(0, 20), (20, 36)]
NBLK = 56
NT = NBLK * 128
NOWN = 6144
EPS = 1e-6
NEG = -1e30
SCALE = 128 ** -0.5


class Eng:
    def __init__(self, e, name, sem):
        self.e, self.name, self.sem, self.n, self.seen = e, name, sem, 0, {}

    def wait_tok(self, tok):
        if tok is None:
            return
        key, sem, cnt = tok
        if self.seen.get(key, 0) >= cnt:
            return
        self.e.wait_ge(sem, cnt)
        self.seen[key] = cnt


class B:
    def __init__(self):
        self.w, self.r = {}, {}

    def read(self, eng):
        for t in self.w.values():
            if t[0] == "PE" and eng.name == "PE":
                continue
            eng.wait_tok(t)

    def write(self, eng):
        for k, t in self.r.items():
            if k != eng.name:
                eng.wait_tok(t)
        for k, t in self.w.items():
            if k != eng.name:
                eng.wait_tok(t)

    def did_read(self, tok):
        k = tok[0]
        if k not in self.r or self.r[k][2] < tok[2]:
            self.r[k] = tok

    def did_write(self, tok):
        self.w = {tok[0]: tok}
        self.r = {}


class K:
    def __init__(self, nc, es):
        self.nc = nc
        E = es.enter_context
        self.PE = Eng(nc.tensor, "PE", E(nc.semaphore("sem_pe")))
        self.ACT = Eng(nc.scalar, "ACT", E(nc.semaphore("sem_act")))
        self.DVE = Eng(nc.vector, "DVE", E(nc.semaphore("sem_dve")))
        self.POOL = Eng(nc.gpsimd, "POOL", E(nc.semaphore("sem_pool")))
        self.SP = Eng(nc.sync, "SP", E(nc.semaphore("sem_sp")))
        self.es = es
        self.dsem = {}
        self.slot_of = {}

    def do(self, eng, fn, R=(), W=(), inc=True):
        for b in R:
            b.read(eng)
        for b in W:
            b.write(eng)
        ins = fn()
        if inc:
            ins.then_inc(eng.sem, 1)
            eng.n += 1
            tok = (eng.name, eng.sem, eng.n)
        else:
            tok = (eng.name, eng.sem, eng.n + 1)
        for b in R:
            b.did_read(tok)
        for b in W:
            b.did_write(tok)
        return tok

    def dma(self, slot, out, in_, R=(), W=(), q=None):
        q = q or self.SP
        if slot not in self.dsem:
            self.dsem[slot] = [self.es.enter_context(self.nc.semaphore("dq_" + slot)), 0]
        s = self.dsem[slot]
        for b in R:
            b.read(q)
        for b in W:
            b.write(q)
        q.e.dma_start(out=out, in_=in_).then_inc(s[0], 16)
        s[1] += 16
        tok = ("dma:" + slot, s[0], s[1])
        for b in R:
            b.did_read(tok)
        for b in W:
            b.did_write(tok)
        return tok

    def all_tokens(self, final=False):
        toks = []
        for e in (self.PE, self.ACT, self.DVE, self.POOL):
            if e.n:
                toks.append((e.name, e.sem, e.n))
        for name, s in self.dsem.items():
            if s[1] and (final or not name.startswith("cv_")):
                toks.append(("dma:" + name, s[0], s[1]))
        return toks

    def barrier(self, final=False):
        toks = self.all_tokens(final)
        for e in (self.PE, self.ACT, self.DVE, self.POOL, self.SP):
            for t in toks:
                if t[0] != e.name:
                    e.wait_tok(t)


def Bs(n):
    return [B() for _ in range(n)]


def build_program():
    nc = bass.Bass("TRN2", target_bir_lowering=False)

    _uid = [0]

    def sbt(name, shape, dt):
        _uid[0] += 1
        return nc.sbuf_tensor("sb%d_%s" % (_uid[0], name), shape, dt)

    def pst(name, shape, dt):
        _uid[0] += 1
        return nc.psum_tensor("ps%d_%s" % (_uid[0], name), shape, dt)

    def din(name, shape, dt=F32):
        return nc.dram_tensor(name, list(shape), dt, kind="ExternalInput").ap()

    def dscr(name, shape, dt):
        return nc.dram_tensor(name, list(shape), dt, kind="Internal").ap()

    x_local = din("x_local", [NT, D])
    flags_d = din("flags", [128, 8])
    cT_d = din("cT", [128, 16, 2])
    ident_d = din("ident", [128, 128], BF16)
    ones_d = din("ones", [128, 128], BF16)
    bias_d = din("biasmat", [128, 8, 384])
    nmg_d = din("norm_mix_g", [2, D])
    nfg_d = din("norm_ffn_g", [2, D])
    wada_d = din("w_ada", [2, D, 6 * D])
    bada_d = din("b_ada", [2, 6 * D])
    win_d = din("w_in", [2, D, 4096])
    wout_d = din("w_out", [2, D, D])
    caw_d = din("caw", [2, 128, 4, 31])
    cab_d = din("cab", [2, 128, 4])
    lng_d = din("lng", [2, 128, 4])
    lnb_d = din("lnb", [2, 128, 4])
    sink_d = din("sink", [2, 1, 8])
    ccw_d = din("ccw", [2, 128, 4, 3])
    wr_d = din("wr", [2, 128, 16, 20])
    br_d = din("br", [2, 1, 20])
    wg_d = din("w_gate", [2, 16, D, 512])
    wu_d = din("w_up", [2, 16, D, 512])
    wd_d = din("w_down", [2, 16, 512, D])
    fng_d = din("final_norm_g", [1, D])
    y_d = nc.dram_tensor("y_local", [NOWN, D], F32, kind="ExternalOutput").ap()

    wb_in = dscr("wb_in", [2, D, 4096], BF16)
    wb_out = dscr("wb_out", [2, D, D], BF16)
    wb_g = dscr("wb_g", [2, 16, D, 512], BF16)
    wb_u = dscr("wb_u", [2, 16, D, 512], BF16)
    wb_d = dscr("wb_d", [2, 16, 512, D], BF16)
    mod_d = dscr("mod_d", [2, 2, 6 * D], F32)
    xa = dscr("xa", [NT, D], F32)
    xb = dscr("xb", [NT, D], F32)
    aT_d = dscr("aT", [4, 128, NT], BF16)
    qT_d = dscr("qT", [8, 128, NT], BF16)
    kT_d = dscr("kT", [2, 128, NT], BF16)
    v_d = dscr("v", [NT, 256], BF16)
    uT_d = dscr("uT", [4, 128, NT], F32)
    cbT_d = dscr("cbT", [4, 128, NT], F32)
    mix_d = dscr("mixT", [16, 128, NT], BF16)

    with ExitStack() as es_all:
        k = K(nc, es_all)
        PE, ACT, DVE, POOL, SP = k.PE, k.ACT, k.DVE, k.POOL, k.SP
        T, A, V, G = nc.tensor, nc.scalar, nc.vector, nc.gpsimd

        conv_tok = {}

        def cast(name, dst, src):
            conv_tok[name] = k.dma("cv_" + name, dst, src, q=POOL)

        for l in range(2):
            for i in range(8):
                cast("in%d" % l, wb_in[l, i * 256:(i + 1) * 256, :], win_d[l, i * 256:(i + 1) * 256, :])
            for i in range(4):
                cast("out%d" % l, wb_out[l, i * 512:(i + 1) * 512, :], wout_d[l, i * 512:(i + 1) * 512, :])
            for e in range(16):
                cast("g%d" % l, wb_g[l, e], wg_d[l, e])
                cast("u%d" % l, wb_u[l, e], wu_d[l, e])
                cast("d%d" % l, wb_d[l, e], wd_d[l, e])

        with ExitStack() as es:
            E = es.enter_context
            siluT = E(sbt("siluT", [128, 16, 2], F32))
            wt = [E(sbt("wadat%d" % i, [128, 16, 512], F32)) for i in range(2)]
            modrow = E(sbt("modrow", [2, 6 * D], F32))
            badar = E(sbt("badar", [2, 6 * D], F32))
            grow = E(sbt("grow", [2, 2, D], F32))
            ps = [E(pst("pps%d" % i, [128, 512], F32)) for i in range(2)]
            b_silu, b_mod, b_bada, b_grow = B(), B(), B(), B()
            b_wt, b_ps = Bs(2), Bs(2)
            k.dma("siluT", siluT[:], cT_d, W=[b_silu])
            k.do(ACT, lambda: A.activation(out=siluT[:], in_=siluT[:], func=AF.Silu), R=[b_silu], W=[b_silu])
            it = 0
            for l in range(2):
                k.dma("bada", badar[:], bada_d[l:l + 1, :].partition_broadcast(2), W=[b_bada])
                k.dma("grow", grow[:, 0, :], nmg_d[l:l + 1, :].partition_broadcast(2), W=[b_grow])
                k.dma("grow", grow[:, 1, :], nfg_d[l:l + 1, :].partition_broadcast(2), W=[b_grow])
                for j in range(24):
                    s = it % 2
                    it += 1
                    k.dma("wadat%d" % s, wt[s][:],
                          wada_d[l, :, j * 512:(j + 1) * 512].rearrange("(c p) n -> p c n", p=128), W=[b_wt[s]])
                    for c in range(16):
                        k.do(PE, lambda: T.matmul(ps[s][0:2, :], lhsT=siluT[:, c, :], rhs=wt[s][:, c, :],
                                                  start=(c == 0), stop=(c == 15)),
                             R=[b_silu, b_wt[s]], W=[b_ps[s]], inc=(c == 15))
                    k.do(DVE, lambda: V.tensor_tensor(out=modrow[:, j * 512:(j + 1) * 512], in0=ps[s][0:2, :],
                                                      in1=badar[:, j * 512:(j + 1) * 512], op=ALU.add),
                         R=[b_ps[s], b_bada], W=[b_mod])
                for (off, gi) in ((D, 0), (4 * D, 1)):
                    k.do(DVE, lambda: V.scalar_tensor_tensor(out=modrow[:, off:off + D], in0=modrow[:, off:off + D],
                                                             scalar=1.0, in1=grow[:, gi, :], op0=ALU.add,
                                                             op1=ALU.mult), R=[b_mod, b_grow], W=[b_mod])
                k.dma("modout", mod_d[l], modrow[:], R=[b_mod], q=POOL)
            k.barrier()

        def load_const(E, name, shape, dt, src, b):
            t = E(sbt(name, shape, dt))
            k.dma("c_" + name, t[:], src, W=[b])
            return t

        def seg_of_block(gb):
            return 0 if gb < 20 else 1

        def tiles_full(l):
            res = []
            for s, (bs, nbk) in enumerate(SEGS):
                lo, hi = (1, nbk - 1) if l == 0 else (2, nbk - 2)
                b = lo
                while b < hi:
                    nb = min(4, hi - b)
                    res.append((s, bs + b, nb))
                    b += nb
            return res

        def load_fm_mod(E, name, l, off, b):
            t = E(sbt(name, [128, 2, 16], F32))
            with nc.allow_non_contiguous_dma(reason="tiny feature-major load of a modulation vector"):
                for s in range(2):
                    k.dma("c_" + name, t[:, s, :], mod_d[l, s, off:off + D].rearrange("(c p) -> p c", p=128), W=[b])
            return t

        def norm_transpose(x_src, gb0, nb, seg, xs, b_xs, xsi, junk, b_junk, ss, b_ss, xh, b_xh, pT, b_pT,
                           hT, b_hT, afm, bfm, b_const, ident):
            n = nb * 128
            k.do(DVE, lambda: V.memset(ss[:], 0.0), W=[b_ss])
            for b in range(nb):
                si = xsi[0] % len(xs)
                xsi[0] += 1
                k.dma("xs%d" % si, xs[si][:], x_src[(gb0 + b) * 128:(gb0 + b + 1) * 128, :], W=[b_xs[si]])
                k.do(ACT, lambda: A.activation(out=junk[:], in_=xs[si][:], func=AF.Square,
                                               accum_out=ss[:, b:b + 1]), R=[b_xs[si], b_ss], W=[b_junk, b_ss])
                k.do(ACT, lambda: A.activation(out=ss[:, 4 + b:5 + b], in_=ss[:, b:b + 1], func=AF.Sqrt,
                                               scale=1.0 / D, bias=EPS), R=[b_ss], W=[b_ss])
                k.do(DVE, lambda: V.reciprocal(out=ss[:, 8 + b:9 + b], in_=ss[:, 4 + b:5 + b]), R=[b_ss], W=[b_ss])
                k.do(DVE, lambda: V.tensor_scalar(out=xh[:, b, :], in0=xs[si][:], scalar1=ss[:, 8 + b:9 + b],
                                                  scalar2=None, op0=ALU.mult), R=[b_xs[si], b_ss], W=[b_xh])
            for c in range(16):
                pi = c % 2
                for b in range(nb):
                    k.do(PE, lambda: T.transpose(out=pT[pi][:, b * 128:(b + 1) * 128],
                                                 in_=xh[:, b, c * 128:(c + 1) * 128], identity=ident[:]),
                         R=[b_xh, b_const], W=[b_pT[pi]], inc=(b == nb - 1))
                k.do(ACT, lambda: A.activation(out=hT[:, c, 0:n], in_=pT[pi][:, 0:n], func=AF.Identity,
                                               scale=afm[:, seg, c:c + 1], bias=bfm[:, seg, c:c + 1]),
                     R=[b_pT[pi], b_const], W=[b_hT])

        for l in range(2):
            x_src1 = x_local if l == 0 else xb
            x_mid = xa
            x_dst = xb

            with ExitStack() as es:
                E = es.enter_context
                b_const = B()
                ident = load_const(E, "ident", [128, 128], BF16, ident_d, b_const)
                flg = load_const(E, "flg", [128, 8], F32, flags_d, b_const)
                afm = load_fm_mod(E, "afm", l, D, b_const)
                bfm = load_fm_mod(E, "bfm", l, 0, b_const)
                xs = [E(sbt("xs%d" % i, [128, D], F32)) for i in range(2)]
                junk = E(sbt("junk", [128, D], BF16))
                ss = E(sbt("ss", [128, 12], F32))
                xh = [E(sbt("xh%d" % i, [128, 4, D], BF16)) for i in range(2)]
                hT = [E(sbt("hT%d" % i, [128, 16, 512], BF16)) for i in range(2)]
                wt = [E(sbt("wt%d" % i, [128, 16, 512], BF16)) for i in range(3)]
                sig = E(sbt("sig", [128, 4, 512], F32))
                o16 = [E(sbt("o16_%d" % i, [128, 4, 512], BF16)) for i in range(3)]
                o32 = [E(sbt("o32_%d" % i, [128, 4, 512], F32)) for i in range(2)]
                vo = E(sbt("vo", [128, 4, 256], BF16))
                pT = [E(pst("pT%d" % i, [128, 1024], BF16)) for i in range(2)]
                pz = [E(pst("pz%d" % i, [128, 512], F32)) for i in range(4)]
                b_xs, b_xh, b_hT, b_wt, b_pT, b_pz = Bs(2), Bs(2), Bs(2), Bs(3), Bs(2), Bs(4)
                b_junk, b_ss, b_sig, b_vo = B(), B(), B(), B()
                b_o16, b_o32 = Bs(3), Bs(2)
                xsi = [0]
                wi = 0
                pzi = 0
                o16i = 0
                o32i = 0
                first_w = True
                for t in range(14):
                    seg = 0 if t < 5 else 1
                    sl = t % 2
                    tok0 = t * 512
                    halo = None
                    if t in (0, 5):
                        halo = (0, 256, 0 if t == 0 else 2)
                    if t in (4, 13):
                        halo = (256, 512, 1 if t == 4 else 3)
                    norm_transpose(x_src1, t * 4, 4, seg, xs, b_xs, xsi, junk, b_junk, ss, b_ss, xh[sl], b_xh[sl],
                                   pT, b_pT, hT[sl], b_hT[sl], afm, bfm, b_const, ident)

                    def mask_halo(buf, bb, ngrp):
                        if halo is None:
                            return
                        lo, hi, fi = halo
                        k.do(POOL, lambda: G.tensor_scalar(out=buf[:, 0:ngrp, lo:hi], in0=buf[:, 0:ngrp, lo:hi],
                                                           scalar1=flg[:, fi:fi + 1], scalar2=None, op0=ALU.mult),
                             R=[bb, b_const], W=[bb])

                    for g in (1, 0, 2, 3, 4, 7, 5, 6):
                        ws = wi % 3
                        wi += 1
                        if first_w:
                            SP.wait_tok(conv_tok["in%d" % l])
                            first_w = False
                        k.dma("wt%d" % ws, wt[ws][:],
                              wb_in[l, :, g * 512:(g + 1) * 512].rearrange("(c p) n -> p c n", p=128), W=[b_wt[ws]])
                        nfm = 2 if g == 4 else 4
                        if g in (0, 2, 3, 4):
                            oi = o16i % 3
                            o16i += 1
                            ob, bo = o16[oi], b_o16[oi]
                        elif g in (5, 6):
                            oi = o32i % 2
                            o32i += 1
                            ob, bo = o32[oi], b_o32[oi]
                        for j in range(nfm):
                            p = pzi % 4
                            pzi += 1
                            for c in range(16):
                                k.do(PE, lambda: T.matmul(pz[p][:], lhsT=wt[ws][:, c, j * 128:(j + 1) * 128],
                                                          rhs=hT[sl][:, c, :], start=(c == 0), stop=(c == 15)),
                                     R=[b_wt[ws], b_hT[sl]], W=[b_pz[p]], inc=(c == 15))
                            if g == 1:
                                k.do(ACT, lambda: A.activation(out=sig[:, j, :], in_=pz[p][:], func=AF.Sigmoid),
                                     R=[b_pz[p]], W=[b_sig])
                            elif g == 0:
                                k.do(DVE, lambda: V.tensor_tensor(out=ob[:, j, :], in0=pz[p][:], in1=sig[:, j, :],
                                                                  op=ALU.mult), R=[b_pz[p], b_sig], W=[bo])
                            elif g in (2, 3, 4):
                                k.do(ACT, lambda: A.activation(out=ob[:, j, :], in_=pz[p][:], func=AF.Identity),
                                     R=[b_pz[p]], W=[bo])
                            elif g == 7:
                                k.do(ACT, lambda: A.activation(out=sig[:, j, :], in_=pz[p][:], func=AF.Identity),
                                     R=[b_pz[p]], W=[b_sig])
                            elif g == 5:
                                k.do(DVE, lambda: V.tensor_tensor(out=ob[:, j, :], in0=pz[p][:], in1=sig[:, j, :],
                                                                  op=ALU.mult), R=[b_pz[p], b_sig], W=[bo])
                            elif g == 6:
                                k.do(ACT, lambda: A.activation(out=ob[:, j, :], in_=pz[p][:], func=AF.Identity),
                                     R=[b_pz[p]], W=[bo])
                        if g == 4:
                            for b in range(4):
                                p = pzi % 4
                                pzi += 1
                                for c in range(16):
                                    k.do(PE, lambda: T.matmul(pz[p][:, 0:256], lhsT=hT[sl][:, c, b * 128:(b + 1) * 128],
                                                              rhs=wt[ws][:, c, 256:512], start=(c == 0),
                                                              stop=(c == 15)),
                                         R=[b_wt[ws], b_hT[sl]], W=[b_pz[p]], inc=(c == 15))
                                k.do(DVE, lambda: V.tensor_copy(out=vo[:, b, :], in_=pz[p][:, 0:256]),
                                     R=[b_pz[p]], W=[b_vo])
                            if halo is not None:
                                lo, hi, fi = halo
                                k.do(POOL, lambda: G.tensor_scalar(out=vo[:, lo // 128:hi // 128, :],
                                                                   in0=vo[:, lo // 128:hi // 128, :],
                                                                   scalar1=flg[:, fi:fi + 1], scalar2=None,
                                                                   op0=ALU.mult), R=[b_vo, b_const], W=[b_vo])
                            k.dma("st_v", v_d[tok0:tok0 + 512, :].rearrange("(b p) d -> p b d", p=128), vo[:],
                                  R=[b_vo], q=POOL)
                        if g == 0:
                            mask_halo(ob, bo, 4)
                            k.dma("st_a", aT_d[:, :, tok0:tok0 + 512].rearrange("g p t -> p g t"), ob[:], R=[bo], q=POOL)
                        elif g in (2, 3):
                            h0 = (g - 2) * 4
                            k.dma("st_q%d" % g, qT_d[h0:h0 + 4, :, tok0:tok0 + 512].rearrange("g p t -> p g t"), ob[:],
                                  R=[bo], q=POOL)
                        elif g == 4:
                            mask_halo(ob, bo, 2)
                            k.dma("st_k", kT_d[:, :, tok0:tok0 + 512].rearrange("g p t -> p g t"), ob[:, 0:2, :],
                                  R=[bo], q=POOL)
                        elif g == 5:
                            mask_halo(ob, bo, 4)
                            k.dma("st_u", uT_d[:, :, tok0:tok0 + 512].rearrange("g p t -> p g t"), ob[:], R=[bo], q=POOL)
                        elif g == 6:
                            k.dma("st_cb", cbT_d[:, :, tok0:tok0 + 512].rearrange("g p t -> p g t"), ob[:], R=[bo],
                                  q=POOL)
                k.barrier()

            tl = tiles_full(l)
            with ExitStack() as es:
                E = es.enter_context
                b_const = B()
                ident = load_const(E, "ident", [128, 128], BF16, ident_d, b_const)
                onesb = load_const(E, "onesb", [128, 128], BF16, ones_d, b_const)
                flg = load_const(E, "flg", [128, 8], F32, flags_d, b_const)
                bias8 = load_const(E, "bias8", [128, 8, 384], F32, bias_d, b_const)
                caw = load_const(E, "caw", [128, 4, 31], F32, caw_d[l], b_const)
                cab = load_const(E, "cab", [128, 4], F32, cab_d[l], b_const)
                lng = load_const(E, "lng", [128, 4], F32, lng_d[l], b_const)
                lnb = load_const(E, "lnb", [128, 4], F32, lnb_d[l], b_const)
                ccw = load_const(E, "ccw", [128, 4, 3], F32, ccw_d[l], b_const)
                sinkb = load_const(E, "sinkb", [128, 8], F32, sink_d[l].partition_broadcast(128), b_const)
                Dg = E(sbt("Dg", [128, 31, 4, 128], BF16))
                b_Dg = B()
                for kk in range(31):
                    for g in range(4):
                        k.do(DVE, lambda: V.tensor_scalar(out=Dg[:, kk, g, :], in0=ident[:],
                                                          scalar1=caw[:, g, kk:kk + 1], scalar2=None, op0=ALU.mult),
                             R=[b_const], W=[b_Dg])
                a_sb = [E(sbt("a_sb%d" % i, [128, 4, 542], BF16)) for i in range(2)]
                q_sb = [E(sbt("q_sb%d" % i, [128, 8, 512], BF16)) for i in range(2)]
                k_sb = [E(sbt("k_sb%d" % i, [128, 2, 768], BF16)) for i in range(2)]
                v_sb = [E(sbt("v_sb%d" % i, [128, 6, 256], BF16)) for i in range(2)]
                u_sb = [E(sbt("u_sb%d" % i, [128, 4, 514], F32)) for i in range(2)]
                cb_sb = [E(sbt("cb_sb%d" % i, [128, 4, 512], F32)) for i in range(2)]
                mixT = [E(sbt("mixT%d" % i, [128, 16, 512], BF16)) for i in range(2)]
                cv = E(sbt("cv", [128, 4, 512], F32))
                cvb = E(sbt("cvb", [128, 4, 512], BF16))
                cv2 = E(sbt("cv2", [128, 4, 512], BF16))
                mean_sb = E(sbt("mean_sb", [128, 512], F32))
                var_sb = E(sbt("var_sb", [128, 512], F32))
                rstd_sb = E(sbt("rstd_sb", [128, 512], F32))
                t1 = [E(sbt("t1_%d" % i, [128, 512], F32)) for i in range(2)]
                s_sb = [E(sbt("s_sb%d" % i, [128, 384], F32)) for i in range(2)]
                p_sb = [E(sbt("p_sb%d" % i, [128, 384], F32)) for i in range(2)]
                pn_sb = [E(sbt("pn_sb%d" % i, [128, 384], BF16)) for i in range(2)]
                pT_sb = [E(sbt("pT_sb%d" % i, [128, 384], BF16)) for i in range(2)]
                st = [E(sbt("st%d" % i, [128, 8], F32)) for i in range(2)]
                acc = [E(sbt("acc%d" % i, [128, 512], F32)) for i in range(2)]
                acc2 = E(sbt("acc2", [128, 512], F32))
                b_acc2 = B()
                pc = [E(pst("pc%d" % i, [128, 512], F32)) for i in range(2)]
                pmean = E(pst("pmean", [128, 512], F32))
                pex2 = E(pst("pex2", [128, 512], F32))
                s_ps = [E(pst("s_ps%d" % i, [128, 512], F32)) for i in range(2)]
                pT_ps = E(pst("pT_ps", [128, 1024], BF16))
                o_ps = E(pst("o_ps", [128, 512], F32))
                b_a, b_q, b_k, b_v, b_u, b_cb, b_mix = Bs(2), Bs(2), Bs(2), Bs(2), Bs(2), Bs(2), Bs(2)
                b_cv, b_cvb, b_cv2, b_mean, b_var, b_rstd = B(), B(), B(), B(), B(), B()
                b_t1, b_s, b_p, b_pn, b_pTs, b_st, b_acc = Bs(2), Bs(2), Bs(2), Bs(2), Bs(2), Bs(2), Bs(2)
                b_pc, b_sps = Bs(2), Bs(2)
                b_pmean, b_pex2, b_pTp, b_ops = B(), B(), B(), B()
                hi_ = 0
                gi_ = 0
                for ti, (seg, gb0, nb) in enumerate(tl):
                    sl = ti % 2
                    n = nb * 128
                    t0 = gb0 * 128
                    bs, nbk = SEGS[seg]
                    k.dma("a_sb%d" % sl, a_sb[sl][:, :, 0:n + 30],
                          aT_d[:, :, t0 - 15:t0 + n + 15].rearrange("g p t -> p g t"), W=[b_a[sl]])
                    k.dma("q_sb%d" % sl, q_sb[sl][:, :, 0:n], qT_d[:, :, t0:t0 + n].rearrange("g p t -> p g t"),
                          W=[b_q[sl]])
                    k.dma("k_sb%d" % sl, k_sb[sl][:, :, 0:n + 256],
                          kT_d[:, :, t0 - 128:t0 + n + 128].rearrange("g p t -> p g t"), W=[b_k[sl]])
                    k.dma("v_sb%d" % sl, v_sb[sl][:, 0:nb + 2, :],
                          v_d[t0 - 128:t0 + n + 128, :].rearrange("(b p) d -> p b d", p=128), W=[b_v[sl]])
                    k.dma("u_sb%d" % sl, u_sb[sl][:, :, 0:n + 2],
                          uT_d[:, :, t0 - 1:t0 + n + 1].rearrange("g p t -> p g t"), W=[b_u[sl]])
                    k.dma("cb_sb%d" % sl, cb_sb[sl][:, :, 0:n], cbT_d[:, :, t0:t0 + n].rearrange("g p t -> p g t"),
                          W=[b_cb[sl]])
                    mx = mixT[sl]
                    bm = b_mix[sl]
                    for g in range(4):
                        pi = g % 2
                        for kk in range(31):
                            k.do(PE, lambda: T.matmul(pc[pi][:, 0:n], lhsT=Dg[:, kk, g, :],
                                                      rhs=a_sb[sl][:, g, kk:kk + n], start=(kk == 0), stop=(kk == 30)),
                                 R=[b_Dg, b_a[sl]], W=[b_pc[pi]], inc=(kk == 30))
                        k.do(ACT, lambda: A.activation(out=cv[:, g, 0:n], in_=pc[pi][:, 0:n], func=AF.Identity,
                                                       bias=cab[:, g:g + 1]), R=[b_pc[pi], b_const], W=[b_cv])
                        k.do(ACT, lambda: A.activation(out=cvb[:, g, 0:n], in_=pc[pi][:, 0:n], func=AF.Identity,
                                                       bias=cab[:, g:g + 1]), R=[b_pc[pi], b_const], W=[b_cvb])
                        k.do(ACT, lambda: A.activation(out=cv2[:, g, 0:n], in_=pc[pi][:, 0:n], func=AF.Square,
                                                       bias=cab[:, g:g + 1]), R=[b_pc[pi], b_const], W=[b_cv2])
                    for g in range(4):
                        k.do(PE, lambda: T.matmul(pmean[:, 0:n], lhsT=onesb[:], rhs=cvb[:, g, 0:n], start=(g == 0),
                                                  stop=(g == 3)), R=[b_const, b_cvb], W=[b_pmean], inc=(g == 3))
                    for g in range(4):
                        k.do(PE, lambda: T.matmul(pex2[:, 0:n], lhsT=onesb[:], rhs=cv2[:, g, 0:n], start=(g == 0),
                                                  stop=(g == 3)), R=[b_const, b_cv2], W=[b_pex2], inc=(g == 3))
                    k.do(DVE, lambda: V.tensor_copy(out=mean_sb[:, 0:n], in_=pmean[:, 0:n]), R=[b_pmean], W=[b_mean])
                    k.do(DVE, lambda: V.tensor_tensor(out=var_sb[:, 0:n], in0=mean_sb[:, 0:n], in1=mean_sb[:, 0:n],
                                                      op=ALU.mult), R=[b_mean], W=[b_var])
                    k.do(DVE, lambda: V.tensor_tensor(out=var_sb[:, 0:n], in0=pex2[:, 0:n], in1=var_sb[:, 0:n],
                                                      op=ALU.subtract), R=[b_pex2, b_var], W=[b_var])
                    k.do(DVE, lambda: V.tensor_scalar(out=var_sb[:, 0:n], in0=var_sb[:, 0:n], scalar1=0.0,
                                                      scalar2=None, op0=ALU.max), R=[b_var], W=[b_var])
                    k.do(ACT, lambda: A.activation(out=var_sb[:, 0:n], in_=var_sb[:, 0:n], func=AF.Sqrt, bias=EPS),
                         R=[b_var], W=[b_var])
                    k.do(DVE, lambda: V.reciprocal(out=rstd_sb[:, 0:n], in_=var_sb[:, 0:n]), R=[b_var], W=[b_rstd])
                    for g in range(4):
                        ti_ = gi_ % 2
                        gi_ += 1
                        k.do(DVE, lambda: V.tensor_tensor(out=t1[ti_][:, 0:n], in0=cv[:, g, 0:n], in1=mean_sb[:, 0:n],
                                                          op=ALU.subtract), R=[b_cv, b_mean], W=[b_t1[ti_]])
                        k.do(DVE, lambda: V.tensor_tensor(out=t1[ti_][:, 0:n], in0=t1[ti_][:, 0:n],
                                                          in1=rstd_sb[:, 0:n], op=ALU.mult),
                             R=[b_t1[ti_], b_rstd], W=[b_t1[ti_]])
                        k.do(ACT, lambda: A.activation(out=mx[:, g, 0:n], in_=t1[ti_][:, 0:n], func=AF.Silu,
                                                       scale=lng[:, g:g + 1], bias=lnb[:, g:g + 1]),
                             R=[b_t1[ti_], b_const], W=[bm])
                    for g in range(4):
                        ai = g % 2
                        k.do(POOL, lambda: G.tensor_scalar(out=acc[ai][:, 0:n], in0=u_sb[sl][:, g, 0:n],
                                                           scalar1=ccw[:, g, 0:1], scalar2=None, op0=ALU.mult),
                             R=[b_u[sl], b_const], W=[b_acc[ai]])
                        for kk in (1, 2):
                            k.do(POOL, lambda: G.tensor_scalar(out=acc2[:, 0:n], in0=u_sb[sl][:, g, kk:kk + n],
                                                               scalar1=ccw[:, g, kk:kk + 1], scalar2=None, op0=ALU.mult),
                                 R=[b_u[sl], b_const], W=[b_acc2])
                            k.do(POOL, lambda: G.tensor_tensor(out=acc[ai][:, 0:n], in0=acc[ai][:, 0:n],
                                                               in1=acc2[:, 0:n], op=ALU.add),
                                 R=[b_acc2, b_acc[ai]], W=[b_acc[ai]])
                        k.do(POOL, lambda: G.tensor_tensor(out=mx[:, 12 + g, 0:n], in0=acc[ai][:, 0:n],
                                                           in1=cb_sb[sl][:, g, 0:n], op=ALU.mult),
                             R=[b_acc[ai], b_cb[sl]], W=[bm])
                    for b in range(nb):
                        gb = gb0 + b
                        left_edge = (gb == bs + 2)
                        right_edge = (gb == bs + nbk - 3)
                        for h in range(8):
                            i2 = hi_ % 2
                            hi_ += 1
                            kv = h // 4
                            sp, bsp = s_ps[i2], b_sps[i2]
                            ssb, bss = s_sb[i2], b_s[i2]
                            stt, bst = st[i2], b_st[i2]
                            k.do(PE, lambda: T.matmul(sp[:, 0:384], lhsT=q_sb[sl][:, h, b * 128:(b + 1) * 128],
                                                      rhs=k_sb[sl][:, kv, b * 128:b * 128 + 384], start=True, stop=True),
                                 R=[b_q[sl], b_k[sl]], W=[bsp])
                            k.do(DVE, lambda: V.scalar_tensor_tensor(out=ssb[:], in0=sp[:, 0:384], scalar=SCALE,
                                                                     in1=bias8[:, h, :], op0=ALU.mult, op1=ALU.add),
                                 R=[bsp, b_const], W=[bss])
                            if left_edge:
                                fi = 4 + (0 if seg == 0 else 2)
                                k.do(DVE, lambda: V.tensor_scalar(out=ssb[:, 0:128], in0=ssb[:, 0:128],
                                                                  scalar1=flg[:, fi:fi + 1], scalar2=None, op0=ALU.add),
                                     R=[bss, b_const], W=[bss])
                            if right_edge:
                                fi = 4 + (1 if seg == 0 else 3)
                                k.do(DVE, lambda: V.tensor_scalar(out=ssb[:, 256:384], in0=ssb[:, 256:384],
                                                                  scalar1=flg[:, fi:fi + 1], scalar2=None, op0=ALU.add),
                                     R=[bss, b_const], W=[bss])
                            k.do(DVE, lambda: V.reduce_max(out=stt[:, 0:1], in_=ssb[:], axis=AX.X), R=[bss], W=[bst])
                            k.do(DVE, lambda: V.tensor_scalar(out=stt[:, 1:2], in0=stt[:, 0:1], scalar1=sinkb[:, h:h + 1],
                                                              scalar2=-1.0, op0=ALU.max, op1=ALU.mult),
                                 R=[bst, b_const], W=[bst])
                            k.do(DVE, lambda: V.memset(stt[:, 2:3], 0.0), R=[bst], W=[bst])
                            k.do(ACT, lambda: A.activation(out=p_sb[i2][:], in_=ssb[:], func=AF.Exp, bias=stt[:, 1:2],
                                                           accum_out=stt[:, 2:3]), R=[bss, bst], W=[b_p[i2], bst])
                            k.do(ACT, lambda: A.activation(out=stt[:, 3:4], in_=sinkb[:, h:h + 1], func=AF.Exp,
                                                           bias=stt[:, 1:2]), R=[bst, b_const], W=[bst])
                            k.do(DVE, lambda: V.tensor_tensor(out=stt[:, 4:5], in0=stt[:, 2:3], in1=stt[:, 3:4],
                                                              op=ALU.add), R=[bst], W=[bst])
                            k.do(DVE, lambda: V.reciprocal(out=stt[:, 5:6], in_=stt[:, 4:5]), R=[bst], W=[bst])
                            k.do(DVE, lambda: V.tensor_scalar(out=pn_sb[i2][:], in0=p_sb[i2][:], scalar1=stt[:, 5:6],
                                                              scalar2=None, op0=ALU.mult),
                                 R=[b_p[i2], bst], W=[b_pn[i2]])
                            for j in range(3):
                                k.do(PE, lambda: T.transpose(out=pT_ps[:, j * 128:(j + 1) * 128],
                                                             in_=pn_sb[i2][:, j * 128:(j + 1) * 128], identity=ident[:]),
                                     R=[b_pn[i2], b_const], W=[b_pTp], inc=(j == 2))
                            k.do(ACT, lambda: A.activation(out=pT_sb[i2][:],
                                                           in_=pT_ps[:, 0:384], func=AF.Identity),
                                 R=[b_pTp], W=[b_pTs[i2]])
                            for j in range(3):
                                k.do(PE, lambda: T.matmul(o_ps[:, 0:128], lhsT=v_sb[sl][:, b + j, kv * 128:(kv + 1) * 128],
                                                          rhs=pT_sb[i2][:, j * 128:(j + 1) * 128], start=(j == 0), stop=(j == 2)),
                                     R=[b_v[sl], b_pTs[i2]], W=[b_ops], inc=(j == 2))
                            k.do(DVE, lambda: V.tensor_copy(out=mx[:, 4 + h, b * 128:(b + 1) * 128], in_=o_ps[:, 0:128]),
                                 R=[b_ops], W=[bm])
                    k.dma("st_mix%d" % sl, mix_d[:, :, t0:t0 + n].rearrange("g p t -> p g t"), mx[:, :, 0:n], R=[bm],
                          q=POOL)
                k.barrier()

            with ExitStack() as es:
                E = es.enter_context
                b_const = B()
                wo = E(sbt("wo", [128, 16, D], BF16))
                SP.wait_tok(conv_tok["out%d" % l])
                k.dma("c_wo", wo[:], wb_out[l].rearrange("(c p) n -> p c n", p=128), W=[b_const])
                gm = E(sbt("gm", [128, 2, D], F32))
                for s in range(2):
                    k.dma("c_gm", gm[:, s, :], mod_d[l, s:s + 1, 2 * D:3 * D].partition_broadcast(128), W=[b_const])
                mixs = [E(sbt("mixs%d" % i, [128, 16, 512], BF16)) for i in range(2)]
                xs = [E(sbt("xs%d" % i, [128, D], F32)) for i in range(2)]
                tmp = [E(sbt("tmp%d" % i, [128, 512], F32)) for i in range(2)]
                xo = [E(sbt("xo%d" % i, [128, D], F32)) for i in range(2)]
                po = [E(pst("po%d" % i, [128, 512], F32)) for i in range(8)]
                b_mixs, b_xs, b_tmp, b_xo, b_po = Bs(2), Bs(2), Bs(2), Bs(2), Bs(8)
                bi_ = 0
                tmi = 0
                for ti, (seg, gb0, nb) in enumerate(tl):
                    sl = ti % 2
                    n = nb * 128
                    t0 = gb0 * 128
                    k.dma("mixs%d" % sl, mixs[sl][:, :, 0:n], mix_d[:, :, t0:t0 + n].rearrange("g p t -> p g t"),
                          W=[b_mixs[sl]])
                    for b in range(nb):
                        i2 = bi_ % 2
                        bi_ += 1
                        r0 = (gb0 + b) * 128
                        k.dma("xs%d" % i2, xs[i2][:], x_src1[r0:r0 + 128, :], W=[b_xs[i2]])
                        for c in range(16):
                            for ft in range(4):
                                p = i2 * 4 + ft
                                k.do(PE, lambda: T.matmul(po[p][:], lhsT=mixs[sl][:, c, b * 128:(b + 1) * 128],
                                                          rhs=wo[:, c, ft * 512:(ft + 1) * 512], start=(c == 0),
                                                          stop=(c == 15)),
                                     R=[b_mixs[sl], b_const], W=[b_po[p]], inc=(c == 15 and ft == 3))
                        for ft in range(4):
                            p = i2 * 4 + ft
                            tm = tmi % 2
                            tmi += 1
                            k.do(DVE, lambda: V.tensor_tensor(out=tmp[tm][:], in0=po[p][:],
                                                              in1=gm[:, seg, ft * 512:(ft + 1) * 512], op=ALU.mult),
                                 R=[b_po[p], b_const], W=[b_tmp[tm]])
                            k.do(POOL, lambda: G.tensor_tensor(out=xo[i2][:, ft * 512:(ft + 1) * 512], in0=tmp[tm][:],
                                                               in1=xs[i2][:, ft * 512:(ft + 1) * 512], op=ALU.add),
                                 R=[b_tmp[tm], b_xs[i2]], W=[b_xo[i2]])
                        k.dma("st_xo%d" % i2, x_mid[r0:r0 + 128, :], xo[i2][:], R=[b_xo[i2]], q=POOL)
                k.barrier()

            with ExitStack() as es:
                E = es.enter_context
                b_const = B()
                ident = load_const(E, "ident", [128, 128], BF16, ident_d, b_const)
                afm = load_fm_mod(E, "afm", l, 4 * D, b_const)
                bfm = load_fm_mod(E, "bfm", l, 3 * D, b_const)
                gf = E(sbt("gf", [128, 2, D], F32))
                for s in range(2):
                    k.dma("c_gf", gf[:, s, :], mod_d[l, s:s + 1, 5 * D:6 * D].partition_broadcast(128), W=[b_const])
                wr32 = load_const(E, "wr32", [128, 16, 20], F32, wr_d[l], b_const)
                wrb = E(sbt("wrb", [128, 16, 20], BF16))
                k.do(DVE, lambda: V.tensor_copy(out=wrb[:], in_=wr32[:]), R=[b_const], W=[b_const])
                brb = load_const(E, "brb", [128, 20], F32, br_d[l].partition_broadcast(128), b_const)
                if l == 1:
                    fg = load_const(E, "fg", [128, D], F32, fng_d.partition_broadcast(128), b_const)
                xs = [E(sbt("xs%d" % i, [128, D], F32)) for i in range(2)]
                junk = E(sbt("junk", [128, D], BF16))
                ss = E(sbt("ss", [128, 12], F32))
                xh = E(sbt("xh", [128, 4, D], BF16))
                hT = E(sbt("hT", [128, 16, 512], BF16))
                wq = [E(sbt("wq%d" % i, [128, 8192], BF16)) for i in range(4)]
                yacc = E(sbt("yacc", [128, 4, D], F32))
                he = [E(sbt("he%d" % i, [128, 4, 512], BF16)) for i in range(2)]
                sg = [E(sbt("sg%d" % i, [128, 512], F32)) for i in range(2)]
                rt = E(sbt("rt", [128, 64], F32))
                comb = E(sbt("comb", [128, 4, 16], F32))
                xo = E(sbt("xo", [128, D], F32))
                pT = [E(pst("pT%d" % i, [128, 1024], BF16)) for i in range(2)]
                pg = [E(pst("pg%d" % i, [128, 512], F32)) for i in range(2)]
                pu = [E(pst("pu%d" % i, [128, 512], F32)) for i in range(2)]
                pd = [E(pst("pd%d" % i, [128, 512], F32)) for i in range(2)]
                b_xs, b_pT, b_pg, b_pu, b_pd, b_wq, b_he, b_sg = Bs(2), Bs(2), Bs(2), Bs(2), Bs(2), Bs(4), Bs(2), Bs(2)
                b_junk, b_ss, b_xh, b_hT, b_yacc, b_rt, b_comb, b_xo = B(), B(), B(), B(), B(), B(), B(), B()
                xsi = [0]
                wqi = 0
                ji_ = 0
                di_ = 0
                first_w = True
                for ti, (seg, gb0, nb) in enumerate(tl):
                    n = nb * 128
                    norm_transpose(x_mid, gb0, nb, seg, xs, b_xs, xsi, junk, b_junk, ss, b_ss, xh, b_xh, pT, b_pT,
                                   hT, b_hT, afm, bfm, b_const, ident)
                    for b in range(nb):
                        rp = pg[0]
                        for c in range(16):
                            k.do(PE, lambda: T.matmul(rp[:, 0:20], lhsT=hT[:, c, b * 128:(b + 1) * 128], rhs=wrb[:, c, :],
                                                      start=(c == 0), stop=(c == 15)),
                                 R=[b_hT, b_const], W=[b_pg[0]], inc=(c == 15))

                        def dv(fn, extraR=(), extraW=()):
                            k.do(DVE, fn, R=[b_rt] + list(extraR), W=[b_rt] + list(extraW))
                        lg = rt[:, 0:20]
                        dv(lambda: V.tensor_tensor(out=lg, in0=rp[:, 0:20], in1=brb[:], op=ALU.add),
                           extraR=[b_pg[0], b_const])
                        dv(lambda: V.reduce_max(out=rt[:, 20:21], in_=rt[:, 0:4], axis=AX.X))
                        dv(lambda: V.tensor_scalar(out=rt[:, 24:28], in0=rt[:, 0:4], scalar1=rt[:, 20:21], scalar2=None,
                                                   op0=ALU.is_equal))
                        dv(lambda: V.tensor_scalar(out=rt[:, 21:22], in0=rt[:, 20:21], scalar1=-1.0, scalar2=None,
                                                   op0=ALU.mult))
                        dv(lambda: V.memset(rt[:, 22:23], 0.0))
                        k.do(ACT, lambda: A.activation(out=rt[:, 28:32], in_=rt[:, 0:4], func=AF.Exp, bias=rt[:, 21:22],
                                                       accum_out=rt[:, 22:23]), R=[b_rt], W=[b_rt])
                        dv(lambda: V.reciprocal(out=rt[:, 23:24], in_=rt[:, 22:23]))
                        dv(lambda: V.tensor_scalar(out=rt[:, 32:36], in0=rt[:, 4:8], scalar1=rt[:, 24:25], scalar2=None,
                                                   op0=ALU.mult))
                        for g in range(1, 4):
                            dv(lambda: V.scalar_tensor_tensor(out=rt[:, 32:36], in0=rt[:, 4 + 4 * g:8 + 4 * g],
                                                              scalar=rt[:, 24 + g:25 + g], in1=rt[:, 32:36],
                                                              op0=ALU.mult, op1=ALU.add))
                        dv(lambda: V.reduce_max(out=rt[:, 36:37], in_=rt[:, 32:36], axis=AX.X))
                        dv(lambda: V.tensor_scalar(out=rt[:, 40:44], in0=rt[:, 32:36], scalar1=rt[:, 36:37], scalar2=None,
                                                   op0=ALU.is_equal))
                        dv(lambda: V.scalar_tensor_tensor(out=rt[:, 44:48], in0=rt[:, 40:44], scalar=NEG,
                                                          in1=rt[:, 32:36], op0=ALU.mult, op1=ALU.add))
                        dv(lambda: V.reduce_max(out=rt[:, 37:38], in_=rt[:, 44:48], axis=AX.X))
                        dv(lambda: V.tensor_scalar(out=rt[:, 48:52], in0=rt[:, 44:48], scalar1=rt[:, 37:38], scalar2=None,
                                                   op0=ALU.is_equal))
                        dv(lambda: V.tensor_tensor(out=rt[:, 40:44], in0=rt[:, 40:44], in1=rt[:, 48:52], op=ALU.add))
                        dv(lambda: V.tensor_scalar(out=rt[:, 38:39], in0=rt[:, 36:37], scalar1=-1.0, scalar2=None,
                                                   op0=ALU.mult))
                        k.do(ACT, lambda: A.activation(out=rt[:, 52:56], in_=rt[:, 32:36], func=AF.Exp, bias=rt[:, 38:39]),
                             R=[b_rt], W=[b_rt])
                        dv(lambda: V.tensor_tensor(out=rt[:, 52:56], in0=rt[:, 52:56], in1=rt[:, 40:44], op=ALU.mult))
                        dv(lambda: V.reduce_sum(out=rt[:, 39:40], in_=rt[:, 52:56], axis=AX.X))
                        dv(lambda: V.reciprocal(out=rt[:, 56:57], in_=rt[:, 39:40]))
                        dv(lambda: V.tensor_tensor(out=rt[:, 56:57], in0=rt[:, 56:57], in1=rt[:, 23:24], op=ALU.mult))
                        dv(lambda: V.tensor_scalar(out=rt[:, 52:56], in0=rt[:, 52:56], scalar1=rt[:, 56:57], scalar2=None,
                                                   op0=ALU.mult))
                        for g in range(4):
                            dv(lambda: V.tensor_scalar(out=comb[:, b, 4 * g:4 * g + 4], in0=rt[:, 52:56],
                                                       scalar1=rt[:, 24 + g:25 + g], scalar2=None, op0=ALU.mult),
                               extraW=[b_comb])
                    for e in range(16):
                        wslots = []
                        for (nm, src) in (("g", wb_g[l, e]), ("u", wb_u[l, e]), ("d", wb_d[l, e])):
                            ws = wqi % 4
                            wqi += 1
                            if first_w:
                                SP.wait_tok(conv_tok["g%d" % l])
                                SP.wait_tok(conv_tok["u%d" % l])
                                SP.wait_tok(conv_tok["d%d" % l])
                                first_w = False
                            if nm == "d":
                                dst = wq[ws][:].rearrange("p (c n) -> p c n", c=4)
                            else:
                                dst = wq[ws][:].rearrange("p (c n) -> p c n", c=16)
                            k.dma("wq%d" % ws, dst, src.rearrange("(c p) n -> p c n", p=128), W=[b_wq[ws]])
                            wslots.append(ws)
                        wgs, wus, wds = wslots
                        wgv = wq[wgs][:].rearrange("p (c n) -> p c n", c=16)
                        wuv = wq[wus][:].rearrange("p (c n) -> p c n", c=16)
                        wdv = wq[wds][:].rearrange("p (c n) -> p c n", c=4)
                        hi2 = e % 2
                        for j in range(4):
                            pi = ji_ % 2
                            ji_ += 1
                            for c in range(16):
                                k.do(PE, lambda: T.matmul(pg[pi][:, 0:n], lhsT=wgv[:, c, j * 128:(j + 1) * 128],
                                                          rhs=hT[:, c, 0:n], start=(c == 0), stop=(c == 15)),
                                     R=[b_wq[wgs], b_hT], W=[b_pg[pi]], inc=(c == 15))
                            for c in range(16):
                                k.do(PE, lambda: T.matmul(pu[pi][:, 0:n], lhsT=wuv[:, c, j * 128:(j + 1) * 128],
                                                          rhs=hT[:, c, 0:n], start=(c == 0), stop=(c == 15)),
                                     R=[b_wq[wus], b_hT], W=[b_pu[pi]], inc=(c == 15))
                            k.do(ACT, lambda: A.activation(out=sg[pi][:, 0:n], in_=pg[pi][:, 0:n], func=AF.Silu),
                                 R=[b_pg[pi]], W=[b_sg[pi]])
                            k.do(DVE, lambda: V.tensor_tensor(out=he[hi2][:, j, 0:n], in0=sg[pi][:, 0:n], in1=pu[pi][:, 0:n],
                                                              op=ALU.mult), R=[b_sg[pi], b_pu[pi]], W=[b_he[hi2]])
                        for b in range(nb):
                            for ft in range(4):
                                pi = di_ % 2
                                di_ += 1
                                for j in range(4):
                                    k.do(PE, lambda: T.matmul(pd[pi][:], lhsT=he[hi2][:, j, b * 128:(b + 1) * 128],
                                                              rhs=wdv[:, j, ft * 512:(ft + 1) * 512], start=(j == 0),
                                                              stop=(j == 3)),
                                         R=[b_he[hi2], b_wq[wds]], W=[b_pd[pi]], inc=(j == 3))
                                ya = yacc[:, b, ft * 512:(ft + 1) * 512]
                                if e == 0:
                                    k.do(DVE, lambda: V.tensor_scalar(out=ya, in0=pd[pi][:], scalar1=comb[:, b, e:e + 1],
                                                                      scalar2=None, op0=ALU.mult),
                                         R=[b_pd[pi], b_comb], W=[b_yacc])
                                else:
                                    k.do(DVE, lambda: V.scalar_tensor_tensor(out=ya, in0=pd[pi][:],
                                                                             scalar=comb[:, b, e:e + 1], in1=ya,
                                                                             op0=ALU.mult, op1=ALU.add),
                                         R=[b_pd[pi], b_comb, b_yacc], W=[b_yacc])
                    for b in range(nb):
                        si = xsi[0] % 2
                        xsi[0] += 1
                        r0 = (gb0 + b) * 128
                        k.dma("xs%d" % si, xs[si][:], x_mid[r0:r0 + 128, :], W=[b_xs[si]])
                        k.do(POOL, lambda: G.tensor_tensor(out=xo[:], in0=yacc[:, b, :], in1=gf[:, seg, :], op=ALU.mult),
                             R=[b_yacc, b_const], W=[b_xo])
                        k.do(POOL, lambda: G.tensor_tensor(out=xo[:], in0=xo[:], in1=xs[si][:], op=ALU.add),
                             R=[b_xo, b_xs[si]], W=[b_xo])
                        if l == 0:
                            k.dma("st_xo", x_dst[r0:r0 + 128, :], xo[:], R=[b_xo], q=POOL)
                        else:
                            k.do(DVE, lambda: V.memset(ss[:, 0:1], 0.0), W=[b_ss])
                            k.do(ACT, lambda: A.activation(out=junk[:], in_=xo[:], func=AF.Square, accum_out=ss[:, 0:1]),
                                 R=[b_xo, b_ss], W=[b_junk, b_ss])
                            k.do(ACT, lambda: A.activation(out=ss[:, 4:5], in_=ss[:, 0:1], func=AF.Sqrt, scale=1.0 / D,
                                                           bias=EPS), R=[b_ss], W=[b_ss])
                            k.do(DVE, lambda: V.reciprocal(out=ss[:, 8:9], in_=ss[:, 4:5]), R=[b_ss], W=[b_ss])
                            k.do(DVE, lambda: V.scalar_tensor_tensor(out=xo[:], in0=xo[:], scalar=ss[:, 8:9], in1=fg[:],
                                                                     op0=ALU.mult, op1=ALU.mult),
                                 R=[b_xo, b_ss, b_const], W=[b_xo])
                            gb = gb0 + b
                            orow = (gb - 2) * 128 if seg == 0 else 2048 + (gb - 22) * 128
                            k.dma("st_y", y_d[orow:orow + 128, :], xo[:], R=[b_xo], q=POOL)
                k.barrier(final=(l == 1))
    return nc


_NC_CACHE = {}


def _alibi_bias():
    slopes = 2.0 ** (-8.0 * np.arange(1, 9, dtype=np.float64) / 8.0)
    q = np.arange(128)[:, None]
    s = np.arange(384)[None, :]
    dist = np.abs(s - 128 - q)
    out = np.empty((128, 8, 384), np.float32)
    for h in range(8):
        out[:, h, :] = np.where(dist <= 128, -slopes[h] * dist, NEG)
    return out


def kernel(x_prompt, x_sample, c_prompt, c_sample, norm_mix_g, norm_ffn_g, w_ada, b_ada, w_in, w_out,
           conv_a_w, conv_a_b, ln_a_g, ln_a_b, attn_sink, conv_c_w, w_router_group, b_router_group,
           w_router_expert, b_router_expert, w_gate, w_up, w_down, final_norm_g):
    f = lambda a: np.ascontiguousarray(np.asarray(a, dtype=np.float32))
    x_prompt, x_sample, c_prompt, c_sample = f(x_prompt), f(x_sample), f(c_prompt), f(c_sample)
    if "nc" not in _NC_CACHE:
        _NC_CACHE["nc"] = build_program()
    nc = _NC_CACHE["nc"]

    caw = f(np.transpose(f(conv_a_w).reshape(2, 31, 4, 128), (0, 3, 2, 1)))
    cab = f(np.transpose(f(conv_a_b).reshape(2, 4, 128), (0, 2, 1)))
    lng = f(np.transpose(f(ln_a_g).reshape(2, 4, 128), (0, 2, 1)))
    lnb = f(np.transpose(f(ln_a_b).reshape(2, 4, 128), (0, 2, 1)))
    ccw = f(np.transpose(f(conv_c_w).reshape(2, 3, 4, 128), (0, 3, 2, 1)))
    wr = np.concatenate([f(w_router_group), f(w_router_expert)], axis=-1)
    wr = f(np.transpose(wr.reshape(2, 16, 128, 20), (0, 2, 1, 3)))
    br = f(np.concatenate([f(b_router_group), f(b_router_expert)], axis=-1).reshape(2, 1, 20))
    shared = dict(
        ident=np.eye(128, dtype=np.float32).astype(ml_dtypes.bfloat16),
        ones=np.full((128, 128), 1.0 / 512, np.float32).astype(ml_dtypes.bfloat16),
        biasmat=_alibi_bias(),
        norm_mix_g=f(norm_mix_g), norm_ffn_g=f(norm_ffn_g), w_ada=f(w_ada), b_ada=f(b_ada), w_in=f(w_in),
        w_out=f(w_out), caw=caw, cab=cab, lng=lng, lnb=lnb, sink=f(attn_sink).reshape(2, 1, 8), ccw=ccw, wr=wr, br=br,
        w_gate=f(w_gate), w_up=f(w_up), w_down=f(w_down), final_norm_g=f(final_norm_g).reshape(1, D),
    )
    in_maps = []
    for c in range(NCORES):
        sb, half = c // 2, c % 2
        xl = np.zeros((NT, D), np.float32)
        lo, hi = 2048 * c - 256, 2048 * c + 2048 + 256
        a, b = max(lo, 0), min(hi, 16384)
        xl[a - lo:b - lo] = x_prompt[0, a:b]
        lo, hi = 4096 * half - 256, 4096 * half + 4096 + 256
        a, b = max(lo, 0), min(hi, 8192)
        xl[2560 + a - lo:2560 + b - lo] = x_sample[sb, a:b]
        fl = np.array([c > 0, c < 7, half == 1, half == 0], np.float32)
        flags = np.zeros((128, 8), np.float32)
        flags[:, 0:4] = fl[None]
        flags[:, 4:8] = np.where(fl > 0, 0.0, NEG)[None]
        cc = np.stack([c_prompt[0], c_sample[sb]], axis=-1)
        cT = f(np.transpose(cc.reshape(16, 128, 2), (1, 0, 2)))
        in_maps.append(dict(shared, x_local=xl, flags=flags, cT=cT))

    res = run_bass_kernel_spmd(nc, in_maps, core_ids=list(range(NCORES)))
    y_prompt = np.empty((1, 16384, D), np.float32)
    y_sample = np.empty((4, 8192, D), np.float32)
    for c in range(NCORES):
        y = res.results[c]["y_local"]
        sb, half = c // 2, c % 2
        y_prompt[0, 2048 * c:2048 * c + 2048] = y[0:2048]
        y_sample[sb, 4096 * half:4096 * half + 4096] = y[2048:6144]
    return (y_prompt, y_sample)
```

```python
import numpy as np
import ml_dtypes
from contextlib import ExitStack
import concourse.bass as bass
import concourse.mybir as mybir
from concourse.bass_utils import run_bass_kernel_spmd

F32 = mybir.dt.float32
BF16 = mybir.dt.bfloat16
I32 = mybir.dt.int32
AF = mybir.ActivationFunctionType
ALU = mybir.AluOpType
AX = mybir.AxisListType

D = 2048
NCORES = 8
SEGS = [(0, 20), (20, 36)]
NBLK = 56
NT = NBLK * 128
NOWN = 6144
EPS = 1e-6
NEG = -1e30
SCALE = 128 ** -0.5


class Eng:
    def __init__(self, e, name, sem):
        self.e, self.name, self.sem, self.n, self.seen = e, name, sem, 0, {}

    def wait_tok(self, tok):
        if tok is None:
            return
        _, sem, cnt = tok
        key = id(sem)
        if self.seen.get(key, 0) >= cnt:
            return
        self.e.wait_ge(sem, cnt)
        self.seen[key] = cnt


class B:
    def __init__(self):
        self.w, self.r = {}, {}

    def read(self, eng):
        for t in self.w.values():
            if t[0] == "PE" and eng.name == "PE":
                continue
            eng.wait_tok(t)

    def write(self, eng):
        for k, t in self.r.items():
            if k != eng.name:
                eng.wait_tok(t)
        for k, t in self.w.items():
            if k != eng.name:
                eng.wait_tok(t)

    def did_read(self, tok):
        k = tok[0]
        if k not in self.r or self.r[k][2] < tok[2]:
            self.r[k] = tok

    def did_write(self, tok):
        self.w = {tok[0]: tok}
        self.r = {}


class K:
    def __init__(self, nc, es):
        self.nc = nc
        E = es.enter_context
        self.PE = Eng(nc.tensor, "PE", E(nc.semaphore("sem_pe")))
        self.ACT = Eng(nc.scalar, "ACT", E(nc.semaphore("sem_act")))
        self.DVE = Eng(nc.vector, "DVE", E(nc.semaphore("sem_dve")))
        self.POOL = Eng(nc.gpsimd, "POOL", E(nc.semaphore("sem_pool")))
        self.SP = Eng(nc.sync, "SP", E(nc.semaphore("sem_sp")))
        self.es = es
        self.dsem = {}
        self.slot_of = {}
        self.sem_pool = []
        self.nsem = 0

    def do(self, eng, fn, R=(), W=(), inc=True):
        for b in R:
            b.read(eng)
        for b in W:
            b.write(eng)
        ins = fn()
        if inc:
            ins.then_inc(eng.sem, 1)
            eng.n += 1
            tok = (eng.name, eng.sem, eng.n)
        else:
            tok = (eng.name, eng.sem, eng.n + 1)
        for b in R:
            b.did_read(tok)
        for b in W:
            b.did_write(tok)
        return tok

    def dma(self, slot, out, in_, R=(), W=(), q=None):
        q = q or self.SP
        if slot not in self.dsem:
            self.dsem[slot] = self.new_sem(slot)
        s = self.dsem[slot]
        for b in R:
            b.read(q)
        for b in W:
            b.write(q)
        q.e.dma_start(out=out, in_=in_).then_inc(s[0], 16)
        s[1] += 16
        tok = ("dma:" + slot, s[0], s[1])
        for b in R:
            b.did_read(tok)
        for b in W:
            b.did_write(tok)
        return tok

    def new_sem(self, slot):
        if self.sem_pool and not slot.startswith("cv_"):
            return self.sem_pool.pop()
        self.nsem += 1
        return [self.es.enter_context(self.nc.semaphore("dq%d" % self.nsem)), 0]

    def dma_ind(self, slot, out, out_off, in_, in_off, nrows, R=(), W=()):
        q = self.POOL
        if slot not in self.dsem:
            self.dsem[slot] = self.new_sem(slot)
        s = self.dsem[slot]
        for b in R:
            b.read(q)
        for b in W:
            b.write(q)
        q.e.indirect_dma_start(out=out, out_offset=out_off, in_=in_, in_offset=in_off).then_inc(s[0], 16)
        s[1] += 16
        tok = ("dma:" + slot, s[0], s[1])
        for b in R:
            b.did_read(tok)
        for b in W:
            b.did_write(tok)
        return tok

    def all_tokens(self, final=False):
        toks = []
        for e in (self.PE, self.ACT, self.DVE, self.POOL):
            if e.n:
                toks.append((e.name, e.sem, e.n))
        for name, s in self.dsem.items():
            if s[1] and (final or not name.startswith("cv_")):
                toks.append(("dma:" + name, s[0], s[1]))
        return toks

    def barrier(self, final=False):
        toks = self.all_tokens(final)
        for e in (self.PE, self.ACT, self.DVE, self.POOL, self.SP):
            for t in toks:
                if t[0] != e.name:
                    e.wait_tok(t)
        for name in list(self.dsem.keys()):
            if not name.startswith("cv_"):
                self.sem_pool.append(self.dsem.pop(name))


def Bs(n):
    return [B() for _ in range(n)]


def build_program():
    nc = bass.Bass("TRN2", target_bir_lowering=False)

    _uid = [0]

    def sbt(name, shape, dt):
        _uid[0] += 1
        return nc.sbuf_tensor("sb%d_%s" % (_uid[0], name), shape, dt)

    def pst(name, shape, dt):
        _uid[0] += 1
        return nc.psum_tensor("ps%d_%s" % (_uid[0], name), shape, dt)

    def din(name, shape, dt=F32):
        return nc.dram_tensor(name, list(shape), dt, kind="ExternalInput").ap()

    def dscr(name, shape, dt):
        return nc.dram_tensor(name, list(shape), dt, kind="Internal").ap()

    x_local = din("x_local", [NT, D])
    flags_d = din("flags", [128, 8])
    cT_d = din("cT", [128, 16, 2])
    ident_d = din("ident", [128, 128], BF16)
    ones_d = din("ones", [128, 128], BF16)
    bias_d = din("biasmat", [128, 8, 384])
    nmg_d = din("norm_mix_g", [2, D])
    nfg_d = din("norm_ffn_g", [2, D])
    wada_d = din("w_ada", [2, D, 6 * D])
    bada_d = din("b_ada", [2, 6 * D])
    win_d = din("w_in", [2, D, 4096])
    wout_d = din("w_out", [2, D, D])
    caw_d = din("caw", [2, 128, 4, 31])
    cab_d = din("cab", [2, 128, 4])
    lng_d = din("lng", [2, 128, 4])
    lnb_d = din("lnb", [2, 128, 4])
    sink_d = din("sink", [2, 1, 8])
    ccw_d = din("ccw", [2, 128, 4, 3])
    wr_d = din("wr", [2, 128, 16, 20])
    br_d = din("br", [2, 1, 20])
    wg_d = din("w_gate", [2, 16, D, 512])
    wu_d = din("w_up", [2, 16, D, 512])
    wd_d = din("w_down", [2, 16, 512, D])
    fng_d = din("final_norm_g", [1, D])
    utri_d = din("utri", [128, 128], BF16)
    ones1_d = din("ones1", [128, 128], BF16)
    thr_d = din("thr", [128, 80])
    iotap_d = din("iotap", [128, 1])
    y_d = nc.dram_tensor("y_local", [NOWN, D], F32, kind="ExternalOutput").ap()

    wb_in = dscr("wb_in", [2, D, 4096], BF16)
    wb_out = dscr("wb_out", [2, D, D], BF16)
    wb_g = [dscr("wb_g%d" % l, [2048, 8192], BF16) for l in range(2)]
    wb_u = [dscr("wb_u%d" % l, [2048, 8192], BF16) for l in range(2)]
    wb_d = [dscr("wb_d%d" % l, [2048, 8192], BF16) for l in range(2)]
    Hrows = dscr("Hrows", [NT, D], BF16)
    Hslots = dscr("Hslots", [68 * 256, D], BF16)
    Yslots = dscr("Yslots", [68 * 256, D], F32)
    mod_d = dscr("mod_d", [2, 2, 6 * D], F32)
    xa = dscr("xa", [NT, D], F32)
    xb = dscr("xb", [NT, D], F32)
    aT_d = dscr("aT", [4, 128, NT], BF16)
    qT_d = dscr("qT", [8, 128, NT], BF16)
    kT_d = dscr("kT", [2, 128, NT], BF16)
    v_d = dscr("v", [NT, 256], BF16)
    uT_d = dscr("uT", [4, 128, NT], F32)
    cbT_d = dscr("cbT", [4, 128, NT], F32)
    mix_d = dscr("mixT", [16, 128, NT], BF16)

    with ExitStack() as es_all:
        k = K(nc, es_all)
        PE, ACT, DVE, POOL, SP = k.PE, k.ACT, k.DVE, k.POOL, k.SP
        T, A, V, G = nc.tensor, nc.scalar, nc.vector, nc.gpsimd

        conv_tok = {}

        def cast(name, dst, src):
            conv_tok[name] = k.dma("cv_" + name, dst, src, q=POOL)

        for l in range(2):
            for i in range(8):
                cast("in%d" % l, wb_in[l, i * 256:(i + 1) * 256, :], win_d[l, i * 256:(i + 1) * 256, :])
            for i in range(4):
                cast("out%d" % l, wb_out[l, i * 512:(i + 1) * 512, :], wout_d[l, i * 512:(i + 1) * 512, :])
            for e in range(16):
                cast("g%d" % l, wb_g[l][e * 128:(e + 1) * 128, :].rearrange("p (c n) -> p c n", c=16),
                     wg_d[l, e].rearrange("(c p) n -> p c n", p=128))
                cast("u%d" % l, wb_u[l][e * 128:(e + 1) * 128, :].rearrange("p (c n) -> p c n", c=16),
                     wu_d[l, e].rearrange("(c p) n -> p c n", p=128))
                cast("d%d" % l, wb_d[l][e * 128:(e + 1) * 128, :].rearrange("p (c n) -> p c n", c=4),
                     wd_d[l, e].rearrange("(c p) n -> p c n", p=128))

        with ExitStack() as es:
            E = es.enter_context
            siluT = E(sbt("siluT", [128, 16, 2], F32))
            wt = [E(sbt("wadat%d" % i, [128, 16, 512], F32)) for i in range(2)]
            modrow = E(sbt("modrow", [2, 6 * D], F32))
            badar = E(sbt("badar", [2, 6 * D], F32))
            grow = E(sbt("grow", [2, 2, D], F32))
            ps = [E(pst("pps%d" % i, [128, 512], F32)) for i in range(2)]
            b_silu, b_mod, b_bada, b_grow = B(), B(), B(), B()
            b_wt, b_ps = Bs(2), Bs(2)
            k.dma("siluT", siluT[:], cT_d, W=[b_silu])
            k.do(ACT, lambda: A.activation(out=siluT[:], in_=siluT[:], func=AF.Silu), R=[b_silu], W=[b_silu])
            it = 0
            for l in range(2):
                k.dma("bada", badar[:], bada_d[l:l + 1, :].partition_broadcast(2), W=[b_bada])
                k.dma("grow", grow[:, 0, :], nmg_d[l:l + 1, :].partition_broadcast(2), W=[b_grow])
                k.dma("grow", grow[:, 1, :], nfg_d[l:l + 1, :].partition_broadcast(2), W=[b_grow])
                for j in range(24):
                    s = it % 2
                    it += 1
                    k.dma("wadat%d" % s, wt[s][:],
                          wada_d[l, :, j * 512:(j + 1) * 512].rearrange("(c p) n -> p c n", p=128), W=[b_wt[s]])
                    for c in range(16):
                        k.do(PE, lambda: T.matmul(ps[s][0:2, :], lhsT=siluT[:, c, :], rhs=wt[s][:, c, :],
                                                  start=(c == 0), stop=(c == 15)),
                             R=[b_silu, b_wt[s]], W=[b_ps[s]], inc=(c == 15))
                    k.do(DVE, lambda: V.tensor_tensor(out=modrow[:, j * 512:(j + 1) * 512], in0=ps[s][0:2, :],
                                                      in1=badar[:, j * 512:(j + 1) * 512], op=ALU.add),
                         R=[b_ps[s], b_bada], W=[b_mod])
                for (off, gi) in ((D, 0), (4 * D, 1)):
                    k.do(DVE, lambda: V.scalar_tensor_tensor(out=modrow[:, off:off + D], in0=modrow[:, off:off + D],
                                                             scalar=1.0, in1=grow[:, gi, :], op0=ALU.add,
                                                             op1=ALU.mult), R=[b_mod, b_grow], W=[b_mod])
                k.dma("modout", mod_d[l], modrow[:], R=[b_mod], q=POOL)
            k.barrier()

        def load_const(E, name, shape, dt, src, b):
            t = E(sbt(name, shape, dt))
            k.dma("c_" + name, t[:], src, W=[b])
            return t

        def seg_of_block(gb):
            return 0 if gb < 20 else 1

        def tiles_full(l):
            res = []
            for s, (bs, nbk) in enumerate(SEGS):
                lo, hi = (1, nbk - 1) if l == 0 else (2, nbk - 2)
                b = lo
                while b < hi:
                    nb = min(4, hi - b)
                    res.append((s, bs + b, nb))
                    b += nb
            return res

        def load_fm_mod(E, name, l, off, b):
            t = E(sbt(name, [128, 2, 16], F32))
            with nc.allow_non_contiguous_dma(reason="tiny feature-major load of a modulation vector"):
                for s in range(2):
                    k.dma("c_" + name, t[:, s, :], mod_d[l, s, off:off + D].rearrange("(c p) -> p c", p=128), W=[b])
            return t

        def norm_transpose(x_src, gb0, nb, seg, xs, b_xs, xsi, junk, b_junk, ss, b_ss, xh, b_xh, pT, b_pT,
                           hT, b_hT, afm, bfm, b_const, ident):
            n = nb * 128
            k.do(DVE, lambda: V.memset(ss[:], 0.0), W=[b_ss])
            for b in range(nb):
                si = xsi[0] % len(xs)
                xsi[0] += 1
                k.dma("xs%d" % si, xs[si][:], x_src[(gb0 + b) * 128:(gb0 + b + 1) * 128, :], W=[b_xs[si]])
                k.do(ACT, lambda: A.activation(out=junk[:], in_=xs[si][:], func=AF.Square,
                                               accum_out=ss[:, b:b + 1]), R=[b_xs[si], b_ss], W=[b_junk, b_ss])
                k.do(ACT, lambda: A.activation(out=ss[:, 4 + b:5 + b], in_=ss[:, b:b + 1], func=AF.Sqrt,
                                               scale=1.0 / D, bias=EPS), R=[b_ss], W=[b_ss])
                k.do(DVE, lambda: V.reciprocal(out=ss[:, 8 + b:9 + b], in_=ss[:, 4 + b:5 + b]), R=[b_ss], W=[b_ss])
                k.do(DVE, lambda: V.tensor_scalar(out=xh[:, b, :], in0=xs[si][:], scalar1=ss[:, 8 + b:9 + b],
                                                  scalar2=None, op0=ALU.mult), R=[b_xs[si], b_ss], W=[b_xh])
            for c in range(16):
                pi = c % 2
                for b in range(nb):
                    k.do(PE, lambda: T.transpose(out=pT[pi][:, b * 128:(b + 1) * 128],
                                                 in_=xh[:, b, c * 128:(c + 1) * 128], identity=ident[:]),
                         R=[b_xh, b_const], W=[b_pT[pi]], inc=(b == nb - 1))
                k.do(ACT, lambda: A.activation(out=hT[:, c, 0:n], in_=pT[pi][:, 0:n], func=AF.Identity,
                                               scale=afm[:, seg, c:c + 1], bias=bfm[:, seg, c:c + 1]),
                     R=[b_pT[pi], b_const], W=[b_hT])

        for l in range(2):
            x_src1 = x_local if l == 0 else xb
            x_mid = xa
            x_dst = xb

            with ExitStack() as es:
                E = es.enter_context
                b_const = B()
                ident = load_const(E, "ident", [128, 128], BF16, ident_d, b_const)
                flg = load_const(E, "flg", [128, 8], F32, flags_d, b_const)
                afm = load_fm_mod(E, "afm", l, D, b_const)
                bfm = load_fm_mod(E, "bfm", l, 0, b_const)
                xs = [E(sbt("xs%d" % i, [128, D], F32)) for i in range(2)]
                junk = E(sbt("junk", [128, D], BF16))
                ss = E(sbt("ss", [128, 12], F32))
                xh = [E(sbt("xh%d" % i, [128, 4, D], BF16)) for i in range(2)]
                hT = [E(sbt("hT%d" % i, [128, 16, 512], BF16)) for i in range(2)]
                wt = [E(sbt("wt%d" % i, [128, 16, 512], BF16)) for i in range(3)]
                sig = E(sbt("sig", [128, 4, 512], F32))
                o16 = [E(sbt("o16_%d" % i, [128, 4, 512], BF16)) for i in range(3)]
                o32 = [E(sbt("o32_%d" % i, [128, 4, 512], F32)) for i in range(2)]
                vo = E(sbt("vo", [128, 4, 256], BF16))
                pT = [E(pst("pT%d" % i, [128, 1024], BF16)) for i in range(2)]
                pz = [E(pst("pz%d" % i, [128, 512], F32)) for i in range(4)]
                b_xs, b_xh, b_hT, b_wt, b_pT, b_pz = Bs(2), Bs(2), Bs(2), Bs(3), Bs(2), Bs(4)
                b_junk, b_ss, b_sig, b_vo = B(), B(), B(), B()
                b_o16, b_o32 = Bs(3), Bs(2)
                xsi = [0]
                wi = 0
                pzi = 0
                o16i = 0
                o32i = 0
                first_w = True
                for t in range(14):
                    seg = 0 if t < 5 else 1
                    sl = t % 2
                    tok0 = t * 512
                    halo = None
                    if t in (0, 5):
                        halo = (0, 256, 0 if t == 0 else 2)
                    if t in (4, 13):
                        halo = (256, 512, 1 if t == 4 else 3)
                    norm_transpose(x_src1, t * 4, 4, seg, xs, b_xs, xsi, junk, b_junk, ss, b_ss, xh[sl], b_xh[sl],
                                   pT, b_pT, hT[sl], b_hT[sl], afm, bfm, b_const, ident)

                    def mask_halo(buf, bb, ngrp):
                        if halo is None:
                            return
                        lo, hi, fi = halo
                        k.do(POOL, lambda: G.tensor_scalar(out=buf[:, 0:ngrp, lo:hi], in0=buf[:, 0:ngrp, lo:hi],
                                                           scalar1=flg[:, fi:fi + 1], scalar2=None, op0=ALU.mult),
                             R=[bb, b_const], W=[bb])

                    for g in (1, 0, 2, 3, 4, 7, 5, 6):
                        ws = wi % 3
                        wi += 1
                        if first_w:
                            SP.wait_tok(conv_tok["in%d" % l])
                            first_w = False
                        k.dma("wt%d" % ws, wt[ws][:],
                              wb_in[l, :, g * 512:(g + 1) * 512].rearrange("(c p) n -> p c n", p=128), W=[b_wt[ws]])
                        nfm = 2 if g == 4 else 4
                        if g in (0, 2, 3, 4):
                            oi = o16i % 3
                            o16i += 1
                            ob, bo = o16[oi], b_o16[oi]
                        elif g in (5, 6):
                            oi = o32i % 2
                            o32i += 1
                            ob, bo = o32[oi], b_o32[oi]
                        for j in range(nfm):
                            p = pzi % 4
                            pzi += 1
                            for c in range(16):
                                k.do(PE, lambda: T.matmul(pz[p][:], lhsT=wt[ws][:, c, j * 128:(j + 1) * 128],
                                                          rhs=hT[sl][:, c, :], start=(c == 0), stop=(c == 15)),
                                     R=[b_wt[ws], b_hT[sl]], W=[b_pz[p]], inc=(c == 15))
                            if g == 1:
                                k.do(ACT, lambda: A.activation(out=sig[:, j, :], in_=pz[p][:], func=AF.Sigmoid),
                                     R=[b_pz[p]], W=[b_sig])
                            elif g == 0:
                                k.do(DVE, lambda: V.tensor_tensor(out=ob[:, j, :], in0=pz[p][:], in1=sig[:, j, :],
                                                                  op=ALU.mult), R=[b_pz[p], b_sig], W=[bo])
                            elif g in (2, 3, 4):
                                k.do(ACT, lambda: A.activation(out=ob[:, j, :], in_=pz[p][:], func=AF.Identity),
                                     R=[b_pz[p]], W=[bo])
                            elif g == 7:
                                k.do(ACT, lambda: A.activation(out=sig[:, j, :], in_=pz[p][:], func=AF.Identity),
                                     R=[b_pz[p]], W=[b_sig])
                            elif g == 5:
                                k.do(DVE, lambda: V.tensor_tensor(out=ob[:, j, :], in0=pz[p][:], in1=sig[:, j, :],
                                                                  op=ALU.mult), R=[b_pz[p], b_sig], W=[bo])
                            elif g == 6:
                                k.do(ACT, lambda: A.activation(out=ob[:, j, :], in_=pz[p][:], func=AF.Identity),
                                     R=[b_pz[p]], W=[bo])
                        if g == 4:
                            for b in range(4):
                                p = pzi % 4
                                pzi += 1
                                for c in range(16):
                                    k.do(PE, lambda: T.matmul(pz[p][:, 0:256], lhsT=hT[sl][:, c, b * 128:(b + 1) * 128],
                                                              rhs=wt[ws][:, c, 256:512], start=(c == 0),
                                                              stop=(c == 15)),
                                         R=[b_wt[ws], b_hT[sl]], W=[b_pz[p]], inc=(c == 15))
                                k.do(DVE, lambda: V.tensor_copy(out=vo[:, b, :], in_=pz[p][:, 0:256]),
                                     R=[b_pz[p]], W=[b_vo])
                            if halo is not None:
                                lo, hi, fi = halo
                                k.do(POOL, lambda: G.tensor_scalar(out=vo[:, lo // 128:hi // 128, :],
                                                                   in0=vo[:, lo // 128:hi // 128, :],
                                                                   scalar1=flg[:, fi:fi + 1], scalar2=None,
                                                                   op0=ALU.mult), R=[b_vo, b_const], W=[b_vo])
                            k.dma("st_v", v_d[tok0:tok0 + 512, :].rearrange("(b p) d -> p b d", p=128), vo[:],
                                  R=[b_vo], q=POOL)
                        if g == 0:
                            mask_halo(ob, bo, 4)
                            k.dma("st_a", aT_d[:, :, tok0:tok0 + 512].rearrange("g p t -> p g t"), ob[:], R=[bo], q=POOL)
                        elif g in (2, 3):
                            h0 = (g - 2) * 4
                            k.dma("st_q%d" % g, qT_d[h0:h0 + 4, :, tok0:tok0 + 512].rearrange("g p t -> p g t"), ob[:],
                                  R=[bo], q=POOL)
                        elif g == 4:
                            mask_halo(ob, bo, 2)
                            k.dma("st_k", kT_d[:, :, tok0:tok0 + 512].rearrange("g p t -> p g t"), ob[:, 0:2, :],
                                  R=[bo], q=POOL)
                        elif g == 5:
                            mask_halo(ob, bo, 4)
                            k.dma("st_u", uT_d[:, :, tok0:tok0 + 512].rearrange("g p t -> p g t"), ob[:], R=[bo], q=POOL)
                        elif g == 6:
                            k.dma("st_cb", cbT_d[:, :, tok0:tok0 + 512].rearrange("g p t -> p g t"), ob[:], R=[bo],
                                  q=POOL)
                k.barrier()

            tl = tiles_full(l)
            with ExitStack() as es:
                E = es.enter_context
                b_const = B()
                ident = load_const(E, "ident", [128, 128], BF16, ident_d, b_const)
                onesb = load_const(E, "onesb", [128, 128], BF16, ones_d, b_const)
                flg = load_const(E, "flg", [128, 8], F32, flags_d, b_const)
                bias8 = load_const(E, "bias8", [128, 8, 384], F32, bias_d, b_const)
                caw = load_const(E, "caw", [128, 4, 31], F32, caw_d[l], b_const)
                cab = load_const(E, "cab", [128, 4], F32, cab_d[l], b_const)
                lng = load_const(E, "lng", [128, 4], F32, lng_d[l], b_const)
                lnb = load_const(E, "lnb", [128, 4], F32, lnb_d[l], b_const)
                ccw = load_const(E, "ccw", [128, 4, 3], F32, ccw_d[l], b_const)
                sinkb = load_const(E, "sinkb", [128, 8], F32, sink_d[l].partition_broadcast(128), b_const)
                Dg = E(sbt("Dg", [128, 31, 4, 128], BF16))
                b_Dg = B()
                for kk in range(31):
                    for g in range(4):
                        k.do(DVE, lambda: V.tensor_scalar(out=Dg[:, kk, g, :], in0=ident[:],
                                                          scalar1=caw[:, g, kk:kk + 1], scalar2=None, op0=ALU.mult),
                             R=[b_const], W=[b_Dg])
                a_sb = [E(sbt("a_sb%d" % i, [128, 4, 542], BF16)) for i in range(2)]
                q_sb = [E(sbt("q_sb%d" % i, [128, 8, 512], BF16)) for i in range(2)]
                k_sb = [E(sbt("k_sb%d" % i, [128, 2, 768], BF16)) for i in range(2)]
                v_sb = [E(sbt("v_sb%d" % i, [128, 6, 256], BF16)) for i in range(2)]
                u_sb = [E(sbt("u_sb%d" % i, [128, 4, 514], F32)) for i in range(2)]
                cb_sb = [E(sbt("cb_sb%d" % i, [128, 4, 512], F32)) for i in range(2)]
                mixT = [E(sbt("mixT%d" % i, [128, 16, 512], BF16)) for i in range(2)]
                cv = E(sbt("cv", [128, 4, 512], F32))
                cvb = E(sbt("cvb", [128, 4, 512], BF16))
                cv2 = E(sbt("cv2", [128, 4, 512], BF16))
                mean_sb = E(sbt("mean_sb", [128, 512], F32))
                var_sb = E(sbt("var_sb", [128, 512], F32))
                rstd_sb = E(sbt("rstd_sb", [128, 512], F32))
                t1 = [E(sbt("t1_%d" % i, [128, 512], F32)) for i in range(2)]
                s_sb = [E(sbt("s_sb%d" % i, [128, 384], F32)) for i in range(2)]
                p_sb = [E(sbt("p_sb%d" % i, [128, 384], F32)) for i in range(2)]
                pn_sb = [E(sbt("pn_sb%d" % i, [128, 384], BF16)) for i in range(2)]
                pT_sb = [E(sbt("pT_sb%d" % i, [128, 384], BF16)) for i in range(2)]
                st = [E(sbt("st%d" % i, [128, 8], F32)) for i in range(2)]
                acc = [E(sbt("acc%d" % i, [128, 512], F32)) for i in range(2)]
                acc2 = E(sbt("acc2", [128, 512], F32))
                b_acc2 = B()
                pc = [E(pst("pc%d" % i, [128, 512], F32)) for i in range(2)]
                pmean = E(pst("pmean", [128, 512], F32))
                pex2 = E(pst("pex2", [128, 512], F32))
                s_ps = [E(pst("s_ps%d" % i, [128, 512], F32)) for i in range(2)]
                pT_ps = E(pst("pT_ps", [128, 1024], BF16))
                o_ps = E(pst("o_ps", [128, 512], F32))
                b_a, b_q, b_k, b_v, b_u, b_cb, b_mix = Bs(2), Bs(2), Bs(2), Bs(2), Bs(2), Bs(2), Bs(2)
                b_cv, b_cvb, b_cv2, b_mean, b_var, b_rstd = B(), B(), B(), B(), B(), B()
                b_t1, b_s, b_p, b_pn, b_pTs, b_st, b_acc = Bs(2), Bs(2), Bs(2), Bs(2), Bs(2), Bs(2), Bs(2)
                b_pc, b_sps = Bs(2), Bs(2)
                b_pmean, b_pex2, b_pTp, b_ops = B(), B(), B(), B()
                hi_ = 0
                gi_ = 0
                for ti, (seg, gb0, nb) in enumerate(tl):
                    sl = ti % 2
                    n = nb * 128
                    t0 = gb0 * 128
                    bs, nbk = SEGS[seg]
                    k.dma("a_sb%d" % sl, a_sb[sl][:, :, 0:n + 30],
                          aT_d[:, :, t0 - 15:t0 + n + 15].rearrange("g p t -> p g t"), W=[b_a[sl]])
                    k.dma("q_sb%d" % sl, q_sb[sl][:, :, 0:n], qT_d[:, :, t0:t0 + n].rearrange("g p t -> p g t"),
                          W=[b_q[sl]])
                    k.dma("k_sb%d" % sl, k_sb[sl][:, :, 0:n + 256],
                          kT_d[:, :, t0 - 128:t0 + n + 128].rearrange("g p t -> p g t"), W=[b_k[sl]])
                    k.dma("v_sb%d" % sl, v_sb[sl][:, 0:nb + 2, :],
                          v_d[t0 - 128:t0 + n + 128, :].rearrange("(b p) d -> p b d", p=128), W=[b_v[sl]])
                    k.dma("u_sb%d" % sl, u_sb[sl][:, :, 0:n + 2],
                          uT_d[:, :, t0 - 1:t0 + n + 1].rearrange("g p t -> p g t"), W=[b_u[sl]])
                    k.dma("cb_sb%d" % sl, cb_sb[sl][:, :, 0:n], cbT_d[:, :, t0:t0 + n].rearrange("g p t -> p g t"),
                          W=[b_cb[sl]])
                    mx = mixT[sl]
                    bm = b_mix[sl]
                    for g in range(4):
                        pi = g % 2
                        for kk in range(31):
                            k.do(PE, lambda: T.matmul(pc[pi][:, 0:n], lhsT=Dg[:, kk, g, :],
                                                      rhs=a_sb[sl][:, g, kk:kk + n], start=(kk == 0), stop=(kk == 30)),
                                 R=[b_Dg, b_a[sl]], W=[b_pc[pi]], inc=(kk == 30))
                        k.do(ACT, lambda: A.activation(out=cv[:, g, 0:n], in_=pc[pi][:, 0:n], func=AF.Identity,
                                                       bias=cab[:, g:g + 1]), R=[b_pc[pi], b_const], W=[b_cv])
                        k.do(ACT, lambda: A.activation(out=cvb[:, g, 0:n], in_=pc[pi][:, 0:n], func=AF.Identity,
                                                       bias=cab[:, g:g + 1]), R=[b_pc[pi], b_const], W=[b_cvb])
                        k.do(ACT, lambda: A.activation(out=cv2[:, g, 0:n], in_=pc[pi][:, 0:n], func=AF.Square,
                                                       bias=cab[:, g:g + 1]), R=[b_pc[pi], b_const], W=[b_cv2])
                    for g in range(4):
                        k.do(PE, lambda: T.matmul(pmean[:, 0:n], lhsT=onesb[:], rhs=cvb[:, g, 0:n], start=(g == 0),
                                                  stop=(g == 3)), R=[b_const, b_cvb], W=[b_pmean], inc=(g == 3))
                    for g in range(4):
                        k.do(PE, lambda: T.matmul(pex2[:, 0:n], lhsT=onesb[:], rhs=cv2[:, g, 0:n], start=(g == 0),
                                                  stop=(g == 3)), R=[b_const, b_cv2], W=[b_pex2], inc=(g == 3))
                    k.do(DVE, lambda: V.tensor_copy(out=mean_sb[:, 0:n], in_=pmean[:, 0:n]), R=[b_pmean], W=[b_mean])
                    k.do(DVE, lambda: V.tensor_tensor(out=var_sb[:, 0:n], in0=mean_sb[:, 0:n], in1=mean_sb[:, 0:n],
                                                      op=ALU.mult), R=[b_mean], W=[b_var])
                    k.do(DVE, lambda: V.tensor_tensor(out=var_sb[:, 0:n], in0=pex2[:, 0:n], in1=var_sb[:, 0:n],
                                                      op=ALU.subtract), R=[b_pex2, b_var], W=[b_var])
                    k.do(DVE, lambda: V.tensor_scalar(out=var_sb[:, 0:n], in0=var_sb[:, 0:n], scalar1=0.0,
                                                      scalar2=None, op0=ALU.max), R=[b_var], W=[b_var])
                    k.do(ACT, lambda: A.activation(out=var_sb[:, 0:n], in_=var_sb[:, 0:n], func=AF.Sqrt, bias=EPS),
                         R=[b_var], W=[b_var])
                    k.do(DVE, lambda: V.reciprocal(out=rstd_sb[:, 0:n], in_=var_sb[:, 0:n]), R=[b_var], W=[b_rstd])
                    for g in range(4):
                        ti_ = gi_ % 2
                        gi_ += 1
                        k.do(DVE, lambda: V.tensor_tensor(out=t1[ti_][:, 0:n], in0=cv[:, g, 0:n], in1=mean_sb[:, 0:n],
                                                          op=ALU.subtract), R=[b_cv, b_mean], W=[b_t1[ti_]])
                        k.do(DVE, lambda: V.tensor_tensor(out=t1[ti_][:, 0:n], in0=t1[ti_][:, 0:n],
                                                          in1=rstd_sb[:, 0:n], op=ALU.mult),
                             R=[b_t1[ti_], b_rstd], W=[b_t1[ti_]])
                        k.do(ACT, lambda: A.activation(out=mx[:, g, 0:n], in_=t1[ti_][:, 0:n], func=AF.Silu,
                                                       scale=lng[:, g:g + 1], bias=lnb[:, g:g + 1]),
                             R=[b_t1[ti_], b_const], W=[bm])
                    for g in range(4):
                        ai = g % 2
                        k.do(POOL, lambda: G.tensor_scalar(out=acc[ai][:, 0:n], in0=u_sb[sl][:, g, 0:n],
                                                           scalar1=ccw[:, g, 0:1], scalar2=None, op0=ALU.mult),
                             R=[b_u[sl], b_const], W=[b_acc[ai]])
                        for kk in (1, 2):
                            k.do(POOL, lambda: G.tensor_scalar(out=acc2[:, 0:n], in0=u_sb[sl][:, g, kk:kk + n],
                                                               scalar1=ccw[:, g, kk:kk + 1], scalar2=None, op0=ALU.mult),
                                 R=[b_u[sl], b_const], W=[b_acc2])
                            k.do(POOL, lambda: G.tensor_tensor(out=acc[ai][:, 0:n], in0=acc[ai][:, 0:n],
                                                               in1=acc2[:, 0:n], op=ALU.add),
                                 R=[b_acc2, b_acc[ai]], W=[b_acc[ai]])
                        k.do(POOL, lambda: G.tensor_tensor(out=mx[:, 12 + g, 0:n], in0=acc[ai][:, 0:n],
                                                           in1=cb_sb[sl][:, g, 0:n], op=ALU.mult),
                             R=[b_acc[ai], b_cb[sl]], W=[bm])
                    for b in range(nb):
                        gb = gb0 + b
                        left_edge = (gb == bs + 2)
                        right_edge = (gb == bs + nbk - 3)
                        for h in range(8):
                            i2 = hi_ % 2
                            hi_ += 1
                            kv = h // 4
                            sp, bsp = s_ps[i2], b_sps[i2]
                            ssb, bss = s_sb[i2], b_s[i2]
                            stt, bst = st[i2], b_st[i2]
                            k.do(PE, lambda: T.matmul(sp[:, 0:384], lhsT=q_sb[sl][:, h, b * 128:(b + 1) * 128],
                                                      rhs=k_sb[sl][:, kv, b * 128:b * 128 + 384], start=True, stop=True),
                                 R=[b_q[sl], b_k[sl]], W=[bsp])
                            k.do(DVE, lambda: V.scalar_tensor_tensor(out=ssb[:], in0=sp[:, 0:384], scalar=SCALE,
                                                                     in1=bias8[:, h, :], op0=ALU.mult, op1=ALU.add),
                                 R=[bsp, b_const], W=[bss])
                            if left_edge:
                                fi = 4 + (0 if seg == 0 else 2)
                                k.do(DVE, lambda: V.tensor_scalar(out=ssb[:, 0:128], in0=ssb[:, 0:128],
                                                                  scalar1=flg[:, fi:fi + 1], scalar2=None, op0=ALU.add),
                                     R=[bss, b_const], W=[bss])
                            if right_edge:
                                fi = 4 + (1 if seg == 0 else 3)
                                k.do(DVE, lambda: V.tensor_scalar(out=ssb[:, 256:384], in0=ssb[:, 256:384],
                                                                  scalar1=flg[:, fi:fi + 1], scalar2=None, op0=ALU.add),
                                     R=[bss, b_const], W=[bss])
                            k.do(DVE, lambda: V.reduce_max(out=stt[:, 0:1], in_=ssb[:], axis=AX.X), R=[bss], W=[bst])
                            k.do(DVE, lambda: V.tensor_scalar(out=stt[:, 1:2], in0=stt[:, 0:1], scalar1=sinkb[:, h:h + 1],
                                                              scalar2=-1.0, op0=ALU.max, op1=ALU.mult),
                                 R=[bst, b_const], W=[bst])
                            k.do(DVE, lambda: V.memset(stt[:, 2:3], 0.0), R=[bst], W=[bst])
                            k.do(ACT, lambda: A.activation(out=p_sb[i2][:], in_=ssb[:], func=AF.Exp, bias=stt[:, 1:2],
                                                           accum_out=stt[:, 2:3]), R=[bss, bst], W=[b_p[i2], bst])
                            k.do(ACT, lambda: A.activation(out=stt[:, 3:4], in_=sinkb[:, h:h + 1], func=AF.Exp,
                                                           bias=stt[:, 1:2]), R=[bst, b_const], W=[bst])
                            k.do(DVE, lambda: V.tensor_tensor(out=stt[:, 4:5], in0=stt[:, 2:3], in1=stt[:, 3:4],
                                                              op=ALU.add), R=[bst], W=[bst])
                            k.do(DVE, lambda: V.reciprocal(out=stt[:, 5:6], in_=stt[:, 4:5]), R=[bst], W=[bst])
                            k.do(DVE, lambda: V.tensor_scalar(out=pn_sb[i2][:], in0=p_sb[i2][:], scalar1=stt[:, 5:6],
                                                              scalar2=None, op0=ALU.mult),
                                 R=[b_p[i2], bst], W=[b_pn[i2]])
                            for j in range(3):
                                k.do(PE, lambda: T.transpose(out=pT_ps[:, j * 128:(j + 1) * 128],
                                                             in_=pn_sb[i2][:, j * 128:(j + 1) * 128], identity=ident[:]),
                                     R=[b_pn[i2], b_const], W=[b_pTp], inc=(j == 2))
                            k.do(ACT, lambda: A.activation(out=pT_sb[i2][:],
                                                           in_=pT_ps[:, 0:384], func=AF.Identity),
                                 R=[b_pTp], W=[b_pTs[i2]])
                            for j in range(3):
                                k.do(PE, lambda: T.matmul(o_ps[:, 0:128], lhsT=v_sb[sl][:, b + j, kv * 128:(kv + 1) * 128],
                                                          rhs=pT_sb[i2][:, j * 128:(j + 1) * 128], start=(j == 0), stop=(j == 2)),
                                     R=[b_v[sl], b_pTs[i2]], W=[b_ops], inc=(j == 2))
                            k.do(DVE, lambda: V.tensor_copy(out=mx[:, 4 + h, b * 128:(b + 1) * 128], in_=o_ps[:, 0:128]),
                                 R=[b_ops], W=[bm])
                    k.dma("st_mix%d" % sl, mix_d[:, :, t0:t0 + n].rearrange("g p t -> p g t"), mx[:, :, 0:n], R=[bm],
                          q=POOL)
                k.barrier()

            with ExitStack() as es:
                E = es.enter_context
                b_const = B()
                wo = E(sbt("wo", [128, 16, D], BF16))
                SP.wait_tok(conv_tok["out%d" % l])
                k.dma("c_wo", wo[:], wb_out[l].rearrange("(c p) n -> p c n", p=128), W=[b_const])
                gm = E(sbt("gm", [128, 2, D], F32))
                for s in range(2):
                    k.dma("c_gm", gm[:, s, :], mod_d[l, s:s + 1, 2 * D:3 * D].partition_broadcast(128), W=[b_const])
                mixs = [E(sbt("mixs%d" % i, [128, 16, 512], BF16)) for i in range(2)]
                xs = [E(sbt("xs%d" % i, [128, D], F32)) for i in range(2)]
                tmp = [E(sbt("tmp%d" % i, [128, 512], F32)) for i in range(2)]
                xo = [E(sbt("xo%d" % i, [128, D], F32)) for i in range(2)]
                po = [E(pst("po%d" % i, [128, 512], F32)) for i in range(8)]
                b_mixs, b_xs, b_tmp, b_xo, b_po = Bs(2), Bs(2), Bs(2), Bs(2), Bs(8)
                bi_ = 0
                tmi = 0
                for ti, (seg, gb0, nb) in enumerate(tl):
                    sl = ti % 2
                    n = nb * 128
                    t0 = gb0 * 128
                    k.dma("mixs%d" % sl, mixs[sl][:, :, 0:n], mix_d[:, :, t0:t0 + n].rearrange("g p t -> p g t"),
                          W=[b_mixs[sl]])
                    for b in range(nb):
                        i2 = bi_ % 2
                        bi_ += 1
                        r0 = (gb0 + b) * 128
                        k.dma("xs%d" % i2, xs[i2][:], x_src1[r0:r0 + 128, :], W=[b_xs[i2]])
                        for c in range(16):
                            for ft in range(4):
                                p = i2 * 4 + ft
                                k.do(PE, lambda: T.matmul(po[p][:], lhsT=mixs[sl][:, c, b * 128:(b + 1) * 128],
                                                          rhs=wo[:, c, ft * 512:(ft + 1) * 512], start=(c == 0),
                                                          stop=(c == 15)),
                                     R=[b_mixs[sl], b_const], W=[b_po[p]], inc=(c == 15 and ft == 3))
                        for ft in range(4):
                            p = i2 * 4 + ft
                            tm = tmi % 2
                            tmi += 1
                            k.do(DVE, lambda: V.tensor_tensor(out=tmp[tm][:], in0=po[p][:],
                                                              in1=gm[:, seg, ft * 512:(ft + 1) * 512], op=ALU.mult),
                                 R=[b_po[p], b_const], W=[b_tmp[tm]])
                            k.do(POOL, lambda: G.tensor_tensor(out=xo[i2][:, ft * 512:(ft + 1) * 512], in0=tmp[tm][:],
                                                               in1=xs[i2][:, ft * 512:(ft + 1) * 512], op=ALU.add),
                                 R=[b_tmp[tm], b_xs[i2]], W=[b_xo[i2]])
                        k.dma("st_xo%d" % i2, x_mid[r0:r0 + 128, :], xo[i2][:], R=[b_xo[i2]], q=POOL)
                k.barrier()

            blocks = [(seg, gb0 + b) for (seg, gb0, nb) in tl for b in range(nb)]
            NB3 = len(blocks)
            NTL = NB3 + 16
            NSLOT = NTL * 256
            Hs_l = Hslots[0:NSLOT, :]
            Ys_l = Yslots[0:NSLOT, :]
            with ExitStack() as es3:
                E3 = es3.enter_context
                b_rout = B()
                selA = E3(sbt("selA", [128, NB3, 16], F32))
                selB = E3(sbt("selB", [128, NB3, 16], F32))
                Rg = E3(sbt("Rg", [128, NB3, 16], F32))
                cA = E3(sbt("cA", [128, NB3], F32))
                cB = E3(sbt("cB", [128, NB3], F32))
                posA_i = E3(sbt("posA_i", [128, NB3], I32))
                posB_i = E3(sbt("posB_i", [128, NB3], I32))
                idxw = E3(sbt("idxw", [128, NTL], I32))
                base = E3(sbt("base", [128, 16], F32))

                with ExitStack() as es:
                    E = es.enter_context
                    b_const = B()
                    ident = load_const(E, "ident", [128, 128], BF16, ident_d, b_const)
                    utri = load_const(E, "utri", [128, 128], BF16, utri_d, b_const)
                    ones1 = load_const(E, "ones1", [128, 128], BF16, ones1_d, b_const)
                    thr = load_const(E, "thr", [128, 80], F32, thr_d, b_const)
                    iotap = load_const(E, "iotap", [128, 1], F32, iotap_d, b_const)
                    abc = E(sbt("abc", [128, 2, D], F32))
                    bbc = E(sbt("bbc", [128, 2, D], F32))
                    for s in range(2):
                        k.dma("c_abc", abc[:, s, :], mod_d[l, s:s + 1, 4 * D:5 * D].partition_broadcast(128), W=[b_const])
                        k.dma("c_bbc", bbc[:, s, :], mod_d[l, s:s + 1, 3 * D:4 * D].partition_broadcast(128), W=[b_const])
                    wr32 = load_const(E, "wr32", [128, 16, 20], F32, wr_d[l], b_const)
                    wrb = E(sbt("wrb", [128, 16, 20], BF16))
                    k.do(DVE, lambda: V.tensor_copy(out=wrb[:], in_=wr32[:]), R=[b_const], W=[b_const])
                    brb = load_const(E, "brb", [128, 20], F32, br_d[l].partition_broadcast(128), b_const)
                    xs = [E(sbt("xs%d" % i, [128, D], F32)) for i in range(2)]
                    tmpf = [E(sbt("tmpf%d" % i, [128, D], F32)) for i in range(2)]
                    junk = E(sbt("junk", [128, D], BF16))
                    ss = E(sbt("ss", [128, 12], F32))
                    xh = E(sbt("xh", [128, 4, D], BF16))
                    hT = E(sbt("hT", [128, 16, 512], BF16))
                    rt = E(sbt("rt", [128, 64], F32))
                    s16 = E(sbt("s16", [128, 16], BF16))
                    cmp = E(sbt("cmp", [128, 80], F32))
                    q16 = E(sbt("q16", [128, 64], F32))
                    etf = E(sbt("etf", [128, NTL], F32))
                    posf = E(sbt("posf", [128, 2, NB3], F32))
                    pT = [E(pst("pT%d" % i, [128, 1024], BF16)) for i in range(2)]
                    prt = E(pst("prt", [128, 512], F32))
                    prk = E(pst("prk", [128, 512], F32))
                    ptt = E(pst("ptt", [128, 512], F32))
                    b_xs, b_tmpf, b_pT = Bs(2), Bs(2), Bs(2)
                    b_junk, b_ss, b_xh, b_hT, b_rt, b_s16, b_prt, b_prk, b_ptt = B(), B(), B(), B(), B(), B(), B(), B(), B()
                    k.do(DVE, lambda: V.memset(base[:], 0.0), W=[b_rout])
                    zt = E(sbt("zt", [128, 4, D], BF16))
                    b_zt = B()
                    k.do(DVE, lambda: V.memset(zt[:], 0.0), W=[b_zt])
                    for j0 in range(0, 2 * NTL, 4):
                        k.dma("zero_hs", Hs_l[j0 * 128:(j0 + 4) * 128, :].rearrange("(j p) f -> p j f", p=128), zt[:],
                              R=[b_zt], q=POOL)
                    xi = 0
                    bi = 0
                    for ti, (seg, gb0, nb) in enumerate(tl):
                        n = nb * 128
                        k.do(DVE, lambda: V.memset(ss[:], 0.0), W=[b_ss])
                        for b in range(nb):
                            si = xi % 2
                            xi += 1
                            r0 = (gb0 + b) * 128
                            k.dma("xs%d" % si, xs[si][:], x_mid[r0:r0 + 128, :], W=[b_xs[si]])
                            k.do(ACT, lambda: A.activation(out=junk[:], in_=xs[si][:], func=AF.Square,
                                                           accum_out=ss[:, b:b + 1]), R=[b_xs[si], b_ss], W=[b_junk, b_ss])
                            k.do(ACT, lambda: A.activation(out=ss[:, 4 + b:5 + b], in_=ss[:, b:b + 1], func=AF.Sqrt,
                                                           scale=1.0 / D, bias=EPS), R=[b_ss], W=[b_ss])
                            k.do(DVE, lambda: V.reciprocal(out=ss[:, 8 + b:9 + b], in_=ss[:, 4 + b:5 + b]), R=[b_ss], W=[b_ss])
                            k.do(DVE, lambda: V.scalar_tensor_tensor(out=tmpf[si][:], in0=xs[si][:], scalar=ss[:, 8 + b:9 + b],
                                                                     in1=abc[:, seg, :], op0=ALU.mult, op1=ALU.mult),
                                 R=[b_xs[si], b_ss, b_const], W=[b_tmpf[si]])
                            k.do(POOL, lambda: G.tensor_tensor(out=xh[:, b, :], in0=tmpf[si][:], in1=bbc[:, seg, :], op=ALU.add),
                                 R=[b_tmpf[si], b_const], W=[b_xh])
                            k.dma("st_hrow", Hrows[r0:r0 + 128, :], xh[:, b, :], R=[b_xh], q=POOL)
                        for c in range(16):
                            pi = c % 2
                            for b in range(nb):
                                k.do(PE, lambda: T.transpose(out=pT[pi][:, b * 128:(b + 1) * 128],
                                                             in_=xh[:, b, c * 128:(c + 1) * 128], identity=ident[:]),
                                     R=[b_xh, b_const], W=[b_pT[pi]], inc=(b == nb - 1))
                            k.do(ACT, lambda: A.activation(out=hT[:, c, 0:n], in_=pT[pi][:, 0:n], func=AF.Identity),
                                 R=[b_pT[pi]], W=[b_hT])
                        for b in range(nb):
                            for c in range(16):
                                k.do(PE, lambda: T.matmul(prt[:, 0:20], lhsT=hT[:, c, b * 128:(b + 1) * 128], rhs=wrb[:, c, :],
                                                          start=(c == 0), stop=(c == 15)),
                                     R=[b_hT, b_const], W=[b_prt], inc=(c == 15))

                            def dv(fn, extraR=(), extraW=()):
                                k.do(DVE, fn, R=[b_rt] + list(extraR), W=[b_rt] + list(extraW))
                            dv(lambda: V.tensor_tensor(out=rt[:, 0:20], in0=prt[:, 0:20], in1=brb[:], op=ALU.add),
                               extraR=[b_prt, b_const])
                            dv(lambda: V.reduce_max(out=rt[:, 20:21], in_=rt[:, 0:4], axis=AX.X))
                            dv(lambda: V.tensor_scalar(out=rt[:, 24:28], in0=rt[:, 0:4], scalar1=rt[:, 20:21], scalar2=None,
                                                       op0=ALU.is_equal))
                            dv(lambda: V.tensor_scalar(out=rt[:, 21:22], in0=rt[:, 20:21], scalar1=-1.0, scalar2=None,
                                                       op0=ALU.mult))
                            dv(lambda: V.memset(rt[:, 22:23], 0.0))
                            k.do(ACT, lambda: A.activation(out=rt[:, 28:32], in_=rt[:, 0:4], func=AF.Exp, bias=rt[:, 21:22],
                                                           accum_out=rt[:, 22:23]), R=[b_rt], W=[b_rt])
                            dv(lambda: V.reciprocal(out=rt[:, 23:24], in_=rt[:, 22:23]))
                            dv(lambda: V.tensor_scalar(out=rt[:, 32:36], in0=rt[:, 4:8], scalar1=rt[:, 24:25], scalar2=None,
                                                       op0=ALU.mult))
                            for g in range(1, 4):
                                dv(lambda: V.scalar_tensor_tensor(out=rt[:, 32:36], in0=rt[:, 4 + 4 * g:8 + 4 * g],
                                                                  scalar=rt[:, 24 + g:25 + g], in1=rt[:, 32:36],
                                                                  op0=ALU.mult, op1=ALU.add))
                            dv(lambda: V.reduce_max(out=rt[:, 36:37], in_=rt[:, 32:36], axis=AX.X))
                            dv(lambda: V.tensor_scalar(out=rt[:, 40:44], in0=rt[:, 32:36], scalar1=rt[:, 36:37], scalar2=None,
                                                       op0=ALU.is_equal))
                            dv(lambda: V.scalar_tensor_tensor(out=rt[:, 44:48], in0=rt[:, 40:44], scalar=NEG,
                                                              in1=rt[:, 32:36], op0=ALU.mult, op1=ALU.add))
                            dv(lambda: V.reduce_max(out=rt[:, 37:38], in_=rt[:, 44:48], axis=AX.X))
                            dv(lambda: V.tensor_scalar(out=rt[:, 48:52], in0=rt[:, 44:48], scalar1=rt[:, 37:38], scalar2=None,
                                                       op0=ALU.is_equal))
                            dv(lambda: V.tensor_scalar(out=rt[:, 38:39], in0=rt[:, 36:37], scalar1=-1.0, scalar2=None,
                                                       op0=ALU.mult))
                            k.do(ACT, lambda: A.activation(out=rt[:, 52:56], in_=rt[:, 32:36], func=AF.Exp, bias=rt[:, 38:39]),
                                 R=[b_rt], W=[b_rt])
                            dv(lambda: V.tensor_tensor(out=rt[:, 56:60], in0=rt[:, 52:56], in1=rt[:, 48:52], op=ALU.mult))
                            dv(lambda: V.reduce_sum(out=rt[:, 39:40], in_=rt[:, 56:60], axis=AX.X))
                            dv(lambda: V.tensor_scalar(out=rt[:, 60:61], in0=rt[:, 39:40], scalar1=1.0, scalar2=None,
                                                       op0=ALU.add))
                            dv(lambda: V.reciprocal(out=rt[:, 61:62], in_=rt[:, 60:61]))
                            dv(lambda: V.tensor_tensor(out=cA[:, bi:bi + 1], in0=rt[:, 61:62], in1=rt[:, 23:24], op=ALU.mult),
                               extraW=[b_rout])
                            dv(lambda: V.tensor_tensor(out=cB[:, bi:bi + 1], in0=cA[:, bi:bi + 1], in1=rt[:, 39:40], op=ALU.mult),
                               extraR=[b_rout], extraW=[b_rout])
                            for g in range(4):
                                dv(lambda: V.tensor_scalar(out=selA[:, bi, 4 * g:4 * g + 4], in0=rt[:, 40:44],
                                                           scalar1=rt[:, 24 + g:25 + g], scalar2=None, op0=ALU.mult),
                                   extraW=[b_rout])
                                dv(lambda: V.tensor_scalar(out=selB[:, bi, 4 * g:4 * g + 4], in0=rt[:, 48:52],
                                                           scalar1=rt[:, 24 + g:25 + g], scalar2=None, op0=ALU.mult),
                                   extraW=[b_rout])
                            k.do(DVE, lambda: V.tensor_tensor(out=s16[:], in0=selA[:, bi, :], in1=selB[:, bi, :], op=ALU.add),
                                 R=[b_rout], W=[b_s16])
                            k.do(PE, lambda: T.matmul(prk[:, 0:16], lhsT=utri[:], rhs=s16[:], start=True, stop=True),
                                 R=[b_const, b_s16], W=[b_prk])
                            k.do(PE, lambda: T.matmul(ptt[:, 0:16], lhsT=ones1[:], rhs=s16[:], start=True, stop=True),
                                 R=[b_const, b_s16], W=[b_ptt])
                            k.do(DVE, lambda: V.tensor_tensor(out=Rg[:, bi, :], in0=prk[:, 0:16], in1=base[:], op=ALU.add),
                                 R=[b_prk, b_rout], W=[b_rout])
                            k.do(DVE, lambda: V.tensor_tensor(out=base[:], in0=ptt[:, 0:16], in1=base[:], op=ALU.add),
                                 R=[b_ptt, b_rout], W=[b_rout])
                            bi += 1
                    def dr(fn):
                        k.do(DVE, fn, R=[b_rout, b_rt, b_const], W=[b_rout, b_rt])
                    for e in range(16):
                        dr(lambda: V.tensor_scalar(out=cmp[:, 0:80], in0=thr[:, 0:80], scalar1=base[:, e:e + 1], scalar2=None,
                                                   op0=ALU.is_lt))
                        dr(lambda: V.reduce_sum(out=q16[:, e:e + 1], in_=cmp[:, 0:80], axis=AX.X))
                    dr(lambda: V.tensor_scalar(out=q16[:, 0:16], in0=q16[:, 0:16], scalar1=256.0, scalar2=None, op0=ALU.mult))
                    dr(lambda: V.memset(q16[:, 16:17], 0.0))
                    for e in range(1, 16):
                        dr(lambda: V.tensor_tensor(out=q16[:, 16 + e:17 + e], in0=q16[:, 15 + e:16 + e], in1=q16[:, e - 1:e],
                                                   op=ALU.add))
                    dr(lambda: V.tensor_tensor(out=q16[:, 32:48], in0=q16[:, 16:32], in1=q16[:, 0:16], op=ALU.add))
                    dr(lambda: V.memset(etf[:], 0.0))
                    for e in range(16):
                        dr(lambda: V.tensor_scalar(out=cmp[:, 0:NTL], in0=thr[:, 0:NTL], scalar1=q16[:, 32 + e:33 + e],
                                                   scalar2=None, op0=ALU.is_ge))
                        dr(lambda: V.tensor_tensor(out=etf[:], in0=etf[:], in1=cmp[:, 0:NTL], op=ALU.add))
                    dr(lambda: V.tensor_scalar(out=etf[:], in0=etf[:], scalar1=15.0, scalar2=None, op0=ALU.min))
                    dr(lambda: V.tensor_scalar(out=etf[:], in0=etf[:], scalar1=128.0, scalar2=iotap[:, 0:1], op0=ALU.mult,
                                               op1=ALU.add))
                    dr(lambda: V.tensor_copy(out=idxw[:], in_=etf[:]))
                    for bi in range(NB3):
                        dr(lambda: V.tensor_tensor(out=rt[:, 0:16], in0=Rg[:, bi, :], in1=q16[:, 16:32], op=ALU.add))
                        dr(lambda: V.tensor_tensor(out=rt[:, 16:32], in0=rt[:, 0:16], in1=selA[:, bi, :], op=ALU.mult))
                        dr(lambda: V.reduce_sum(out=posf[:, 0, bi:bi + 1], in_=rt[:, 16:32], axis=AX.X))
                        dr(lambda: V.tensor_tensor(out=rt[:, 32:48], in0=rt[:, 0:16], in1=selB[:, bi, :], op=ALU.mult))
                        dr(lambda: V.reduce_sum(out=posf[:, 1, bi:bi + 1], in_=rt[:, 32:48], axis=AX.X))
                    dr(lambda: V.tensor_copy(out=posA_i[:], in_=posf[:, 0, :]))
                    dr(lambda: V.tensor_copy(out=posB_i[:], in_=posf[:, 1, :]))
                    k.barrier()

                with ExitStack() as es:
                    E = es.enter_context
                    hr = [E(sbt("hr%d" % i, [128, D], BF16)) for i in range(4)]
                    b_hr = Bs(4)
                    for bi, (seg, gb) in enumerate(blocks):
                        hi = bi % 4
                        k.dma("hr%d" % hi, hr[hi][:], Hrows[gb * 128:(gb + 1) * 128, :], W=[b_hr[hi]])
                        k.dma_ind("sc_a%d" % hi, Hs_l, bass.IndirectOffsetOnAxis(ap=posA_i[:, bi:bi + 1], axis=0), hr[hi][:], None,
                                  NSLOT, R=[b_hr[hi], b_rout])
                        k.dma_ind("sc_b%d" % hi, Hs_l, bass.IndirectOffsetOnAxis(ap=posB_i[:, bi:bi + 1], axis=0), hr[hi][:], None,
                                  NSLOT, R=[b_hr[hi], b_rout])
                    k.barrier()

                with ExitStack() as es:
                    E = es.enter_context
                    b_const = B()
                    ident = load_const(E, "ident", [128, 128], BF16, ident_d, b_const)
                    wq = [E(sbt("wq%d" % i, [128, 8192], BF16)) for i in range(6)]
                    hs = [E(sbt("hs%d" % i, [128, 2, D], BF16)) for i in range(2)]
                    hsT = [E(sbt("hsT%d" % i, [128, 16, 256], BF16)) for i in range(2)]
                    he = [E(sbt("he%d" % i, [128, 4, 256], BF16)) for i in range(2)]
                    sg = [E(sbt("sg%d" % i, [128, 256], F32)) for i in range(2)]
                    yo = [E(sbt("yo%d" % i, [128, D], F32)) for i in range(2)]
                    pT = [E(pst("pT%d" % i, [128, 1024], BF16)) for i in range(2)]
                    pg = [E(pst("pg%d" % i, [128, 512], F32)) for i in range(2)]
                    pu = [E(pst("pu%d" % i, [128, 512], F32)) for i in range(2)]
                    pd = [E(pst("pd%d" % i, [128, 512], F32)) for i in range(2)]
                    b_wq, b_hs, b_hsT, b_he, b_sg, b_yo = Bs(6), Bs(2), Bs(2), Bs(2), Bs(2), Bs(2)
                    b_pT, b_pg, b_pu, b_pd = Bs(2), Bs(2), Bs(2), Bs(2)
                    POOL.wait_tok(conv_tok["g%d" % l])
                    POOL.wait_tok(conv_tok["u%d" % l])
                    POOL.wait_tok(conv_tok["d%d" % l])
                    wsrc = [wb_g[l], wb_u[l], wb_d[l]]

                    def load_w(i):
                        for m in range(3):
                            ws = (3 * i + m) % 6
                            k.dma_ind("wq%d" % ws, wq[ws][:], None, wsrc[m],
                                      bass.IndirectOffsetOnAxis(ap=idxw[:, i:i + 1], axis=0), 2048, R=[b_rout], W=[b_wq[ws]])
                    load_w(0)
                    ji_ = 0
                    di_ = 0
                    ev_ = 0
                    for i in range(NTL):
                        sl = i % 2
                        if i + 1 < NTL:
                            load_w(i + 1)
                        k.dma("hs%d" % sl, hs[sl][:], Hs_l[i * 256:(i + 1) * 256, :].rearrange("(s p) f -> p s f", p=128),
                              W=[b_hs[sl]])
                        wgs, wus, wds = [(3 * i + m) % 6 for m in range(3)]
                        wgv = wq[wgs][:].rearrange("p (c n) -> p c n", c=16)
                        wuv = wq[wus][:].rearrange("p (c n) -> p c n", c=16)
                        wdv = wq[wds][:].rearrange("p (c n) -> p c n", c=4)
                        for c in range(16):
                            pi = c % 2
                            for sb_ in range(2):
                                k.do(PE, lambda: T.transpose(out=pT[pi][:, sb_ * 128:(sb_ + 1) * 128],
                                                             in_=hs[sl][:, sb_, c * 128:(c + 1) * 128], identity=ident[:]),
                                     R=[b_hs[sl], b_const], W=[b_pT[pi]], inc=(sb_ == 1))
                            if c % 2 == 0:
                                k.do(ACT, lambda: A.activation(out=hsT[sl][:, c, :], in_=pT[pi][:, 0:256], func=AF.Identity),
                                     R=[b_pT[pi]], W=[b_hsT[sl]])
                            else:
                                k.do(DVE, lambda: V.tensor_copy(out=hsT[sl][:, c, :], in_=pT[pi][:, 0:256]),
                                     R=[b_pT[pi]], W=[b_hsT[sl]])
                        for j in range(4):
                            pi = ji_ % 2
                            ji_ += 1
                            for c in range(16):
                                k.do(PE, lambda: T.matmul(pg[pi][:, 0:256], lhsT=wgv[:, c, j * 128:(j + 1) * 128],
                                                          rhs=hsT[sl][:, c, :], start=(c == 0), stop=(c == 15)),
                                     R=[b_wq[wgs], b_hsT[sl]], W=[b_pg[pi]], inc=(c == 15))
                            for c in range(16):
                                k.do(PE, lambda: T.matmul(pu[pi][:, 0:256], lhsT=wuv[:, c, j * 128:(j + 1) * 128],
                                                          rhs=hsT[sl][:, c, :], start=(c == 0), stop=(c == 15)),
                                     R=[b_wq[wus], b_hsT[sl]], W=[b_pu[pi]], inc=(c == 15))
                            k.do(ACT, lambda: A.activation(out=sg[pi][:], in_=pg[pi][:, 0:256], func=AF.Silu),
                                 R=[b_pg[pi]], W=[b_sg[pi]])
                            k.do(DVE, lambda: V.tensor_tensor(out=he[sl][:, j, :], in0=sg[pi][:], in1=pu[pi][:, 0:256],
                                                              op=ALU.mult), R=[b_sg[pi], b_pu[pi]], W=[b_he[sl]])
                        for sb_ in range(2):
                            yi = (2 * i + sb_) % 2
                            for ft in range(4):
                                pi = di_ % 2
                                di_ += 1
                                for j in range(4):
                                    k.do(PE, lambda: T.matmul(pd[pi][:], lhsT=he[sl][:, j, sb_ * 128:(sb_ + 1) * 128],
                                                              rhs=wdv[:, j, ft * 512:(ft + 1) * 512], start=(j == 0),
                                                              stop=(j == 3)),
                                         R=[b_he[sl], b_wq[wds]], W=[b_pd[pi]], inc=(j == 3))
                                ev_ += 1
                                if ev_ % 2 == 0:
                                    k.do(ACT, lambda: A.activation(out=yo[yi][:, ft * 512:(ft + 1) * 512], in_=pd[pi][:],
                                                                   func=AF.Identity), R=[b_pd[pi]], W=[b_yo[yi]])
                                else:
                                    k.do(DVE, lambda: V.tensor_copy(out=yo[yi][:, ft * 512:(ft + 1) * 512], in_=pd[pi][:]),
                                         R=[b_pd[pi]], W=[b_yo[yi]])
                            r0 = (2 * i + sb_) * 128
                            k.dma("st_yo%d" % yi, Ys_l[r0:r0 + 128, :], yo[yi][:], R=[b_yo[yi]])
                    k.barrier()

                with ExitStack() as es:
                    E = es.enter_context
                    b_const = B()
                    gf = E(sbt("gf", [128, 2, D], F32))
                    for s in range(2):
                        k.dma("c_gf", gf[:, s, :], mod_d[l, s:s + 1, 5 * D:6 * D].partition_broadcast(128), W=[b_const])
                    if l == 1:
                        fg = load_const(E, "fg", [128, D], F32, fng_d.partition_broadcast(128), b_const)
                        junk = E(sbt("junk", [128, D], BF16))
                        ss = E(sbt("ss", [128, 12], F32))
                        b_junk, b_ss = B(), B()
                    xs = [E(sbt("xs%d" % i, [128, D], F32)) for i in range(2)]
                    yA = [E(sbt("yA%d" % i, [128, D], F32)) for i in range(2)]
                    yB = [E(sbt("yB%d" % i, [128, D], F32)) for i in range(2)]
                    tt_ = [E(sbt("tt%d" % i, [128, D], F32)) for i in range(2)]
                    xo = [E(sbt("xo%d" % i, [128, D], F32)) for i in range(2)]
                    b_xs, b_yA, b_yB, b_tt, b_xo = Bs(2), Bs(2), Bs(2), Bs(2), Bs(2)
                    for bi, (seg, gb) in enumerate(blocks):
                        i2 = bi % 2
                        r0 = gb * 128
                        k.dma("xs%d" % i2, xs[i2][:], x_mid[r0:r0 + 128, :], W=[b_xs[i2]])
                        k.dma_ind("ga%d" % i2, yA[i2][:], None, Ys_l, bass.IndirectOffsetOnAxis(ap=posA_i[:, bi:bi + 1], axis=0),
                                  NSLOT, R=[b_rout], W=[b_yA[i2]])
                        k.dma_ind("gb%d" % i2, yB[i2][:], None, Ys_l, bass.IndirectOffsetOnAxis(ap=posB_i[:, bi:bi + 1], axis=0),
                                  NSLOT, R=[b_rout], W=[b_yB[i2]])
                        k.do(DVE, lambda: V.tensor_scalar(out=tt_[i2][:], in0=yA[i2][:], scalar1=cA[:, bi:bi + 1], scalar2=None,
                                                          op0=ALU.mult), R=[b_yA[i2], b_rout], W=[b_tt[i2]])
                        k.do(DVE, lambda: V.scalar_tensor_tensor(out=tt_[i2][:], in0=yB[i2][:], scalar=cB[:, bi:bi + 1],
                                                                 in1=tt_[i2][:], op0=ALU.mult, op1=ALU.add),
                             R=[b_yB[i2], b_rout, b_tt[i2]], W=[b_tt[i2]])
                        k.do(POOL, lambda: G.tensor_tensor(out=tt_[i2][:], in0=tt_[i2][:], in1=gf[:, seg, :], op=ALU.mult),
                             R=[b_tt[i2], b_const], W=[b_tt[i2]])
                        k.do(DVE, lambda: V.tensor_tensor(out=xo[i2][:], in0=tt_[i2][:], in1=xs[i2][:], op=ALU.add),
                             R=[b_tt[i2], b_xs[i2]], W=[b_xo[i2]])
                        if l == 0:
                            k.dma("st_xo%d" % i2, x_dst[r0:r0 + 128, :], xo[i2][:], R=[b_xo[i2]])
                        else:
                            k.do(DVE, lambda: V.memset(ss[:, 0:1], 0.0), W=[b_ss])
                            k.do(ACT, lambda: A.activation(out=junk[:], in_=xo[i2][:], func=AF.Square, accum_out=ss[:, 0:1]),
                                 R=[b_xo[i2], b_ss], W=[b_junk, b_ss])
                            k.do(ACT, lambda: A.activation(out=ss[:, 4:5], in_=ss[:, 0:1], func=AF.Sqrt, scale=1.0 / D,
                                                           bias=EPS), R=[b_ss], W=[b_ss])
                            k.do(DVE, lambda: V.reciprocal(out=ss[:, 8:9], in_=ss[:, 4:5]), R=[b_ss], W=[b_ss])
                            k.do(DVE, lambda: V.scalar_tensor_tensor(out=xo[i2][:], in0=xo[i2][:], scalar=ss[:, 8:9], in1=fg[:],
                                                                     op0=ALU.mult, op1=ALU.mult),
                                 R=[b_xo[i2], b_ss, b_const], W=[b_xo[i2]])
                            orow = (gb - 2) * 128 if seg == 0 else 2048 + (gb - 22) * 128
                            k.dma("st_y%d" % i2, y_d[orow:orow + 128, :], xo[i2][:], R=[b_xo[i2]])
                    k.barrier(final=(l == 1))
    return nc


_NC_CACHE = {}


def _alibi_bias():
    slopes = 2.0 ** (-8.0 * np.arange(1, 9, dtype=np.float64) / 8.0)
    q = np.arange(128)[:, None]
    s = np.arange(384)[None, :]
    dist = np.abs(s - 128 - q)
    out = np.empty((128, 8, 384), np.float32)
    for h in range(8):
        out[:, h, :] = np.where(dist <= 128, -slopes[h] * dist, NEG)
    return out


def kernel(x_prompt, x_sample, c_prompt, c_sample, norm_mix_g, norm_ffn_g, w_ada, b_ada, w_in, w_out,
           conv_a_w, conv_a_b, ln_a_g, ln_a_b, attn_sink, conv_c_w, w_router_group, b_router_group,
           w_router_expert, b_router_expert, w_gate, w_up, w_down, final_norm_g):
    f = lambda a: np.ascontiguousarray(np.asarray(a, dtype=np.float32))
    x_prompt, x_sample, c_prompt, c_sample = f(x_prompt), f(x_sample), f(c_prompt), f(c_sample)
    if "nc" not in _NC_CACHE:
        _NC_CACHE["nc"] = build_program()
    nc = _NC_CACHE["nc"]

    caw = f(np.transpose(f(conv_a_w).reshape(2, 31, 4, 128), (0, 3, 2, 1)))
    cab = f(np.transpose(f(conv_a_b).reshape(2, 4, 128), (0, 2, 1)))
    lng = f(np.transpose(f(ln_a_g).reshape(2, 4, 128), (0, 2, 1)))
    lnb = f(np.transpose(f(ln_a_b).reshape(2, 4, 128), (0, 2, 1)))
    ccw = f(np.transpose(f(conv_c_w).reshape(2, 3, 4, 128), (0, 3, 2, 1)))
    wr = np.concatenate([f(w_router_group), f(w_router_expert)], axis=-1)
    wr = f(np.transpose(wr.reshape(2, 16, 128, 20), (0, 2, 1, 3)))
    br = f(np.concatenate([f(b_router_group), f(b_router_expert)], axis=-1).reshape(2, 1, 20))
    shared = dict(
        ident=np.eye(128, dtype=np.float32).astype(ml_dtypes.bfloat16),
        ones=np.full((128, 128), 1.0 / 512, np.float32).astype(ml_dtypes.bfloat16),
        biasmat=_alibi_bias(),
        utri=np.triu(np.ones((128, 128), np.float32), 1).astype(ml_dtypes.bfloat16),
        ones1=np.ones((128, 128), np.float32).astype(ml_dtypes.bfloat16),
        thr=np.tile((256.0 * np.arange(80, dtype=np.float32))[None], (128, 1)),
        iotap=np.arange(128, dtype=np.float32).reshape(128, 1),
        norm_mix_g=f(norm_mix_g), norm_ffn_g=f(norm_ffn_g), w_ada=f(w_ada), b_ada=f(b_ada), w_in=f(w_in),
        w_out=f(w_out), caw=caw, cab=cab, lng=lng, lnb=lnb, sink=f(attn_sink).reshape(2, 1, 8), ccw=ccw, wr=wr, br=br,
        w_gate=f(w_gate), w_up=f(w_up), w_down=f(w_down), final_norm_g=f(final_norm_g).reshape(1, D),
    )
    in_maps = []
    for c in range(NCORES):
        sb, half = c // 2, c % 2
        xl = np.zeros((NT, D), np.float32)
        lo, hi = 2048 * c - 256, 2048 * c + 2048 + 256
        a, b = max(lo, 0), min(hi, 16384)
        xl[a - lo:b - lo] = x_prompt[0, a:b]
        lo, hi = 4096 * half - 256, 4096 * half + 4096 + 256
        a, b = max(lo, 0), min(hi, 8192)
        xl[2560 + a - lo:2560 + b - lo] = x_sample[sb, a:b]
        fl = np.array([c > 0, c < 7, half == 1, half == 0], np.float32)
        flags = np.zeros((128, 8), np.float32)
        flags[:, 0:4] = fl[None]
        flags[:, 4:8] = np.where(fl > 0, 0.0, NEG)[None]
        cc = np.stack([c_prompt[0], c_sample[sb]], axis=-1)
        cT = f(np.transpose(cc.reshape(16, 128, 2), (1, 0, 2)))
        in_maps.append(dict(shared, x_local=xl, flags=flags, cT=cT))

    res = run_bass_kernel_spmd(nc, in_maps, core_ids=list(range(NCORES)))
    y_prompt = np.empty((1, 16384, D), np.float32)
    y_sample = np.empty((4, 8192, D), np.float32)
    for c in range(NCORES):
        y = res.results[c]["y_local"]
        sb, half = c // 2, c % 2
        y_prompt[0, 2048 * c:2048 * c + 2048] = y[0:2048]
        y_sample[sb, 4096 * half:4096 * half + 4096] = y[2048:6144]
    return (y_prompt, y_sample)
```

```python
import numpy as np
import ml_dtypes
from contextlib import ExitStack
import concourse.bass as bass
import concourse.mybir as mybir
from concourse.bass_utils import run_bass_kernel_spmd

F32 = mybir.dt.float32
BF16 = mybir.dt.bfloat16
I32 = mybir.dt.int32
AF = mybir.ActivationFunctionType
ALU = mybir.AluOpType
AX = mybir.AxisListType

D = 2048
NCORES = 8
SEGS = [(0, 20), (20, 36)]
NBLK = 56
NT = NBLK * 128
NOWN = 6144
EPS = 1e-6
NEG = -1e30
SCALE = 128 ** -0.5


class Eng:
    def __init__(self, e, name, sem):
        self.e, self.name, self.sem, self.n, self.seen = e, name, sem, 0, {}

    def wait_tok(self, tok):
        if tok is None:
            return
        _, sem, cnt = tok
        key = id(sem)
        if self.seen.get(key, 0) >= cnt:
            return
        self.e.wait_ge(sem, cnt)
        self.seen[key] = cnt


class B:
    def __init__(self):
        self.w, self.r = {}, {}

    def read(self, eng):
        for t in self.w.values():
            if t[0] == "PE" and eng.name == "PE":
                continue
            eng.wait_tok(t)

    def write(self, eng):
        for k, t in self.r.items():
            if k != eng.name:
                eng.wait_tok(t)
        for k, t in self.w.items():
            if k != eng.name:
                eng.wait_tok(t)

    def did_read(self, tok):
        k = tok[0]
        if k not in self.r or self.r[k][2] < tok[2]:
            self.r[k] = tok

    def did_write(self, tok):
        self.w = {tok[0]: tok}
        self.r = {}


class K:
    def __init__(self, nc, es):
        self.nc = nc
        E = es.enter_context
        self.PE = Eng(nc.tensor, "PE", E(nc.semaphore("sem_pe")))
        self.ACT = Eng(nc.scalar, "ACT", E(nc.semaphore("sem_act")))
        self.DVE = Eng(nc.vector, "DVE", E(nc.semaphore("sem_dve")))
        self.POOL = Eng(nc.gpsimd, "POOL", E(nc.semaphore("sem_pool")))
        self.SP = Eng(nc.sync, "SP", E(nc.semaphore("sem_sp")))
        self.es = es
        self.dsem = {}
        self.slot_of = {}
        self.sem_pool = []
        self.nsem = 0

    def do(self, eng, fn, R=(), W=(), inc=True):
        for b in R:
            b.read(eng)
        for b in W:
            b.write(eng)
        ins = fn()
        if inc:
            ins.then_inc(eng.sem, 1)
            eng.n += 1
            tok = (eng.name, eng.sem, eng.n)
        else:
            tok = (eng.name, eng.sem, eng.n + 1)
        for b in R:
            b.did_read(tok)
        for b in W:
            b.did_write(tok)
        return tok

    def dma(self, slot, out, in_, R=(), W=(), q=None):
        q = q or self.SP
        if slot not in self.dsem:
            self.dsem[slot] = self.new_sem(slot)
        s = self.dsem[slot]
        for b in R:
            b.read(q)
        for b in W:
            b.write(q)
        q.e.dma_start(out=out, in_=in_).then_inc(s[0], 16)
        s[1] += 16
        tok = ("dma:" + slot, s[0], s[1])
        for b in R:
            b.did_read(tok)
        for b in W:
            b.did_write(tok)
        return tok

    def new_sem(self, slot):
        if self.sem_pool and not slot.startswith("cv_"):
            return self.sem_pool.pop()
        self.nsem += 1
        return [self.es.enter_context(self.nc.semaphore("dq%d" % self.nsem)), 0]

    def dma_ind(self, slot, out, out_off, in_, in_off, nrows, R=(), W=()):
        q = self.POOL
        if slot not in self.dsem:
            self.dsem[slot] = self.new_sem(slot)
        s = self.dsem[slot]
        for b in R:
            b.read(q)
        for b in W:
            b.write(q)
        q.e.indirect_dma_start(out=out, out_offset=out_off, in_=in_, in_offset=in_off).then_inc(s[0], 16)
        s[1] += 16
        tok = ("dma:" + slot, s[0], s[1])
        for b in R:
            b.did_read(tok)
        for b in W:
            b.did_write(tok)
        return tok

    def all_tokens(self, final=False):
        toks = []
        for e in (self.PE, self.ACT, self.DVE, self.POOL):
            if e.n:
                toks.append((e.name, e.sem, e.n))
        for name, s in self.dsem.items():
            if s[1] and (final or not name.startswith("cv_")):
                toks.append(("dma:" + name, s[0], s[1]))
        return toks

    def barrier(self, final=False):
        toks = self.all_tokens(final)
        for e in (self.PE, self.ACT, self.DVE, self.POOL, self.SP):
            for t in toks:
                if t[0] != e.name:
                    e.wait_tok(t)
        for name in list(self.dsem.keys()):
            if not name.startswith("cv_"):
                self.sem_pool.append(self.dsem.pop(name))


def Bs(n):
    return [B() for _ in range(n)]


def build_program():
    nc = bass.Bass("TRN2", target_bir_lowering=False)

    _uid = [0]

    def sbt(name, shape, dt):
        _uid[0] += 1
        return nc.sbuf_tensor("sb%d_%s" % (_uid[0], name), shape, dt)

    def pst(name, shape, dt):
        _uid[0] += 1
        return nc.psum_tensor("ps%d_%s" % (_uid[0], name), shape, dt)

    def din(name, shape, dt=F32):
        return nc.dram_tensor(name, list(shape), dt, kind="ExternalInput").ap()

    def dscr(name, shape, dt):
        return nc.dram_tensor(name, list(shape), dt, kind="Internal").ap()

    x_local = din("x_local", [NT, D])
    flags_d = din("flags", [128, 8])
    cT_d = din("cT", [128, 16, 2])
    ident_d = din("ident", [128, 128], BF16)
    ones_d = din("ones", [128, 128], BF16)
    bias_d = din("biasmat", [128, 8, 384])
    nmg_d = din("norm_mix_g", [2, D])
    nfg_d = din("norm_ffn_g", [2, D])
    wada_d = din("w_ada", [2, D, 6 * D])
    bada_d = din("b_ada", [2, 6 * D])
    win_d = din("w_in", [2, D, 4096])
    wout_d = din("w_out", [2, D, D])
    caw_d = din("caw", [2, 128, 4, 31])
    cab_d = din("cab", [2, 128, 4])
    lng_d = din("lng", [2, 128, 4])
    lnb_d = din("lnb", [2, 128, 4])
    sink_d = din("sink", [2, 1, 8])
    ccw_d = din("ccw", [2, 128, 4, 3])
    wr_d = din("wr", [2, 128, 16, 20])
    br_d = din("br", [2, 1, 20])
    wg_d = din("w_gate", [2, 16, D, 512])
    wu_d = din("w_up", [2, 16, D, 512])
    wd_d = din("w_down", [2, 16, 512, D])
    fng_d = din("final_norm_g", [1, D])
    utri_d = din("utri", [128, 128], BF16)
    ones1_d = din("ones1", [128, 128], BF16)
    thr_d = din("thr", [128, 80])
    iotap_d = din("iotap", [128, 1])
    y_d = nc.dram_tensor("y_local", [NOWN, D], F32, kind="ExternalOutput").ap()

    wb_in = dscr("wb_in", [2, D, 4096], BF16)
    wb_out = dscr("wb_out", [2, D, D], BF16)
    wb_g = [dscr("wb_g%d" % l, [2048, 8192], BF16) for l in range(2)]
    wb_u = [dscr("wb_u%d" % l, [2048, 8192], BF16) for l in range(2)]
    wb_d = [dscr("wb_d%d" % l, [2048, 8192], BF16) for l in range(2)]
    Hrows = dscr("Hrows", [NT, D], BF16)
    Hslots = dscr("Hslots", [68 * 256, D], BF16)
    Yslots = dscr("Yslots", [68 * 256, D], F32)
    mod_d = dscr("mod_d", [2, 2, 6 * D], F32)
    xa = dscr("xa", [NT, D], F32)
    xb = dscr("xb", [NT, D], F32)
    aT_d = dscr("aT", [4, 128, NT], BF16)
    qT_d = dscr("qT", [8, 128, NT], BF16)
    kT_d = dscr("kT", [2, 128, NT], BF16)
    v_d = dscr("v", [NT, 256], BF16)
    uT_d = dscr("uT", [4, 128, NT], F32)
    cbT_d = dscr("cbT", [4, 128, NT], F32)
    mix_d = dscr("mixT", [16, 128, NT], BF16)

    with ExitStack() as es_all:
        k = K(nc, es_all)
        PE, ACT, DVE, POOL, SP = k.PE, k.ACT, k.DVE, k.POOL, k.SP
        T, A, V, G = nc.tensor, nc.scalar, nc.vector, nc.gpsimd

        conv_tok = {}

        def cast(name, dst, src):
            conv_tok[name] = k.dma("cv_" + name, dst, src, q=POOL)

        for l in range(2):
            for i in range(8):
                cast("in%d" % l, wb_in[l, i * 256:(i + 1) * 256, :], win_d[l, i * 256:(i + 1) * 256, :])
            for i in range(4):
                cast("out%d" % l, wb_out[l, i * 512:(i + 1) * 512, :], wout_d[l, i * 512:(i + 1) * 512, :])
            for e in range(16):
                cast("g%d" % l, wb_g[l][e * 128:(e + 1) * 128, :].rearrange("p (c n) -> p c n", c=16),
                     wg_d[l, e].rearrange("(c p) n -> p c n", p=128))
                cast("u%d" % l, wb_u[l][e * 128:(e + 1) * 128, :].rearrange("p (c n) -> p c n", c=16),
                     wu_d[l, e].rearrange("(c p) n -> p c n", p=128))
                cast("d%d" % l, wb_d[l][e * 128:(e + 1) * 128, :].rearrange("p (c n) -> p c n", c=4),
                     wd_d[l, e].rearrange("(c p) n -> p c n", p=128))

        with ExitStack() as es:
            E = es.enter_context
            siluT = E(sbt("siluT", [128, 16, 2], F32))
            wt = [E(sbt("wadat%d" % i, [128, 16, 512], F32)) for i in range(2)]
            modrow = E(sbt("modrow", [2, 6 * D], F32))
            badar = E(sbt("badar", [2, 6 * D], F32))
            grow = E(sbt("grow", [2, 2, D], F32))
            ps = [E(pst("pps%d" % i, [128, 512], F32)) for i in range(2)]
            b_silu, b_mod, b_bada, b_grow = B(), B(), B(), B()
            b_wt, b_ps = Bs(2), Bs(2)
            k.dma("siluT", siluT[:], cT_d, W=[b_silu])
            k.do(ACT, lambda: A.activation(out=siluT[:], in_=siluT[:], func=AF.Silu), R=[b_silu], W=[b_silu])
            it = 0
            for l in range(2):
                k.dma("bada", badar[:], bada_d[l:l + 1, :].partition_broadcast(2), W=[b_bada])
                k.dma("grow", grow[:, 0, :], nmg_d[l:l + 1, :].partition_broadcast(2), W=[b_grow])
                k.dma("grow", grow[:, 1, :], nfg_d[l:l + 1, :].partition_broadcast(2), W=[b_grow])
                for j in range(24):
                    s = it % 2
                    it += 1
                    k.dma("wadat%d" % s, wt[s][:],
                          wada_d[l, :, j * 512:(j + 1) * 512].rearrange("(c p) n -> p c n", p=128), W=[b_wt[s]])
                    for c in range(16):
                        k.do(PE, lambda: T.matmul(ps[s][0:2, :], lhsT=siluT[:, c, :], rhs=wt[s][:, c, :],
                                                  start=(c == 0), stop=(c == 15)),
                             R=[b_silu, b_wt[s]], W=[b_ps[s]], inc=(c == 15))
                    k.do(DVE, lambda: V.tensor_tensor(out=modrow[:, j * 512:(j + 1) * 512], in0=ps[s][0:2, :],
                                                      in1=badar[:, j * 512:(j + 1) * 512], op=ALU.add),
                         R=[b_ps[s], b_bada], W=[b_mod])
                for (off, gi) in ((D, 0), (4 * D, 1)):
                    k.do(DVE, lambda: V.scalar_tensor_tensor(out=modrow[:, off:off + D], in0=modrow[:, off:off + D],
                                                             scalar=1.0, in1=grow[:, gi, :], op0=ALU.add,
                                                             op1=ALU.mult), R=[b_mod, b_grow], W=[b_mod])
                k.dma("modout", mod_d[l], modrow[:], R=[b_mod], q=POOL)
            k.barrier()

        def load_const(E, name, shape, dt, src, b):
            t = E(sbt(name, shape, dt))
            k.dma("c_" + name, t[:], src, W=[b])
            return t

        def seg_of_block(gb):
            return 0 if gb < 20 else 1

        def tiles_full(l):
            res = []
            for s, (bs, nbk) in enumerate(SEGS):
                lo, hi = (1, nbk - 1) if l == 0 else (2, nbk - 2)
                b = lo
                while b < hi:
                    nb = min(4, hi - b)
                    res.append((s, bs + b, nb))
                    b += nb
            return res

        def load_fm_mod(E, name, l, off, b):
            t = E(sbt(name, [128, 2, 16], F32))
            with nc.allow_non_contiguous_dma(reason="tiny feature-major load of a modulation vector"):
                for s in range(2):
                    k.dma("c_" + name, t[:, s, :], mod_d[l, s, off:off + D].rearrange("(c p) -> p c", p=128), W=[b])
            return t

        def norm_transpose(x_src, gb0, nb, seg, xs, b_xs, xsi, junk, b_junk, ss, b_ss, xh, b_xh, pT, b_pT,
                           hT, b_hT, afm, bfm, b_const, ident):
            n = nb * 128
            k.do(DVE, lambda: V.memset(ss[:], 0.0), W=[b_ss])
            for b in range(nb):
                si = xsi[0] % len(xs)
                xsi[0] += 1
                k.dma("xs%d" % si, xs[si][:], x_src[(gb0 + b) * 128:(gb0 + b + 1) * 128, :], W=[b_xs[si]])
                k.do(ACT, lambda: A.activation(out=junk[:], in_=xs[si][:], func=AF.Square,
                                               accum_out=ss[:, b:b + 1]), R=[b_xs[si], b_ss], W=[b_junk, b_ss])
                k.do(ACT, lambda: A.activation(out=ss[:, 4 + b:5 + b], in_=ss[:, b:b + 1], func=AF.Sqrt,
                                               scale=1.0 / D, bias=EPS), R=[b_ss], W=[b_ss])
                k.do(DVE, lambda: V.reciprocal(out=ss[:, 8 + b:9 + b], in_=ss[:, 4 + b:5 + b]), R=[b_ss], W=[b_ss])
                k.do(DVE, lambda: V.tensor_scalar(out=xh[:, b, :], in0=xs[si][:], scalar1=ss[:, 8 + b:9 + b],
                                                  scalar2=None, op0=ALU.mult), R=[b_xs[si], b_ss], W=[b_xh])
            for c in range(16):
                pi = c % 2
                for b in range(nb):
                    k.do(PE, lambda: T.transpose(out=pT[pi][:, b * 128:(b + 1) * 128],
                                                 in_=xh[:, b, c * 128:(c + 1) * 128], identity=ident[:]),
                         R=[b_xh, b_const], W=[b_pT[pi]], inc=(b == nb - 1))
                k.do(ACT, lambda: A.activation(out=hT[:, c, 0:n], in_=pT[pi][:, 0:n], func=AF.Identity,
                                               scale=afm[:, seg, c:c + 1], bias=bfm[:, seg, c:c + 1]),
                     R=[b_pT[pi], b_const], W=[b_hT])

        for l in range(2):
            x_src1 = x_local if l == 0 else xb
            x_mid = xa
            x_dst = xb

            with ExitStack() as es:
                E = es.enter_context
                b_const = B()
                ident = load_const(E, "ident", [128, 128], BF16, ident_d, b_const)
                flg = load_const(E, "flg", [128, 8], F32, flags_d, b_const)
                afm = load_fm_mod(E, "afm", l, D, b_const)
                bfm = load_fm_mod(E, "bfm", l, 0, b_const)
                xs = [E(sbt("xs%d" % i, [128, D], F32)) for i in range(2)]
                junk = E(sbt("junk", [128, D], BF16))
                ss = E(sbt("ss", [128, 12], F32))
                xh = [E(sbt("xh%d" % i, [128, 4, D], BF16)) for i in range(2)]
                hT = [E(sbt("hT%d" % i, [128, 16, 512], BF16)) for i in range(2)]
                wt = [E(sbt("wt%d" % i, [128, 16, 512], BF16)) for i in range(3)]
                sig = E(sbt("sig", [128, 4, 512], F32))
                o16 = [E(sbt("o16_%d" % i, [128, 4, 512], BF16)) for i in range(3)]
                o32 = [E(sbt("o32_%d" % i, [128, 4, 512], F32)) for i in range(2)]
                vo = E(sbt("vo", [128, 4, 256], BF16))
                pT = [E(pst("pT%d" % i, [128, 1024], BF16)) for i in range(2)]
                pz = [E(pst("pz%d" % i, [128, 512], F32)) for i in range(4)]
                b_xs, b_xh, b_hT, b_wt, b_pT, b_pz = Bs(2), Bs(2), Bs(2), Bs(3), Bs(2), Bs(4)
                b_junk, b_ss, b_sig, b_vo = B(), B(), B(), B()
                b_o16, b_o32 = Bs(3), Bs(2)
                xsi = [0]
                wi = 0
                pzi = 0
                o16i = 0
                o32i = 0
                first_w = True
                for t in range(14):
                    seg = 0 if t < 5 else 1
                    sl = t % 2
                    tok0 = t * 512
                    halo = None
                    if t in (0, 5):
                        halo = (0, 256, 0 if t == 0 else 2)
                    if t in (4, 13):
                        halo = (256, 512, 1 if t == 4 else 3)
                    norm_transpose(x_src1, t * 4, 4, seg, xs, b_xs, xsi, junk, b_junk, ss, b_ss, xh[sl], b_xh[sl],
                                   pT, b_pT, hT[sl], b_hT[sl], afm, bfm, b_const, ident)

                    def mask_halo(buf, bb, ngrp):
                        if halo is None:
                            return
                        lo, hi, fi = halo
                        k.do(POOL, lambda: G.tensor_scalar(out=buf[:, 0:ngrp, lo:hi], in0=buf[:, 0:ngrp, lo:hi],
                                                           scalar1=flg[:, fi:fi + 1], scalar2=None, op0=ALU.mult),
                             R=[bb, b_const], W=[bb])

                    for g in (1, 0, 2, 3, 4, 7, 5, 6):
                        ws = wi % 3
                        wi += 1
                        if first_w:
                            SP.wait_tok(conv_tok["in%d" % l])
                            first_w = False
                        k.dma("wt%d" % ws, wt[ws][:],
                              wb_in[l, :, g * 512:(g + 1) * 512].rearrange("(c p) n -> p c n", p=128), W=[b_wt[ws]])
                        nfm = 2 if g == 4 else 4
                        if g in (0, 2, 3, 4):
                            oi = o16i % 3
                            o16i += 1
                            ob, bo = o16[oi], b_o16[oi]
                        elif g in (5, 6):
                            oi = o32i % 2
                            o32i += 1
                            ob, bo = o32[oi], b_o32[oi]
                        for j in range(nfm):
                            p = pzi % 4
                            pzi += 1
                            for c in range(16):
                                k.do(PE, lambda: T.matmul(pz[p][:], lhsT=wt[ws][:, c, j * 128:(j + 1) * 128],
                                                          rhs=hT[sl][:, c, :], start=(c == 0), stop=(c == 15)),
                                     R=[b_wt[ws], b_hT[sl]], W=[b_pz[p]], inc=(c == 15))
                            if g == 1:
                                k.do(ACT, lambda: A.activation(out=sig[:, j, :], in_=pz[p][:], func=AF.Sigmoid),
                                     R=[b_pz[p]], W=[b_sig])
                            elif g == 0:
                                k.do(DVE, lambda: V.tensor_tensor(out=ob[:, j, :], in0=pz[p][:], in1=sig[:, j, :],
                                                                  op=ALU.mult), R=[b_pz[p], b_sig], W=[bo])
                            elif g in (2, 3, 4):
                                k.do(ACT, lambda: A.activation(out=ob[:, j, :], in_=pz[p][:], func=AF.Identity),
                                     R=[b_pz[p]], W=[bo])
                            elif g == 7:
                                k.do(ACT, lambda: A.activation(out=sig[:, j, :], in_=pz[p][:], func=AF.Identity),
                                     R=[b_pz[p]], W=[b_sig])
                            elif g == 5:
                                k.do(DVE, lambda: V.tensor_tensor(out=ob[:, j, :], in0=pz[p][:], in1=sig[:, j, :],
                                                                  op=ALU.mult), R=[b_pz[p], b_sig], W=[bo])
                            elif g == 6:
                                k.do(ACT, lambda: A.activation(out=ob[:, j, :], in_=pz[p][:], func=AF.Identity),
                                     R=[b_pz[p]], W=[bo])
                        if g == 4:
                            for b in range(4):
                                p = pzi % 4
                                pzi += 1
                                for c in range(16):
                                    k.do(PE, lambda: T.matmul(pz[p][:, 0:256], lhsT=hT[sl][:, c, b * 128:(b + 1) * 128],
                                                              rhs=wt[ws][:, c, 256:512], start=(c == 0),
                                                              stop=(c == 15)),
                                         R=[b_wt[ws], b_hT[sl]], W=[b_pz[p]], inc=(c == 15))
                                k.do(DVE, lambda: V.tensor_copy(out=vo[:, b, :], in_=pz[p][:, 0:256]),
                                     R=[b_pz[p]], W=[b_vo])
                            if halo is not None:
                                lo, hi, fi = halo
                                k.do(POOL, lambda: G.tensor_scalar(out=vo[:, lo // 128:hi // 128, :],
                                                                   in0=vo[:, lo // 128:hi // 128, :],
                                                                   scalar1=flg[:, fi:fi + 1], scalar2=None,
                                                                   op0=ALU.mult), R=[b_vo, b_const], W=[b_vo])
                            k.dma("st_v", v_d[tok0:tok0 + 512, :].rearrange("(b p) d -> p b d", p=128), vo[:],
                                  R=[b_vo], q=POOL)
                        if g == 0:
                            mask_halo(ob, bo, 4)
                            k.dma("st_a", aT_d[:, :, tok0:tok0 + 512].rearrange("g p t -> p g t"), ob[:], R=[bo], q=POOL)
                        elif g in (2, 3):
                            h0 = (g - 2) * 4
                            k.dma("st_q%d" % g, qT_d[h0:h0 + 4, :, tok0:tok0 + 512].rearrange("g p t -> p g t"), ob[:],
                                  R=[bo], q=POOL)
                        elif g == 4:
                            mask_halo(ob, bo, 2)
                            k.dma("st_k", kT_d[:, :, tok0:tok0 + 512].rearrange("g p t -> p g t"), ob[:, 0:2, :],
                                  R=[bo], q=POOL)
                        elif g == 5:
                            mask_halo(ob, bo, 4)
                            k.dma("st_u", uT_d[:, :, tok0:tok0 + 512].rearrange("g p t -> p g t"), ob[:], R=[bo], q=POOL)
                        elif g == 6:
                            k.dma("st_cb", cbT_d[:, :, tok0:tok0 + 512].rearrange("g p t -> p g t"), ob[:], R=[bo],
                                  q=POOL)
                k.barrier()

            tl = tiles_full(l)
            with ExitStack() as es:
                E = es.enter_context
                b_const = B()
                ident = load_const(E, "ident", [128, 128], BF16, ident_d, b_const)
                onesb = load_const(E, "onesb", [128, 128], BF16, ones_d, b_const)
                flg = load_const(E, "flg", [128, 8], F32, flags_d, b_const)
                bias8 = load_const(E, "bias8", [128, 8, 384], F32, bias_d, b_const)
                caw = load_const(E, "caw", [128, 4, 31], F32, caw_d[l], b_const)
                cab = load_const(E, "cab", [128, 4], F32, cab_d[l], b_const)
                lng = load_const(E, "lng", [128, 4], F32, lng_d[l], b_const)
                lnb = load_const(E, "lnb", [128, 4], F32, lnb_d[l], b_const)
                ccw = load_const(E, "ccw", [128, 4, 3], F32, ccw_d[l], b_const)
                sinkb = load_const(E, "sinkb", [128, 8], F32, sink_d[l].partition_broadcast(128), b_const)
                Dg = E(sbt("Dg", [128, 31, 4, 128], BF16))
                b_Dg = B()
                for kk in range(31):
                    for g in range(4):
                        k.do(DVE, lambda: V.tensor_scalar(out=Dg[:, kk, g, :], in0=ident[:],
                                                          scalar1=caw[:, g, kk:kk + 1], scalar2=None, op0=ALU.mult),
                             R=[b_const], W=[b_Dg])
                a_sb = [E(sbt("a_sb%d" % i, [128, 4, 542], BF16)) for i in range(2)]
                q_sb = [E(sbt("q_sb%d" % i, [128, 8, 512], BF16)) for i in range(2)]
                k_sb = [E(sbt("k_sb%d" % i, [128, 2, 768], BF16)) for i in range(2)]
                v_sb = [E(sbt("v_sb%d" % i, [128, 6, 256], BF16)) for i in range(2)]
                u_sb = [E(sbt("u_sb%d" % i, [128, 4, 514], F32)) for i in range(2)]
                cb_sb = [E(sbt("cb_sb%d" % i, [128, 4, 512], F32)) for i in range(2)]
                mixT = [E(sbt("mixT%d" % i, [128, 16, 512], BF16)) for i in range(2)]
                cv = E(sbt("cv", [128, 4, 512], F32))
                cvb = E(sbt("cvb", [128, 4, 512], BF16))
                cv2 = E(sbt("cv2", [128, 4, 512], BF16))
                mean_sb = E(sbt("mean_sb", [128, 512], F32))
                var_sb = E(sbt("var_sb", [128, 512], F32))
                rstd_sb = E(sbt("rstd_sb", [128, 512], F32))
                t1 = [E(sbt("t1_%d" % i, [128, 512], F32)) for i in range(2)]
                s_sb = [E(sbt("s_sb%d" % i, [128, 384], F32)) for i in range(2)]
                p_sb = [E(sbt("p_sb%d" % i, [128, 384], F32)) for i in range(2)]
                pn_sb = [E(sbt("pn_sb%d" % i, [128, 384], BF16)) for i in range(2)]
                pT_sb = [E(sbt("pT_sb%d" % i, [128, 384], BF16)) for i in range(2)]
                st = [E(sbt("st%d" % i, [128, 8], F32)) for i in range(2)]
                acc = [E(sbt("acc%d" % i, [128, 512], F32)) for i in range(2)]
                acc2 = E(sbt("acc2", [128, 512], F32))
                b_acc2 = B()
                pc0 = E(pst("pc0", [128, 512], F32))
                pc = [pc0, pc0]
                pmean = E(pst("pmean", [128, 512], F32))
                pex2 = pmean
                s_ps = [E(pst("s_ps%d" % i, [128, 512], F32)) for i in range(2)]
                pT_ps = [E(pst("pT_ps%d" % i, [128, 1024], BF16)) for i in range(2)]
                o_ps = [E(pst("o_ps%d" % i, [128, 512], F32)) for i in range(2)]
                b_a, b_q, b_k, b_v, b_u, b_cb, b_mix = Bs(2), Bs(2), Bs(2), Bs(2), Bs(2), Bs(2), Bs(2)
                b_cv, b_cvb, b_cv2, b_mean, b_var, b_rstd = B(), B(), B(), B(), B(), B()
                b_t1, b_s, b_p, b_pn, b_pTs, b_st, b_acc = Bs(2), Bs(2), Bs(2), Bs(2), Bs(2), Bs(2), Bs(2)
                b_pc0, b_sps = B(), Bs(2)
                b_pc = [b_pc0, b_pc0]
                b_pmean = B()
                b_pex2 = b_pmean
                b_pTp, b_ops = Bs(2), Bs(2)
                hi_ = 0
                gi_ = 0
                for ti, (seg, gb0, nb) in enumerate(tl):
                    sl = ti % 2
                    n = nb * 128
                    t0 = gb0 * 128
                    bs, nbk = SEGS[seg]
                    k.dma("a_sb%d" % sl, a_sb[sl][:, :, 0:n + 30],
                          aT_d[:, :, t0 - 15:t0 + n + 15].rearrange("g p t -> p g t"), W=[b_a[sl]])
                    k.dma("q_sb%d" % sl, q_sb[sl][:, :, 0:n], qT_d[:, :, t0:t0 + n].rearrange("g p t -> p g t"),
                          W=[b_q[sl]])
                    k.dma("k_sb%d" % sl, k_sb[sl][:, :, 0:n + 256],
                          kT_d[:, :, t0 - 128:t0 + n + 128].rearrange("g p t -> p g t"), W=[b_k[sl]])
                    k.dma("v_sb%d" % sl, v_sb[sl][:, 0:nb + 2, :],
                          v_d[t0 - 128:t0 + n + 128, :].rearrange("(b p) d -> p b d", p=128), W=[b_v[sl]])
                    k.dma("u_sb%d" % sl, u_sb[sl][:, :, 0:n + 2],
                          uT_d[:, :, t0 - 1:t0 + n + 1].rearrange("g p t -> p g t"), W=[b_u[sl]])
                    k.dma("cb_sb%d" % sl, cb_sb[sl][:, :, 0:n], cbT_d[:, :, t0:t0 + n].rearrange("g p t -> p g t"),
                          W=[b_cb[sl]])
                    mx = mixT[sl]
                    bm = b_mix[sl]
                    for g in range(4):
                        pi = g % 2
                        for kk in range(31):
                            k.do(PE, lambda: T.matmul(pc[pi][:, 0:n], lhsT=Dg[:, kk, g, :],
                                                      rhs=a_sb[sl][:, g, kk:kk + n], start=(kk == 0), stop=(kk == 30)),
                                 R=[b_Dg, b_a[sl]], W=[b_pc[pi]], inc=(kk == 30))
                        k.do(ACT, lambda: A.activation(out=cv[:, g, 0:n], in_=pc[pi][:, 0:n], func=AF.Identity,
                                                       bias=cab[:, g:g + 1]), R=[b_pc[pi], b_const], W=[b_cv])
                        k.do(ACT, lambda: A.activation(out=cvb[:, g, 0:n], in_=pc[pi][:, 0:n], func=AF.Identity,
                                                       bias=cab[:, g:g + 1]), R=[b_pc[pi], b_const], W=[b_cvb])
                        k.do(ACT, lambda: A.activation(out=cv2[:, g, 0:n], in_=pc[pi][:, 0:n], func=AF.Square,
                                                       bias=cab[:, g:g + 1]), R=[b_pc[pi], b_const], W=[b_cv2])
                    for g in range(4):
                        k.do(PE, lambda: T.matmul(pmean[:, 0:n], lhsT=onesb[:], rhs=cvb[:, g, 0:n], start=(g == 0),
                                                  stop=(g == 3)), R=[b_const, b_cvb], W=[b_pmean], inc=(g == 3))
                    k.do(DVE, lambda: V.tensor_copy(out=mean_sb[:, 0:n], in_=pmean[:, 0:n]), R=[b_pmean], W=[b_mean])
                    for g in range(4):
                        k.do(PE, lambda: T.matmul(pex2[:, 0:n], lhsT=onesb[:], rhs=cv2[:, g, 0:n], start=(g == 0),
                                                  stop=(g == 3)), R=[b_const, b_cv2], W=[b_pex2], inc=(g == 3))
                    k.do(DVE, lambda: V.tensor_tensor(out=var_sb[:, 0:n], in0=mean_sb[:, 0:n], in1=mean_sb[:, 0:n],
                                                      op=ALU.mult), R=[b_mean], W=[b_var])
                    k.do(DVE, lambda: V.tensor_tensor(out=var_sb[:, 0:n], in0=pex2[:, 0:n], in1=var_sb[:, 0:n],
                                                      op=ALU.subtract), R=[b_pex2, b_var], W=[b_var])
                    k.do(DVE, lambda: V.tensor_scalar(out=var_sb[:, 0:n], in0=var_sb[:, 0:n], scalar1=0.0,
                                                      scalar2=None, op0=ALU.max), R=[b_var], W=[b_var])
                    k.do(ACT, lambda: A.activation(out=var_sb[:, 0:n], in_=var_sb[:, 0:n], func=AF.Sqrt, bias=EPS),
                         R=[b_var], W=[b_var])
                    k.do(DVE, lambda: V.reciprocal(out=rstd_sb[:, 0:n], in_=var_sb[:, 0:n]), R=[b_var], W=[b_rstd])
                    for g in range(4):
                        ti_ = gi_ % 2
                        gi_ += 1
                        k.do(DVE, lambda: V.tensor_tensor(out=t1[ti_][:, 0:n], in0=cv[:, g, 0:n], in1=mean_sb[:, 0:n],
                                                          op=ALU.subtract), R=[b_cv, b_mean], W=[b_t1[ti_]])
                        k.do(DVE, lambda: V.tensor_tensor(out=t1[ti_][:, 0:n], in0=t1[ti_][:, 0:n],
                                                          in1=rstd_sb[:, 0:n], op=ALU.mult),
                             R=[b_t1[ti_], b_rstd], W=[b_t1[ti_]])
                        k.do(ACT, lambda: A.activation(out=mx[:, g, 0:n], in_=t1[ti_][:, 0:n], func=AF.Silu,
                                                       scale=lng[:, g:g + 1], bias=lnb[:, g:g + 1]),
                             R=[b_t1[ti_], b_const], W=[bm])
                    for g in range(4):
                        ai = g % 2
                        k.do(POOL, lambda: G.tensor_scalar(out=acc[ai][:, 0:n], in0=u_sb[sl][:, g, 0:n],
                                                           scalar1=ccw[:, g, 0:1], scalar2=None, op0=ALU.mult),
                             R=[b_u[sl], b_const], W=[b_acc[ai]])
                        for kk in (1, 2):
                            k.do(POOL, lambda: G.tensor_scalar(out=acc2[:, 0:n], in0=u_sb[sl][:, g, kk:kk + n],
                                                               scalar1=ccw[:, g, kk:kk + 1], scalar2=None, op0=ALU.mult),
                                 R=[b_u[sl], b_const], W=[b_acc2])
                            k.do(POOL, lambda: G.tensor_tensor(out=acc[ai][:, 0:n], in0=acc[ai][:, 0:n],
                                                               in1=acc2[:, 0:n], op=ALU.add),
                                 R=[b_acc2, b_acc[ai]], W=[b_acc[ai]])
                        k.do(POOL, lambda: G.tensor_tensor(out=mx[:, 12 + g, 0:n], in0=acc[ai][:, 0:n],
                                                           in1=cb_sb[sl][:, g, 0:n], op=ALU.mult),
                             R=[b_acc[ai], b_cb[sl]], W=[bm])
                    for b in range(nb):
                        gb = gb0 + b
                        left_edge = (gb == bs + 2)
                        right_edge = (gb == bs + nbk - 3)
                        for hp in range(4):
                            kv = hp // 2
                            pair = [(0, 2 * hp), (1, 2 * hp + 1)]
                            for i2, h in pair:
                                k.do(PE, lambda: T.matmul(s_ps[i2][:, 0:384], lhsT=q_sb[sl][:, h, b * 128:(b + 1) * 128],
                                                          rhs=k_sb[sl][:, kv, b * 128:b * 128 + 384], start=True, stop=True),
                                     R=[b_q[sl], b_k[sl]], W=[b_sps[i2]])
                            for i2, h in pair:
                                sp, bsp = s_ps[i2], b_sps[i2]
                                ssb, bss = s_sb[i2], b_s[i2]
                                stt, bst = st[i2], b_st[i2]
                                k.do(DVE, lambda: V.scalar_tensor_tensor(out=ssb[:], in0=sp[:, 0:384], scalar=SCALE,
                                                                         in1=bias8[:, h, :], op0=ALU.mult, op1=ALU.add),
                                     R=[bsp, b_const], W=[bss])
                                if left_edge:
                                    fi = 4 + (0 if seg == 0 else 2)
                                    k.do(DVE, lambda: V.tensor_scalar(out=ssb[:, 0:128], in0=ssb[:, 0:128],
                                                                      scalar1=flg[:, fi:fi + 1], scalar2=None, op0=ALU.add),
                                         R=[bss, b_const], W=[bss])
                                if right_edge:
                                    fi = 4 + (1 if seg == 0 else 3)
                                    k.do(DVE, lambda: V.tensor_scalar(out=ssb[:, 256:384], in0=ssb[:, 256:384],
                                                                      scalar1=flg[:, fi:fi + 1], scalar2=None, op0=ALU.add),
                                         R=[bss, b_const], W=[bss])
                                k.do(DVE, lambda: V.reduce_max(out=stt[:, 0:1], in_=ssb[:], axis=AX.X), R=[bss], W=[bst])
                                k.do(DVE, lambda: V.tensor_scalar(out=stt[:, 1:2], in0=stt[:, 0:1], scalar1=sinkb[:, h:h + 1],
                                                                  scalar2=-1.0, op0=ALU.max, op1=ALU.mult),
                                     R=[bst, b_const], W=[bst])
                                k.do(DVE, lambda: V.memset(stt[:, 2:3], 0.0), R=[bst], W=[bst])
                            for i2, h in pair:
                                ssb, bss = s_sb[i2], b_s[i2]
                                stt, bst = st[i2], b_st[i2]
                                k.do(ACT, lambda: A.activation(out=p_sb[i2][:], in_=ssb[:], func=AF.Exp, bias=stt[:, 1:2],
                                                               accum_out=stt[:, 2:3]), R=[bss, bst], W=[b_p[i2], bst])
                                k.do(ACT, lambda: A.activation(out=stt[:, 3:4], in_=sinkb[:, h:h + 1], func=AF.Exp,
                                                               bias=stt[:, 1:2]), R=[bst, b_const], W=[bst])
                            for i2, h in pair:
                                stt, bst = st[i2], b_st[i2]
                                k.do(DVE, lambda: V.tensor_tensor(out=stt[:, 4:5], in0=stt[:, 2:3], in1=stt[:, 3:4],
                                                                  op=ALU.add), R=[bst], W=[bst])
                                k.do(DVE, lambda: V.reciprocal(out=stt[:, 5:6], in_=stt[:, 4:5]), R=[bst], W=[bst])
                                k.do(DVE, lambda: V.tensor_scalar(out=pn_sb[i2][:], in0=p_sb[i2][:], scalar1=stt[:, 5:6],
                                                                  scalar2=None, op0=ALU.mult),
                                     R=[b_p[i2], bst], W=[b_pn[i2]])
                            for i2, h in pair:
                                for j in range(3):
                                    k.do(PE, lambda: T.transpose(out=pT_ps[i2][:, j * 128:(j + 1) * 128],
                                                                 in_=pn_sb[i2][:, j * 128:(j + 1) * 128], identity=ident[:]),
                                         R=[b_pn[i2], b_const], W=[b_pTp[i2]], inc=(j == 2))
                            for i2, h in pair:
                                k.do(ACT, lambda: A.activation(out=pT_sb[i2][:],
                                                               in_=pT_ps[i2][:, 0:384], func=AF.Identity),
                                     R=[b_pTp[i2]], W=[b_pTs[i2]])
                            for i2, h in pair:
                                for j in range(3):
                                    k.do(PE, lambda: T.matmul(o_ps[i2][:, 0:128],
                                                              lhsT=v_sb[sl][:, b + j, kv * 128:(kv + 1) * 128],
                                                              rhs=pT_sb[i2][:, j * 128:(j + 1) * 128], start=(j == 0),
                                                              stop=(j == 2)),
                                         R=[b_v[sl], b_pTs[i2]], W=[b_ops[i2]], inc=(j == 2))
                            for i2, h in pair:
                                k.do(DVE, lambda: V.tensor_copy(out=mx[:, 4 + h, b * 128:(b + 1) * 128],
                                                                in_=o_ps[i2][:, 0:128]),
                                     R=[b_ops[i2]], W=[bm])
                    k.dma("st_mix%d" % sl, mix_d[:, :, t0:t0 + n].rearrange("g p t -> p g t"), mx[:, :, 0:n], R=[bm],
                          q=POOL)
                k.barrier()

            with ExitStack() as es:
                E = es.enter_context
                b_const = B()
                wo = E(sbt("wo", [128, 16, D], BF16))
                SP.wait_tok(conv_tok["out%d" % l])
                k.dma("c_wo", wo[:], wb_out[l].rearrange("(c p) n -> p c n", p=128), W=[b_const])
                gm = E(sbt("gm", [128, 2, D], F32))
                for s in range(2):
                    k.dma("c_gm", gm[:, s, :], mod_d[l, s:s + 1, 2 * D:3 * D].partition_broadcast(128), W=[b_const])
                mixs = [E(sbt("mixs%d" % i, [128, 16, 512], BF16)) for i in range(2)]
                xs = [E(sbt("xs%d" % i, [128, D], F32)) for i in range(2)]
                tmp = [E(sbt("tmp%d" % i, [128, 512], F32)) for i in range(2)]
                xo = [E(sbt("xo%d" % i, [128, D], F32)) for i in range(2)]
                po = [E(pst("po%d" % i, [128, 512], F32)) for i in range(8)]
                b_mixs, b_xs, b_tmp, b_xo, b_po = Bs(2), Bs(2), Bs(2), Bs(2), Bs(8)
                bi_ = 0
                tmi = 0
                for ti, (seg, gb0, nb) in enumerate(tl):
                    sl = ti % 2
                    n = nb * 128
                    t0 = gb0 * 128
                    k.dma("mixs%d" % sl, mixs[sl][:, :, 0:n], mix_d[:, :, t0:t0 + n].rearrange("g p t -> p g t"),
                          W=[b_mixs[sl]])
                    for b in range(nb):
                        i2 = bi_ % 2
                        bi_ += 1
                        r0 = (gb0 + b) * 128
                        k.dma("xs%d" % i2, xs[i2][:], x_src1[r0:r0 + 128, :], W=[b_xs[i2]])
                        for c in range(16):
                            for ft in range(4):
                                p = i2 * 4 + ft
                                k.do(PE, lambda: T.matmul(po[p][:], lhsT=mixs[sl][:, c, b * 128:(b + 1) * 128],
                                                          rhs=wo[:, c, ft * 512:(ft + 1) * 512], start=(c == 0),
                                                          stop=(c == 15)),
                                     R=[b_mixs[sl], b_const], W=[b_po[p]], inc=(c == 15 and ft == 3))
                        for ft in range(4):
                            p = i2 * 4 + ft
                            tm = tmi % 2
                            tmi += 1
                            k.do(DVE, lambda: V.tensor_tensor(out=tmp[tm][:], in0=po[p][:],
                                                              in1=gm[:, seg, ft * 512:(ft + 1) * 512], op=ALU.mult),
                                 R=[b_po[p], b_const], W=[b_tmp[tm]])
                            k.do(POOL, lambda: G.tensor_tensor(out=xo[i2][:, ft * 512:(ft + 1) * 512], in0=tmp[tm][:],
                                                               in1=xs[i2][:, ft * 512:(ft + 1) * 512], op=ALU.add),
                                 R=[b_tmp[tm], b_xs[i2]], W=[b_xo[i2]])
                        k.dma("st_xo%d" % i2, x_mid[r0:r0 + 128, :], xo[i2][:], R=[b_xo[i2]], q=POOL)
                k.barrier()

            blocks = [(seg, gb0 + b) for (seg, gb0, nb) in tl for b in range(nb)]
            NB3 = len(blocks)
            NTL = NB3 + 16
            NSLOT = NTL * 256
            Hs_l = Hslots[0:NSLOT, :]
            Ys_l = Yslots[0:NSLOT, :]
            with ExitStack() as es3:
                E3 = es3.enter_context
                b_rout = B()
                selA = E3(sbt("selA", [128, NB3, 16], F32))
                selB = E3(sbt("selB", [128, NB3, 16], F32))
                Rg = E3(sbt("Rg", [128, NB3, 16], F32))
                cA = E3(sbt("cA", [128, NB3], F32))
                cB = E3(sbt("cB", [128, NB3], F32))
                posA_i = E3(sbt("posA_i", [128, NB3], I32))
                posB_i = E3(sbt("posB_i", [128, NB3], I32))
                idxw = E3(sbt("idxw", [128, NTL], I32))
                base = E3(sbt("base", [128, 16], F32))

                with ExitStack() as es:
                    E = es.enter_context
                    b_const = B()
                    ident = load_const(E, "ident", [128, 128], BF16, ident_d, b_const)
                    utri = load_const(E, "utri", [128, 128], BF16, utri_d, b_const)
                    ones1 = load_const(E, "ones1", [128, 128], BF16, ones1_d, b_const)
                    thr = load_const(E, "thr", [128, 80], F32, thr_d, b_const)
                    iotap = load_const(E, "iotap", [128, 1], F32, iotap_d, b_const)
                    abc = E(sbt("abc", [128, 2, D], F32))
                    bbc = E(sbt("bbc", [128, 2, D], F32))
                    for s in range(2):
                        k.dma("c_abc", abc[:, s, :], mod_d[l, s:s + 1, 4 * D:5 * D].partition_broadcast(128), W=[b_const])
                        k.dma("c_bbc", bbc[:, s, :], mod_d[l, s:s + 1, 3 * D:4 * D].partition_broadcast(128), W=[b_const])
                    wr32 = load_const(E, "wr32", [128, 16, 20], F32, wr_d[l], b_const)
                    wrb = E(sbt("wrb", [128, 16, 20], BF16))
                    k.do(DVE, lambda: V.tensor_copy(out=wrb[:], in_=wr32[:]), R=[b_const], W=[b_const])
                    brb = load_const(E, "brb", [128, 20], F32, br_d[l].partition_broadcast(128), b_const)
                    xs = [E(sbt("xs%d" % i, [128, D], F32)) for i in range(2)]
                    tmpf = [E(sbt("tmpf%d" % i, [128, D], F32)) for i in range(2)]
                    junk = E(sbt("junk", [128, D], BF16))
                    ss = E(sbt("ss", [128, 12], F32))
                    xh = E(sbt("xh", [128, 4, D], BF16))
                    hT = E(sbt("hT", [128, 16, 512], BF16))
                    rt = E(sbt("rt", [128, 64], F32))
                    s16 = E(sbt("s16", [128, 16], BF16))
                    cmp = E(sbt("cmp", [128, 80], F32))
                    q16 = E(sbt("q16", [128, 64], F32))
                    etf = E(sbt("etf", [128, NTL], F32))
                    posf = E(sbt("posf", [128, 2, NB3], F32))
                    pT = [E(pst("pT%d" % i, [128, 1024], BF16)) for i in range(2)]
                    prt = E(pst("prt", [128, 512], F32))
                    prk = E(pst("prk", [128, 512], F32))
                    ptt = E(pst("ptt", [128, 512], F32))
                    b_xs, b_tmpf, b_pT = Bs(2), Bs(2), Bs(2)
                    b_junk, b_ss, b_xh, b_hT, b_rt, b_s16, b_prt, b_prk, b_ptt = B(), B(), B(), B(), B(), B(), B(), B(), B()
                    k.do(DVE, lambda: V.memset(base[:], 0.0), W=[b_rout])
                    zt = E(sbt("zt", [128, 4, D], BF16))
                    b_zt = B()
                    k.do(DVE, lambda: V.memset(zt[:], 0.0), W=[b_zt])
                    for j0 in range(0, 2 * NTL, 4):
                        k.dma("zero_hs", Hs_l[j0 * 128:(j0 + 4) * 128, :].rearrange("(j p) f -> p j f", p=128), zt[:],
                              R=[b_zt], q=POOL)
                    xi = 0
                    bi = 0
                    for ti, (seg, gb0, nb) in enumerate(tl):
                        n = nb * 128
                        k.do(DVE, lambda: V.memset(ss[:], 0.0), W=[b_ss])
                        for b in range(nb):
                            si = xi % 2
                            xi += 1
                            r0 = (gb0 + b) * 128
                            k.dma("xs%d" % si, xs[si][:], x_mid[r0:r0 + 128, :], W=[b_xs[si]])
                            k.do(ACT, lambda: A.activation(out=junk[:], in_=xs[si][:], func=AF.Square,
                                                           accum_out=ss[:, b:b + 1]), R=[b_xs[si], b_ss], W=[b_junk, b_ss])
                            k.do(ACT, lambda: A.activation(out=ss[:, 4 + b:5 + b], in_=ss[:, b:b + 1], func=AF.Sqrt,
                                                           scale=1.0 / D, bias=EPS), R=[b_ss], W=[b_ss])
                            k.do(DVE, lambda: V.reciprocal(out=ss[:, 8 + b:9 + b], in_=ss[:, 4 + b:5 + b]), R=[b_ss], W=[b_ss])
                            k.do(DVE, lambda: V.scalar_tensor_tensor(out=tmpf[si][:], in0=xs[si][:], scalar=ss[:, 8 + b:9 + b],
                                                                     in1=abc[:, seg, :], op0=ALU.mult, op1=ALU.mult),
                                 R=[b_xs[si], b_ss, b_const], W=[b_tmpf[si]])
                            k.do(POOL, lambda: G.tensor_tensor(out=xh[:, b, :], in0=tmpf[si][:], in1=bbc[:, seg, :], op=ALU.add),
                                 R=[b_tmpf[si], b_const], W=[b_xh])
                            k.dma("st_hrow", Hrows[r0:r0 + 128, :], xh[:, b, :], R=[b_xh], q=POOL)
                        for c in range(16):
                            pi = c % 2
                            for b in range(nb):
                                k.do(PE, lambda: T.transpose(out=pT[pi][:, b * 128:(b + 1) * 128],
                                                             in_=xh[:, b, c * 128:(c + 1) * 128], identity=ident[:]),
                                     R=[b_xh, b_const], W=[b_pT[pi]], inc=(b == nb - 1))
                            k.do(ACT, lambda: A.activation(out=hT[:, c, 0:n], in_=pT[pi][:, 0:n], func=AF.Identity),
                                 R=[b_pT[pi]], W=[b_hT])
                        for b in range(nb):
                            for c in range(16):
                                k.do(PE, lambda: T.matmul(prt[:, 0:20], lhsT=hT[:, c, b * 128:(b + 1) * 128], rhs=wrb[:, c, :],
                                                          start=(c == 0), stop=(c == 15)),
                                     R=[b_hT, b_const], W=[b_prt], inc=(c == 15))

                            def dv(fn, extraR=(), extraW=()):
                                k.do(DVE, fn, R=[b_rt] + list(extraR), W=[b_rt] + list(extraW))
                            dv(lambda: V.tensor_tensor(out=rt[:, 0:20], in0=prt[:, 0:20], in1=brb[:], op=ALU.add),
                               extraR=[b_prt, b_const])
                            dv(lambda: V.reduce_max(out=rt[:, 20:21], in_=rt[:, 0:4], axis=AX.X))
                            dv(lambda: V.tensor_scalar(out=rt[:, 24:28], in0=rt[:, 0:4], scalar1=rt[:, 20:21], scalar2=None,
                                                       op0=ALU.is_equal))
                            dv(lambda: V.tensor_scalar(out=rt[:, 21:22], in0=rt[:, 20:21], scalar1=-1.0, scalar2=None,
                                                       op0=ALU.mult))
                            dv(lambda: V.memset(rt[:, 22:23], 0.0))
                            k.do(ACT, lambda: A.activation(out=rt[:, 28:32], in_=rt[:, 0:4], func=AF.Exp, bias=rt[:, 21:22],
                                                           accum_out=rt[:, 22:23]), R=[b_rt], W=[b_rt])
                            dv(lambda: V.reciprocal(out=rt[:, 23:24], in_=rt[:, 22:23]))
                            dv(lambda: V.tensor_scalar(out=rt[:, 32:36], in0=rt[:, 4:8], scalar1=rt[:, 24:25], scalar2=None,
                                                       op0=ALU.mult))
                            for g in range(1, 4):
                                dv(lambda: V.scalar_tensor_tensor(out=rt[:, 32:36], in0=rt[:, 4 + 4 * g:8 + 4 * g],
                                                                  scalar=rt[:, 24 + g:25 + g], in1=rt[:, 32:36],
                                                                  op0=ALU.mult, op1=ALU.add))
                            dv(lambda: V.reduce_max(out=rt[:, 36:37], in_=rt[:, 32:36], axis=AX.X))
                            dv(lambda: V.tensor_scalar(out=rt[:, 40:44], in0=rt[:, 32:36], scalar1=rt[:, 36:37], scalar2=None,
                                                       op0=ALU.is_equal))
                            dv(lambda: V.scalar_tensor_tensor(out=rt[:, 44:48], in0=rt[:, 40:44], scalar=NEG,
                                                              in1=rt[:, 32:36], op0=ALU.mult, op1=ALU.add))
                            dv(lambda: V.reduce_max(out=rt[:, 37:38], in_=rt[:, 44:48], axis=AX.X))
                            dv(lambda: V.tensor_scalar(out=rt[:, 48:52], in0=rt[:, 44:48], scalar1=rt[:, 37:38], scalar2=None,
                                                       op0=ALU.is_equal))
                            dv(lambda: V.tensor_scalar(out=rt[:, 38:39], in0=rt[:, 36:37], scalar1=-1.0, scalar2=None,
                                                       op0=ALU.mult))
                            k.do(ACT, lambda: A.activation(out=rt[:, 52:56], in_=rt[:, 32:36], func=AF.Exp, bias=rt[:, 38:39]),
                                 R=[b_rt], W=[b_rt])
                            dv(lambda: V.tensor_tensor(out=rt[:, 56:60], in0=rt[:, 52:56], in1=rt[:, 48:52], op=ALU.mult))
                            dv(lambda: V.reduce_sum(out=rt[:, 39:40], in_=rt[:, 56:60], axis=AX.X))
                            dv(lambda: V.tensor_scalar(out=rt[:, 60:61], in0=rt[:, 39:40], scalar1=1.0, scalar2=None,
                                                       op0=ALU.add))
                            dv(lambda: V.reciprocal(out=rt[:, 61:62], in_=rt[:, 60:61]))
                            dv(lambda: V.tensor_tensor(out=cA[:, bi:bi + 1], in0=rt[:, 61:62], in1=rt[:, 23:24], op=ALU.mult),
                               extraW=[b_rout])
                            dv(lambda: V.tensor_tensor(out=cB[:, bi:bi + 1], in0=cA[:, bi:bi + 1], in1=rt[:, 39:40], op=ALU.mult),
                               extraR=[b_rout], extraW=[b_rout])
                            for g in range(4):
                                dv(lambda: V.tensor_scalar(out=selA[:, bi, 4 * g:4 * g + 4], in0=rt[:, 40:44],
                                                           scalar1=rt[:, 24 + g:25 + g], scalar2=None, op0=ALU.mult),
                                   extraW=[b_rout])
                                dv(lambda: V.tensor_scalar(out=selB[:, bi, 4 * g:4 * g + 4], in0=rt[:, 48:52],
                                                           scalar1=rt[:, 24 + g:25 + g], scalar2=None, op0=ALU.mult),
                                   extraW=[b_rout])
                            k.do(DVE, lambda: V.tensor_tensor(out=s16[:], in0=selA[:, bi, :], in1=selB[:, bi, :], op=ALU.add),
                                 R=[b_rout], W=[b_s16])
                            k.do(PE, lambda: T.matmul(prk[:, 0:16], lhsT=utri[:], rhs=s16[:], start=True, stop=True),
                                 R=[b_const, b_s16], W=[b_prk])
                            k.do(PE, lambda: T.matmul(ptt[:, 0:16], lhsT=ones1[:], rhs=s16[:], start=True, stop=True),
                                 R=[b_const, b_s16], W=[b_ptt])
                            k.do(DVE, lambda: V.tensor_tensor(out=Rg[:, bi, :], in0=prk[:, 0:16], in1=base[:], op=ALU.add),
                                 R=[b_prk, b_rout], W=[b_rout])
                            k.do(DVE, lambda: V.tensor_tensor(out=base[:], in0=ptt[:, 0:16], in1=base[:], op=ALU.add),
                                 R=[b_ptt, b_rout], W=[b_rout])
                            bi += 1
                    def dr(fn):
                        k.do(DVE, fn, R=[b_rout, b_rt, b_const], W=[b_rout, b_rt])
                    for e in range(16):
                        dr(lambda: V.tensor_scalar(out=cmp[:, 0:80], in0=thr[:, 0:80], scalar1=base[:, e:e + 1], scalar2=None,
                                                   op0=ALU.is_lt))
                        dr(lambda: V.reduce_sum(out=q16[:, e:e + 1], in_=cmp[:, 0:80], axis=AX.X))
                    dr(lambda: V.tensor_scalar(out=q16[:, 0:16], in0=q16[:, 0:16], scalar1=256.0, scalar2=None, op0=ALU.mult))
                    dr(lambda: V.memset(q16[:, 16:17], 0.0))
                    for e in range(1, 16):
                        dr(lambda: V.tensor_tensor(out=q16[:, 16 + e:17 + e], in0=q16[:, 15 + e:16 + e], in1=q16[:, e - 1:e],
                                                   op=ALU.add))
                    dr(lambda: V.tensor_tensor(out=q16[:, 32:48], in0=q16[:, 16:32], in1=q16[:, 0:16], op=ALU.add))
                    dr(lambda: V.memset(etf[:], 0.0))
                    for e in range(16):
                        dr(lambda: V.tensor_scalar(out=cmp[:, 0:NTL], in0=thr[:, 0:NTL], scalar1=q16[:, 32 + e:33 + e],
                                                   scalar2=None, op0=ALU.is_ge))
                        dr(lambda: V.tensor_tensor(out=etf[:], in0=etf[:], in1=cmp[:, 0:NTL], op=ALU.add))
                    dr(lambda: V.tensor_scalar(out=etf[:], in0=etf[:], scalar1=15.0, scalar2=None, op0=ALU.min))
                    dr(lambda: V.tensor_scalar(out=etf[:], in0=etf[:], scalar1=128.0, scalar2=iotap[:, 0:1], op0=ALU.mult,
                                               op1=ALU.add))
                    dr(lambda: V.tensor_copy(out=idxw[:], in_=etf[:]))
                    for bi in range(NB3):
                        dr(lambda: V.tensor_tensor(out=rt[:, 0:16], in0=Rg[:, bi, :], in1=q16[:, 16:32], op=ALU.add))
                        dr(lambda: V.tensor_tensor(out=rt[:, 16:32], in0=rt[:, 0:16], in1=selA[:, bi, :], op=ALU.mult))
                        dr(lambda: V.reduce_sum(out=posf[:, 0, bi:bi + 1], in_=rt[:, 16:32], axis=AX.X))
                        dr(lambda: V.tensor_tensor(out=rt[:, 32:48], in0=rt[:, 0:16], in1=selB[:, bi, :], op=ALU.mult))
                        dr(lambda: V.reduce_sum(out=posf[:, 1, bi:bi + 1], in_=rt[:, 32:48], axis=AX.X))
                    dr(lambda: V.tensor_copy(out=posA_i[:], in_=posf[:, 0, :]))
                    dr(lambda: V.tensor_copy(out=posB_i[:], in_=posf[:, 1, :]))
                    k.barrier()

                with ExitStack() as es:
                    E = es.enter_context
                    hr = [E(sbt("hr%d" % i, [128, D], BF16)) for i in range(4)]
                    b_hr = Bs(4)
                    for bi, (seg, gb) in enumerate(blocks):
                        hi = bi % 4
                        k.dma("hr%d" % hi, hr[hi][:], Hrows[gb * 128:(gb + 1) * 128, :], W=[b_hr[hi]])
                        k.dma_ind("sc_a%d" % hi, Hs_l, bass.IndirectOffsetOnAxis(ap=posA_i[:, bi:bi + 1], axis=0), hr[hi][:], None,
                                  NSLOT, R=[b_hr[hi], b_rout])
                        k.dma_ind("sc_b%d" % hi, Hs_l, bass.IndirectOffsetOnAxis(ap=posB_i[:, bi:bi + 1], axis=0), hr[hi][:], None,
                                  NSLOT, R=[b_hr[hi], b_rout])
                    k.barrier()

                with ExitStack() as es:
                    E = es.enter_context
                    b_const = B()
                    ident = load_const(E, "ident", [128, 128], BF16, ident_d, b_const)
                    wq = [E(sbt("wq%d" % i, [128, 8192], BF16)) for i in range(6)]
                    hs = [E(sbt("hs%d" % i, [128, 2, D], BF16)) for i in range(2)]
                    hsT = [E(sbt("hsT%d" % i, [128, 16, 256], BF16)) for i in range(2)]
                    he = [E(sbt("he%d" % i, [128, 4, 256], BF16)) for i in range(2)]
                    sg = [E(sbt("sg%d" % i, [128, 256], F32)) for i in range(2)]
                    yo = [E(sbt("yo%d" % i, [128, D], F32)) for i in range(2)]
                    pT = [E(pst("pT%d" % i, [128, 1024], BF16)) for i in range(2)]
                    pg = [E(pst("pg%d" % i, [128, 512], F32)) for i in range(2)]
                    pu = [E(pst("pu%d" % i, [128, 512], F32)) for i in range(2)]
                    pd = [E(pst("pd%d" % i, [128, 512], F32)) for i in range(2)]
                    b_wq, b_hs, b_hsT, b_he, b_sg, b_yo = Bs(6), Bs(2), Bs(2), Bs(2), Bs(2), Bs(2)
                    b_pT, b_pg, b_pu, b_pd = Bs(2), Bs(2), Bs(2), Bs(2)
                    POOL.wait_tok(conv_tok["g%d" % l])
                    POOL.wait_tok(conv_tok["u%d" % l])
                    POOL.wait_tok(conv_tok["d%d" % l])
                    wsrc = [wb_g[l], wb_u[l], wb_d[l]]

                    def load_w(i):
                        for m in range(3):
                            ws = (3 * i + m) % 6
                            k.dma_ind("wq%d" % ws, wq[ws][:], None, wsrc[m],
                                      bass.IndirectOffsetOnAxis(ap=idxw[:, i:i + 1], axis=0), 2048, R=[b_rout], W=[b_wq[ws]])
                    load_w(0)
                    ji_ = 0
                    di_ = 0
                    ev_ = 0
                    for i in range(NTL):
                        sl = i % 2
                        if i + 1 < NTL:
                            load_w(i + 1)
                        k.dma("hs%d" % sl, hs[sl][:], Hs_l[i * 256:(i + 1) * 256, :].rearrange("(s p) f -> p s f", p=128),
                              W=[b_hs[sl]])
                        wgs, wus, wds = [(3 * i + m) % 6 for m in range(3)]
                        wgv = wq[wgs][:].rearrange("p (c n) -> p c n", c=16)
                        wuv = wq[wus][:].rearrange("p (c n) -> p c n", c=16)
                        wdv = wq[wds][:].rearrange("p (c n) -> p c n", c=4)
                        for c in range(16):
                            pi = c % 2
                            for sb_ in range(2):
                                k.do(PE, lambda: T.transpose(out=pT[pi][:, sb_ * 128:(sb_ + 1) * 128],
                                                             in_=hs[sl][:, sb_, c * 128:(c + 1) * 128], identity=ident[:]),
                                     R=[b_hs[sl], b_const], W=[b_pT[pi]], inc=(sb_ == 1))
                            if c % 2 == 0:
                                k.do(ACT, lambda: A.activation(out=hsT[sl][:, c, :], in_=pT[pi][:, 0:256], func=AF.Identity),
                                     R=[b_pT[pi]], W=[b_hsT[sl]])
                            else:
                                k.do(DVE, lambda: V.tensor_copy(out=hsT[sl][:, c, :], in_=pT[pi][:, 0:256]),
                                     R=[b_pT[pi]], W=[b_hsT[sl]])
                        for j in range(4):
                            pi = ji_ % 2
                            ji_ += 1
                            for c in range(16):
                                k.do(PE, lambda: T.matmul(pg[pi][:, 0:256], lhsT=wgv[:, c, j * 128:(j + 1) * 128],
                                                          rhs=hsT[sl][:, c, :], start=(c == 0), stop=(c == 15)),
                                     R=[b_wq[wgs], b_hsT[sl]], W=[b_pg[pi]], inc=(c == 15))
                            for c in range(16):
                                k.do(PE, lambda: T.matmul(pu[pi][:, 0:256], lhsT=wuv[:, c, j * 128:(j + 1) * 128],
                                                          rhs=hsT[sl][:, c, :], start=(c == 0), stop=(c == 15)),
                                     R=[b_wq[wus], b_hsT[sl]], W=[b_pu[pi]], inc=(c == 15))
                            k.do(ACT, lambda: A.activation(out=sg[pi][:], in_=pg[pi][:, 0:256], func=AF.Silu),
                                 R=[b_pg[pi]], W=[b_sg[pi]])
                            k.do(DVE, lambda: V.tensor_tensor(out=he[sl][:, j, :], in0=sg[pi][:], in1=pu[pi][:, 0:256],
                                                              op=ALU.mult), R=[b_sg[pi], b_pu[pi]], W=[b_he[sl]])
                        for sb_ in range(2):
                            yi = (2 * i + sb_) % 2
                            for ft in range(4):
                                pi = di_ % 2
                                di_ += 1
                                for j in range(4):
                                    k.do(PE, lambda: T.matmul(pd[pi][:], lhsT=he[sl][:, j, sb_ * 128:(sb_ + 1) * 128],
                                                              rhs=wdv[:, j, ft * 512:(ft + 1) * 512], start=(j == 0),
                                                              stop=(j == 3)),
                                         R=[b_he[sl], b_wq[wds]], W=[b_pd[pi]], inc=(j == 3))
                                ev_ += 1
                                if ev_ % 2 == 0:
                                    k.do(ACT, lambda: A.activation(out=yo[yi][:, ft * 512:(ft + 1) * 512], in_=pd[pi][:],
                                                                   func=AF.Identity), R=[b_pd[pi]], W=[b_yo[yi]])
                                else:
                                    k.do(DVE, lambda: V.tensor_copy(out=yo[yi][:, ft * 512:(ft + 1) * 512], in_=pd[pi][:]),
                                         R=[b_pd[pi]], W=[b_yo[yi]])
                            r0 = (2 * i + sb_) * 128
                            k.dma("st_yo%d" % yi, Ys_l[r0:r0 + 128, :], yo[yi][:], R=[b_yo[yi]])
                    k.barrier()

                with ExitStack() as es:
                    E = es.enter_context
                    b_const = B()
                    gf = E(sbt("gf", [128, 2, D], F32))
                    for s in range(2):
                        k.dma("c_gf", gf[:, s, :], mod_d[l, s:s + 1, 5 * D:6 * D].partition_broadcast(128), W=[b_const])
                    if l == 1:
                        fg = load_const(E, "fg", [128, D], F32, fng_d.partition_broadcast(128), b_const)
                        junk = E(sbt("junk", [128, D], BF16))
                        ss = E(sbt("ss", [128, 12], F32))
                        b_junk, b_ss = B(), B()
                    xs = [E(sbt("xs%d" % i, [128, D], F32)) for i in range(2)]
                    yA = [E(sbt("yA%d" % i, [128, D], F32)) for i in range(2)]
                    yB = [E(sbt("yB%d" % i, [128, D], F32)) for i in range(2)]
                    tt_ = [E(sbt("tt%d" % i, [128, D], F32)) for i in range(2)]
                    xo = [E(sbt("xo%d" % i, [128, D], F32)) for i in range(2)]
                    b_xs, b_yA, b_yB, b_tt, b_xo = Bs(2), Bs(2), Bs(2), Bs(2), Bs(2)
                    for bi, (seg, gb) in enumerate(blocks):
                        i2 = bi % 2
                        r0 = gb * 128
                        k.dma("xs%d" % i2, xs[i2][:], x_mid[r0:r0 + 128, :], W=[b_xs[i2]])
                        k.dma_ind("ga%d" % i2, yA[i2][:], None, Ys_l, bass.IndirectOffsetOnAxis(ap=posA_i[:, bi:bi + 1], axis=0),
                                  NSLOT, R=[b_rout], W=[b_yA[i2]])
                        k.dma_ind("gb%d" % i2, yB[i2][:], None, Ys_l, bass.IndirectOffsetOnAxis(ap=posB_i[:, bi:bi + 1], axis=0),
                                  NSLOT, R=[b_rout], W=[b_yB[i2]])
                        k.do(DVE, lambda: V.tensor_scalar(out=tt_[i2][:], in0=yA[i2][:], scalar1=cA[:, bi:bi + 1], scalar2=None,
                                                          op0=ALU.mult), R=[b_yA[i2], b_rout], W=[b_tt[i2]])
                        k.do(DVE, lambda: V.scalar_tensor_tensor(out=tt_[i2][:], in0=yB[i2][:], scalar=cB[:, bi:bi + 1],
                                                                 in1=tt_[i2][:], op0=ALU.mult, op1=ALU.add),
                             R=[b_yB[i2], b_rout, b_tt[i2]], W=[b_tt[i2]])
                        k.do(POOL, lambda: G.tensor_tensor(out=tt_[i2][:], in0=tt_[i2][:], in1=gf[:, seg, :], op=ALU.mult),
                             R=[b_tt[i2], b_const], W=[b_tt[i2]])
                        k.do(DVE, lambda: V.tensor_tensor(out=xo[i2][:], in0=tt_[i2][:], in1=xs[i2][:], op=ALU.add),
                             R=[b_tt[i2], b_xs[i2]], W=[b_xo[i2]])
                        if l == 0:
                            k.dma("st_xo%d" % i2, x_dst[r0:r0 + 128, :], xo[i2][:], R=[b_xo[i2]])
                        else:
                            k.do(DVE, lambda: V.memset(ss[:, 0:1], 0.0), W=[b_ss])
                            k.do(ACT, lambda: A.activation(out=junk[:], in_=xo[i2][:], func=AF.Square, accum_out=ss[:, 0:1]),
                                 R=[b_xo[i2], b_ss], W=[b_junk, b_ss])
                            k.do(ACT, lambda: A.activation(out=ss[:, 4:5], in_=ss[:, 0:1], func=AF.Sqrt, scale=1.0 / D,
                                                           bias=EPS), R=[b_ss], W=[b_ss])
                            k.do(DVE, lambda: V.reciprocal(out=ss[:, 8:9], in_=ss[:, 4:5]), R=[b_ss], W=[b_ss])
                            k.do(DVE, lambda: V.scalar_tensor_tensor(out=xo[i2][:], in0=xo[i2][:], scalar=ss[:, 8:9], in1=fg[:],
                                                                     op0=ALU.mult, op1=ALU.mult),
                                 R=[b_xo[i2], b_ss, b_const], W=[b_xo[i2]])
                            orow = (gb - 2) * 128 if seg == 0 else 2048 + (gb - 22) * 128
                            k.dma("st_y%d" % i2, y_d[orow:orow + 128, :], xo[i2][:], R=[b_xo[i2]])
                    k.barrier(final=(l == 1))
    return nc


_NC_CACHE = {}


def _alibi_bias():
    slopes = 2.0 ** (-8.0 * np.arange(1, 9, dtype=np.float64) / 8.0)
    q = np.arange(128)[:, None]
    s = np.arange(384)[None, :]
    dist = np.abs(s - 128 - q)
    out = np.empty((128, 8, 384), np.float32)
    for h in range(8):
        out[:, h, :] = np.where(dist <= 128, -slopes[h] * dist, NEG)
    return out


def kernel(x_prompt, x_sample, c_prompt, c_sample, norm_mix_g, norm_ffn_g, w_ada, b_ada, w_in, w_out,
           conv_a_w, conv_a_b, ln_a_g, ln_a_b, attn_sink, conv_c_w, w_router_group, b_router_group,
           w_router_expert, b_router_expert, w_gate, w_up, w_down, final_norm_g):
    f = lambda a: np.ascontiguousarray(np.asarray(a, dtype=np.float32))
    x_prompt, x_sample, c_prompt, c_sample = f(x_prompt), f(x_sample), f(c_prompt), f(c_sample)
    if "nc" not in _NC_CACHE:
        _NC_CACHE["nc"] = build_program()
    nc = _NC_CACHE["nc"]

    caw = f(np.transpose(f(conv_a_w).reshape(2, 31, 4, 128), (0, 3, 2, 1)))
    cab = f(np.transpose(f(conv_a_b).reshape(2, 4, 128), (0, 2, 1)))
    lng = f(np.transpose(f(ln_a_g).reshape(2, 4, 128), (0, 2, 1)))
    lnb = f(np.transpose(f(ln_a_b).reshape(2, 4, 128), (0, 2, 1)))
    ccw = f(np.transpose(f(conv_c_w).reshape(2, 3, 4, 128), (0, 3, 2, 1)))
    wr = np.concatenate([f(w_router_group), f(w_router_expert)], axis=-1)
    wr = f(np.transpose(wr.reshape(2, 16, 128, 20), (0, 2, 1, 3)))
    br = f(np.concatenate([f(b_router_group), f(b_router_expert)], axis=-1).reshape(2, 1, 20))
    shared = dict(
        ident=np.eye(128, dtype=np.float32).astype(ml_dtypes.bfloat16),
        ones=np.full((128, 128), 1.0 / 512, np.float32).astype(ml_dtypes.bfloat16),
        biasmat=_alibi_bias(),
        utri=np.triu(np.ones((128, 128), np.float32), 1).astype(ml_dtypes.bfloat16),
        ones1=np.ones((128, 128), np.float32).astype(ml_dtypes.bfloat16),
        thr=np.tile((256.0 * np.arange(80, dtype=np.float32))[None], (128, 1)),
        iotap=np.arange(128, dtype=np.float32).reshape(128, 1),
        norm_mix_g=f(norm_mix_g), norm_ffn_g=f(norm_ffn_g), w_ada=f(w_ada), b_ada=f(b_ada), w_in=f(w_in),
        w_out=f(w_out), caw=caw, cab=cab, lng=lng, lnb=lnb, sink=f(attn_sink).reshape(2, 1, 8), ccw=ccw, wr=wr, br=br,
        w_gate=f(w_gate), w_up=f(w_up), w_down=f(w_down), final_norm_g=f(final_norm_g).reshape(1, D),
    )
    in_maps = []
    for c in range(NCORES):
        sb, half = c // 2, c % 2
        xl = np.zeros((NT, D), np.float32)
        lo, hi = 2048 * c - 256, 2048 * c + 2048 + 256
        a, b = max(lo, 0), min(hi, 16384)
        xl[a - lo:b - lo] = x_prompt[0, a:b]
        lo, hi = 4096 * half - 256, 4096 * half + 4096 + 256
        a, b = max(lo, 0), min(hi, 8192)
        xl[2560 + a - lo:2560 + b - lo] = x_sample[sb, a:b]
        fl = np.array([c > 0, c < 7, half == 1, half == 0], np.float32)
        flags = np.zeros((128, 8), np.float32)
        flags[:, 0:4] = fl[None]
        flags[:, 4:8] = np.where(fl > 0, 0.0, NEG)[None]
        cc = np.stack([c_prompt[0], c_sample[sb]], axis=-1)
        cT = f(np.transpose(cc.reshape(16, 128, 2), (1, 0, 2)))
        in_maps.append(dict(shared, x_local=xl, flags=flags, cT=cT))

    res = run_bass_kernel_spmd(nc, in_maps, core_ids=list(range(NCORES)))
    y_prompt = np.empty((1, 16384, D), np.float32)
    y_sample = np.empty((4, 8192, D), np.float32)
    for c in range(NCORES):
        y = res.results[c]["y_local"]
        sb, half = c // 2, c % 2
        y_prompt[0, 2048 * c:2048 * c + 2048] = y[0:2048]
        y_sample[sb, 4096 * half:4096 * half + 4096] = y[2048:6144]
    return (y_prompt, y_sample)
```

```python
import numpy as np
import ml_dtypes
from contextlib import ExitStack
import concourse.bass as bass
import concourse.mybir as mybir
from concourse.bass_utils import run_bass_kernel_spmd

F32 = mybir.dt.float32
BF16 = mybir.dt.bfloat16
I32 = mybir.dt.int32
AF = mybir.ActivationFunctionType
ALU = mybir.AluOpType
AX = mybir.AxisListType

D = 2048
NCORES = 8
SEGS = [(0, 20), (20, 36)]
NBLK = 56
NT = NBLK * 128
NOWN = 6144
EPS = 1e-6
NEG = -1e30
SCALE = 128 ** -0.5


class Eng:
    def __init__(self, e, name, sem):
        self.e, self.name, self.sem, self.n, self.seen = e, name, sem, 0, {}

    def wait_tok(self, tok):
        if tok is None:
            return
        _, sem, cnt = tok
        key = id(sem)
        if self.seen.get(key, 0) >= cnt:
            return
        self.e.wait_ge(sem, cnt)
        self.seen[key] = cnt


class B:
    def __init__(self):
        self.w, self.r = {}, {}

    def read(self, eng):
        for t in self.w.values():
            if t[0] == "PE" and eng.name == "PE":
                continue
            eng.wait_tok(t)

    def write(self, eng):
        for k, t in self.r.items():
            if k != eng.name:
                eng.wait_tok(t)
        for k, t in self.w.items():
            if k != eng.name:
                eng.wait_tok(t)

    def did_read(self, tok):
        k = tok[0]
        if k not in self.r or self.r[k][2] < tok[2]:
            self.r[k] = tok

    def did_write(self, tok):
        self.w = {tok[0]: tok}
        self.r = {}


class K:
    def __init__(self, nc, es):
        self.nc = nc
        E = es.enter_context
        self.PE = Eng(nc.tensor, "PE", E(nc.semaphore("sem_pe")))
        self.ACT = Eng(nc.scalar, "ACT", E(nc.semaphore("sem_act")))
        self.DVE = Eng(nc.vector, "DVE", E(nc.semaphore("sem_dve")))
        self.POOL = Eng(nc.gpsimd, "POOL", E(nc.semaphore("sem_pool")))
        self.SP = Eng(nc.sync, "SP", E(nc.semaphore("sem_sp")))
        self.es = es
        self.dsem = {}
        self.slot_of = {}
        self.sem_pool = []
        self.nsem = 0

    def do(self, eng, fn, R=(), W=(), inc=True):
        for b in R:
            b.read(eng)
        for b in W:
            b.write(eng)
        ins = fn()
        if inc:
            ins.then_inc(eng.sem, 1)
            eng.n += 1
            tok = (eng.name, eng.sem, eng.n)
        else:
            tok = (eng.name, eng.sem, eng.n + 1)
        for b in R:
            b.did_read(tok)
        for b in W:
            b.did_write(tok)
        return tok

    def dma(self, slot, out, in_, R=(), W=(), q=None):
        q = q or self.SP
        if slot not in self.dsem:
            self.dsem[slot] = self.new_sem(slot)
        s = self.dsem[slot]
        for b in R:
            b.read(q)
        for b in W:
            b.write(q)
        q.e.dma_start(out=out, in_=in_).then_inc(s[0], 16)
        s[1] += 16
        tok = ("dma:" + slot, s[0], s[1])
        for b in R:
            b.did_read(tok)
        for b in W:
            b.did_write(tok)
        return tok

    def new_sem(self, slot):
        if self.sem_pool and not slot.startswith("cv_"):
            return self.sem_pool.pop()
        self.nsem += 1
        return [self.es.enter_context(self.nc.semaphore("dq%d" % self.nsem)), 0]

    def dma_ind(self, slot, out, out_off, in_, in_off, nrows, R=(), W=()):
        q = self.POOL
        if slot not in self.dsem:
            self.dsem[slot] = self.new_sem(slot)
        s = self.dsem[slot]
        for b in R:
            b.read(q)
        for b in W:
            b.write(q)
        q.e.indirect_dma_start(out=out, out_offset=out_off, in_=in_, in_offset=in_off).then_inc(s[0], 16)
        s[1] += 16
        tok = ("dma:" + slot, s[0], s[1])
        for b in R:
            b.did_read(tok)
        for b in W:
            b.did_write(tok)
        return tok

    def all_tokens(self, final=False):
        toks = []
        for e in (self.PE, self.ACT, self.DVE, self.POOL):
            if e.n:
                toks.append((e.name, e.sem, e.n))
        for name, s in self.dsem.items():
            if s[1] and (final or not name.startswith("cv_")):
                toks.append(("dma:" + name, s[0], s[1]))
        return toks

    def barrier(self, final=False):
        toks = self.all_tokens(final)
        for e in (self.PE, self.ACT, self.DVE, self.POOL, self.SP):
            for t in toks:
                if t[0] != e.name:
                    e.wait_tok(t)
        for name in list(self.dsem.keys()):
            if not name.startswith("cv_"):
                self.sem_pool.append(self.dsem.pop(name))


def Bs(n):
    return [B() for _ in range(n)]


def build_program():
    nc = bass.Bass("TRN2", target_bir_lowering=False)

    _uid = [0]

    def sbt(name, shape, dt):
        _uid[0] += 1
        return nc.sbuf_tensor("sb%d_%s" % (_uid[0], name), shape, dt)

    def pst(name, shape, dt):
        _uid[0] += 1
        return nc.psum_tensor("ps%d_%s" % (_uid[0], name), shape, dt)

    def din(name, shape, dt=F32):
        return nc.dram_tensor(name, list(shape), dt, kind="ExternalInput").ap()

    def dscr(name, shape, dt):
        return nc.dram_tensor(name, list(shape), dt, kind="Internal").ap()

    x_local = din("x_local", [NT, D])
    flags_d = din("flags", [128, 8])
    cT_d = din("cT", [128, 16, 2])
    ident_d = din("ident", [128, 128], BF16)
    ones_d = din("ones", [128, 128], BF16)
    bias_d = din("biasmat", [128, 8, 384])
    nmg_d = din("norm_mix_g", [2, D])
    nfg_d = din("norm_ffn_g", [2, D])
    wada_d = din("w_ada", [2, D, 6 * D])
    bada_d = din("b_ada", [2, 6 * D])
    win_d = din("w_in", [2, D, 4096])
    wout_d = din("w_out", [2, D, D])
    caw_d = din("caw", [2, 128, 4, 31])
    cab_d = din("cab", [2, 128, 4])
    lng_d = din("lng", [2, 128, 4])
    lnb_d = din("lnb", [2, 128, 4])
    sink_d = din("sink", [2, 1, 8])
    ccw_d = din("ccw", [2, 128, 4, 3])
    wr_d = din("wr", [2, 128, 16, 20])
    br_d = din("br", [2, 1, 20])
    wg_d = din("w_gate", [2, 16, D, 512])
    wu_d = din("w_up", [2, 16, D, 512])
    wd_d = din("w_down", [2, 16, 512, D])
    fng_d = din("final_norm_g", [1, D])
    utri_d = din("utri", [128, 128], BF16)
    ones1_d = din("ones1", [128, 128], BF16)
    thr_d = din("thr", [128, 80])
    iotap_d = din("iotap", [128, 1])
    y_d = nc.dram_tensor("y_local", [NOWN, D], F32, kind="ExternalOutput").ap()

    wb_in = dscr("wb_in", [2, D, 4096], BF16)
    wb_out = dscr("wb_out", [2, D, D], BF16)
    wb_g = [dscr("wb_g%d" % l, [2048, 8192], BF16) for l in range(2)]
    wb_u = [dscr("wb_u%d" % l, [2048, 8192], BF16) for l in range(2)]
    wb_d = [dscr("wb_d%d" % l, [2048, 8192], BF16) for l in range(2)]
    Hrows = dscr("Hrows", [NT, D], BF16)
    Hslots = dscr("Hslots", [68 * 256, D], BF16)
    Yslots = dscr("Yslots", [68 * 256, D], F32)
    mod_d = dscr("mod_d", [2, 2, 6 * D], F32)
    xa = dscr("xa", [NT, D], F32)
    xb = dscr("xb", [NT, D], F32)
    aT_d = dscr("aT", [4, 128, NT], BF16)
    qT_d = dscr("qT", [8, 128, NT], BF16)
    kT_d = dscr("kT", [2, 128, NT], BF16)
    v_d = dscr("v", [NT, 256], BF16)
    uT_d = dscr("uT", [4, 128, NT], F32)
    cbT_d = dscr("cbT", [4, 128, NT], F32)
    mix_d = dscr("mixT", [16, 128, NT], BF16)

    with ExitStack() as es_all:
        k = K(nc, es_all)
        PE, ACT, DVE, POOL, SP = k.PE, k.ACT, k.DVE, k.POOL, k.SP
        T, A, V, G = nc.tensor, nc.scalar, nc.vector, nc.gpsimd

        conv_tok = {}

        def cast(name, dst, src):
            conv_tok[name] = k.dma("cv_" + name, dst, src, q=POOL)

        def cast_in(l, i):
            cast("in%d" % l, wb_in[l, i * 256:(i + 1) * 256, :], win_d[l, i * 256:(i + 1) * 256, :])

        def cast_out(l, i):
            cast("out%d" % l, wb_out[l, i * 512:(i + 1) * 512, :], wout_d[l, i * 512:(i + 1) * 512, :])

        def cast_moe(l, e):
            cast("g%d" % l, wb_g[l][e * 128:(e + 1) * 128, :].rearrange("p (c n) -> p c n", c=16),
                 wg_d[l, e].rearrange("(c p) n -> p c n", p=128))
            cast("u%d" % l, wb_u[l][e * 128:(e + 1) * 128, :].rearrange("p (c n) -> p c n", c=16),
                 wu_d[l, e].rearrange("(c p) n -> p c n", p=128))
            cast("d%d" % l, wb_d[l][e * 128:(e + 1) * 128, :].rearrange("p (c n) -> p c n", c=4),
                 wd_d[l, e].rearrange("(c p) n -> p c n", p=128))

        for i in range(8):
            cast_in(0, i)
        for i in range(4):
            cast_out(0, i)

        with ExitStack() as es:
            E = es.enter_context
            siluT = E(sbt("siluT", [128, 16, 2], F32))
            wt = [E(sbt("wadat%d" % i, [128, 16, 512], F32)) for i in range(2)]
            modrow = E(sbt("modrow", [2, 6 * D], F32))
            badar = E(sbt("badar", [2, 6 * D], F32))
            grow = E(sbt("grow", [2, 2, D], F32))
            ps = [E(pst("pps%d" % i, [128, 512], F32)) for i in range(2)]
            b_silu, b_mod, b_bada, b_grow = B(), B(), B(), B()
            b_wt, b_ps = Bs(2), Bs(2)
            k.dma("siluT", siluT[:], cT_d, W=[b_silu])
            k.do(ACT, lambda: A.activation(out=siluT[:], in_=siluT[:], func=AF.Silu), R=[b_silu], W=[b_silu])
            it = 0
            for l in range(2):
                k.dma("bada", badar[:], bada_d[l:l + 1, :].partition_broadcast(2), W=[b_bada])
                k.dma("grow", grow[:, 0, :], nmg_d[l:l + 1, :].partition_broadcast(2), W=[b_grow])
                k.dma("grow", grow[:, 1, :], nfg_d[l:l + 1, :].partition_broadcast(2), W=[b_grow])
                for j in range(24):
                    s = it % 2
                    it += 1
                    k.dma("wadat%d" % s, wt[s][:],
                          wada_d[l, :, j * 512:(j + 1) * 512].rearrange("(c p) n -> p c n", p=128), W=[b_wt[s]])
                    for c in range(16):
                        k.do(PE, lambda: T.matmul(ps[s][0:2, :], lhsT=siluT[:, c, :], rhs=wt[s][:, c, :],
                                                  start=(c == 0), stop=(c == 15)),
                             R=[b_silu, b_wt[s]], W=[b_ps[s]], inc=(c == 15))
                    k.do(DVE, lambda: V.tensor_tensor(out=modrow[:, j * 512:(j + 1) * 512], in0=ps[s][0:2, :],
                                                      in1=badar[:, j * 512:(j + 1) * 512], op=ALU.add),
                         R=[b_ps[s], b_bada], W=[b_mod])
                for (off, gi) in ((D, 0), (4 * D, 1)):
                    k.do(DVE, lambda: V.scalar_tensor_tensor(out=modrow[:, off:off + D], in0=modrow[:, off:off + D],
                                                             scalar=1.0, in1=grow[:, gi, :], op0=ALU.add,
                                                             op1=ALU.mult), R=[b_mod, b_grow], W=[b_mod])
                k.dma("modout", mod_d[l], modrow[:], R=[b_mod], q=POOL)
            k.barrier()

        def load_const(E, name, shape, dt, src, b):
            t = E(sbt(name, shape, dt))
            k.dma("c_" + name, t[:], src, W=[b])
            return t

        def seg_of_block(gb):
            return 0 if gb < 20 else 1

        def tiles_full(l):
            res = []
            for s, (bs, nbk) in enumerate(SEGS):
                lo, hi = (1, nbk - 1) if l == 0 else (2, nbk - 2)
                b = lo
                while b < hi:
                    nb = min(4, hi - b)
                    res.append((s, bs + b, nb))
                    b += nb
            return res

        def load_fm_mod(E, name, l, off, b):
            t = E(sbt(name, [128, 2, 16], F32))
            with nc.allow_non_contiguous_dma(reason="tiny feature-major load of a modulation vector"):
                for s in range(2):
                    k.dma("c_" + name, t[:, s, :], mod_d[l, s, off:off + D].rearrange("(c p) -> p c", p=128), W=[b])
            return t

        def norm_transpose(x_src, gb0, nb, seg, xs, b_xs, xsi, junk, b_junk, ss, b_ss, xh, b_xh, pT, b_pT,
                           hT, b_hT, afm, bfm, b_const, ident):
            n = nb * 128
            k.do(DVE, lambda: V.memset(ss[:], 0.0), W=[b_ss])
            for b in range(nb):
                si = xsi[0] % len(xs)
                xsi[0] += 1
                k.dma("xs%d" % si, xs[si][:], x_src[(gb0 + b) * 128:(gb0 + b + 1) * 128, :], W=[b_xs[si]])
                k.do(ACT, lambda: A.activation(out=junk[:], in_=xs[si][:], func=AF.Square,
                                               accum_out=ss[:, b:b + 1]), R=[b_xs[si], b_ss], W=[b_junk, b_ss])
                k.do(ACT, lambda: A.activation(out=ss[:, 4 + b:5 + b], in_=ss[:, b:b + 1], func=AF.Sqrt,
                                               scale=1.0 / D, bias=EPS), R=[b_ss], W=[b_ss])
                k.do(DVE, lambda: V.reciprocal(out=ss[:, 8 + b:9 + b], in_=ss[:, 4 + b:5 + b]), R=[b_ss], W=[b_ss])
                k.do(DVE, lambda: V.tensor_scalar(out=xh[:, b, :], in0=xs[si][:], scalar1=ss[:, 8 + b:9 + b],
                                                  scalar2=None, op0=ALU.mult), R=[b_xs[si], b_ss], W=[b_xh])
            for c in range(16):
                pi = c % 2
                for b in range(nb):
                    k.do(PE, lambda: T.transpose(out=pT[pi][:, b * 128:(b + 1) * 128],
                                                 in_=xh[:, b, c * 128:(c + 1) * 128], identity=ident[:]),
                         R=[b_xh, b_const], W=[b_pT[pi]], inc=(b == nb - 1))
                k.do(ACT, lambda: A.activation(out=hT[:, c, 0:n], in_=pT[pi][:, 0:n], func=AF.Identity,
                                               scale=afm[:, seg, c:c + 1], bias=bfm[:, seg, c:c + 1]),
                     R=[b_pT[pi], b_const], W=[b_hT])

        for l in range(2):
            x_src1 = x_local if l == 0 else xb
            x_mid = xa
            x_dst = xb

            with ExitStack() as es:
                E = es.enter_context
                b_const = B()
                ident = load_const(E, "ident", [128, 128], BF16, ident_d, b_const)
                flg = load_const(E, "flg", [128, 8], F32, flags_d, b_const)
                afm = load_fm_mod(E, "afm", l, D, b_const)
                bfm = load_fm_mod(E, "bfm", l, 0, b_const)
                xs = [E(sbt("xs%d" % i, [128, D], F32)) for i in range(2)]
                junk = E(sbt("junk", [128, D], BF16))
                ss = E(sbt("ss", [128, 12], F32))
                xh = [E(sbt("xh%d" % i, [128, 4, D], BF16)) for i in range(2)]
                hT = [E(sbt("hT%d" % i, [128, 16, 512], BF16)) for i in range(2)]
                wt = [E(sbt("wt%d" % i, [128, 16, 512], BF16)) for i in range(3)]
                sig = E(sbt("sig", [128, 4, 512], F32))
                o16 = [E(sbt("o16_%d" % i, [128, 4, 512], BF16)) for i in range(3)]
                o32 = [E(sbt("o32_%d" % i, [128, 4, 512], F32)) for i in range(2)]
                vo = E(sbt("vo", [128, 4, 256], BF16))
                pT = [E(pst("pT%d" % i, [128, 1024], BF16)) for i in range(2)]
                pz = [E(pst("pz%d" % i, [128, 512], F32)) for i in range(4)]
                b_xs, b_xh, b_hT, b_wt, b_pT, b_pz = Bs(2), Bs(2), Bs(2), Bs(3), Bs(2), Bs(4)
                b_junk, b_ss, b_sig, b_vo = B(), B(), B(), B()
                b_o16, b_o32 = Bs(3), Bs(2)
                xsi = [0]
                wi = 0
                pzi = 0
                o16i = 0
                o32i = 0
                first_w = True
                for t in range(14):
                    seg = 0 if t < 5 else 1
                    sl = t % 2
                    tok0 = t * 512
                    halo = None
                    if t in (0, 5):
                        halo = (0, 256, 0 if t == 0 else 2)
                    if t in (4, 13):
                        halo = (256, 512, 1 if t == 4 else 3)
                    norm_transpose(x_src1, t * 4, 4, seg, xs, b_xs, xsi, junk, b_junk, ss, b_ss, xh[sl], b_xh[sl],
                                   pT, b_pT, hT[sl], b_hT[sl], afm, bfm, b_const, ident)

                    def mask_halo(buf, bb, ngrp):
                        if halo is None:
                            return
                        lo, hi, fi = halo
                        k.do(POOL, lambda: G.tensor_scalar(out=buf[:, 0:ngrp, lo:hi], in0=buf[:, 0:ngrp, lo:hi],
                                                           scalar1=flg[:, fi:fi + 1], scalar2=None, op0=ALU.mult),
                             R=[bb, b_const], W=[bb])

                    for g in (1, 0, 2, 3, 4, 7, 5, 6):
                        ws = wi % 3
                        wi += 1
                        if first_w:
                            SP.wait_tok(conv_tok["in%d" % l])
                            first_w = False
                        k.dma("wt%d" % ws, wt[ws][:],
                              wb_in[l, :, g * 512:(g + 1) * 512].rearrange("(c p) n -> p c n", p=128), W=[b_wt[ws]])
                        nfm = 2 if g == 4 else 4
                        if g in (0, 2, 3, 4):
                            oi = o16i % 3
                            o16i += 1
                            ob, bo = o16[oi], b_o16[oi]
                        elif g in (5, 6):
                            oi = o32i % 2
                            o32i += 1
                            ob, bo = o32[oi], b_o32[oi]
                        for j in range(nfm):
                            p = pzi % 4
                            pzi += 1
                            for c in range(16):
                                k.do(PE, lambda: T.matmul(pz[p][:], lhsT=wt[ws][:, c, j * 128:(j + 1) * 128],
                                                          rhs=hT[sl][:, c, :], start=(c == 0), stop=(c == 15)),
                                     R=[b_wt[ws], b_hT[sl]], W=[b_pz[p]], inc=(c == 15))
                            if g == 1:
                                k.do(ACT, lambda: A.activation(out=sig[:, j, :], in_=pz[p][:], func=AF.Sigmoid),
                                     R=[b_pz[p]], W=[b_sig])
                            elif g == 0:
                                k.do(DVE, lambda: V.tensor_tensor(out=ob[:, j, :], in0=pz[p][:], in1=sig[:, j, :],
                                                                  op=ALU.mult), R=[b_pz[p], b_sig], W=[bo])
                            elif g in (2, 3, 4):
                                k.do(ACT, lambda: A.activation(out=ob[:, j, :], in_=pz[p][:], func=AF.Identity),
                                     R=[b_pz[p]], W=[bo])
                            elif g == 7:
                                k.do(ACT, lambda: A.activation(out=sig[:, j, :], in_=pz[p][:], func=AF.Identity),
                                     R=[b_pz[p]], W=[b_sig])
                            elif g == 5:
                                k.do(DVE, lambda: V.tensor_tensor(out=ob[:, j, :], in0=pz[p][:], in1=sig[:, j, :],
                                                                  op=ALU.mult), R=[b_pz[p], b_sig], W=[bo])
                            elif g == 6:
                                k.do(ACT, lambda: A.activation(out=ob[:, j, :], in_=pz[p][:], func=AF.Identity),
                                     R=[b_pz[p]], W=[bo])
                        if g == 4:
                            for b in range(4):
                                p = pzi % 4
                                pzi += 1
                                for c in range(16):
                                    k.do(PE, lambda: T.matmul(pz[p][:, 0:256], lhsT=hT[sl][:, c, b * 128:(b + 1) * 128],
                                                              rhs=wt[ws][:, c, 256:512], start=(c == 0),
                                                              stop=(c == 15)),
                                         R=[b_wt[ws], b_hT[sl]], W=[b_pz[p]], inc=(c == 15))
                                k.do(DVE, lambda: V.tensor_copy(out=vo[:, b, :], in_=pz[p][:, 0:256]),
                                     R=[b_pz[p]], W=[b_vo])
                            if halo is not None:
                                lo, hi, fi = halo
                                k.do(POOL, lambda: G.tensor_scalar(out=vo[:, lo // 128:hi // 128, :],
                                                                   in0=vo[:, lo // 128:hi // 128, :],
                                                                   scalar1=flg[:, fi:fi + 1], scalar2=None,
                                                                   op0=ALU.mult), R=[b_vo, b_const], W=[b_vo])
                            k.dma("st_v", v_d[tok0:tok0 + 512, :].rearrange("(b p) d -> p b d", p=128), vo[:],
                                  R=[b_vo], q=POOL)
                        if g == 0:
                            mask_halo(ob, bo, 4)
                            k.dma("st_a", aT_d[:, :, tok0:tok0 + 512].rearrange("g p t -> p g t"), ob[:], R=[bo], q=POOL)
                        elif g in (2, 3):
                            h0 = (g - 2) * 4
                            k.dma("st_q%d" % g, qT_d[h0:h0 + 4, :, tok0:tok0 + 512].rearrange("g p t -> p g t"), ob[:],
                                  R=[bo], q=POOL)
                        elif g == 4:
                            mask_halo(ob, bo, 2)
                            k.dma("st_k", kT_d[:, :, tok0:tok0 + 512].rearrange("g p t -> p g t"), ob[:, 0:2, :],
                                  R=[bo], q=POOL)
                        elif g == 5:
                            mask_halo(ob, bo, 4)
                            k.dma("st_u", uT_d[:, :, tok0:tok0 + 512].rearrange("g p t -> p g t"), ob[:], R=[bo], q=POOL)
                        elif g == 6:
                            k.dma("st_cb", cbT_d[:, :, tok0:tok0 + 512].rearrange("g p t -> p g t"), ob[:], R=[bo],
                                  q=POOL)
                k.barrier()

            tl = tiles_full(l)
            with ExitStack() as es:
                E = es.enter_context
                b_const = B()
                ident = load_const(E, "ident", [128, 128], BF16, ident_d, b_const)
                onesb = load_const(E, "onesb", [128, 128], BF16, ones_d, b_const)
                flg = load_const(E, "flg", [128, 8], F32, flags_d, b_const)
                bias8 = load_const(E, "bias8", [128, 8, 384], F32, bias_d, b_const)
                caw = load_const(E, "caw", [128, 4, 31], F32, caw_d[l], b_const)
                cab = load_const(E, "cab", [128, 4], F32, cab_d[l], b_const)
                lng = load_const(E, "lng", [128, 4], F32, lng_d[l], b_const)
                lnb = load_const(E, "lnb", [128, 4], F32, lnb_d[l], b_const)
                ccw = load_const(E, "ccw", [128, 4, 3], F32, ccw_d[l], b_const)
                sinkb = load_const(E, "sinkb", [128, 8], F32, sink_d[l].partition_broadcast(128), b_const)
                Dg = E(sbt("Dg", [128, 31, 4, 128], BF16))
                b_Dg = B()
                for kk in range(31):
                    for g in range(4):
                        k.do(DVE, lambda: V.tensor_scalar(out=Dg[:, kk, g, :], in0=ident[:],
                                                          scalar1=caw[:, g, kk:kk + 1], scalar2=None, op0=ALU.mult),
                             R=[b_const], W=[b_Dg])
                a_sb = [E(sbt("a_sb%d" % i, [128, 4, 542], BF16)) for i in range(2)]
                q_sb = [E(sbt("q_sb%d" % i, [128, 8, 512], BF16)) for i in range(2)]
                k_sb = [E(sbt("k_sb%d" % i, [128, 2, 768], BF16)) for i in range(2)]
                v_sb = [E(sbt("v_sb%d" % i, [128, 6, 256], BF16)) for i in range(2)]
                u_sb = [E(sbt("u_sb%d" % i, [128, 4, 514], F32)) for i in range(2)]
                cb_sb = [E(sbt("cb_sb%d" % i, [128, 4, 512], F32)) for i in range(2)]
                mixT = [E(sbt("mixT%d" % i, [128, 16, 512], BF16)) for i in range(2)]
                cv = E(sbt("cv", [128, 4, 512], F32))
                cvb = E(sbt("cvb", [128, 4, 512], BF16))
                cv2 = E(sbt("cv2", [128, 4, 512], BF16))
                mean_sb = E(sbt("mean_sb", [128, 512], F32))
                var_sb = E(sbt("var_sb", [128, 512], F32))
                rstd_sb = E(sbt("rstd_sb", [128, 512], F32))
                t1 = [E(sbt("t1_%d" % i, [128, 512], F32)) for i in range(2)]
                s_sb = [E(sbt("s_sb%d" % i, [128, 384], F32)) for i in range(2)]
                p_sb = [E(sbt("p_sb%d" % i, [128, 384], F32)) for i in range(2)]
                pn_sb = [E(sbt("pn_sb%d" % i, [128, 384], BF16)) for i in range(2)]
                pT_sb = [E(sbt("pT_sb%d" % i, [128, 384], BF16)) for i in range(2)]
                st = [E(sbt("st%d" % i, [128, 8], F32)) for i in range(2)]
                acc = [E(sbt("acc%d" % i, [128, 512], F32)) for i in range(2)]
                acc2 = E(sbt("acc2", [128, 512], F32))
                b_acc2 = B()
                pc0 = E(pst("pc0", [128, 512], F32))
                pc = [pc0, pc0]
                pmean = E(pst("pmean", [128, 512], F32))
                pex2 = pmean
                s_ps = [E(pst("s_ps%d" % i, [128, 512], F32)) for i in range(2)]
                pT_ps = [E(pst("pT_ps%d" % i, [128, 1024], BF16)) for i in range(2)]
                o_ps = [E(pst("o_ps%d" % i, [128, 512], F32)) for i in range(2)]
                b_a, b_q, b_k, b_v, b_u, b_cb, b_mix = Bs(2), Bs(2), Bs(2), Bs(2), Bs(2), Bs(2), Bs(2)
                b_cv, b_cvb, b_cv2, b_mean, b_var, b_rstd = B(), B(), B(), B(), B(), B()
                b_t1, b_s, b_p, b_pn, b_pTs, b_st, b_acc = Bs(2), Bs(2), Bs(2), Bs(2), Bs(2), Bs(2), Bs(2)
                b_pc0, b_sps = B(), Bs(2)
                b_pc = [b_pc0, b_pc0]
                b_pmean = B()
                b_pex2 = b_pmean
                b_pTp, b_ops = Bs(2), Bs(2)
                hi_ = 0
                gi_ = 0
                for ti, (seg, gb0, nb) in enumerate(tl):
                    sl = ti % 2
                    n = nb * 128
                    t0 = gb0 * 128
                    bs, nbk = SEGS[seg]
                    k.dma("a_sb%d" % sl, a_sb[sl][:, :, 0:n + 30],
                          aT_d[:, :, t0 - 15:t0 + n + 15].rearrange("g p t -> p g t"), W=[b_a[sl]])
                    k.dma("q_sb%d" % sl, q_sb[sl][:, :, 0:n], qT_d[:, :, t0:t0 + n].rearrange("g p t -> p g t"),
                          W=[b_q[sl]])
                    k.dma("k_sb%d" % sl, k_sb[sl][:, :, 0:n + 256],
                          kT_d[:, :, t0 - 128:t0 + n + 128].rearrange("g p t -> p g t"), W=[b_k[sl]])
                    k.dma("v_sb%d" % sl, v_sb[sl][:, 0:nb + 2, :],
                          v_d[t0 - 128:t0 + n + 128, :].rearrange("(b p) d -> p b d", p=128), W=[b_v[sl]])
                    k.dma("u_sb%d" % sl, u_sb[sl][:, :, 0:n + 2],
                          uT_d[:, :, t0 - 1:t0 + n + 1].rearrange("g p t -> p g t"), W=[b_u[sl]])
                    k.dma("cb_sb%d" % sl, cb_sb[sl][:, :, 0:n], cbT_d[:, :, t0:t0 + n].rearrange("g p t -> p g t"),
                          W=[b_cb[sl]])
                    mx = mixT[sl]
                    bm = b_mix[sl]
                    for g in range(4):
                        pi = g % 2
                        for kk in range(31):
                            k.do(PE, lambda: T.matmul(pc[pi][:, 0:n], lhsT=Dg[:, kk, g, :],
                                                      rhs=a_sb[sl][:, g, kk:kk + n], start=(kk == 0), stop=(kk == 30)),
                                 R=[b_Dg, b_a[sl]], W=[b_pc[pi]], inc=(kk == 30))
                        k.do(ACT, lambda: A.activation(out=cv[:, g, 0:n], in_=pc[pi][:, 0:n], func=AF.Identity,
                                                       bias=cab[:, g:g + 1]), R=[b_pc[pi], b_const], W=[b_cv])
                        k.do(ACT, lambda: A.activation(out=cvb[:, g, 0:n], in_=pc[pi][:, 0:n], func=AF.Identity,
                                                       bias=cab[:, g:g + 1]), R=[b_pc[pi], b_const], W=[b_cvb])
                        k.do(ACT, lambda: A.activation(out=cv2[:, g, 0:n], in_=pc[pi][:, 0:n], func=AF.Square,
                                                       bias=cab[:, g:g + 1]), R=[b_pc[pi], b_const], W=[b_cv2])
                    for g in range(4):
                        k.do(PE, lambda: T.matmul(pmean[:, 0:n], lhsT=onesb[:], rhs=cvb[:, g, 0:n], start=(g == 0),
                                                  stop=(g == 3)), R=[b_const, b_cvb], W=[b_pmean], inc=(g == 3))
                    k.do(DVE, lambda: V.tensor_copy(out=mean_sb[:, 0:n], in_=pmean[:, 0:n]), R=[b_pmean], W=[b_mean])
                    for g in range(4):
                        k.do(PE, lambda: T.matmul(pex2[:, 0:n], lhsT=onesb[:], rhs=cv2[:, g, 0:n], start=(g == 0),
                                                  stop=(g == 3)), R=[b_const, b_cv2], W=[b_pex2], inc=(g == 3))
                    k.do(DVE, lambda: V.tensor_tensor(out=var_sb[:, 0:n], in0=mean_sb[:, 0:n], in1=mean_sb[:, 0:n],
                                                      op=ALU.mult), R=[b_mean], W=[b_var])
                    k.do(DVE, lambda: V.tensor_tensor(out=var_sb[:, 0:n], in0=pex2[:, 0:n], in1=var_sb[:, 0:n],
                                                      op=ALU.subtract), R=[b_pex2, b_var], W=[b_var])
                    k.do(DVE, lambda: V.tensor_scalar(out=var_sb[:, 0:n], in0=var_sb[:, 0:n], scalar1=0.0,
                                                      scalar2=None, op0=ALU.max), R=[b_var], W=[b_var])
                    k.do(ACT, lambda: A.activation(out=var_sb[:, 0:n], in_=var_sb[:, 0:n], func=AF.Sqrt, bias=EPS),
                         R=[b_var], W=[b_var])
                    k.do(DVE, lambda: V.reciprocal(out=rstd_sb[:, 0:n], in_=var_sb[:, 0:n]), R=[b_var], W=[b_rstd])
                    for g in range(4):
                        ti_ = gi_ % 2
                        gi_ += 1
                        k.do(DVE, lambda: V.tensor_tensor(out=t1[ti_][:, 0:n], in0=cv[:, g, 0:n], in1=mean_sb[:, 0:n],
                                                          op=ALU.subtract), R=[b_cv, b_mean], W=[b_t1[ti_]])
                        k.do(DVE, lambda: V.tensor_tensor(out=t1[ti_][:, 0:n], in0=t1[ti_][:, 0:n],
                                                          in1=rstd_sb[:, 0:n], op=ALU.mult),
                             R=[b_t1[ti_], b_rstd], W=[b_t1[ti_]])
                        k.do(ACT, lambda: A.activation(out=mx[:, g, 0:n], in_=t1[ti_][:, 0:n], func=AF.Silu,
                                                       scale=lng[:, g:g + 1], bias=lnb[:, g:g + 1]),
                             R=[b_t1[ti_], b_const], W=[bm])
                    for g in range(4):
                        ai = g % 2
                        k.do(POOL, lambda: G.tensor_scalar(out=acc[ai][:, 0:n], in0=u_sb[sl][:, g, 0:n],
                                                           scalar1=ccw[:, g, 0:1], scalar2=None, op0=ALU.mult),
                             R=[b_u[sl], b_const], W=[b_acc[ai]])
                        for kk in (1, 2):
                            k.do(POOL, lambda: G.tensor_scalar(out=acc2[:, 0:n], in0=u_sb[sl][:, g, kk:kk + n],
                                                               scalar1=ccw[:, g, kk:kk + 1], scalar2=None, op0=ALU.mult),
                                 R=[b_u[sl], b_const], W=[b_acc2])
                            k.do(POOL, lambda: G.tensor_tensor(out=acc[ai][:, 0:n], in0=acc[ai][:, 0:n],
                                                               in1=acc2[:, 0:n], op=ALU.add),
                                 R=[b_acc2, b_acc[ai]], W=[b_acc[ai]])
                        k.do(POOL, lambda: G.tensor_tensor(out=mx[:, 12 + g, 0:n], in0=acc[ai][:, 0:n],
                                                           in1=cb_sb[sl][:, g, 0:n], op=ALU.mult),
                             R=[b_acc[ai], b_cb[sl]], W=[bm])
                    ntl_ = len(tl)
                    for e in range(ti * 16 // ntl_, (ti + 1) * 16 // ntl_):
                        cast_moe(l, e)
                    if l == 0:
                        for i in range(ti * 12 // ntl_, (ti + 1) * 12 // ntl_):
                            if i < 8:
                                cast_in(1, i)
                            else:
                                cast_out(1, i - 8)
                    for b in range(nb):
                        gb = gb0 + b
                        left_edge = (gb == bs + 2)
                        right_edge = (gb == bs + nbk - 3)
                        for hp in range(4):
                            kv = hp // 2
                            pair = [(0, 2 * hp), (1, 2 * hp + 1)]
                            for i2, h in pair:
                                k.do(PE, lambda: T.matmul(s_ps[i2][:, 0:384], lhsT=q_sb[sl][:, h, b * 128:(b + 1) * 128],
                                                          rhs=k_sb[sl][:, kv, b * 128:b * 128 + 384], start=True, stop=True),
                                     R=[b_q[sl], b_k[sl]], W=[b_sps[i2]])
                            for i2, h in pair:
                                sp, bsp = s_ps[i2], b_sps[i2]
                                ssb, bss = s_sb[i2], b_s[i2]
                                stt, bst = st[i2], b_st[i2]
                                k.do(DVE, lambda: V.scalar_tensor_tensor(out=ssb[:], in0=sp[:, 0:384], scalar=SCALE,
                                                                         in1=bias8[:, h, :], op0=ALU.mult, op1=ALU.add),
                                     R=[bsp, b_const], W=[bss])
                                if left_edge:
                                    fi = 4 + (0 if seg == 0 else 2)
                                    k.do(DVE, lambda: V.tensor_scalar(out=ssb[:, 0:128], in0=ssb[:, 0:128],
                                                                      scalar1=flg[:, fi:fi + 1], scalar2=None, op0=ALU.add),
                                         R=[bss, b_const], W=[bss])
                                if right_edge:
                                    fi = 4 + (1 if seg == 0 else 3)
                                    k.do(DVE, lambda: V.tensor_scalar(out=ssb[:, 256:384], in0=ssb[:, 256:384],
                                                                      scalar1=flg[:, fi:fi + 1], scalar2=None, op0=ALU.add),
                                         R=[bss, b_const], W=[bss])
                                k.do(DVE, lambda: V.reduce_max(out=stt[:, 0:1], in_=ssb[:], axis=AX.X), R=[bss], W=[bst])
                                k.do(DVE, lambda: V.tensor_scalar(out=stt[:, 1:2], in0=stt[:, 0:1], scalar1=sinkb[:, h:h + 1],
                                                                  scalar2=-1.0, op0=ALU.max, op1=ALU.mult),
                                     R=[bst, b_const], W=[bst])
                                k.do(DVE, lambda: V.memset(stt[:, 2:3], 0.0), R=[bst], W=[bst])
                            for i2, h in pair:
                                ssb, bss = s_sb[i2], b_s[i2]
                                stt, bst = st[i2], b_st[i2]
                                k.do(ACT, lambda: A.activation(out=p_sb[i2][:], in_=ssb[:], func=AF.Exp, bias=stt[:, 1:2],
                                                               accum_out=stt[:, 2:3]), R=[bss, bst], W=[b_p[i2], bst])
                                k.do(ACT, lambda: A.activation(out=stt[:, 3:4], in_=sinkb[:, h:h + 1], func=AF.Exp,
                                                               bias=stt[:, 1:2]), R=[bst, b_const], W=[bst])
                            for i2, h in pair:
                                stt, bst = st[i2], b_st[i2]
                                k.do(DVE, lambda: V.tensor_tensor(out=stt[:, 4:5], in0=stt[:, 2:3], in1=stt[:, 3:4],
                                                                  op=ALU.add), R=[bst], W=[bst])
                                k.do(DVE, lambda: V.reciprocal(out=stt[:, 5:6], in_=stt[:, 4:5]), R=[bst], W=[bst])
                                k.do(DVE, lambda: V.tensor_scalar(out=pn_sb[i2][:], in0=p_sb[i2][:], scalar1=stt[:, 5:6],
                                                                  scalar2=None, op0=ALU.mult),
                                     R=[b_p[i2], bst], W=[b_pn[i2]])
                            for i2, h in pair:
                                for j in range(3):
                                    k.do(PE, lambda: T.transpose(out=pT_ps[i2][:, j * 128:(j + 1) * 128],
                                                                 in_=pn_sb[i2][:, j * 128:(j + 1) * 128], identity=ident[:]),
                                         R=[b_pn[i2], b_const], W=[b_pTp[i2]], inc=(j == 2))
                            for i2, h in pair:
                                k.do(ACT, lambda: A.activation(out=pT_sb[i2][:],
                                                               in_=pT_ps[i2][:, 0:384], func=AF.Identity),
                                     R=[b_pTp[i2]], W=[b_pTs[i2]])
                            for i2, h in pair:
                                for j in range(3):
                                    k.do(PE, lambda: T.matmul(o_ps[i2][:, 0:128],
                                                              lhsT=v_sb[sl][:, b + j, kv * 128:(kv + 1) * 128],
                                                              rhs=pT_sb[i2][:, j * 128:(j + 1) * 128], start=(j == 0),
                                                              stop=(j == 2)),
                                         R=[b_v[sl], b_pTs[i2]], W=[b_ops[i2]], inc=(j == 2))
                            for i2, h in pair:
                                k.do(DVE, lambda: V.tensor_copy(out=mx[:, 4 + h, b * 128:(b + 1) * 128],
                                                                in_=o_ps[i2][:, 0:128]),
                                     R=[b_ops[i2]], W=[bm])
                    k.dma("st_mix%d" % sl, mix_d[:, :, t0:t0 + n].rearrange("g p t -> p g t"), mx[:, :, 0:n], R=[bm],
                          q=POOL)
                k.barrier()

            with ExitStack() as es:
                E = es.enter_context
                b_const = B()
                wo = E(sbt("wo", [128, 16, D], BF16))
                SP.wait_tok(conv_tok["out%d" % l])
                k.dma("c_wo", wo[:], wb_out[l].rearrange("(c p) n -> p c n", p=128), W=[b_const])
                gm = E(sbt("gm", [128, 2, D], F32))
                for s in range(2):
                    k.dma("c_gm", gm[:, s, :], mod_d[l, s:s + 1, 2 * D:3 * D].partition_broadcast(128), W=[b_const])
                mixs = [E(sbt("mixs%d" % i, [128, 16, 512], BF16)) for i in range(2)]
                xs = [E(sbt("xs%d" % i, [128, D], F32)) for i in range(2)]
                tmp = [E(sbt("tmp%d" % i, [128, 512], F32)) for i in range(2)]
                xo = [E(sbt("xo%d" % i, [128, D], F32)) for i in range(2)]
                po = [E(pst("po%d" % i, [128, 512], F32)) for i in range(8)]
                b_mixs, b_xs, b_tmp, b_xo, b_po = Bs(2), Bs(2), Bs(2), Bs(2), Bs(8)
                bi_ = 0
                tmi = 0
                for ti, (seg, gb0, nb) in enumerate(tl):
                    sl = ti % 2
                    n = nb * 128
                    t0 = gb0 * 128
                    k.dma("mixs%d" % sl, mixs[sl][:, :, 0:n], mix_d[:, :, t0:t0 + n].rearrange("g p t -> p g t"),
                          W=[b_mixs[sl]])
                    for b in range(nb):
                        i2 = bi_ % 2
                        bi_ += 1
                        r0 = (gb0 + b) * 128
                        k.dma("xs%d" % i2, xs[i2][:], x_src1[r0:r0 + 128, :], W=[b_xs[i2]])
                        for c in range(16):
                            for ft in range(4):
                                p = i2 * 4 + ft
                                k.do(PE, lambda: T.matmul(po[p][:], lhsT=mixs[sl][:, c, b * 128:(b + 1) * 128],
                                                          rhs=wo[:, c, ft * 512:(ft + 1) * 512], start=(c == 0),
                                                          stop=(c == 15)),
                                     R=[b_mixs[sl], b_const], W=[b_po[p]], inc=(c == 15 and ft == 3))
                        for ft in range(4):
                            p = i2 * 4 + ft
                            tm = tmi % 2
                            tmi += 1
                            k.do(DVE, lambda: V.tensor_tensor(out=tmp[tm][:], in0=po[p][:],
                                                              in1=gm[:, seg, ft * 512:(ft + 1) * 512], op=ALU.mult),
                                 R=[b_po[p], b_const], W=[b_tmp[tm]])
                            k.do(POOL, lambda: G.tensor_tensor(out=xo[i2][:, ft * 512:(ft + 1) * 512], in0=tmp[tm][:],
                                                               in1=xs[i2][:, ft * 512:(ft + 1) * 512], op=ALU.add),
                                 R=[b_tmp[tm], b_xs[i2]], W=[b_xo[i2]])
                        k.dma("st_xo%d" % i2, x_mid[r0:r0 + 128, :], xo[i2][:], R=[b_xo[i2]], q=POOL)
                k.barrier()

            blocks = [(seg, gb0 + b) for (seg, gb0, nb) in tl for b in range(nb)]
            NB3 = len(blocks)
            NTL = NB3 + 16
            NSLOT = NTL * 256
            Hs_l = Hslots[0:NSLOT, :]
            Ys_l = Yslots[0:NSLOT, :]
            with ExitStack() as es3:
                E3 = es3.enter_context
                b_rout = B()
                selA = E3(sbt("selA", [128, NB3, 16], F32))
                selB = E3(sbt("selB", [128, NB3, 16], F32))
                Rg = E3(sbt("Rg", [128, NB3, 16], F32))
                cA = E3(sbt("cA", [128, NB3], F32))
                cB = E3(sbt("cB", [128, NB3], F32))
                posA_i = E3(sbt("posA_i", [128, NB3], I32))
                posB_i = E3(sbt("posB_i", [128, NB3], I32))
                idxw = E3(sbt("idxw", [128, NTL], I32))
                base = E3(sbt("base", [128, 16], F32))

                with ExitStack() as es:
                    E = es.enter_context
                    b_const = B()
                    ident = load_const(E, "ident", [128, 128], BF16, ident_d, b_const)
                    utri = load_const(E, "utri", [128, 128], BF16, utri_d, b_const)
                    ones1 = load_const(E, "ones1", [128, 128], BF16, ones1_d, b_const)
                    thr = load_const(E, "thr", [128, 80], F32, thr_d, b_const)
                    iotap = load_const(E, "iotap", [128, 1], F32, iotap_d, b_const)
                    abc = E(sbt("abc", [128, 2, D], F32))
                    bbc = E(sbt("bbc", [128, 2, D], F32))
                    for s in range(2):
                        k.dma("c_abc", abc[:, s, :], mod_d[l, s:s + 1, 4 * D:5 * D].partition_broadcast(128), W=[b_const])
                        k.dma("c_bbc", bbc[:, s, :], mod_d[l, s:s + 1, 3 * D:4 * D].partition_broadcast(128), W=[b_const])
                    wr32 = load_const(E, "wr32", [128, 16, 20], F32, wr_d[l], b_const)
                    wrb = E(sbt("wrb", [128, 16, 20], BF16))
                    k.do(DVE, lambda: V.tensor_copy(out=wrb[:], in_=wr32[:]), R=[b_const], W=[b_const])
                    brb = load_const(E, "brb", [128, 20], F32, br_d[l].partition_broadcast(128), b_const)
                    xs = [E(sbt("xs%d" % i, [128, D], F32)) for i in range(2)]
                    tmpf = [E(sbt("tmpf%d" % i, [128, D], F32)) for i in range(2)]
                    junk = E(sbt("junk", [128, D], BF16))
                    ss = E(sbt("ss", [128, 12], F32))
                    xh = E(sbt("xh", [128, 4, D], BF16))
                    hT = E(sbt("hT", [128, 16, 512], BF16))
                    rt = E(sbt("rt", [128, 64], F32))
                    s16 = E(sbt("s16", [128, 16], BF16))
                    cmp = E(sbt("cmp", [128, 80], F32))
                    q16 = E(sbt("q16", [128, 64], F32))
                    etf = E(sbt("etf", [128, NTL], F32))
                    posf = E(sbt("posf", [128, 2, NB3], F32))
                    pT = [E(pst("pT%d" % i, [128, 1024], BF16)) for i in range(2)]
                    prt = E(pst("prt", [128, 512], F32))
                    prk = E(pst("prk", [128, 512], F32))
                    ptt = E(pst("ptt", [128, 512], F32))
                    b_xs, b_tmpf, b_pT = Bs(2), Bs(2), Bs(2)
                    b_junk, b_ss, b_xh, b_hT, b_rt, b_s16, b_prt, b_prk, b_ptt = B(), B(), B(), B(), B(), B(), B(), B(), B()
                    k.do(DVE, lambda: V.memset(base[:], 0.0), W=[b_rout])
                    zt = E(sbt("zt", [128, 4, D], BF16))
                    b_zt = B()
                    k.do(DVE, lambda: V.memset(zt[:], 0.0), W=[b_zt])
                    for j0 in range(0, 2 * NTL, 4):
                        k.dma("zero_hs", Hs_l[j0 * 128:(j0 + 4) * 128, :].rearrange("(j p) f -> p j f", p=128), zt[:],
                              R=[b_zt], q=POOL)
                    xi = 0
                    bi = 0
                    for ti, (seg, gb0, nb) in enumerate(tl):
                        n = nb * 128
                        k.do(DVE, lambda: V.memset(ss[:], 0.0), W=[b_ss])
                        for b in range(nb):
                            si = xi % 2
                            xi += 1
                            r0 = (gb0 + b) * 128
                            k.dma("xs%d" % si, xs[si][:], x_mid[r0:r0 + 128, :], W=[b_xs[si]])
                            k.do(ACT, lambda: A.activation(out=junk[:], in_=xs[si][:], func=AF.Square,
                                                           accum_out=ss[:, b:b + 1]), R=[b_xs[si], b_ss], W=[b_junk, b_ss])
                            k.do(ACT, lambda: A.activation(out=ss[:, 4 + b:5 + b], in_=ss[:, b:b + 1], func=AF.Sqrt,
                                                           scale=1.0 / D, bias=EPS), R=[b_ss], W=[b_ss])
                            k.do(DVE, lambda: V.reciprocal(out=ss[:, 8 + b:9 + b], in_=ss[:, 4 + b:5 + b]), R=[b_ss], W=[b_ss])
                            k.do(DVE, lambda: V.scalar_tensor_tensor(out=tmpf[si][:], in0=xs[si][:], scalar=ss[:, 8 + b:9 + b],
                                                                     in1=abc[:, seg, :], op0=ALU.mult, op1=ALU.mult),
                                 R=[b_xs[si], b_ss, b_const], W=[b_tmpf[si]])
                            k.do(POOL, lambda: G.tensor_tensor(out=xh[:, b, :], in0=tmpf[si][:], in1=bbc[:, seg, :], op=ALU.add),
                                 R=[b_tmpf[si], b_const], W=[b_xh])
                            k.dma("st_hrow", Hrows[r0:r0 + 128, :], xh[:, b, :], R=[b_xh], q=POOL)
                        for c in range(16):
                            pi = c % 2
                            for b in range(nb):
                                k.do(PE, lambda: T.transpose(out=pT[pi][:, b * 128:(b + 1) * 128],
                                                             in_=xh[:, b, c * 128:(c + 1) * 128], identity=ident[:]),
                                     R=[b_xh, b_const], W=[b_pT[pi]], inc=(b == nb - 1))
                            k.do(ACT, lambda: A.activation(out=hT[:, c, 0:n], in_=pT[pi][:, 0:n], func=AF.Identity),
                                 R=[b_pT[pi]], W=[b_hT])
                        for b in range(nb):
                            for c in range(16):
                                k.do(PE, lambda: T.matmul(prt[:, 0:20], lhsT=hT[:, c, b * 128:(b + 1) * 128], rhs=wrb[:, c, :],
                                                          start=(c == 0), stop=(c == 15)),
                                     R=[b_hT, b_const], W=[b_prt], inc=(c == 15))

                            def dv(fn, extraR=(), extraW=()):
                                k.do(DVE, fn, R=[b_rt] + list(extraR), W=[b_rt] + list(extraW))
                            dv(lambda: V.tensor_tensor(out=rt[:, 0:20], in0=prt[:, 0:20], in1=brb[:], op=ALU.add),
                               extraR=[b_prt, b_const])
                            dv(lambda: V.reduce_max(out=rt[:, 20:21], in_=rt[:, 0:4], axis=AX.X))
                            dv(lambda: V.tensor_scalar(out=rt[:, 24:28], in0=rt[:, 0:4], scalar1=rt[:, 20:21], scalar2=None,
                                                       op0=ALU.is_equal))
                            dv(lambda: V.tensor_scalar(out=rt[:, 21:22], in0=rt[:, 20:21], scalar1=-1.0, scalar2=None,
                                                       op0=ALU.mult))
                            dv(lambda: V.memset(rt[:, 22:23], 0.0))
                            k.do(ACT, lambda: A.activation(out=rt[:, 28:32], in_=rt[:, 0:4], func=AF.Exp, bias=rt[:, 21:22],
                                                           accum_out=rt[:, 22:23]), R=[b_rt], W=[b_rt])
                            dv(lambda: V.reciprocal(out=rt[:, 23:24], in_=rt[:, 22:23]))
                            dv(lambda: V.tensor_scalar(out=rt[:, 32:36], in0=rt[:, 4:8], scalar1=rt[:, 24:25], scalar2=None,
                                                       op0=ALU.mult))
                            for g in range(1, 4):
                                dv(lambda: V.scalar_tensor_tensor(out=rt[:, 32:36], in0=rt[:, 4 + 4 * g:8 + 4 * g],
                                                                  scalar=rt[:, 24 + g:25 + g], in1=rt[:, 32:36],
                                                                  op0=ALU.mult, op1=ALU.add))
                            dv(lambda: V.reduce_max(out=rt[:, 36:37], in_=rt[:, 32:36], axis=AX.X))
                            dv(lambda: V.tensor_scalar(out=rt[:, 40:44], in0=rt[:, 32:36], scalar1=rt[:, 36:37], scalar2=None,
                                                       op0=ALU.is_equal))
                            dv(lambda: V.scalar_tensor_tensor(out=rt[:, 44:48], in0=rt[:, 40:44], scalar=NEG,
                                                              in1=rt[:, 32:36], op0=ALU.mult, op1=ALU.add))
                            dv(lambda: V.reduce_max(out=rt[:, 37:38], in_=rt[:, 44:48], axis=AX.X))
                            dv(lambda: V.tensor_scalar(out=rt[:, 48:52], in0=rt[:, 44:48], scalar1=rt[:, 37:38], scalar2=None,
                                                       op0=ALU.is_equal))
                            dv(lambda: V.tensor_scalar(out=rt[:, 38:39], in0=rt[:, 36:37], scalar1=-1.0, scalar2=None,
                                                       op0=ALU.mult))
                            k.do(ACT, lambda: A.activation(out=rt[:, 52:56], in_=rt[:, 32:36], func=AF.Exp, bias=rt[:, 38:39]),
                                 R=[b_rt], W=[b_rt])
                            dv(lambda: V.tensor_tensor(out=rt[:, 56:60], in0=rt[:, 52:56], in1=rt[:, 48:52], op=ALU.mult))
                            dv(lambda: V.reduce_sum(out=rt[:, 39:40], in_=rt[:, 56:60], axis=AX.X))
                            dv(lambda: V.tensor_scalar(out=rt[:, 60:61], in0=rt[:, 39:40], scalar1=1.0, scalar2=None,
                                                       op0=ALU.add))
                            dv(lambda: V.reciprocal(out=rt[:, 61:62], in_=rt[:, 60:61]))
                            dv(lambda: V.tensor_tensor(out=cA[:, bi:bi + 1], in0=rt[:, 61:62], in1=rt[:, 23:24], op=ALU.mult),
                               extraW=[b_rout])
                            dv(lambda: V.tensor_tensor(out=cB[:, bi:bi + 1], in0=cA[:, bi:bi + 1], in1=rt[:, 39:40], op=ALU.mult),
                               extraR=[b_rout], extraW=[b_rout])
                            for g in range(4):
                                dv(lambda: V.tensor_scalar(out=selA[:, bi, 4 * g:4 * g + 4], in0=rt[:, 40:44],
                                                           scalar1=rt[:, 24 + g:25 + g], scalar2=None, op0=ALU.mult),
                                   extraW=[b_rout])
                                dv(lambda: V.tensor_scalar(out=selB[:, bi, 4 * g:4 * g + 4], in0=rt[:, 48:52],
                                                           scalar1=rt[:, 24 + g:25 + g], scalar2=None, op0=ALU.mult),
                                   extraW=[b_rout])
                            k.do(DVE, lambda: V.tensor_tensor(out=s16[:], in0=selA[:, bi, :], in1=selB[:, bi, :], op=ALU.add),
                                 R=[b_rout], W=[b_s16])
                            k.do(PE, lambda: T.matmul(prk[:, 0:16], lhsT=utri[:], rhs=s16[:], start=True, stop=True),
                                 R=[b_const, b_s16], W=[b_prk])
                            k.do(PE, lambda: T.matmul(ptt[:, 0:16], lhsT=ones1[:], rhs=s16[:], start=True, stop=True),
                                 R=[b_const, b_s16], W=[b_ptt])
                            k.do(DVE, lambda: V.tensor_tensor(out=Rg[:, bi, :], in0=prk[:, 0:16], in1=base[:], op=ALU.add),
                                 R=[b_prk, b_rout], W=[b_rout])
                            k.do(DVE, lambda: V.tensor_tensor(out=base[:], in0=ptt[:, 0:16], in1=base[:], op=ALU.add),
                                 R=[b_ptt, b_rout], W=[b_rout])
                            bi += 1
                    def dr(fn):
                        k.do(DVE, fn, R=[b_rout, b_rt, b_const], W=[b_rout, b_rt])
                    for e in range(16):
                        dr(lambda: V.tensor_scalar(out=cmp[:, 0:80], in0=thr[:, 0:80], scalar1=base[:, e:e + 1], scalar2=None,
                                                   op0=ALU.is_lt))
                        dr(lambda: V.reduce_sum(out=q16[:, e:e + 1], in_=cmp[:, 0:80], axis=AX.X))
                    dr(lambda: V.tensor_scalar(out=q16[:, 0:16], in0=q16[:, 0:16], scalar1=256.0, scalar2=None, op0=ALU.mult))
                    dr(lambda: V.memset(q16[:, 16:17], 0.0))
                    for e in range(1, 16):
                        dr(lambda: V.tensor_tensor(out=q16[:, 16 + e:17 + e], in0=q16[:, 15 + e:16 + e], in1=q16[:, e - 1:e],
                                                   op=ALU.add))
                    dr(lambda: V.tensor_tensor(out=q16[:, 32:48], in0=q16[:, 16:32], in1=q16[:, 0:16], op=ALU.add))
                    dr(lambda: V.memset(etf[:], 0.0))
                    for e in range(16):
                        dr(lambda: V.tensor_scalar(out=cmp[:, 0:NTL], in0=thr[:, 0:NTL], scalar1=q16[:, 32 + e:33 + e],
                                                   scalar2=None, op0=ALU.is_ge))
                        dr(lambda: V.tensor_tensor(out=etf[:], in0=etf[:], in1=cmp[:, 0:NTL], op=ALU.add))
                    dr(lambda: V.tensor_scalar(out=etf[:], in0=etf[:], scalar1=15.0, scalar2=None, op0=ALU.min))
                    dr(lambda: V.tensor_scalar(out=etf[:], in0=etf[:], scalar1=128.0, scalar2=iotap[:, 0:1], op0=ALU.mult,
                                               op1=ALU.add))
                    dr(lambda: V.tensor_copy(out=idxw[:], in_=etf[:]))
                    for bi in range(NB3):
                        dr(lambda: V.tensor_tensor(out=rt[:, 0:16], in0=Rg[:, bi, :], in1=q16[:, 16:32], op=ALU.add))
                        dr(lambda: V.tensor_tensor(out=rt[:, 16:32], in0=rt[:, 0:16], in1=selA[:, bi, :], op=ALU.mult))
                        dr(lambda: V.reduce_sum(out=posf[:, 0, bi:bi + 1], in_=rt[:, 16:32], axis=AX.X))
                        dr(lambda: V.tensor_tensor(out=rt[:, 32:48], in0=rt[:, 0:16], in1=selB[:, bi, :], op=ALU.mult))
                        dr(lambda: V.reduce_sum(out=posf[:, 1, bi:bi + 1], in_=rt[:, 32:48], axis=AX.X))
                    dr(lambda: V.tensor_copy(out=posA_i[:], in_=posf[:, 0, :]))
                    dr(lambda: V.tensor_copy(out=posB_i[:], in_=posf[:, 1, :]))
                    k.barrier()

                with ExitStack() as es:
                    E = es.enter_context
                    hr = [E(sbt("hr%d" % i, [128, D], BF16)) for i in range(4)]
                    b_hr = Bs(4)
                    for bi, (seg, gb) in enumerate(blocks):
                        hi = bi % 4
                        k.dma("hr%d" % hi, hr[hi][:], Hrows[gb * 128:(gb + 1) * 128, :], W=[b_hr[hi]])
                        k.dma_ind("sc_a%d" % hi, Hs_l, bass.IndirectOffsetOnAxis(ap=posA_i[:, bi:bi + 1], axis=0), hr[hi][:], None,
                                  NSLOT, R=[b_hr[hi], b_rout])
                        k.dma_ind("sc_b%d" % hi, Hs_l, bass.IndirectOffsetOnAxis(ap=posB_i[:, bi:bi + 1], axis=0), hr[hi][:], None,
                                  NSLOT, R=[b_hr[hi], b_rout])
                    k.barrier()

                with ExitStack() as es:
                    E = es.enter_context
                    b_const = B()
                    ident = load_const(E, "ident", [128, 128], BF16, ident_d, b_const)
                    wq = [E(sbt("wq%d" % i, [128, 8192], BF16)) for i in range(6)]
                    hs = [E(sbt("hs%d" % i, [128, 2, D], BF16)) for i in range(2)]
                    hsT = [E(sbt("hsT%d" % i, [128, 16, 256], BF16)) for i in range(2)]
                    he = [E(sbt("he%d" % i, [128, 4, 256], BF16)) for i in range(2)]
                    sg = [E(sbt("sg%d" % i, [128, 256], F32)) for i in range(2)]
                    yo = [E(sbt("yo%d" % i, [128, D], F32)) for i in range(2)]
                    pT = [E(pst("pT%d" % i, [128, 1024], BF16)) for i in range(2)]
                    pg = [E(pst("pg%d" % i, [128, 512], F32)) for i in range(2)]
                    pu = [E(pst("pu%d" % i, [128, 512], F32)) for i in range(2)]
                    pd = [E(pst("pd%d" % i, [128, 512], F32)) for i in range(2)]
                    b_wq, b_hs, b_hsT, b_he, b_sg, b_yo = Bs(6), Bs(2), Bs(2), Bs(2), Bs(2), Bs(2)
                    b_pT, b_pg, b_pu, b_pd = Bs(2), Bs(2), Bs(2), Bs(2)
                    POOL.wait_tok(conv_tok["g%d" % l])
                    POOL.wait_tok(conv_tok["u%d" % l])
                    POOL.wait_tok(conv_tok["d%d" % l])
                    wsrc = [wb_g[l], wb_u[l], wb_d[l]]

                    def load_w(i):
                        for m in range(3):
                            ws = (3 * i + m) % 6
                            k.dma_ind("wq%d" % ws, wq[ws][:], None, wsrc[m],
                                      bass.IndirectOffsetOnAxis(ap=idxw[:, i:i + 1], axis=0), 2048, R=[b_rout], W=[b_wq[ws]])
                    load_w(0)
                    ji_ = 0
                    di_ = 0
                    ev_ = 0
                    for i in range(NTL):
                        sl = i % 2
                        if i + 1 < NTL:
                            load_w(i + 1)
                        k.dma("hs%d" % sl, hs[sl][:], Hs_l[i * 256:(i + 1) * 256, :].rearrange("(s p) f -> p s f", p=128),
                              W=[b_hs[sl]])
                        wgs, wus, wds = [(3 * i + m) % 6 for m in range(3)]
                        wgv = wq[wgs][:].rearrange("p (c n) -> p c n", c=16)
                        wuv = wq[wus][:].rearrange("p (c n) -> p c n", c=16)
                        wdv = wq[wds][:].rearrange("p (c n) -> p c n", c=4)
                        for c in range(16):
                            pi = c % 2
                            for sb_ in range(2):
                                k.do(PE, lambda: T.transpose(out=pT[pi][:, sb_ * 128:(sb_ + 1) * 128],
                                                             in_=hs[sl][:, sb_, c * 128:(c + 1) * 128], identity=ident[:]),
                                     R=[b_hs[sl], b_const], W=[b_pT[pi]], inc=(sb_ == 1))
                            if c % 2 == 0:
                                k.do(ACT, lambda: A.activation(out=hsT[sl][:, c, :], in_=pT[pi][:, 0:256], func=AF.Identity),
                                     R=[b_pT[pi]], W=[b_hsT[sl]])
                            else:
                                k.do(DVE, lambda: V.tensor_copy(out=hsT[sl][:, c, :], in_=pT[pi][:, 0:256]),
                                     R=[b_pT[pi]], W=[b_hsT[sl]])
                        for j in range(4):
                            pi = ji_ % 2
                            ji_ += 1
                            for c in range(16):
                                k.do(PE, lambda: T.matmul(pg[pi][:, 0:256], lhsT=wgv[:, c, j * 128:(j + 1) * 128],
                                                          rhs=hsT[sl][:, c, :], start=(c == 0), stop=(c == 15)),
                                     R=[b_wq[wgs], b_hsT[sl]], W=[b_pg[pi]], inc=(c == 15))
                            for c in range(16):
                                k.do(PE, lambda: T.matmul(pu[pi][:, 0:256], lhsT=wuv[:, c, j * 128:(j + 1) * 128],
                                                          rhs=hsT[sl][:, c, :], start=(c == 0), stop=(c == 15)),
                                     R=[b_wq[wus], b_hsT[sl]], W=[b_pu[pi]], inc=(c == 15))
                            k.do(ACT, lambda: A.activation(out=sg[pi][:], in_=pg[pi][:, 0:256], func=AF.Silu),
                                 R=[b_pg[pi]], W=[b_sg[pi]])
                            k.do(DVE, lambda: V.tensor_tensor(out=he[sl][:, j, :], in0=sg[pi][:], in1=pu[pi][:, 0:256],
                                                              op=ALU.mult), R=[b_sg[pi], b_pu[pi]], W=[b_he[sl]])
                        for sb_ in range(2):
                            yi = (2 * i + sb_) % 2
                            for ft in range(4):
                                pi = di_ % 2
                                di_ += 1
                                for j in range(4):
                                    k.do(PE, lambda: T.matmul(pd[pi][:], lhsT=he[sl][:, j, sb_ * 128:(sb_ + 1) * 128],
                                                              rhs=wdv[:, j, ft * 512:(ft + 1) * 512], start=(j == 0),
                                                              stop=(j == 3)),
                                         R=[b_he[sl], b_wq[wds]], W=[b_pd[pi]], inc=(j == 3))
                                ev_ += 1
                                if ev_ % 2 == 0:
                                    k.do(ACT, lambda: A.activation(out=yo[yi][:, ft * 512:(ft + 1) * 512], in_=pd[pi][:],
                                                                   func=AF.Identity), R=[b_pd[pi]], W=[b_yo[yi]])
                                else:
                                    k.do(DVE, lambda: V.tensor_copy(out=yo[yi][:, ft * 512:(ft + 1) * 512], in_=pd[pi][:]),
                                         R=[b_pd[pi]], W=[b_yo[yi]])
                            r0 = (2 * i + sb_) * 128
                            k.dma("st_yo%d" % yi, Ys_l[r0:r0 + 128, :], yo[yi][:], R=[b_yo[yi]])
                    k.barrier()

                with ExitStack() as es:
                    E = es.enter_context
                    b_const = B()
                    gf = E(sbt("gf", [128, 2, D], F32))
                    for s in range(2):
                        k.dma("c_gf", gf[:, s, :], mod_d[l, s:s + 1, 5 * D:6 * D].partition_broadcast(128), W=[b_const])
                    if l == 1:
                        fg = load_const(E, "fg", [128, D], F32, fng_d.partition_broadcast(128), b_const)
                        junk = E(sbt("junk", [128, D], BF16))
                        ss = E(sbt("ss", [128, 12], F32))
                        b_junk, b_ss = B(), B()
                    xs = [E(sbt("xs%d" % i, [128, D], F32)) for i in range(2)]
                    yA = [E(sbt("yA%d" % i, [128, D], F32)) for i in range(2)]
                    yB = [E(sbt("yB%d" % i, [128, D], F32)) for i in range(2)]
                    tt_ = [E(sbt("tt%d" % i, [128, D], F32)) for i in range(2)]
                    xo = [E(sbt("xo%d" % i, [128, D], F32)) for i in range(2)]
                    b_xs, b_yA, b_yB, b_tt, b_xo = Bs(2), Bs(2), Bs(2), Bs(2), Bs(2)
                    for bi, (seg, gb) in enumerate(blocks):
                        i2 = bi % 2
                        r0 = gb * 128
                        k.dma("xs%d" % i2, xs[i2][:], x_mid[r0:r0 + 128, :], W=[b_xs[i2]])
                        k.dma_ind("ga%d" % i2, yA[i2][:], None, Ys_l, bass.IndirectOffsetOnAxis(ap=posA_i[:, bi:bi + 1], axis=0),
                                  NSLOT, R=[b_rout], W=[b_yA[i2]])
                        k.dma_ind("gb%d" % i2, yB[i2][:], None, Ys_l, bass.IndirectOffsetOnAxis(ap=posB_i[:, bi:bi + 1], axis=0),
                                  NSLOT, R=[b_rout], W=[b_yB[i2]])
                        k.do(DVE, lambda: V.tensor_scalar(out=tt_[i2][:], in0=yA[i2][:], scalar1=cA[:, bi:bi + 1], scalar2=None,
                                                          op0=ALU.mult), R=[b_yA[i2], b_rout], W=[b_tt[i2]])
                        k.do(DVE, lambda: V.scalar_tensor_tensor(out=tt_[i2][:], in0=yB[i2][:], scalar=cB[:, bi:bi + 1],
                                                                 in1=tt_[i2][:], op0=ALU.mult, op1=ALU.add),
                             R=[b_yB[i2], b_rout, b_tt[i2]], W=[b_tt[i2]])
                        k.do(POOL, lambda: G.tensor_tensor(out=tt_[i2][:], in0=tt_[i2][:], in1=gf[:, seg, :], op=ALU.mult),
                             R=[b_tt[i2], b_const], W=[b_tt[i2]])
                        k.do(DVE, lambda: V.tensor_tensor(out=xo[i2][:], in0=tt_[i2][:], in1=xs[i2][:], op=ALU.add),
                             R=[b_tt[i2], b_xs[i2]], W=[b_xo[i2]])
                        if l == 0:
                            k.dma("st_xo%d" % i2, x_dst[r0:r0 + 128, :], xo[i2][:], R=[b_xo[i2]])
                        else:
                            k.do(DVE, lambda: V.memset(ss[:, 0:1], 0.0), W=[b_ss])
                            k.do(ACT, lambda: A.activation(out=junk[:], in_=xo[i2][:], func=AF.Square, accum_out=ss[:, 0:1]),
                                 R=[b_xo[i2], b_ss], W=[b_junk, b_ss])
                            k.do(ACT, lambda: A.activation(out=ss[:, 4:5], in_=ss[:, 0:1], func=AF.Sqrt, scale=1.0 / D,
                                                           bias=EPS), R=[b_ss], W=[b_ss])
                            k.do(DVE, lambda: V.reciprocal(out=ss[:, 8:9], in_=ss[:, 4:5]), R=[b_ss], W=[b_ss])
                            k.do(DVE, lambda: V.scalar_tensor_tensor(out=xo[i2][:], in0=xo[i2][:], scalar=ss[:, 8:9], in1=fg[:],
                                                                     op0=ALU.mult, op1=ALU.mult),
                                 R=[b_xo[i2], b_ss, b_const], W=[b_xo[i2]])
                            orow = (gb - 2) * 128 if seg == 0 else 2048 + (gb - 22) * 128
                            k.dma("st_y%d" % i2, y_d[orow:orow + 128, :], xo[i2][:], R=[b_xo[i2]])
                    k.barrier(final=(l == 1))
    return nc


_NC_CACHE = {}


def _alibi_bias():
    slopes = 2.0 ** (-8.0 * np.arange(1, 9, dtype=np.float64) / 8.0)
    q = np.arange(128)[:, None]
    s = np.arange(384)[None, :]
    dist = np.abs(s - 128 - q)
    out = np.empty((128, 8, 384), np.float32)
    for h in range(8):
        out[:, h, :] = np.where(dist <= 128, -slopes[h] * dist, NEG)
    return out


def kernel(x_prompt, x_sample, c_prompt, c_sample, norm_mix_g, norm_ffn_g, w_ada, b_ada, w_in, w_out,
           conv_a_w, conv_a_b, ln_a_g, ln_a_b, attn_sink, conv_c_w, w_router_group, b_router_group,
           w_router_expert, b_router_expert, w_gate, w_up, w_down, final_norm_g):
    f = lambda a: np.ascontiguousarray(np.asarray(a, dtype=np.float32))
    x_prompt, x_sample, c_prompt, c_sample = f(x_prompt), f(x_sample), f(c_prompt), f(c_sample)
    if "nc" not in _NC_CACHE:
        _NC_CACHE["nc"] = build_program()
    nc = _NC_CACHE["nc"]

    caw = f(np.transpose(f(conv_a_w).reshape(2, 31, 4, 128), (0, 3, 2, 1)))
    cab = f(np.transpose(f(conv_a_b).reshape(2, 4, 128), (0, 2, 1)))
    lng = f(np.transpose(f(ln_a_g).reshape(2, 4, 128), (0, 2, 1)))
    lnb = f(np.transpose(f(ln_a_b).reshape(2, 4, 128), (0, 2, 1)))
    ccw = f(np.transpose(f(conv_c_w).reshape(2, 3, 4, 128), (0, 3, 2, 1)))
    wr = np.concatenate([f(w_router_group), f(w_router_expert)], axis=-1)
    wr = f(np.transpose(wr.reshape(2, 16, 128, 20), (0, 2, 1, 3)))
    br = f(np.concatenate([f(b_router_group), f(b_router_expert)], axis=-1).reshape(2, 1, 20))
    shared = dict(
        ident=np.eye(128, dtype=np.float32).astype(ml_dtypes.bfloat16),
        ones=np.full((128, 128), 1.0 / 512, np.float32).astype(ml_dtypes.bfloat16),
        biasmat=_alibi_bias(),
        utri=np.triu(np.ones((128, 128), np.float32), 1).astype(ml_dtypes.bfloat16),
        ones1=np.ones((128, 128), np.float32).astype(ml_dtypes.bfloat16),
        thr=np.tile((256.0 * np.arange(80, dtype=np.float32))[None], (128, 1)),
        iotap=np.arange(128, dtype=np.float32).reshape(128, 1),
        norm_mix_g=f(norm_mix_g), norm_ffn_g=f(norm_ffn_g), w_ada=f(w_ada), b_ada=f(b_ada), w_in=f(w_in),
        w_out=f(w_out), caw=caw, cab=cab, lng=lng, lnb=lnb, sink=f(attn_sink).reshape(2, 1, 8), ccw=ccw, wr=wr, br=br,
        w_gate=f(w_gate), w_up=f(w_up), w_down=f(w_down), final_norm_g=f(final_norm_g).reshape(1, D),
    )
    in_maps = []
    for c in range(NCORES):
        sb, half = c // 2, c % 2
        xl = np.zeros((NT, D), np.float32)
        lo, hi = 2048 * c - 256, 2048 * c + 2048 + 256
        a, b = max(lo, 0), min(hi, 16384)
        xl[a - lo:b - lo] = x_prompt[0, a:b]
        lo, hi = 4096 * half - 256, 4096 * half + 4096 + 256
        a, b = max(lo, 0), min(hi, 8192)
        xl[2560 + a - lo:2560 + b - lo] = x_sample[sb, a:b]
        fl = np.array([c > 0, c < 7, half == 1, half == 0], np.float32)
        flags = np.zeros((128, 8), np.float32)
        flags[:, 0:4] = fl[None]
        flags[:, 4:8] = np.where(fl > 0, 0.0, NEG)[None]
        cc = np.stack([c_prompt[0], c_sample[sb]], axis=-1)
        cT = f(np.transpose(cc.reshape(16, 128, 2), (1, 0, 2)))
        in_maps.append(dict(shared, x_local=xl, flags=flags, cT=cT))

    res = run_bass_kernel_spmd(nc, in_maps, core_ids=list(range(NCORES)))
    y_prompt = np.empty((1, 16384, D), np.float32)
    y_sample = np.empty((4, 8192, D), np.float32)
    for c in range(NCORES):
        y = res.results[c]["y_local"]
        sb, half = c // 2, c % 2
        y_prompt[0, 2048 * c:2048 * c + 2048] = y[0:2048]
        y_sample[sb, 4096 * half:4096 * half + 4096] = y[2048:6144]
    return (y_prompt, y_sample)
```

```python
import numpy as np
import ml_dtypes
from contextlib import ExitStack
import concourse.bass as bass
import concourse.mybir as mybir
from concourse.bass_utils import run_bass_kernel_spmd

F32 = mybir.dt.float32
BF16 = mybir.dt.bfloat16
I32 = mybir.dt.int32
AF = mybir.ActivationFunctionType
ALU = mybir.AluOpType
AX = mybir.AxisListType

D = 2048
NCORES = 8
SEGS = [(0, 20), (20, 36)]
NBLK = 56
NT = NBLK * 128
NOWN = 6144
EPS = 1e-6
NEG = -1e30
SCALE = 128 ** -0.5


class Eng:
    def __init__(self, e, name, sem):
        self.e, self.name, self.sem, self.n, self.seen = e, name, sem, 0, {}

    def wait_tok(self, tok):
        if tok is None:
            return
        _, sem, cnt = tok
        key = id(sem)
        if self.seen.get(key, 0) >= cnt:
            return
        self.e.wait_ge(sem, cnt)
        self.seen[key] = cnt


class B:
    def __init__(self):
        self.w, self.r = {}, {}

    def read(self, eng):
        for t in self.w.values():
            if t[0] == "PE" and eng.name == "PE":
                continue
            eng.wait_tok(t)

    def write(self, eng):
        for k, t in self.r.items():
            if k != eng.name:
                eng.wait_tok(t)
        for k, t in self.w.items():
            if k != eng.name:
                eng.wait_tok(t)

    def did_read(self, tok):
        k = tok[0]
        if k not in self.r or self.r[k][2] < tok[2]:
            self.r[k] = tok

    def did_write(self, tok):
        self.w = {tok[0]: tok}
        self.r = {}


class K:
    def __init__(self, nc, es):
        self.nc = nc
        E = es.enter_context
        self.PE = Eng(nc.tensor, "PE", E(nc.semaphore("sem_pe")))
        self.ACT = Eng(nc.scalar, "ACT", E(nc.semaphore("sem_act")))
        self.DVE = Eng(nc.vector, "DVE", E(nc.semaphore("sem_dve")))
        self.POOL = Eng(nc.gpsimd, "POOL", E(nc.semaphore("sem_pool")))
        self.SP = Eng(nc.sync, "SP", E(nc.semaphore("sem_sp")))
        self.es = es
        self.dsem = {}
        self.slot_of = {}
        self.sem_pool = []
        self.nsem = 0

    def do(self, eng, fn, R=(), W=(), inc=True):
        for b in R:
            b.read(eng)
        for b in W:
            b.write(eng)
        ins = fn()
        if inc:
            ins.then_inc(eng.sem, 1)
            eng.n += 1
            tok = (eng.name, eng.sem, eng.n)
        else:
            tok = (eng.name, eng.sem, eng.n + 1)
        for b in R:
            b.did_read(tok)
        for b in W:
            b.did_write(tok)
        return tok

    def dma(self, slot, out, in_, R=(), W=(), q=None):
        q = q or self.SP
        if slot not in self.dsem:
            self.dsem[slot] = self.new_sem(slot)
        s = self.dsem[slot]
        for b in R:
            b.read(q)
        for b in W:
            b.write(q)
        q.e.dma_start(out=out, in_=in_).then_inc(s[0], 16)
        s[1] += 16
        tok = ("dma:" + slot, s[0], s[1])
        for b in R:
            b.did_read(tok)
        for b in W:
            b.did_write(tok)
        return tok

    def new_sem(self, slot):
        if self.sem_pool and not slot.startswith("cv_"):
            return self.sem_pool.pop()
        self.nsem += 1
        return [self.es.enter_context(self.nc.semaphore("dq%d" % self.nsem)), 0]

    def dma_ind(self, slot, out, out_off, in_, in_off, nrows, R=(), W=()):
        q = self.POOL
        if slot not in self.dsem:
            self.dsem[slot] = self.new_sem(slot)
        s = self.dsem[slot]
        for b in R:
            b.read(q)
        for b in W:
            b.write(q)
        q.e.indirect_dma_start(out=out, out_offset=out_off, in_=in_, in_offset=in_off).then_inc(s[0], 16)
        s[1] += 16
        tok = ("dma:" + slot, s[0], s[1])
        for b in R:
            b.did_read(tok)
        for b in W:
            b.did_write(tok)
        return tok

    def all_tokens(self, final=False):
        toks = []
        for e in (self.PE, self.ACT, self.DVE, self.POOL):
            if e.n:
                toks.append((e.name, e.sem, e.n))
        for name, s in self.dsem.items():
            if s[1] and (final or not name.startswith("cv_")):
                toks.append(("dma:" + name, s[0], s[1]))
        return toks

    def barrier(self, final=False):
        toks = self.all_tokens(final)
        for e in (self.PE, self.ACT, self.DVE, self.POOL, self.SP):
            for t in toks:
                if t[0] != e.name:
                    e.wait_tok(t)
        for name in list(self.dsem.keys()):
            if not name.startswith("cv_"):
                self.sem_pool.append(self.dsem.pop(name))


def Bs(n):
    return [B() for _ in range(n)]


def build_program():
    nc = bass.Bass("TRN2", target_bir_lowering=False)

    _uid = [0]

    def sbt(name, shape, dt):
        _uid[0] += 1
        return nc.sbuf_tensor("sb%d_%s" % (_uid[0], name), shape, dt)

    def pst(name, shape, dt):
        _uid[0] += 1
        return nc.psum_tensor("ps%d_%s" % (_uid[0], name), shape, dt)

    def din(name, shape, dt=F32):
        return nc.dram_tensor(name, list(shape), dt, kind="ExternalInput").ap()

    def dscr(name, shape, dt):
        return nc.dram_tensor(name, list(shape), dt, kind="Internal").ap()

    x_local = din("x_local", [NT, D])
    flags_d = din("flags", [128, 8])
    cT_d = din("cT", [128, 16, 2])
    ident_d = din("ident", [128, 128], BF16)
    ones_d = din("ones", [128, 128], BF16)
    bias_d = din("biasmat", [128, 8, 384])
    nmg_d = din("norm_mix_g", [2, D])
    nfg_d = din("norm_ffn_g", [2, D])
    wada_d = din("w_ada", [2, D, 6 * D])
    bada_d = din("b_ada", [2, 6 * D])
    win_d = din("w_in", [2, D, 4096])
    wout_d = din("w_out", [2, D, D])
    caw_d = din("caw", [2, 128, 4, 31])
    cab_d = din("cab", [2, 128, 4])
    lng_d = din("lng", [2, 128, 4])
    lnb_d = din("lnb", [2, 128, 4])
    sink_d = din("sink", [2, 1, 8])
    ccw_d = din("ccw", [2, 128, 4, 3])
    wr_d = din("wr", [2, 128, 16, 20])
    br_d = din("br", [2, 1, 20])
    wg_d = din("w_gate", [2, 16, D, 512])
    wu_d = din("w_up", [2, 16, D, 512])
    wd_d = din("w_down", [2, 16, 512, D])
    fng_d = din("final_norm_g", [1, D])
    utri_d = din("utri", [128, 128], BF16)
    ones1_d = din("ones1", [128, 128], BF16)
    thr_d = din("thr", [128, 80])
    iotap_d = din("iotap", [128, 1])
    y_d = nc.dram_tensor("y_local", [NOWN, D], F32, kind="ExternalOutput").ap()

    wb_in = dscr("wb_in", [2, D, 4096], BF16)
    wb_out = dscr("wb_out", [2, D, D], BF16)
    wb_g = [dscr("wb_g%d" % l, [2048, 8192], BF16) for l in range(2)]
    wb_u = [dscr("wb_u%d" % l, [2048, 8192], BF16) for l in range(2)]
    wb_d = [dscr("wb_d%d" % l, [2048, 8192], BF16) for l in range(2)]
    Hrows = dscr("Hrows", [NT, D], BF16)
    Hslots = dscr("Hslots", [68 * 256, D], BF16)
    Yslots = dscr("Yslots", [68 * 256, D], F32)
    mod_d = dscr("mod_d", [2, 2, 6 * D], F32)
    xa = dscr("xa", [NT, D], F32)
    xb = dscr("xb", [NT, D], F32)
    aT_d = dscr("aT", [4, 128, NT], BF16)
    qT_d = dscr("qT", [8, 128, NT], BF16)
    kT_d = dscr("kT", [2, 128, NT], BF16)
    v_d = dscr("v", [NT, 256], BF16)
    uT_d = dscr("uT", [4, 128, NT], F32)
    cbT_d = dscr("cbT", [4, 128, NT], F32)
    mix_d = dscr("mixT", [16, 128, NT], BF16)

    with ExitStack() as es_all:
        k = K(nc, es_all)
        PE, ACT, DVE, POOL, SP = k.PE, k.ACT, k.DVE, k.POOL, k.SP
        T, A, V, G = nc.tensor, nc.scalar, nc.vector, nc.gpsimd

        conv_tok = {}

        def cast(name, dst, src):
            conv_tok[name] = k.dma("cv_" + name, dst, src, q=POOL)

        def cast_in(l, i):
            cast("in%d" % l, wb_in[l, i * 256:(i + 1) * 256, :], win_d[l, i * 256:(i + 1) * 256, :])

        def cast_out(l, i):
            cast("out%d" % l, wb_out[l, i * 512:(i + 1) * 512, :], wout_d[l, i * 512:(i + 1) * 512, :])

        def cast_moe(l, e):
            cast("g%d" % l, wb_g[l][e * 128:(e + 1) * 128, :].rearrange("p (c n) -> p c n", c=16),
                 wg_d[l, e].rearrange("(c p) n -> p c n", p=128))
            cast("u%d" % l, wb_u[l][e * 128:(e + 1) * 128, :].rearrange("p (c n) -> p c n", c=16),
                 wu_d[l, e].rearrange("(c p) n -> p c n", p=128))
            cast("d%d" % l, wb_d[l][e * 128:(e + 1) * 128, :].rearrange("p (c n) -> p c n", c=4),
                 wd_d[l, e].rearrange("(c p) n -> p c n", p=128))

        for i in range(8):
            cast_in(0, i)
        for i in range(4):
            cast_out(0, i)

        with ExitStack() as es:
            E = es.enter_context
            siluT = E(sbt("siluT", [128, 16, 2], F32))
            wt = [E(sbt("wadat%d" % i, [128, 16, 512], F32)) for i in range(2)]
            modrow = E(sbt("modrow", [2, 6 * D], F32))
            badar = E(sbt("badar", [2, 6 * D], F32))
            grow = E(sbt("grow", [2, 2, D], F32))
            ps = [E(pst("pps%d" % i, [128, 512], F32)) for i in range(2)]
            b_silu, b_mod, b_bada, b_grow = B(), B(), B(), B()
            b_wt, b_ps = Bs(2), Bs(2)
            k.dma("siluT", siluT[:], cT_d, W=[b_silu])
            k.do(ACT, lambda: A.activation(out=siluT[:], in_=siluT[:], func=AF.Silu), R=[b_silu], W=[b_silu])
            it = 0
            for l in range(2):
                k.dma("bada", badar[:], bada_d[l:l + 1, :].partition_broadcast(2), W=[b_bada])
                k.dma("grow", grow[:, 0, :], nmg_d[l:l + 1, :].partition_broadcast(2), W=[b_grow])
                k.dma("grow", grow[:, 1, :], nfg_d[l:l + 1, :].partition_broadcast(2), W=[b_grow])
                for j in range(24):
                    s = it % 2
                    it += 1
                    k.dma("wadat%d" % s, wt[s][:],
                          wada_d[l, :, j * 512:(j + 1) * 512].rearrange("(c p) n -> p c n", p=128), W=[b_wt[s]])
                    for c in range(16):
                        k.do(PE, lambda: T.matmul(ps[s][0:2, :], lhsT=siluT[:, c, :], rhs=wt[s][:, c, :],
                                                  start=(c == 0), stop=(c == 15)),
                             R=[b_silu, b_wt[s]], W=[b_ps[s]], inc=(c == 15))
                    k.do(DVE, lambda: V.tensor_tensor(out=modrow[:, j * 512:(j + 1) * 512], in0=ps[s][0:2, :],
                                                      in1=badar[:, j * 512:(j + 1) * 512], op=ALU.add),
                         R=[b_ps[s], b_bada], W=[b_mod])
                for (off, gi) in ((D, 0), (4 * D, 1)):
                    k.do(DVE, lambda: V.scalar_tensor_tensor(out=modrow[:, off:off + D], in0=modrow[:, off:off + D],
                                                             scalar=1.0, in1=grow[:, gi, :], op0=ALU.add,
                                                             op1=ALU.mult), R=[b_mod, b_grow], W=[b_mod])
                k.dma("modout", mod_d[l], modrow[:], R=[b_mod], q=POOL)
            k.barrier()

        def load_const(E, name, shape, dt, src, b):
            t = E(sbt(name, shape, dt))
            k.dma("c_" + name, t[:], src, W=[b])
            return t

        def seg_of_block(gb):
            return 0 if gb < 20 else 1

        def tiles_full(l):
            res = []
            for s, (bs, nbk) in enumerate(SEGS):
                lo, hi = (1, nbk - 1) if l == 0 else (2, nbk - 2)
                b = lo
                while b < hi:
                    nb = min(4, hi - b)
                    res.append((s, bs + b, nb))
                    b += nb
            return res

        def load_fm_mod(E, name, l, off, b):
            t = E(sbt(name, [128, 2, 16], F32))
            with nc.allow_non_contiguous_dma(reason="tiny feature-major load of a modulation vector"):
                for s in range(2):
                    k.dma("c_" + name, t[:, s, :], mod_d[l, s, off:off + D].rearrange("(c p) -> p c", p=128), W=[b])
            return t

        def norm_transpose(x_src, gb0, nb, seg, xs, b_xs, xsi, junk, b_junk, ss, b_ss, xh, b_xh, pT, b_pT,
                           hT, b_hT, afm, bfm, b_const, ident):
            n = nb * 128
            k.do(DVE, lambda: V.memset(ss[:], 0.0), W=[b_ss])
            for b in range(nb):
                si = xsi[0] % len(xs)
                xsi[0] += 1
                k.dma("xs%d" % si, xs[si][:], x_src[(gb0 + b) * 128:(gb0 + b + 1) * 128, :], W=[b_xs[si]])
                k.do(ACT, lambda: A.activation(out=junk[:], in_=xs[si][:], func=AF.Square,
                                               accum_out=ss[:, b:b + 1]), R=[b_xs[si], b_ss], W=[b_junk, b_ss])
                k.do(ACT, lambda: A.activation(out=ss[:, 4 + b:5 + b], in_=ss[:, b:b + 1], func=AF.Sqrt,
                                               scale=1.0 / D, bias=EPS), R=[b_ss], W=[b_ss])
                k.do(DVE, lambda: V.reciprocal(out=ss[:, 8 + b:9 + b], in_=ss[:, 4 + b:5 + b]), R=[b_ss], W=[b_ss])
                k.do(DVE, lambda: V.tensor_scalar(out=xh[:, b, :], in0=xs[si][:], scalar1=ss[:, 8 + b:9 + b],
                                                  scalar2=None, op0=ALU.mult), R=[b_xs[si], b_ss], W=[b_xh])
            for c in range(16):
                pi = c % 2
                for b in range(nb):
                    k.do(PE, lambda: T.transpose(out=pT[pi][:, b * 128:(b + 1) * 128],
                                                 in_=xh[:, b, c * 128:(c + 1) * 128], identity=ident[:]),
                         R=[b_xh, b_const], W=[b_pT[pi]], inc=(b == nb - 1))
                k.do(ACT, lambda: A.activation(out=hT[:, c, 0:n], in_=pT[pi][:, 0:n], func=AF.Identity,
                                               scale=afm[:, seg, c:c + 1], bias=bfm[:, seg, c:c + 1]),
                     R=[b_pT[pi], b_const], W=[b_hT])

        for l in range(2):
            x_src1 = x_local if l == 0 else xb
            x_mid = xa
            x_dst = xb

            with ExitStack() as es:
                E = es.enter_context
                b_const = B()
                ident = load_const(E, "ident", [128, 128], BF16, ident_d, b_const)
                flg = load_const(E, "flg", [128, 8], F32, flags_d, b_const)
                afm = load_fm_mod(E, "afm", l, D, b_const)
                bfm = load_fm_mod(E, "bfm", l, 0, b_const)
                xs = [E(sbt("xs%d" % i, [128, D], F32)) for i in range(2)]
                junk = E(sbt("junk", [128, D], BF16))
                ss = E(sbt("ss", [128, 12], F32))
                xh = [E(sbt("xh%d" % i, [128, 4, D], BF16)) for i in range(2)]
                hT = [E(sbt("hT%d" % i, [128, 16, 512], BF16)) for i in range(2)]
                wt = [E(sbt("wt%d" % i, [128, 16, 512], BF16)) for i in range(3)]
                sig = E(sbt("sig", [128, 4, 512], F32))
                o16 = [E(sbt("o16_%d" % i, [128, 4, 512], BF16)) for i in range(3)]
                o32 = [E(sbt("o32_%d" % i, [128, 4, 512], F32)) for i in range(2)]
                vo = E(sbt("vo", [128, 4, 256], BF16))
                pT = [E(pst("pT%d" % i, [128, 1024], BF16)) for i in range(2)]
                pz = [E(pst("pz%d" % i, [128, 512], F32)) for i in range(4)]
                b_xs, b_xh, b_hT, b_wt, b_pT, b_pz = Bs(2), Bs(2), Bs(2), Bs(3), Bs(2), Bs(4)
                b_junk, b_ss, b_sig, b_vo = B(), B(), B(), B()
                b_o16, b_o32 = Bs(3), Bs(2)
                xsi = [0]
                wi = 0
                pzi = 0
                o16i = 0
                o32i = 0
                first_w = True
                for t in range(14):
                    seg = 0 if t < 5 else 1
                    sl = t % 2
                    tok0 = t * 512
                    halo = None
                    if t in (0, 5):
                        halo = (0, 256, 0 if t == 0 else 2)
                    if t in (4, 13):
                        halo = (256, 512, 1 if t == 4 else 3)
                    norm_transpose(x_src1, t * 4, 4, seg, xs, b_xs, xsi, junk, b_junk, ss, b_ss, xh[sl], b_xh[sl],
                                   pT, b_pT, hT[sl], b_hT[sl], afm, bfm, b_const, ident)

                    def mask_halo(buf, bb, ngrp):
                        if halo is None:
                            return
                        lo, hi, fi = halo
                        k.do(POOL, lambda: G.tensor_scalar(out=buf[:, 0:ngrp, lo:hi], in0=buf[:, 0:ngrp, lo:hi],
                                                           scalar1=flg[:, fi:fi + 1], scalar2=None, op0=ALU.mult),
                             R=[bb, b_const], W=[bb])

                    for g in (1, 0, 2, 3, 4, 7, 5, 6):
                        ws = wi % 3
                        wi += 1
                        if first_w:
                            SP.wait_tok(conv_tok["in%d" % l])
                            first_w = False
                        k.dma("wt%d" % ws, wt[ws][:],
                              wb_in[l, :, g * 512:(g + 1) * 512].rearrange("(c p) n -> p c n", p=128), W=[b_wt[ws]])
                        nfm = 2 if g == 4 else 4
                        if g in (0, 2, 3, 4):
                            oi = o16i % 3
                            o16i += 1
                            ob, bo = o16[oi], b_o16[oi]
                        elif g in (5, 6):
                            oi = o32i % 2
                            o32i += 1
                            ob, bo = o32[oi], b_o32[oi]
                        for j in range(nfm):
                            p = pzi % 4
                            pzi += 1
                            for c in range(16):
                                k.do(PE, lambda: T.matmul(pz[p][:], lhsT=wt[ws][:, c, j * 128:(j + 1) * 128],
                                                          rhs=hT[sl][:, c, :], start=(c == 0), stop=(c == 15)),
                                     R=[b_wt[ws], b_hT[sl]], W=[b_pz[p]], inc=(c == 15))
                            if g == 1:
                                k.do(ACT, lambda: A.activation(out=sig[:, j, :], in_=pz[p][:], func=AF.Sigmoid),
                                     R=[b_pz[p]], W=[b_sig])
                            elif g == 0:
                                k.do(DVE, lambda: V.tensor_tensor(out=ob[:, j, :], in0=pz[p][:], in1=sig[:, j, :],
                                                                  op=ALU.mult), R=[b_pz[p], b_sig], W=[bo])
                            elif g in (2, 3, 4):
                                k.do(ACT, lambda: A.activation(out=ob[:, j, :], in_=pz[p][:], func=AF.Identity),
                                     R=[b_pz[p]], W=[bo])
                            elif g == 7:
                                k.do(ACT, lambda: A.activation(out=sig[:, j, :], in_=pz[p][:], func=AF.Identity),
                                     R=[b_pz[p]], W=[b_sig])
                            elif g == 5:
                                k.do(DVE, lambda: V.tensor_tensor(out=ob[:, j, :], in0=pz[p][:], in1=sig[:, j, :],
                                                                  op=ALU.mult), R=[b_pz[p], b_sig], W=[bo])
                            elif g == 6:
                                k.do(ACT, lambda: A.activation(out=ob[:, j, :], in_=pz[p][:], func=AF.Identity),
                                     R=[b_pz[p]], W=[bo])
                        if g == 4:
                            for b in range(4):
                                p = pzi % 4
                                pzi += 1
                                for c in range(16):
                                    k.do(PE, lambda: T.matmul(pz[p][:, 0:256], lhsT=hT[sl][:, c, b * 128:(b + 1) * 128],
                                                              rhs=wt[ws][:, c, 256:512], start=(c == 0),
                                                              stop=(c == 15)),
                                         R=[b_wt[ws], b_hT[sl]], W=[b_pz[p]], inc=(c == 15))
                                k.do(DVE, lambda: V.tensor_copy(out=vo[:, b, :], in_=pz[p][:, 0:256]),
                                     R=[b_pz[p]], W=[b_vo])
                            if halo is not None:
                                lo, hi, fi = halo
                                k.do(POOL, lambda: G.tensor_scalar(out=vo[:, lo // 128:hi // 128, :],
                                                                   in0=vo[:, lo // 128:hi // 128, :],
                                                                   scalar1=flg[:, fi:fi + 1], scalar2=None,
                                                                   op0=ALU.mult), R=[b_vo, b_const], W=[b_vo])
                            k.dma("st_v", v_d[tok0:tok0 + 512, :].rearrange("(b p) d -> p b d", p=128), vo[:],
                                  R=[b_vo], q=POOL)
                        if g == 0:
                            mask_halo(ob, bo, 4)
                            k.dma("st_a", aT_d[:, :, tok0:tok0 + 512].rearrange("g p t -> p g t"), ob[:], R=[bo], q=POOL)
                        elif g in (2, 3):
                            h0 = (g - 2) * 4
                            k.dma("st_q%d" % g, qT_d[h0:h0 + 4, :, tok0:tok0 + 512].rearrange("g p t -> p g t"), ob[:],
                                  R=[bo], q=POOL)
                        elif g == 4:
                            mask_halo(ob, bo, 2)
                            k.dma("st_k", kT_d[:, :, tok0:tok0 + 512].rearrange("g p t -> p g t"), ob[:, 0:2, :],
                                  R=[bo], q=POOL)
                        elif g == 5:
                            mask_halo(ob, bo, 4)
                            k.dma("st_u", uT_d[:, :, tok0:tok0 + 512].rearrange("g p t -> p g t"), ob[:], R=[bo], q=POOL)
                        elif g == 6:
                            k.dma("st_cb", cbT_d[:, :, tok0:tok0 + 512].rearrange("g p t -> p g t"), ob[:], R=[bo],
                                  q=POOL)
                k.barrier()

            tl = tiles_full(l)
            with ExitStack() as es:
                E = es.enter_context
                b_const = B()
                ident = load_const(E, "ident", [128, 128], BF16, ident_d, b_const)
                onesb = load_const(E, "onesb", [128, 128], BF16, ones_d, b_const)
                flg = load_const(E, "flg", [128, 8], F32, flags_d, b_const)
                bias8 = load_const(E, "bias8", [128, 8, 384], F32, bias_d, b_const)
                caw = load_const(E, "caw", [128, 4, 31], F32, caw_d[l], b_const)
                cab = load_const(E, "cab", [128, 4], F32, cab_d[l], b_const)
                lng = load_const(E, "lng", [128, 4], F32, lng_d[l], b_const)
                lnb = load_const(E, "lnb", [128, 4], F32, lnb_d[l], b_const)
                ccw = load_const(E, "ccw", [128, 4, 3], F32, ccw_d[l], b_const)
                sinkb = load_const(E, "sinkb", [128, 8], F32, sink_d[l].partition_broadcast(128), b_const)
                Dg = E(sbt("Dg", [128, 31, 4, 128], BF16))
                b_Dg = B()
                for kk in range(31):
                    for g in range(4):
                        k.do(DVE, lambda: V.tensor_scalar(out=Dg[:, kk, g, :], in0=ident[:],
                                                          scalar1=caw[:, g, kk:kk + 1], scalar2=None, op0=ALU.mult),
                             R=[b_const], W=[b_Dg])
                a_sb = [E(sbt("a_sb%d" % i, [128, 4, 542], BF16)) for i in range(2)]
                q_sb = [E(sbt("q_sb%d" % i, [128, 8, 512], BF16)) for i in range(2)]
                k_sb = [E(sbt("k_sb%d" % i, [128, 2, 768], BF16)) for i in range(2)]
                v_sb = [E(sbt("v_sb%d" % i, [128, 6, 256], BF16)) for i in range(2)]
                u_sb = [E(sbt("u_sb%d" % i, [128, 4, 514], F32)) for i in range(2)]
                cb_sb = [E(sbt("cb_sb%d" % i, [128, 4, 512], F32)) for i in range(2)]
                mixT = [E(sbt("mixT%d" % i, [128, 16, 512], BF16)) for i in range(2)]
                cv = E(sbt("cv", [128, 4, 512], F32))
                cvb = E(sbt("cvb", [128, 4, 512], BF16))
                cv2 = E(sbt("cv2", [128, 4, 512], BF16))
                mean_sb = E(sbt("mean_sb", [128, 512], F32))
                var_sb = E(sbt("var_sb", [128, 512], F32))
                rstd_sb = E(sbt("rstd_sb", [128, 512], F32))
                t1 = [E(sbt("t1_%d" % i, [128, 512], F32)) for i in range(2)]
                s_sb = [E(sbt("s_sb%d" % i, [128, 384], F32)) for i in range(2)]
                p_sb = [E(sbt("p_sb%d" % i, [128, 384], F32)) for i in range(2)]
                pn_sb = [E(sbt("pn_sb%d" % i, [128, 384], BF16)) for i in range(2)]
                pT_sb = [E(sbt("pT_sb%d" % i, [128, 384], BF16)) for i in range(2)]
                st = [E(sbt("st%d" % i, [128, 8], F32)) for i in range(2)]
                acc = [E(sbt("acc%d" % i, [128, 512], F32)) for i in range(2)]
                acc2 = [E(sbt("acc2_%d" % i, [128, 512], F32)) for i in range(2)]
                b_acc2 = Bs(2)
                pc0 = E(pst("pc0", [128, 512], F32))
                pc = [pc0, pc0]
                pmean = E(pst("pmean", [128, 512], F32))
                pex2 = pmean
                s_ps = [E(pst("s_ps%d" % i, [128, 512], F32)) for i in range(2)]
                pT_ps = [E(pst("pT_ps%d" % i, [128, 1024], BF16)) for i in range(2)]
                o_ps = [E(pst("o_ps%d" % i, [128, 512], F32)) for i in range(2)]
                b_a, b_q, b_k, b_v, b_u, b_cb, b_mix = Bs(2), Bs(2), Bs(2), Bs(2), Bs(2), Bs(2), Bs(2)
                b_cv, b_cvb, b_cv2, b_mean, b_var, b_rstd = B(), B(), B(), B(), B(), B()
                b_t1, b_s, b_p, b_pn, b_pTs, b_st, b_acc = Bs(2), Bs(2), Bs(2), Bs(2), Bs(2), Bs(2), Bs(2)
                b_pc0, b_sps = B(), Bs(2)
                b_pc = [b_pc0, b_pc0]
                b_pmean = B()
                b_pex2 = b_pmean
                b_pTp, b_ops = Bs(2), Bs(2)
                hi_ = 0
                gi_ = 0
                for ti, (seg, gb0, nb) in enumerate(tl):
                    sl = ti % 2
                    n = nb * 128
                    t0 = gb0 * 128
                    bs, nbk = SEGS[seg]
                    k.dma("a_sb%d" % sl, a_sb[sl][:, :, 0:n + 30],
                          aT_d[:, :, t0 - 15:t0 + n + 15].rearrange("g p t -> p g t"), W=[b_a[sl]])
                    k.dma("q_sb%d" % sl, q_sb[sl][:, :, 0:n], qT_d[:, :, t0:t0 + n].rearrange("g p t -> p g t"),
                          W=[b_q[sl]])
                    k.dma("k_sb%d" % sl, k_sb[sl][:, :, 0:n + 256],
                          kT_d[:, :, t0 - 128:t0 + n + 128].rearrange("g p t -> p g t"), W=[b_k[sl]])
                    k.dma("v_sb%d" % sl, v_sb[sl][:, 0:nb + 2, :],
                          v_d[t0 - 128:t0 + n + 128, :].rearrange("(b p) d -> p b d", p=128), W=[b_v[sl]])
                    k.dma("u_sb%d" % sl, u_sb[sl][:, :, 0:n + 2],
                          uT_d[:, :, t0 - 1:t0 + n + 1].rearrange("g p t -> p g t"), W=[b_u[sl]])
                    k.dma("cb_sb%d" % sl, cb_sb[sl][:, :, 0:n], cbT_d[:, :, t0:t0 + n].rearrange("g p t -> p g t"),
                          W=[b_cb[sl]])
                    mx = mixT[sl]
                    bm = b_mix[sl]
                    for g in range(4):
                        pi = g % 2
                        for kk in range(31):
                            k.do(PE, lambda: T.matmul(pc[pi][:, 0:n], lhsT=Dg[:, kk, g, :],
                                                      rhs=a_sb[sl][:, g, kk:kk + n], start=(kk == 0), stop=(kk == 30)),
                                 R=[b_Dg, b_a[sl]], W=[b_pc[pi]], inc=(kk == 30))
                        k.do(ACT, lambda: A.activation(out=cv[:, g, 0:n], in_=pc[pi][:, 0:n], func=AF.Identity,
                                                       bias=cab[:, g:g + 1]), R=[b_pc[pi], b_const], W=[b_cv])
                        k.do(ACT, lambda: A.activation(out=cvb[:, g, 0:n], in_=pc[pi][:, 0:n], func=AF.Identity,
                                                       bias=cab[:, g:g + 1]), R=[b_pc[pi], b_const], W=[b_cvb])
                        k.do(ACT, lambda: A.activation(out=cv2[:, g, 0:n], in_=pc[pi][:, 0:n], func=AF.Square,
                                                       bias=cab[:, g:g + 1]), R=[b_pc[pi], b_const], W=[b_cv2])
                    for g in range(4):
                        k.do(PE, lambda: T.matmul(pmean[:, 0:n], lhsT=onesb[:], rhs=cvb[:, g, 0:n], start=(g == 0),
                                                  stop=(g == 3)), R=[b_const, b_cvb], W=[b_pmean], inc=(g == 3))
                    k.do(DVE, lambda: V.tensor_copy(out=mean_sb[:, 0:n], in_=pmean[:, 0:n]), R=[b_pmean], W=[b_mean])
                    for g in range(4):
                        k.do(PE, lambda: T.matmul(pex2[:, 0:n], lhsT=onesb[:], rhs=cv2[:, g, 0:n], start=(g == 0),
                                                  stop=(g == 3)), R=[b_const, b_cv2], W=[b_pex2], inc=(g == 3))
                    k.do(DVE, lambda: V.tensor_tensor(out=var_sb[:, 0:n], in0=mean_sb[:, 0:n], in1=mean_sb[:, 0:n],
                                                      op=ALU.mult), R=[b_mean], W=[b_var])
                    k.do(DVE, lambda: V.tensor_tensor(out=var_sb[:, 0:n], in0=pex2[:, 0:n], in1=var_sb[:, 0:n],
                                                      op=ALU.subtract), R=[b_pex2, b_var], W=[b_var])
                    k.do(DVE, lambda: V.tensor_scalar(out=var_sb[:, 0:n], in0=var_sb[:, 0:n], scalar1=0.0,
                                                      scalar2=None, op0=ALU.max), R=[b_var], W=[b_var])
                    k.do(ACT, lambda: A.activation(out=var_sb[:, 0:n], in_=var_sb[:, 0:n], func=AF.Sqrt, bias=EPS),
                         R=[b_var], W=[b_var])
                    k.do(DVE, lambda: V.reciprocal(out=rstd_sb[:, 0:n], in_=var_sb[:, 0:n]), R=[b_var], W=[b_rstd])
                    for g in range(4):
                        ti_ = gi_ % 2
                        gi_ += 1
                        k.do(DVE, lambda: V.tensor_tensor(out=t1[ti_][:, 0:n], in0=cv[:, g, 0:n], in1=mean_sb[:, 0:n],
                                                          op=ALU.subtract), R=[b_cv, b_mean], W=[b_t1[ti_]])
                        k.do(DVE, lambda: V.tensor_tensor(out=t1[ti_][:, 0:n], in0=t1[ti_][:, 0:n],
                                                          in1=rstd_sb[:, 0:n], op=ALU.mult),
                             R=[b_t1[ti_], b_rstd], W=[b_t1[ti_]])
                        k.do(ACT, lambda: A.activation(out=mx[:, g, 0:n], in_=t1[ti_][:, 0:n], func=AF.Silu,
                                                       scale=lng[:, g:g + 1], bias=lnb[:, g:g + 1]),
                             R=[b_t1[ti_], b_const], W=[bm])
                    for g in range(4):
                        ai = g % 2
                        k.do(ACT, lambda: A.activation(out=acc[ai][:, 0:n], in_=u_sb[sl][:, g, 0:n], func=AF.Identity,
                                                       scale=ccw[:, g, 0:1]), R=[b_u[sl], b_const], W=[b_acc[ai]])
                        for kk in (1, 2):
                            k.do(ACT, lambda: A.activation(out=acc2[kk - 1][:, 0:n], in_=u_sb[sl][:, g, kk:kk + n],
                                                           func=AF.Identity, scale=ccw[:, g, kk:kk + 1]),
                                 R=[b_u[sl], b_const], W=[b_acc2[kk - 1]])
                        for kk in (1, 2):
                            k.do(POOL, lambda: G.tensor_tensor(out=acc[ai][:, 0:n], in0=acc[ai][:, 0:n],
                                                               in1=acc2[kk - 1][:, 0:n], op=ALU.add),
                                 R=[b_acc2[kk - 1], b_acc[ai]], W=[b_acc[ai]])
                        k.do(POOL, lambda: G.tensor_tensor(out=mx[:, 12 + g, 0:n], in0=acc[ai][:, 0:n],
                                                           in1=cb_sb[sl][:, g, 0:n], op=ALU.mult),
                             R=[b_acc[ai], b_cb[sl]], W=[bm])
                    ntl_ = len(tl)
                    for e in range(ti * 16 // ntl_, (ti + 1) * 16 // ntl_):
                        cast_moe(l, e)
                    if l == 0:
                        for i in range(ti * 12 // ntl_, (ti + 1) * 12 // ntl_):
                            if i < 8:
                                cast_in(1, i)
                            else:
                                cast_out(1, i - 8)
                    for b in range(nb):
                        gb = gb0 + b
                        left_edge = (gb == bs + 2)
                        right_edge = (gb == bs + nbk - 3)
                        for hp in range(4):
                            kv = hp // 2
                            pair = [(0, 2 * hp), (1, 2 * hp + 1)]
                            for i2, h in pair:
                                k.do(PE, lambda: T.matmul(s_ps[i2][:, 0:384], lhsT=q_sb[sl][:, h, b * 128:(b + 1) * 128],
                                                          rhs=k_sb[sl][:, kv, b * 128:b * 128 + 384], start=True, stop=True),
                                     R=[b_q[sl], b_k[sl]], W=[b_sps[i2]])
                            for i2, h in pair:
                                sp, bsp = s_ps[i2], b_sps[i2]
                                ssb, bss = s_sb[i2], b_s[i2]
                                stt, bst = st[i2], b_st[i2]
                                k.do(DVE, lambda: V.scalar_tensor_tensor(out=ssb[:], in0=sp[:, 0:384], scalar=SCALE,
                                                                         in1=bias8[:, h, :], op0=ALU.mult, op1=ALU.add),
                                     R=[bsp, b_const], W=[bss])
                                if left_edge:
                                    fi = 4 + (0 if seg == 0 else 2)
                                    k.do(DVE, lambda: V.tensor_scalar(out=ssb[:, 0:128], in0=ssb[:, 0:128],
                                                                      scalar1=flg[:, fi:fi + 1], scalar2=None, op0=ALU.add),
                                         R=[bss, b_const], W=[bss])
                                if right_edge:
                                    fi = 4 + (1 if seg == 0 else 3)
                                    k.do(DVE, lambda: V.tensor_scalar(out=ssb[:, 256:384], in0=ssb[:, 256:384],
                                                                      scalar1=flg[:, fi:fi + 1], scalar2=None, op0=ALU.add),
                                         R=[bss, b_const], W=[bss])
                                k.do(DVE, lambda: V.reduce_max(out=stt[:, 0:1], in_=ssb[:], axis=AX.X), R=[bss], W=[bst])
                                k.do(DVE, lambda: V.tensor_scalar(out=stt[:, 1:2], in0=stt[:, 0:1], scalar1=sinkb[:, h:h + 1],
                                                                  scalar2=-1.0, op0=ALU.max, op1=ALU.mult),
                                     R=[bst, b_const], W=[bst])
                                k.do(DVE, lambda: V.memset(stt[:, 2:3], 0.0), R=[bst], W=[bst])
                            for i2, h in pair:
                                ssb, bss = s_sb[i2], b_s[i2]
                                stt, bst = st[i2], b_st[i2]
                                k.do(ACT, lambda: A.activation(out=p_sb[i2][:], in_=ssb[:], func=AF.Exp, bias=stt[:, 1:2],
                                                               accum_out=stt[:, 2:3]), R=[bss, bst], W=[b_p[i2], bst])
                                k.do(ACT, lambda: A.activation(out=stt[:, 3:4], in_=sinkb[:, h:h + 1], func=AF.Exp,
                                                               bias=stt[:, 1:2]), R=[bst, b_const], W=[bst])
                            for i2, h in pair:
                                stt, bst = st[i2], b_st[i2]
                                k.do(DVE, lambda: V.tensor_tensor(out=stt[:, 4:5], in0=stt[:, 2:3], in1=stt[:, 3:4],
                                                                  op=ALU.add), R=[bst], W=[bst])
                                k.do(DVE, lambda: V.reciprocal(out=stt[:, 5:6], in_=stt[:, 4:5]), R=[bst], W=[bst])
                                k.do(DVE, lambda: V.tensor_scalar(out=pn_sb[i2][:], in0=p_sb[i2][:], scalar1=stt[:, 5:6],
                                                                  scalar2=None, op0=ALU.mult),
                                     R=[b_p[i2], bst], W=[b_pn[i2]])
                            for i2, h in pair:
                                for j in range(3):
                                    k.do(PE, lambda: T.transpose(out=pT_ps[i2][:, j * 128:(j + 1) * 128],
                                                                 in_=pn_sb[i2][:, j * 128:(j + 1) * 128], identity=ident[:]),
                                         R=[b_pn[i2], b_const], W=[b_pTp[i2]], inc=(j == 2))
                            for i2, h in pair:
                                k.do(ACT, lambda: A.activation(out=pT_sb[i2][:],
                                                               in_=pT_ps[i2][:, 0:384], func=AF.Identity),
                                     R=[b_pTp[i2]], W=[b_pTs[i2]])
                            for i2, h in pair:
                                for j in range(3):
                                    k.do(PE, lambda: T.matmul(o_ps[i2][:, 0:128],
                                                              lhsT=v_sb[sl][:, b + j, kv * 128:(kv + 1) * 128],
                                                              rhs=pT_sb[i2][:, j * 128:(j + 1) * 128], start=(j == 0),
                                                              stop=(j == 2)),
                                         R=[b_v[sl], b_pTs[i2]], W=[b_ops[i2]], inc=(j == 2))
                            for i2, h in pair:
                                k.do(DVE, lambda: V.tensor_copy(out=mx[:, 4 + h, b * 128:(b + 1) * 128],
                                                                in_=o_ps[i2][:, 0:128]),
                                     R=[b_ops[i2]], W=[bm])
                    k.dma("st_mix%d" % sl, mix_d[:, :, t0:t0 + n].rearrange("g p t -> p g t"), mx[:, :, 0:n], R=[bm],
                          q=POOL)
                k.barrier()

            with ExitStack() as es:
                E = es.enter_context
                b_const = B()
                wo = E(sbt("wo", [128, 16, D], BF16))
                SP.wait_tok(conv_tok["out%d" % l])
                k.dma("c_wo", wo[:], wb_out[l].rearrange("(c p) n -> p c n", p=128), W=[b_const])
                gm = E(sbt("gm", [128, 2, D], F32))
                for s in range(2):
                    k.dma("c_gm", gm[:, s, :], mod_d[l, s:s + 1, 2 * D:3 * D].partition_broadcast(128), W=[b_const])
                mixs = [E(sbt("mixs%d" % i, [128, 16, 512], BF16)) for i in range(2)]
                xs = [E(sbt("xs%d" % i, [128, D], F32)) for i in range(2)]
                tmp = [E(sbt("tmp%d" % i, [128, 512], F32)) for i in range(2)]
                xo = [E(sbt("xo%d" % i, [128, D], F32)) for i in range(2)]
                po = [E(pst("po%d" % i, [128, 512], F32)) for i in range(8)]
                b_mixs, b_xs, b_tmp, b_xo, b_po = Bs(2), Bs(2), Bs(2), Bs(2), Bs(8)
                bi_ = 0
                tmi = 0
                for ti, (seg, gb0, nb) in enumerate(tl):
                    sl = ti % 2
                    n = nb * 128
                    t0 = gb0 * 128
                    k.dma("mixs%d" % sl, mixs[sl][:, :, 0:n], mix_d[:, :, t0:t0 + n].rearrange("g p t -> p g t"),
                          W=[b_mixs[sl]])
                    for b in range(nb):
                        i2 = bi_ % 2
                        bi_ += 1
                        r0 = (gb0 + b) * 128
                        k.dma("xs%d" % i2, xs[i2][:], x_src1[r0:r0 + 128, :], W=[b_xs[i2]])
                        for c in range(16):
                            for ft in range(4):
                                p = i2 * 4 + ft
                                k.do(PE, lambda: T.matmul(po[p][:], lhsT=mixs[sl][:, c, b * 128:(b + 1) * 128],
                                                          rhs=wo[:, c, ft * 512:(ft + 1) * 512], start=(c == 0),
                                                          stop=(c == 15)),
                                     R=[b_mixs[sl], b_const], W=[b_po[p]], inc=(c == 15 and ft == 3))
                        for ft in range(4):
                            p = i2 * 4 + ft
                            tm = tmi % 2
                            tmi += 1
                            k.do(DVE, lambda: V.tensor_tensor(out=tmp[tm][:], in0=po[p][:],
                                                              in1=gm[:, seg, ft * 512:(ft + 1) * 512], op=ALU.mult),
                                 R=[b_po[p], b_const], W=[b_tmp[tm]])
                            k.do(POOL, lambda: G.tensor_tensor(out=xo[i2][:, ft * 512:(ft + 1) * 512], in0=tmp[tm][:],
                                                               in1=xs[i2][:, ft * 512:(ft + 1) * 512], op=ALU.add),
                                 R=[b_tmp[tm], b_xs[i2]], W=[b_xo[i2]])
                        k.dma("st_xo%d" % i2, x_mid[r0:r0 + 128, :], xo[i2][:], R=[b_xo[i2]], q=POOL)
                k.barrier()

            blocks = [(seg, gb0 + b) for (seg, gb0, nb) in tl for b in range(nb)]
            NB3 = len(blocks)
            NTL = NB3 + 16
            NSLOT = NTL * 256
            Hs_l = Hslots[0:NSLOT, :]
            Ys_l = Yslots[0:NSLOT, :]
            with ExitStack() as es3:
                E3 = es3.enter_context
                b_rout = B()
                selA = E3(sbt("selA", [128, NB3, 16], F32))
                selB = E3(sbt("selB", [128, NB3, 16], F32))
                Rg = E3(sbt("Rg", [128, NB3, 16], F32))
                cA = E3(sbt("cA", [128, NB3], F32))
                cB = E3(sbt("cB", [128, NB3], F32))
                posA_i = E3(sbt("posA_i", [128, NB3], I32))
                posB_i = E3(sbt("posB_i", [128, NB3], I32))
                idxw = E3(sbt("idxw", [128, NTL], I32))
                base = E3(sbt("base", [128, 16], F32))

                with ExitStack() as es:
                    E = es.enter_context
                    b_const = B()
                    ident = load_const(E, "ident", [128, 128], BF16, ident_d, b_const)
                    utri = load_const(E, "utri", [128, 128], BF16, utri_d, b_const)
                    ones1 = load_const(E, "ones1", [128, 128], BF16, ones1_d, b_const)
                    thr = load_const(E, "thr", [128, 80], F32, thr_d, b_const)
                    iotap = load_const(E, "iotap", [128, 1], F32, iotap_d, b_const)
                    abc = E(sbt("abc", [128, 2, D], F32))
                    bbc = E(sbt("bbc", [128, 2, D], F32))
                    for s in range(2):
                        k.dma("c_abc", abc[:, s, :], mod_d[l, s:s + 1, 4 * D:5 * D].partition_broadcast(128), W=[b_const])
                        k.dma("c_bbc", bbc[:, s, :], mod_d[l, s:s + 1, 3 * D:4 * D].partition_broadcast(128), W=[b_const])
                    wr32 = load_const(E, "wr32", [128, 16, 20], F32, wr_d[l], b_const)
                    wrb = E(sbt("wrb", [128, 16, 20], BF16))
                    k.do(DVE, lambda: V.tensor_copy(out=wrb[:], in_=wr32[:]), R=[b_const], W=[b_const])
                    brb = load_const(E, "brb", [128, 20], F32, br_d[l].partition_broadcast(128), b_const)
                    xs = [E(sbt("xs%d" % i, [128, D], F32)) for i in range(2)]
                    tmpf = [E(sbt("tmpf%d" % i, [128, D], F32)) for i in range(2)]
                    junk = E(sbt("junk", [128, D], BF16))
                    ss = E(sbt("ss", [128, 12], F32))
                    xh = E(sbt("xh", [128, 4, D], BF16))
                    hT = E(sbt("hT", [128, 16, 512], BF16))
                    rt = E(sbt("rt", [128, 64], F32))
                    s16 = E(sbt("s16", [128, 16], BF16))
                    cmp = E(sbt("cmp", [128, 80], F32))
                    q16 = E(sbt("q16", [128, 64], F32))
                    etf = E(sbt("etf", [128, NTL], F32))
                    posf = E(sbt("posf", [128, 2, NB3], F32))
                    pT = [E(pst("pT%d" % i, [128, 1024], BF16)) for i in range(2)]
                    prt = E(pst("prt", [128, 512], F32))
                    prk = E(pst("prk", [128, 512], F32))
                    ptt = E(pst("ptt", [128, 512], F32))
                    b_xs, b_tmpf, b_pT = Bs(2), Bs(2), Bs(2)
                    b_junk, b_ss, b_xh, b_hT, b_rt, b_s16, b_prt, b_prk, b_ptt = B(), B(), B(), B(), B(), B(), B(), B(), B()
                    k.do(DVE, lambda: V.memset(base[:], 0.0), W=[b_rout])
                    zt = E(sbt("zt", [128, 4, D], BF16))
                    b_zt = B()
                    k.do(DVE, lambda: V.memset(zt[:], 0.0), W=[b_zt])
                    for j0 in range(0, 2 * NTL, 4):
                        k.dma("zero_hs", Hs_l[j0 * 128:(j0 + 4) * 128, :].rearrange("(j p) f -> p j f", p=128), zt[:],
                              R=[b_zt], q=POOL)
                    xi = 0
                    bi = 0
                    for ti, (seg, gb0, nb) in enumerate(tl):
                        n = nb * 128
                        k.do(DVE, lambda: V.memset(ss[:], 0.0), W=[b_ss])
                        for b in range(nb):
                            si = xi % 2
                            xi += 1
                            r0 = (gb0 + b) * 128
                            k.dma("xs%d" % si, xs[si][:], x_mid[r0:r0 + 128, :], W=[b_xs[si]])
                            k.do(ACT, lambda: A.activation(out=junk[:], in_=xs[si][:], func=AF.Square,
                                                           accum_out=ss[:, b:b + 1]), R=[b_xs[si], b_ss], W=[b_junk, b_ss])
                            k.do(ACT, lambda: A.activation(out=ss[:, 4 + b:5 + b], in_=ss[:, b:b + 1], func=AF.Sqrt,
                                                           scale=1.0 / D, bias=EPS), R=[b_ss], W=[b_ss])
                            k.do(DVE, lambda: V.reciprocal(out=ss[:, 8 + b:9 + b], in_=ss[:, 4 + b:5 + b]), R=[b_ss], W=[b_ss])
                            k.do(DVE, lambda: V.scalar_tensor_tensor(out=tmpf[si][:], in0=xs[si][:], scalar=ss[:, 8 + b:9 + b],
                                                                     in1=abc[:, seg, :], op0=ALU.mult, op1=ALU.mult),
                                 R=[b_xs[si], b_ss, b_const], W=[b_tmpf[si]])
                            k.do(POOL, lambda: G.tensor_tensor(out=xh[:, b, :], in0=tmpf[si][:], in1=bbc[:, seg, :], op=ALU.add),
                                 R=[b_tmpf[si], b_const], W=[b_xh])
                            k.dma("st_hrow", Hrows[r0:r0 + 128, :], xh[:, b, :], R=[b_xh], q=POOL)
                        for c in range(16):
                            pi = c % 2
                            for b in range(nb):
                                k.do(PE, lambda: T.transpose(out=pT[pi][:, b * 128:(b + 1) * 128],
                                                             in_=xh[:, b, c * 128:(c + 1) * 128], identity=ident[:]),
                                     R=[b_xh, b_const], W=[b_pT[pi]], inc=(b == nb - 1))
                            k.do(ACT, lambda: A.activation(out=hT[:, c, 0:n], in_=pT[pi][:, 0:n], func=AF.Identity),
                                 R=[b_pT[pi]], W=[b_hT])
                        for b in range(nb):
                            for c in range(16):
                                k.do(PE, lambda: T.matmul(prt[:, 0:20], lhsT=hT[:, c, b * 128:(b + 1) * 128], rhs=wrb[:, c, :],
                                                          start=(c == 0), stop=(c == 15)),
                                     R=[b_hT, b_const], W=[b_prt], inc=(c == 15))

                            def dv(fn, extraR=(), extraW=()):
                                k.do(DVE, fn, R=[b_rt] + list(extraR), W=[b_rt] + list(extraW))
                            dv(lambda: V.tensor_tensor(out=rt[:, 0:20], in0=prt[:, 0:20], in1=brb[:], op=ALU.add),
                               extraR=[b_prt, b_const])
                            dv(lambda: V.reduce_max(out=rt[:, 20:21], in_=rt[:, 0:4], axis=AX.X))
                            dv(lambda: V.tensor_scalar(out=rt[:, 24:28], in0=rt[:, 0:4], scalar1=rt[:, 20:21], scalar2=None,
                                                       op0=ALU.is_equal))
                            dv(lambda: V.tensor_scalar(out=rt[:, 21:22], in0=rt[:, 20:21], scalar1=-1.0, scalar2=None,
                                                       op0=ALU.mult))
                            dv(lambda: V.memset(rt[:, 22:23], 0.0))
                            k.do(ACT, lambda: A.activation(out=rt[:, 28:32], in_=rt[:, 0:4], func=AF.Exp, bias=rt[:, 21:22],
                                                           accum_out=rt[:, 22:23]), R=[b_rt], W=[b_rt])
                            dv(lambda: V.reciprocal(out=rt[:, 23:24], in_=rt[:, 22:23]))
                            dv(lambda: V.tensor_scalar(out=rt[:, 32:36], in0=rt[:, 4:8], scalar1=rt[:, 24:25], scalar2=None,
                                                       op0=ALU.mult))
                            for g in range(1, 4):
                                dv(lambda: V.scalar_tensor_tensor(out=rt[:, 32:36], in0=rt[:, 4 + 4 * g:8 + 4 * g],
                                                                  scalar=rt[:, 24 + g:25 + g], in1=rt[:, 32:36],
                                                                  op0=ALU.mult, op1=ALU.add))
                            dv(lambda: V.reduce_max(out=rt[:, 36:37], in_=rt[:, 32:36], axis=AX.X))
                            dv(lambda: V.tensor_scalar(out=rt[:, 40:44], in0=rt[:, 32:36], scalar1=rt[:, 36:37], scalar2=None,
                                                       op0=ALU.is_equal))
                            dv(lambda: V.scalar_tensor_tensor(out=rt[:, 44:48], in0=rt[:, 40:44], scalar=NEG,
                                                              in1=rt[:, 32:36], op0=ALU.mult, op1=ALU.add))
                            dv(lambda: V.reduce_max(out=rt[:, 37:38], in_=rt[:, 44:48], axis=AX.X))
                            dv(lambda: V.tensor_scalar(out=rt[:, 48:52], in0=rt[:, 44:48], scalar1=rt[:, 37:38], scalar2=None,
                                                       op0=ALU.is_equal))
                            dv(lambda: V.tensor_scalar(out=rt[:, 38:39], in0=rt[:, 36:37], scalar1=-1.0, scalar2=None,
                                                       op0=ALU.mult))
                            k.do(ACT, lambda: A.activation(out=rt[:, 52:56], in_=rt[:, 32:36], func=AF.Exp, bias=rt[:, 38:39]),
                                 R=[b_rt], W=[b_rt])
                            dv(lambda: V.tensor_tensor(out=rt[:, 56:60], in0=rt[:, 52:56], in1=rt[:, 48:52], op=ALU.mult))
                            dv(lambda: V.reduce_sum(out=rt[:, 39:40], in_=rt[:, 56:60], axis=AX.X))
                            dv(lambda: V.tensor_scalar(out=rt[:, 60:61], in0=rt[:, 39:40], scalar1=1.0, scalar2=None,
                                                       op0=ALU.add))
                            dv(lambda: V.reciprocal(out=rt[:, 61:62], in_=rt[:, 60:61]))
                            dv(lambda: V.tensor_tensor(out=cA[:, bi:bi + 1], in0=rt[:, 61:62], in1=rt[:, 23:24], op=ALU.mult),
                               extraW=[b_rout])
                            dv(lambda: V.tensor_tensor(out=cB[:, bi:bi + 1], in0=cA[:, bi:bi + 1], in1=rt[:, 39:40], op=ALU.mult),
                               extraR=[b_rout], extraW=[b_rout])
                            for g in range(4):
                                dv(lambda: V.tensor_scalar(out=selA[:, bi, 4 * g:4 * g + 4], in0=rt[:, 40:44],
                                                           scalar1=rt[:, 24 + g:25 + g], scalar2=None, op0=ALU.mult),
                                   extraW=[b_rout])
                                dv(lambda: V.tensor_scalar(out=selB[:, bi, 4 * g:4 * g + 4], in0=rt[:, 48:52],
                                                           scalar1=rt[:, 24 + g:25 + g], scalar2=None, op0=ALU.mult),
                                   extraW=[b_rout])
                            k.do(DVE, lambda: V.tensor_tensor(out=s16[:], in0=selA[:, bi, :], in1=selB[:, bi, :], op=ALU.add),
                                 R=[b_rout], W=[b_s16])
                            k.do(PE, lambda: T.matmul(prk[:, 0:16], lhsT=utri[:], rhs=s16[:], start=True, stop=True),
                                 R=[b_const, b_s16], W=[b_prk])
                            k.do(PE, lambda: T.matmul(ptt[:, 0:16], lhsT=ones1[:], rhs=s16[:], start=True, stop=True),
                                 R=[b_const, b_s16], W=[b_ptt])
                            k.do(DVE, lambda: V.tensor_tensor(out=Rg[:, bi, :], in0=prk[:, 0:16], in1=base[:], op=ALU.add),
                                 R=[b_prk, b_rout], W=[b_rout])
                            k.do(DVE, lambda: V.tensor_tensor(out=base[:], in0=ptt[:, 0:16], in1=base[:], op=ALU.add),
                                 R=[b_ptt, b_rout], W=[b_rout])
                            bi += 1
                    def dr(fn):
                        k.do(DVE, fn, R=[b_rout, b_rt, b_const], W=[b_rout, b_rt])
                    for e in range(16):
                        dr(lambda: V.tensor_scalar(out=cmp[:, 0:80], in0=thr[:, 0:80], scalar1=base[:, e:e + 1], scalar2=None,
                                                   op0=ALU.is_lt))
                        dr(lambda: V.reduce_sum(out=q16[:, e:e + 1], in_=cmp[:, 0:80], axis=AX.X))
                    dr(lambda: V.tensor_scalar(out=q16[:, 0:16], in0=q16[:, 0:16], scalar1=256.0, scalar2=None, op0=ALU.mult))
                    dr(lambda: V.memset(q16[:, 16:17], 0.0))
                    for e in range(1, 16):
                        dr(lambda: V.tensor_tensor(out=q16[:, 16 + e:17 + e], in0=q16[:, 15 + e:16 + e], in1=q16[:, e - 1:e],
                                                   op=ALU.add))
                    dr(lambda: V.tensor_tensor(out=q16[:, 32:48], in0=q16[:, 16:32], in1=q16[:, 0:16], op=ALU.add))
                    dr(lambda: V.memset(etf[:], 0.0))
                    for e in range(16):
                        dr(lambda: V.tensor_scalar(out=cmp[:, 0:NTL], in0=thr[:, 0:NTL], scalar1=q16[:, 32 + e:33 + e],
                                                   scalar2=None, op0=ALU.is_ge))
                        dr(lambda: V.tensor_tensor(out=etf[:], in0=etf[:], in1=cmp[:, 0:NTL], op=ALU.add))
                    dr(lambda: V.tensor_scalar(out=etf[:], in0=etf[:], scalar1=15.0, scalar2=None, op0=ALU.min))
                    dr(lambda: V.tensor_scalar(out=etf[:], in0=etf[:], scalar1=128.0, scalar2=iotap[:, 0:1], op0=ALU.mult,
                                               op1=ALU.add))
                    dr(lambda: V.tensor_copy(out=idxw[:], in_=etf[:]))
                    for bi in range(NB3):
                        dr(lambda: V.tensor_tensor(out=rt[:, 0:16], in0=Rg[:, bi, :], in1=q16[:, 16:32], op=ALU.add))
                        dr(lambda: V.tensor_tensor(out=rt[:, 16:32], in0=rt[:, 0:16], in1=selA[:, bi, :], op=ALU.mult))
                        dr(lambda: V.reduce_sum(out=posf[:, 0, bi:bi + 1], in_=rt[:, 16:32], axis=AX.X))
                        dr(lambda: V.tensor_tensor(out=rt[:, 32:48], in0=rt[:, 0:16], in1=selB[:, bi, :], op=ALU.mult))
                        dr(lambda: V.reduce_sum(out=posf[:, 1, bi:bi + 1], in_=rt[:, 32:48], axis=AX.X))
                    dr(lambda: V.tensor_copy(out=posA_i[:], in_=posf[:, 0, :]))
                    dr(lambda: V.tensor_copy(out=posB_i[:], in_=posf[:, 1, :]))
                    k.barrier()

                with ExitStack() as es:
                    E = es.enter_context
                    hr = [E(sbt("hr%d" % i, [128, D], BF16)) for i in range(4)]
                    b_hr = Bs(4)
                    for bi, (seg, gb) in enumerate(blocks):
                        hi = bi % 4
                        k.dma("hr%d" % hi, hr[hi][:], Hrows[gb * 128:(gb + 1) * 128, :], W=[b_hr[hi]])
                        k.dma_ind("sc_a%d" % hi, Hs_l, bass.IndirectOffsetOnAxis(ap=posA_i[:, bi:bi + 1], axis=0), hr[hi][:], None,
                                  NSLOT, R=[b_hr[hi], b_rout])
                        k.dma_ind("sc_b%d" % hi, Hs_l, bass.IndirectOffsetOnAxis(ap=posB_i[:, bi:bi + 1], axis=0), hr[hi][:], None,
                                  NSLOT, R=[b_hr[hi], b_rout])
                    k.barrier()

                with ExitStack() as es:
                    E = es.enter_context
                    b_const = B()
                    ident = load_const(E, "ident", [128, 128], BF16, ident_d, b_const)
                    wq = [E(sbt("wq%d" % i, [128, 8192], BF16)) for i in range(6)]
                    hs = [E(sbt("hs%d" % i, [128, 2, D], BF16)) for i in range(2)]
                    hsT = [E(sbt("hsT%d" % i, [128, 16, 256], BF16)) for i in range(2)]
                    he = [E(sbt("he%d" % i, [128, 4, 256], BF16)) for i in range(2)]
                    sg = [E(sbt("sg%d" % i, [128, 256], F32)) for i in range(2)]
                    yo = [E(sbt("yo%d" % i, [128, D], F32)) for i in range(2)]
                    pT = [E(pst("pT%d" % i, [128, 1024], BF16)) for i in range(2)]
                    pg = [E(pst("pg%d" % i, [128, 512], F32)) for i in range(2)]
                    pu = [E(pst("pu%d" % i, [128, 512], F32)) for i in range(2)]
                    pd = [E(pst("pd%d" % i, [128, 512], F32)) for i in range(2)]
                    b_wq, b_hs, b_hsT, b_he, b_sg, b_yo = Bs(6), Bs(2), Bs(2), Bs(2), Bs(2), Bs(2)
                    b_pT, b_pg, b_pu, b_pd = Bs(2), Bs(2), Bs(2), Bs(2)
                    POOL.wait_tok(conv_tok["g%d" % l])
                    POOL.wait_tok(conv_tok["u%d" % l])
                    POOL.wait_tok(conv_tok["d%d" % l])
                    wsrc = [wb_g[l], wb_u[l], wb_d[l]]

                    def load_w(i):
                        for m in range(3):
                            ws = (3 * i + m) % 6
                            k.dma_ind("wq%d" % ws, wq[ws][:], None, wsrc[m],
                                      bass.IndirectOffsetOnAxis(ap=idxw[:, i:i + 1], axis=0), 2048, R=[b_rout], W=[b_wq[ws]])
                    load_w(0)
                    ji_ = 0
                    di_ = 0
                    ev_ = 0
                    for i in range(NTL):
                        sl = i % 2
                        if i + 1 < NTL:
                            load_w(i + 1)
                        k.dma("hs%d" % sl, hs[sl][:], Hs_l[i * 256:(i + 1) * 256, :].rearrange("(s p) f -> p s f", p=128),
                              W=[b_hs[sl]])
                        wgs, wus, wds = [(3 * i + m) % 6 for m in range(3)]
                        wgv = wq[wgs][:].rearrange("p (c n) -> p c n", c=16)
                        wuv = wq[wus][:].rearrange("p (c n) -> p c n", c=16)
                        wdv = wq[wds][:].rearrange("p (c n) -> p c n", c=4)
                        for c in range(16):
                            pi = c % 2
                            for sb_ in range(2):
                                k.do(PE, lambda: T.transpose(out=pT[pi][:, sb_ * 128:(sb_ + 1) * 128],
                                                             in_=hs[sl][:, sb_, c * 128:(c + 1) * 128], identity=ident[:]),
                                     R=[b_hs[sl], b_const], W=[b_pT[pi]], inc=(sb_ == 1))
                            if c % 2 == 0:
                                k.do(ACT, lambda: A.activation(out=hsT[sl][:, c, :], in_=pT[pi][:, 0:256], func=AF.Identity),
                                     R=[b_pT[pi]], W=[b_hsT[sl]])
                            else:
                                k.do(DVE, lambda: V.tensor_copy(out=hsT[sl][:, c, :], in_=pT[pi][:, 0:256]),
                                     R=[b_pT[pi]], W=[b_hsT[sl]])
                        for j in range(4):
                            pi = ji_ % 2
                            ji_ += 1
                            for c in range(16):
                                k.do(PE, lambda: T.matmul(pg[pi][:, 0:256], lhsT=wgv[:, c, j * 128:(j + 1) * 128],
                                                          rhs=hsT[sl][:, c, :], start=(c == 0), stop=(c == 15)),
                                     R=[b_wq[wgs], b_hsT[sl]], W=[b_pg[pi]], inc=(c == 15))
                            for c in range(16):
                                k.do(PE, lambda: T.matmul(pu[pi][:, 0:256], lhsT=wuv[:, c, j * 128:(j + 1) * 128],
                                                          rhs=hsT[sl][:, c, :], start=(c == 0), stop=(c == 15)),
                                     R=[b_wq[wus], b_hsT[sl]], W=[b_pu[pi]], inc=(c == 15))
                            k.do(ACT, lambda: A.activation(out=sg[pi][:], in_=pg[pi][:, 0:256], func=AF.Silu),
                                 R=[b_pg[pi]], W=[b_sg[pi]])
                            k.do(DVE, lambda: V.tensor_tensor(out=he[sl][:, j, :], in0=sg[pi][:], in1=pu[pi][:, 0:256],
                                                              op=ALU.mult), R=[b_sg[pi], b_pu[pi]], W=[b_he[sl]])
                        for sb_ in range(2):
                            yi = (2 * i + sb_) % 2
                            for ft in range(4):
                                pi = di_ % 2
                                di_ += 1
                                for j in range(4):
                                    k.do(PE, lambda: T.matmul(pd[pi][:], lhsT=he[sl][:, j, sb_ * 128:(sb_ + 1) * 128],
                                                              rhs=wdv[:, j, ft * 512:(ft + 1) * 512], start=(j == 0),
                                                              stop=(j == 3)),
                                         R=[b_he[sl], b_wq[wds]], W=[b_pd[pi]], inc=(j == 3))
                                ev_ += 1
                                if ev_ % 2 == 0:
                                    k.do(ACT, lambda: A.activation(out=yo[yi][:, ft * 512:(ft + 1) * 512], in_=pd[pi][:],
                                                                   func=AF.Identity), R=[b_pd[pi]], W=[b_yo[yi]])
                                else:
                                    k.do(DVE, lambda: V.tensor_copy(out=yo[yi][:, ft * 512:(ft + 1) * 512], in_=pd[pi][:]),
                                         R=[b_pd[pi]], W=[b_yo[yi]])
                            r0 = (2 * i + sb_) * 128
                            k.dma("st_yo%d" % yi, Ys_l[r0:r0 + 128, :], yo[yi][:], R=[b_yo[yi]])
                    k.barrier()

                with ExitStack() as es:
                    E = es.enter_context
                    b_const = B()
                    gf = E(sbt("gf", [128, 2, D], F32))
                    for s in range(2):
                        k.dma("c_gf", gf[:, s, :], mod_d[l, s:s + 1, 5 * D:6 * D].partition_broadcast(128), W=[b_const])
                    if l == 1:
                        fg = load_const(E, "fg", [128, D], F32, fng_d.partition_broadcast(128), b_const)
                        junk = E(sbt("junk", [128, D], BF16))
                        ss = E(sbt("ss", [128, 12], F32))
                        b_junk, b_ss = B(), B()
                    xs = [E(sbt("xs%d" % i, [128, D], F32)) for i in range(2)]
                    yA = [E(sbt("yA%d" % i, [128, D], F32)) for i in range(2)]
                    yB = [E(sbt("yB%d" % i, [128, D], F32)) for i in range(2)]
                    tt_ = [E(sbt("tt%d" % i, [128, D], F32)) for i in range(2)]
                    xo = [E(sbt("xo%d" % i, [128, D], F32)) for i in range(2)]
                    b_xs, b_yA, b_yB, b_tt, b_xo = Bs(2), Bs(2), Bs(2), Bs(2), Bs(2)
                    for bi, (seg, gb) in enumerate(blocks):
                        i2 = bi % 2
                        r0 = gb * 128
                        k.dma("xs%d" % i2, xs[i2][:], x_mid[r0:r0 + 128, :], W=[b_xs[i2]])
                        k.dma_ind("ga%d" % i2, yA[i2][:], None, Ys_l, bass.IndirectOffsetOnAxis(ap=posA_i[:, bi:bi + 1], axis=0),
                                  NSLOT, R=[b_rout], W=[b_yA[i2]])
                        k.dma_ind("gb%d" % i2, yB[i2][:], None, Ys_l, bass.IndirectOffsetOnAxis(ap=posB_i[:, bi:bi + 1], axis=0),
                                  NSLOT, R=[b_rout], W=[b_yB[i2]])
                        k.do(DVE, lambda: V.tensor_scalar(out=tt_[i2][:], in0=yA[i2][:], scalar1=cA[:, bi:bi + 1], scalar2=None,
                                                          op0=ALU.mult), R=[b_yA[i2], b_rout], W=[b_tt[i2]])
                        k.do(DVE, lambda: V.scalar_tensor_tensor(out=tt_[i2][:], in0=yB[i2][:], scalar=cB[:, bi:bi + 1],
                                                                 in1=tt_[i2][:], op0=ALU.mult, op1=ALU.add),
                             R=[b_yB[i2], b_rout, b_tt[i2]], W=[b_tt[i2]])
                        k.do(POOL, lambda: G.tensor_tensor(out=tt_[i2][:], in0=tt_[i2][:], in1=gf[:, seg, :], op=ALU.mult),
                             R=[b_tt[i2], b_const], W=[b_tt[i2]])
                        k.do(DVE, lambda: V.tensor_tensor(out=xo[i2][:], in0=tt_[i2][:], in1=xs[i2][:], op=ALU.add),
                             R=[b_tt[i2], b_xs[i2]], W=[b_xo[i2]])
                        if l == 0:
                            k.dma("st_xo%d" % i2, x_dst[r0:r0 + 128, :], xo[i2][:], R=[b_xo[i2]])
                        else:
                            k.do(DVE, lambda: V.memset(ss[:, 0:1], 0.0), W=[b_ss])
                            k.do(ACT, lambda: A.activation(out=junk[:], in_=xo[i2][:], func=AF.Square, accum_out=ss[:, 0:1]),
                                 R=[b_xo[i2], b_ss], W=[b_junk, b_ss])
                            k.do(ACT, lambda: A.activation(out=ss[:, 4:5], in_=ss[:, 0:1], func=AF.Sqrt, scale=1.0 / D,
                                                           bias=EPS), R=[b_ss], W=[b_ss])
                            k.do(DVE, lambda: V.reciprocal(out=ss[:, 8:9], in_=ss[:, 4:5]), R=[b_ss], W=[b_ss])
                            k.do(DVE, lambda: V.scalar_tensor_tensor(out=xo[i2][:], in0=xo[i2][:], scalar=ss[:, 8:9], in1=fg[:],
                                                                     op0=ALU.mult, op1=ALU.mult),
                                 R=[b_xo[i2], b_ss, b_const], W=[b_xo[i2]])
                            orow = (gb - 2) * 128 if seg == 0 else 2048 + (gb - 22) * 128
                            k.dma("st_y%d" % i2, y_d[orow:orow + 128, :], xo[i2][:], R=[b_xo[i2]])
                    k.barrier(final=(l == 1))
    return nc


_NC_CACHE = {}


def _alibi_bias():
    slopes = 2.0 ** (-8.0 * np.arange(1, 9, dtype=np.float64) / 8.0)
    q = np.arange(128)[:, None]
    s = np.arange(384)[None, :]
    dist = np.abs(s - 128 - q)
    out = np.empty((128, 8, 384), np.float32)
    for h in range(8):
        out[:, h, :] = np.where(dist <= 128, -slopes[h] * dist, NEG)
    return out


def kernel(x_prompt, x_sample, c_prompt, c_sample, norm_mix_g, norm_ffn_g, w_ada, b_ada, w_in, w_out,
           conv_a_w, conv_a_b, ln_a_g, ln_a_b, attn_sink, conv_c_w, w_router_group, b_router_group,
           w_router_expert, b_router_expert, w_gate, w_up, w_down, final_norm_g):
    f = lambda a: np.ascontiguousarray(np.asarray(a, dtype=np.float32))
    x_prompt, x_sample, c_prompt, c_sample = f(x_prompt), f(x_sample), f(c_prompt), f(c_sample)
    if "nc" not in _NC_CACHE:
        _NC_CACHE["nc"] = build_program()
    nc = _NC_CACHE["nc"]

    caw = f(np.transpose(f(conv_a_w).reshape(2, 31, 4, 128), (0, 3, 2, 1)))
    cab = f(np.transpose(f(conv_a_b).reshape(2, 4, 128), (0, 2, 1)))
    lng = f(np.transpose(f(ln_a_g).reshape(2, 4, 128), (0, 2, 1)))
    lnb = f(np.transpose(f(ln_a_b).reshape(2, 4, 128), (0, 2, 1)))
    ccw = f(np.transpose(f(conv_c_w).reshape(2, 3, 4, 128), (0, 3, 2, 1)))
    wr = np.concatenate([f(w_router_group), f(w_router_expert)], axis=-1)
    wr = f(np.transpose(wr.reshape(2, 16, 128, 20), (0, 2, 1, 3)))
    br = f(np.concatenate([f(b_router_group), f(b_router_expert)], axis=-1).reshape(2, 1, 20))
    shared = dict(
        ident=np.eye(128, dtype=np.float32).astype(ml_dtypes.bfloat16),
        ones=np.full((128, 128), 1.0 / 512, np.float32).astype(ml_dtypes.bfloat16),
        biasmat=_alibi_bias(),
        utri=np.triu(np.ones((128, 128), np.float32), 1).astype(ml_dtypes.bfloat16),
        ones1=np.ones((128, 128), np.float32).astype(ml_dtypes.bfloat16),
        thr=np.tile((256.0 * np.arange(80, dtype=np.float32))[None], (128, 1)),
        iotap=np.arange(128, dtype=np.float32).reshape(128, 1),
        norm_mix_g=f(norm_mix_g), norm_ffn_g=f(norm_ffn_g), w_ada=f(w_ada), b_ada=f(b_ada), w_in=f(w_in),
        w_out=f(w_out), caw=caw, cab=cab, lng=lng, lnb=lnb, sink=f(attn_sink).reshape(2, 1, 8), ccw=ccw, wr=wr, br=br,
        w_gate=f(w_gate), w_up=f(w_up), w_down=f(w_down), final_norm_g=f(final_norm_g).reshape(1, D),
    )
    in_maps = []
    for c in range(NCORES):
        sb, half = c // 2, c % 2
        xl = np.zeros((NT, D), np.float32)
        lo, hi = 2048 * c - 256, 2048 * c + 2048 + 256
        a, b = max(lo, 0), min(hi, 16384)
        xl[a - lo:b - lo] = x_prompt[0, a:b]
        lo, hi = 4096 * half - 256, 4096 * half + 4096 + 256
        a, b = max(lo, 0), min(hi, 8192)
        xl[2560 + a - lo:2560 + b - lo] = x_sample[sb, a:b]
        fl = np.array([c > 0, c < 7, half == 1, half == 0], np.float32)
        flags = np.zeros((128, 8), np.float32)
        flags[:, 0:4] = fl[None]
        flags[:, 4:8] = np.where(fl > 0, 0.0, NEG)[None]
        cc = np.stack([c_prompt[0], c_sample[sb]], axis=-1)
        cT = f(np.transpose(cc.reshape(16, 128, 2), (1, 0, 2)))
        in_maps.append(dict(shared, x_local=xl, flags=flags, cT=cT))

    res = run_bass_kernel_spmd(nc, in_maps, core_ids=list(range(NCORES)))
    y_prompt = np.empty((1, 16384, D), np.float32)
    y_sample = np.empty((4, 8192, D), np.float32)
    for c in range(NCORES):
        y = res.results[c]["y_local"]
        sb, half = c // 2, c % 2
        y_prompt[0, 2048 * c:2048 * c + 2048] = y[0:2048]
        y_sample[sb, 4096 * half:4096 * half + 4096] = y[2048:6144]
    return (y_prompt, y_sample)
```

```python
import numpy as np
import ml_dtypes
from contextlib import ExitStack
import concourse.bass as bass
import concourse.mybir as mybir
from concourse.bass_utils import run_bass_kernel_spmd

F32 = mybir.dt.float32
BF16 = mybir.dt.bfloat16
I32 = mybir.dt.int32
AF = mybir.ActivationFunctionType
ALU = mybir.AluOpType
AX = mybir.AxisListType

D = 2048
NCORES = 8
SEGS = [(0, 20), (20, 36)]
NBLK = 56
NT = NBLK * 128
NOWN = 6144
EPS = 1e-6
NEG = -1e30
SCALE = 128 ** -0.5


class Eng:
    def __init__(self, e, name, sem):
        self.e, self.name, self.sem, self.n, self.seen = e, name, sem, 0, {}

    def wait_tok(self, tok):
        if tok is None:
            return
        _, sem, cnt = tok
        key = id(sem)
        if self.seen.get(key, 0) >= cnt:
            return
        self.e.wait_ge(sem, cnt)
        self.seen[key] = cnt


class B:
    def __init__(self):
        self.w, self.r = {}, {}

    def read(self, eng):
        for t in self.w.values():
            if t[0] == "PE" and eng.name == "PE":
                continue
            eng.wait_tok(t)

    def write(self, eng):
        for k, t in self.r.items():
            if k != eng.name:
                eng.wait_tok(t)
        for k, t in self.w.items():
            if k != eng.name:
                eng.wait_tok(t)

    def did_read(self, tok):
        k = tok[0]
        if k not in self.r or self.r[k][2] < tok[2]:
            self.r[k] = tok

    def did_write(self, tok):
        self.w = {tok[0]: tok}
        self.r = {}


class K:
    def __init__(self, nc, es):
        self.nc = nc
        E = es.enter_context
        self.PE = Eng(nc.tensor, "PE", E(nc.semaphore("sem_pe")))
        self.ACT = Eng(nc.scalar, "ACT", E(nc.semaphore("sem_act")))
        self.DVE = Eng(nc.vector, "DVE", E(nc.semaphore("sem_dve")))
        self.POOL = Eng(nc.gpsimd, "POOL", E(nc.semaphore("sem_pool")))
        self.SP = Eng(nc.sync, "SP", E(nc.semaphore("sem_sp")))
        self.es = es
        self.dsem = {}
        self.slot_of = {}
        self.sem_pool = []
        self.nsem = 0

    def do(self, eng, fn, R=(), W=(), inc=True):
        for b in R:
            b.read(eng)
        for b in W:
            b.write(eng)
        ins = fn()
        if inc:
            ins.then_inc(eng.sem, 1)
            eng.n += 1
            tok = (eng.name, eng.sem, eng.n)
        else:
            tok = (eng.name, eng.sem, eng.n + 1)
        for b in R:
            b.did_read(tok)
        for b in W:
            b.did_write(tok)
        return tok

    def dma(self, slot, out, in_, R=(), W=(), q=None):
        q = q or self.SP
        if slot not in self.dsem:
            self.dsem[slot] = self.new_sem(slot)
        s = self.dsem[slot]
        for b in R:
            b.read(q)
        for b in W:
            b.write(q)
        q.e.dma_start(out=out, in_=in_).then_inc(s[0], 16)
        s[1] += 16
        tok = ("dma:" + slot, s[0], s[1])
        for b in R:
            b.did_read(tok)
        for b in W:
            b.did_write(tok)
        return tok

    def new_sem(self, slot):
        if self.sem_pool and not slot.startswith("cv_"):
            return self.sem_pool.pop()
        self.nsem += 1
        return [self.es.enter_context(self.nc.semaphore("dq%d" % self.nsem)), 0]

    def dma_ind(self, slot, out, out_off, in_, in_off, nrows, R=(), W=()):
        q = self.POOL
        if slot not in self.dsem:
            self.dsem[slot] = self.new_sem(slot)
        s = self.dsem[slot]
        for b in R:
            b.read(q)
        for b in W:
            b.write(q)
        q.e.indirect_dma_start(out=out, out_offset=out_off, in_=in_, in_offset=in_off).then_inc(s[0], 16)
        s[1] += 16
        tok = ("dma:" + slot, s[0], s[1])
        for b in R:
            b.did_read(tok)
        for b in W:
            b.did_write(tok)
        return tok

    def all_tokens(self, final=False):
        toks = []
        for e in (self.PE, self.ACT, self.DVE, self.POOL):
            if e.n:
                toks.append((e.name, e.sem, e.n))
        for name, s in self.dsem.items():
            if s[1] and (final or not name.startswith("cv_")):
                toks.append(("dma:" + name, s[0], s[1]))
        return toks

    def barrier(self, final=False):
        toks = self.all_tokens(final)
        for e in (self.PE, self.ACT, self.DVE, self.POOL, self.SP):
            for t in toks:
                if t[0] != e.name:
                    e.wait_tok(t)
        for name in list(self.dsem.keys()):
            if not name.startswith("cv_"):
                self.sem_pool.append(self.dsem.pop(name))


def Bs(n):
    return [B() for _ in range(n)]


def build_program():
    nc = bass.Bass("TRN2", target_bir_lowering=False)

    _uid = [0]

    def sbt(name, shape, dt):
        _uid[0] += 1
        return nc.sbuf_tensor("sb%d_%s" % (_uid[0], name), shape, dt)

    def pst(name, shape, dt):
        _uid[0] += 1
        return nc.psum_tensor("ps%d_%s" % (_uid[0], name), shape, dt)

    def din(name, shape, dt=F32):
        return nc.dram_tensor(name, list(shape), dt, kind="ExternalInput").ap()

    def dscr(name, shape, dt):
        return nc.dram_tensor(name, list(shape), dt, kind="Internal").ap()

    x_local = din("x_local", [NT, D])
    flags_d = din("flags", [128, 8])
    cT_d = din("cT", [128, 16, 2])
    ident_d = din("ident", [128, 128], BF16)
    ones_d = din("ones", [128, 128], BF16)
    bias_d = din("biasmat", [128, 8, 384])
    nmg_d = din("norm_mix_g", [2, D])
    nfg_d = din("norm_ffn_g", [2, D])
    wada_d = din("w_ada", [2, D, 6 * D])
    bada_d = din("b_ada", [2, 6 * D])
    win_d = din("w_in", [2, D, 4096])
    wout_d = din("w_out", [2, D, D])
    caw_d = din("caw", [2, 128, 4, 31])
    cab_d = din("cab", [2, 128, 4])
    lng_d = din("lng", [2, 128, 4])
    lnb_d = din("lnb", [2, 128, 4])
    sink_d = din("sink", [2, 1, 8])
    ccw_d = din("ccw", [2, 128, 4, 3])
    wr_d = din("wr", [2, 128, 16, 20])
    br_d = din("br", [2, 1, 20])
    wg_d = din("w_gate", [2, 16, D, 512])
    wu_d = din("w_up", [2, 16, D, 512])
    wd_d = din("w_down", [2, 16, 512, D])
    fng_d = din("final_norm_g", [1, D])
    utri_d = din("utri", [128, 128], BF16)
    ones1_d = din("ones1", [128, 128], BF16)
    thr_d = din("thr", [128, 80])
    iotap_d = din("iotap", [128, 1])
    y_d = nc.dram_tensor("y_local", [NOWN, D], F32, kind="ExternalOutput").ap()

    wb_in = dscr("wb_in", [2, D, 4096], BF16)
    wb_out = dscr("wb_out", [2, D, D], BF16)
    wb_g = [dscr("wb_g%d" % l, [2048, 8192], BF16) for l in range(2)]
    wb_u = [dscr("wb_u%d" % l, [2048, 8192], BF16) for l in range(2)]
    wb_d = [dscr("wb_d%d" % l, [2048, 8192], BF16) for l in range(2)]
    Hrows = dscr("Hrows", [NT, D], BF16)
    Hslots = dscr("Hslots", [68 * 256, D], BF16)
    Yslots = dscr("Yslots", [68 * 256, D], F32)
    mod_d = dscr("mod_d", [2, 2, 6 * D], F32)
    xa = dscr("xa", [NT, D], F32)
    xb = dscr("xb", [NT, D], F32)
    aT_d = dscr("aT", [4, 128, NT], BF16)
    qT_d = dscr("qT", [8, 128, NT], BF16)
    kT_d = dscr("kT", [2, 128, NT], BF16)
    v_d = dscr("v", [NT, 256], BF16)
    uT_d = dscr("uT", [4, 128, NT], F32)
    cbT_d = dscr("cbT", [4, 128, NT], F32)
    mix_d = dscr("mixT", [16, 128, NT], BF16)

    with ExitStack() as es_all:
        k = K(nc, es_all)
        PE, ACT, DVE, POOL, SP = k.PE, k.ACT, k.DVE, k.POOL, k.SP
        T, A, V, G = nc.tensor, nc.scalar, nc.vector, nc.gpsimd

        conv_tok = {}

        def cast(name, dst, src):
            conv_tok[name] = k.dma("cv_" + name, dst, src, q=POOL)

        def cast_in(l, i):
            cast("in%d" % l, wb_in[l, i * 256:(i + 1) * 256, :], win_d[l, i * 256:(i + 1) * 256, :])

        def cast_out(l, i):
            cast("out%d" % l, wb_out[l, i * 512:(i + 1) * 512, :], wout_d[l, i * 512:(i + 1) * 512, :])

        def cast_moe(l, e):
            cast("g%d" % l, wb_g[l][e * 128:(e + 1) * 128, :].rearrange("p (c n) -> p c n", c=16),
                 wg_d[l, e].rearrange("(c p) n -> p c n", p=128))
            cast("u%d" % l, wb_u[l][e * 128:(e + 1) * 128, :].rearrange("p (c n) -> p c n", c=16),
                 wu_d[l, e].rearrange("(c p) n -> p c n", p=128))
            cast("d%d" % l, wb_d[l][e * 128:(e + 1) * 128, :].rearrange("p (c n) -> p c n", c=4),
                 wd_d[l, e].rearrange("(c p) n -> p c n", p=128))

        for i in range(8):
            cast_in(0, i)
        for i in range(4):
            cast_out(0, i)

        with ExitStack() as es:
            E = es.enter_context
            siluT = E(sbt("siluT", [128, 16, 2], F32))
            wt = [E(sbt("wadat%d" % i, [128, 16, 512], F32)) for i in range(2)]
            modrow = E(sbt("modrow", [2, 6 * D], F32))
            badar = E(sbt("badar", [2, 6 * D], F32))
            grow = E(sbt("grow", [2, 2, D], F32))
            ps = [E(pst("pps%d" % i, [128, 512], F32)) for i in range(2)]
            b_silu, b_mod, b_bada, b_grow = B(), B(), B(), B()
            b_wt, b_ps = Bs(2), Bs(2)
            k.dma("siluT", siluT[:], cT_d, W=[b_silu])
            k.do(ACT, lambda: A.activation(out=siluT[:], in_=siluT[:], func=AF.Silu), R=[b_silu], W=[b_silu])
            it = 0
            for l in range(2):
                k.dma("bada", badar[:], bada_d[l:l + 1, :].partition_broadcast(2), W=[b_bada])
                k.dma("grow", grow[:, 0, :], nmg_d[l:l + 1, :].partition_broadcast(2), W=[b_grow])
                k.dma("grow", grow[:, 1, :], nfg_d[l:l + 1, :].partition_broadcast(2), W=[b_grow])
                for j in range(24):
                    s = it % 2
                    it += 1
                    k.dma("wadat%d" % s, wt[s][:],
                          wada_d[l, :, j * 512:(j + 1) * 512].rearrange("(c p) n -> p c n", p=128), W=[b_wt[s]])
                    for c in range(16):
                        k.do(PE, lambda: T.matmul(ps[s][0:2, :], lhsT=siluT[:, c, :], rhs=wt[s][:, c, :],
                                                  start=(c == 0), stop=(c == 15)),
                             R=[b_silu, b_wt[s]], W=[b_ps[s]], inc=(c == 15))
                    k.do(DVE, lambda: V.tensor_tensor(out=modrow[:, j * 512:(j + 1) * 512], in0=ps[s][0:2, :],
                                                      in1=badar[:, j * 512:(j + 1) * 512], op=ALU.add),
                         R=[b_ps[s], b_bada], W=[b_mod])
                for (off, gi) in ((D, 0), (4 * D, 1)):
                    k.do(DVE, lambda: V.scalar_tensor_tensor(out=modrow[:, off:off + D], in0=modrow[:, off:off + D],
                                                             scalar=1.0, in1=grow[:, gi, :], op0=ALU.add,
                                                             op1=ALU.mult), R=[b_mod, b_grow], W=[b_mod])
                k.dma("modout", mod_d[l], modrow[:], R=[b_mod], q=POOL)
            k.barrier()

        def load_const(E, name, shape, dt, src, b):
            t = E(sbt(name, shape, dt))
            k.dma("c_" + name, t[:], src, W=[b])
            return t

        def seg_of_block(gb):
            return 0 if gb < 20 else 1

        def tiles_full(l):
            res = []
            for s, (bs, nbk) in enumerate(SEGS):
                lo, hi = (1, nbk - 1) if l == 0 else (2, nbk - 2)
                b = lo
                while b < hi:
                    nb = min(4, hi - b)
                    res.append((s, bs + b, nb))
                    b += nb
            return res

        def load_fm_mod(E, name, l, off, b):
            t = E(sbt(name, [128, 2, 16], F32))
            with nc.allow_non_contiguous_dma(reason="tiny feature-major load of a modulation vector"):
                for s in range(2):
                    k.dma("c_" + name, t[:, s, :], mod_d[l, s, off:off + D].rearrange("(c p) -> p c", p=128), W=[b])
            return t

        def norm_transpose(x_src, gb0, nb, seg, xs, b_xs, xsi, junk, b_junk, ss, b_ss, xh, b_xh, pT, b_pT,
                           hT, b_hT, afm, bfm, b_const, ident):
            n = nb * 128
            k.do(DVE, lambda: V.memset(ss[:], 0.0), W=[b_ss])
            for b in range(nb):
                si = xsi[0] % len(xs)
                xsi[0] += 1
                k.dma("xs%d" % si, xs[si][:], x_src[(gb0 + b) * 128:(gb0 + b + 1) * 128, :], W=[b_xs[si]])
                k.do(ACT, lambda: A.activation(out=junk[:], in_=xs[si][:], func=AF.Square,
                                               accum_out=ss[:, b:b + 1]), R=[b_xs[si], b_ss], W=[b_junk, b_ss])
                k.do(ACT, lambda: A.activation(out=ss[:, 4 + b:5 + b], in_=ss[:, b:b + 1], func=AF.Sqrt,
                                               scale=1.0 / D, bias=EPS), R=[b_ss], W=[b_ss])
                k.do(DVE, lambda: V.reciprocal(out=ss[:, 8 + b:9 + b], in_=ss[:, 4 + b:5 + b]), R=[b_ss], W=[b_ss])
                k.do(DVE, lambda: V.tensor_scalar(out=xh[:, b, :], in0=xs[si][:], scalar1=ss[:, 8 + b:9 + b],
                                                  scalar2=None, op0=ALU.mult), R=[b_xs[si], b_ss], W=[b_xh])
            for c in range(16):
                pi = c % 2
                for b in range(nb):
                    k.do(PE, lambda: T.transpose(out=pT[pi][:, b * 128:(b + 1) * 128],
                                                 in_=xh[:, b, c * 128:(c + 1) * 128], identity=ident[:]),
                         R=[b_xh, b_const], W=[b_pT[pi]], inc=(b == nb - 1))
                k.do(ACT, lambda: A.activation(out=hT[:, c, 0:n], in_=pT[pi][:, 0:n], func=AF.Identity,
                                               scale=afm[:, seg, c:c + 1], bias=bfm[:, seg, c:c + 1]),
                     R=[b_pT[pi], b_const], W=[b_hT])

        for l in range(2):
            x_src1 = x_local if l == 0 else xb
            x_mid = xa
            x_dst = xb

            with ExitStack() as es:
                E = es.enter_context
                b_const = B()
                ident = load_const(E, "ident", [128, 128], BF16, ident_d, b_const)
                flg = load_const(E, "flg", [128, 8], F32, flags_d, b_const)
                afm = load_fm_mod(E, "afm", l, D, b_const)
                bfm = load_fm_mod(E, "bfm", l, 0, b_const)
                xs = [E(sbt("xs%d" % i, [128, D], F32)) for i in range(2)]
                junk = E(sbt("junk", [128, D], BF16))
                ss = E(sbt("ss", [128, 12], F32))
                xh = [E(sbt("xh%d" % i, [128, 4, D], BF16)) for i in range(2)]
                hT = [E(sbt("hT%d" % i, [128, 16, 512], BF16)) for i in range(2)]
                wt = [E(sbt("wt%d" % i, [128, 16, 512], BF16)) for i in range(3)]
                sig = E(sbt("sig", [128, 4, 512], F32))
                o16 = [E(sbt("o16_%d" % i, [128, 4, 512], BF16)) for i in range(3)]
                o32 = [E(sbt("o32_%d" % i, [128, 4, 512], F32)) for i in range(2)]
                vo = E(sbt("vo", [128, 4, 256], BF16))
                pT = [E(pst("pT%d" % i, [128, 1024], BF16)) for i in range(2)]
                pz = [E(pst("pz%d" % i, [128, 512], F32)) for i in range(4)]
                b_xs, b_xh, b_hT, b_wt, b_pT, b_pz = Bs(2), Bs(2), Bs(2), Bs(3), Bs(2), Bs(4)
                b_junk, b_ss, b_sig, b_vo = B(), B(), B(), B()
                b_o16, b_o32 = Bs(3), Bs(2)
                xsi = [0]
                wi = 0
                pzi = 0
                o16i = 0
                o32i = 0
                first_w = True
                for t in range(14):
                    seg = 0 if t < 5 else 1
                    sl = t % 2
                    tok0 = t * 512
                    halo = None
                    if t in (0, 5):
                        halo = (0, 256, 0 if t == 0 else 2)
                    if t in (4, 13):
                        halo = (256, 512, 1 if t == 4 else 3)
                    norm_transpose(x_src1, t * 4, 4, seg, xs, b_xs, xsi, junk, b_junk, ss, b_ss, xh[sl], b_xh[sl],
                                   pT, b_pT, hT[sl], b_hT[sl], afm, bfm, b_const, ident)

                    def mask_halo(buf, bb, ngrp):
                        if halo is None:
                            return
                        lo, hi, fi = halo
                        k.do(POOL, lambda: G.tensor_scalar(out=buf[:, 0:ngrp, lo:hi], in0=buf[:, 0:ngrp, lo:hi],
                                                           scalar1=flg[:, fi:fi + 1], scalar2=None, op0=ALU.mult),
                             R=[bb, b_const], W=[bb])

                    for g in (1, 0, 2, 3, 4, 7, 5, 6):
                        ws = wi % 3
                        wi += 1
                        if first_w:
                            SP.wait_tok(conv_tok["in%d" % l])
                            first_w = False
                        k.dma("wt%d" % ws, wt[ws][:],
                              wb_in[l, :, g * 512:(g + 1) * 512].rearrange("(c p) n -> p c n", p=128), W=[b_wt[ws]])
                        nfm = 2 if g == 4 else 4
                        if g in (0, 2, 3, 4):
                            oi = o16i % 3
                            o16i += 1
                            ob, bo = o16[oi], b_o16[oi]
                        elif g in (5, 6):
                            oi = o32i % 2
                            o32i += 1
                            ob, bo = o32[oi], b_o32[oi]
                        for j in range(nfm):
                            p = pzi % 4
                            pzi += 1
                            for c in range(16):
                                k.do(PE, lambda: T.matmul(pz[p][:], lhsT=wt[ws][:, c, j * 128:(j + 1) * 128],
                                                          rhs=hT[sl][:, c, :], start=(c == 0), stop=(c == 15)),
                                     R=[b_wt[ws], b_hT[sl]], W=[b_pz[p]], inc=(c == 15))
                            if g == 1:
                                k.do(ACT, lambda: A.activation(out=sig[:, j, :], in_=pz[p][:], func=AF.Sigmoid),
                                     R=[b_pz[p]], W=[b_sig])
                            elif g == 0:
                                k.do(DVE, lambda: V.tensor_tensor(out=ob[:, j, :], in0=pz[p][:], in1=sig[:, j, :],
                                                                  op=ALU.mult), R=[b_pz[p], b_sig], W=[bo])
                            elif g in (2, 3, 4):
                                k.do(ACT, lambda: A.activation(out=ob[:, j, :], in_=pz[p][:], func=AF.Identity),
                                     R=[b_pz[p]], W=[bo])
                            elif g == 7:
                                k.do(ACT, lambda: A.activation(out=sig[:, j, :], in_=pz[p][:], func=AF.Identity),
                                     R=[b_pz[p]], W=[b_sig])
                            elif g == 5:
                                k.do(DVE, lambda: V.tensor_tensor(out=ob[:, j, :], in0=pz[p][:], in1=sig[:, j, :],
                                                                  op=ALU.mult), R=[b_pz[p], b_sig], W=[bo])
                            elif g == 6:
                                k.do(ACT, lambda: A.activation(out=ob[:, j, :], in_=pz[p][:], func=AF.Identity),
                                     R=[b_pz[p]], W=[bo])
                        if g == 4:
                            for b in range(4):
                                p = pzi % 4
                                pzi += 1
                                for c in range(16):
                                    k.do(PE, lambda: T.matmul(pz[p][:, 0:256], lhsT=hT[sl][:, c, b * 128:(b + 1) * 128],
                                                              rhs=wt[ws][:, c, 256:512], start=(c == 0),
                                                              stop=(c == 15)),
                                         R=[b_wt[ws], b_hT[sl]], W=[b_pz[p]], inc=(c == 15))
                                k.do(DVE, lambda: V.tensor_copy(out=vo[:, b, :], in_=pz[p][:, 0:256]),
                                     R=[b_pz[p]], W=[b_vo])
                            if halo is not None:
                                lo, hi, fi = halo
                                k.do(POOL, lambda: G.tensor_scalar(out=vo[:, lo // 128:hi // 128, :],
                                                                   in0=vo[:, lo // 128:hi // 128, :],
                                                                   scalar1=flg[:, fi:fi + 1], scalar2=None,
                                                                   op0=ALU.mult), R=[b_vo, b_const], W=[b_vo])
                            k.dma("st_v", v_d[tok0:tok0 + 512, :].rearrange("(b p) d -> p b d", p=128), vo[:],
                                  R=[b_vo], q=POOL)
                        if g == 0:
                            mask_halo(ob, bo, 4)
                            k.dma("st_a", aT_d[:, :, tok0:tok0 + 512].rearrange("g p t -> p g t"), ob[:], R=[bo], q=POOL)
                        elif g in (2, 3):
                            h0 = (g - 2) * 4
                            k.dma("st_q%d" % g, qT_d[h0:h0 + 4, :, tok0:tok0 + 512].rearrange("g p t -> p g t"), ob[:],
                                  R=[bo], q=POOL)
                        elif g == 4:
                            mask_halo(ob, bo, 2)
                            k.dma("st_k", kT_d[:, :, tok0:tok0 + 512].rearrange("g p t -> p g t"), ob[:, 0:2, :],
                                  R=[bo], q=POOL)
                        elif g == 5:
                            mask_halo(ob, bo, 4)
                            k.dma("st_u", uT_d[:, :, tok0:tok0 + 512].rearrange("g p t -> p g t"), ob[:], R=[bo], q=POOL)
                        elif g == 6:
                            k.dma("st_cb", cbT_d[:, :, tok0:tok0 + 512].rearrange("g p t -> p g t"), ob[:], R=[bo],
                                  q=POOL)
                k.barrier()

            tl = tiles_full(l)
            with ExitStack() as es:
                E = es.enter_context
                b_const = B()
                ident = load_const(E, "ident", [128, 128], BF16, ident_d, b_const)
                onesb = load_const(E, "onesb", [128, 128], BF16, ones_d, b_const)
                flg = load_const(E, "flg", [128, 8], F32, flags_d, b_const)
                bias8 = load_const(E, "bias8", [128, 8, 384], F32, bias_d, b_const)
                caw = load_const(E, "caw", [128, 4, 31], F32, caw_d[l], b_const)
                cab = load_const(E, "cab", [128, 4], F32, cab_d[l], b_const)
                lng = load_const(E, "lng", [128, 4], F32, lng_d[l], b_const)
                lnb = load_const(E, "lnb", [128, 4], F32, lnb_d[l], b_const)
                ccw = load_const(E, "ccw", [128, 4, 3], F32, ccw_d[l], b_const)
                sinkb = load_const(E, "sinkb", [128, 8], F32, sink_d[l].partition_broadcast(128), b_const)
                Dg = E(sbt("Dg", [128, 31, 4, 128], BF16))
                b_Dg = B()
                for kk in range(31):
                    for g in range(4):
                        k.do(DVE, lambda: V.tensor_scalar(out=Dg[:, kk, g, :], in0=ident[:],
                                                          scalar1=caw[:, g, kk:kk + 1], scalar2=None, op0=ALU.mult),
                             R=[b_const], W=[b_Dg])
                a_sb = [E(sbt("a_sb%d" % i, [128, 4, 542], BF16)) for i in range(2)]
                q_sb = [E(sbt("q_sb%d" % i, [128, 8, 512], BF16)) for i in range(2)]
                k_sb = [E(sbt("k_sb%d" % i, [128, 2, 768], BF16)) for i in range(2)]
                v_sb = [E(sbt("v_sb%d" % i, [128, 6, 256], BF16)) for i in range(2)]
                u_sb = [E(sbt("u_sb%d" % i, [128, 4, 514], F32)) for i in range(2)]
                cb_sb = [E(sbt("cb_sb%d" % i, [128, 4, 512], F32)) for i in range(2)]
                mixT = [E(sbt("mixT%d" % i, [128, 16, 512], BF16)) for i in range(2)]
                cv = E(sbt("cv", [128, 4, 512], F32))
                cvb = E(sbt("cvb", [128, 4, 512], BF16))
                cv2 = E(sbt("cv2", [128, 4, 512], BF16))
                mean_sb = E(sbt("mean_sb", [128, 512], F32))
                var_sb = E(sbt("var_sb", [128, 512], F32))
                rstd_sb = E(sbt("rstd_sb", [128, 512], F32))
                t1 = [E(sbt("t1_%d" % i, [128, 512], F32)) for i in range(2)]
                s_sb = [E(sbt("s_sb%d" % i, [128, 384], F32)) for i in range(2)]
                p_sb = [E(sbt("p_sb%d" % i, [128, 384], F32)) for i in range(2)]
                pn_sb = [E(sbt("pn_sb%d" % i, [128, 384], BF16)) for i in range(2)]
                pT_sb = [E(sbt("pT_sb%d" % i, [128, 384], BF16)) for i in range(2)]
                st = [E(sbt("st%d" % i, [128, 8], F32)) for i in range(2)]
                acc = [E(sbt("acc%d" % i, [128, 512], F32)) for i in range(2)]
                acc2 = [E(sbt("acc2_%d" % i, [128, 512], F32)) for i in range(2)]
                b_acc2 = Bs(2)
                pc0 = E(pst("pc0", [128, 512], F32))
                pc = [pc0, pc0]
                pmean = E(pst("pmean", [128, 512], F32))
                pex2 = pmean
                s_ps = [E(pst("s_ps%d" % i, [128, 512], F32)) for i in range(2)]
                pT_ps = [E(pst("pT_ps%d" % i, [128, 1024], BF16)) for i in range(2)]
                o_ps = [E(pst("o_ps%d" % i, [128, 512], F32)) for i in range(2)]
                b_a, b_q, b_k, b_v, b_u, b_cb, b_mix = Bs(2), Bs(2), Bs(2), Bs(2), Bs(2), Bs(2), Bs(2)
                b_cv, b_cvb, b_cv2, b_mean, b_var, b_rstd = B(), B(), B(), B(), B(), B()
                b_t1, b_s, b_p, b_pn, b_pTs, b_st, b_acc = Bs(2), Bs(2), Bs(2), Bs(2), Bs(2), Bs(2), Bs(2)
                b_pc0, b_sps = B(), Bs(2)
                b_pc = [b_pc0, b_pc0]
                b_pmean = B()
                b_pex2 = b_pmean
                b_pTp, b_ops = Bs(2), Bs(2)
                hi_ = 0
                gi_ = 0
                for ti, (seg, gb0, nb) in enumerate(tl):
                    sl = ti % 2
                    n = nb * 128
                    t0 = gb0 * 128
                    bs, nbk = SEGS[seg]
                    k.dma("a_sb%d" % sl, a_sb[sl][:, :, 0:n + 30],
                          aT_d[:, :, t0 - 15:t0 + n + 15].rearrange("g p t -> p g t"), W=[b_a[sl]])
                    k.dma("q_sb%d" % sl, q_sb[sl][:, :, 0:n], qT_d[:, :, t0:t0 + n].rearrange("g p t -> p g t"),
                          W=[b_q[sl]])
                    k.dma("k_sb%d" % sl, k_sb[sl][:, :, 0:n + 256],
                          kT_d[:, :, t0 - 128:t0 + n + 128].rearrange("g p t -> p g t"), W=[b_k[sl]])
                    k.dma("v_sb%d" % sl, v_sb[sl][:, 0:nb + 2, :],
                          v_d[t0 - 128:t0 + n + 128, :].rearrange("(b p) d -> p b d", p=128), W=[b_v[sl]])
                    k.dma("u_sb%d" % sl, u_sb[sl][:, :, 0:n + 2],
                          uT_d[:, :, t0 - 1:t0 + n + 1].rearrange("g p t -> p g t"), W=[b_u[sl]])
                    k.dma("cb_sb%d" % sl, cb_sb[sl][:, :, 0:n], cbT_d[:, :, t0:t0 + n].rearrange("g p t -> p g t"),
                          W=[b_cb[sl]])
                    mx = mixT[sl]
                    bm = b_mix[sl]
                    for g in range(4):
                        pi = g % 2
                        for kk in range(31):
                            k.do(PE, lambda: T.matmul(pc[pi][:, 0:n], lhsT=Dg[:, kk, g, :],
                                                      rhs=a_sb[sl][:, g, kk:kk + n], start=(kk == 0), stop=(kk == 30)),
                                 R=[b_Dg, b_a[sl]], W=[b_pc[pi]], inc=(kk == 30))
                        k.do(ACT, lambda: A.activation(out=cv[:, g, 0:n], in_=pc[pi][:, 0:n], func=AF.Identity,
                                                       bias=cab[:, g:g + 1]), R=[b_pc[pi], b_const], W=[b_cv])
                        k.do(ACT, lambda: A.activation(out=cvb[:, g, 0:n], in_=pc[pi][:, 0:n], func=AF.Identity,
                                                       bias=cab[:, g:g + 1]), R=[b_pc[pi], b_const], W=[b_cvb])
                        k.do(ACT, lambda: A.activation(out=cv2[:, g, 0:n], in_=pc[pi][:, 0:n], func=AF.Square,
                                                       bias=cab[:, g:g + 1]), R=[b_pc[pi], b_const], W=[b_cv2])
                    for g in range(4):
                        k.do(PE, lambda: T.matmul(pmean[:, 0:n], lhsT=onesb[:], rhs=cvb[:, g, 0:n], start=(g == 0),
                                                  stop=(g == 3)), R=[b_const, b_cvb], W=[b_pmean], inc=(g == 3))
                    k.do(DVE, lambda: V.tensor_copy(out=mean_sb[:, 0:n], in_=pmean[:, 0:n]), R=[b_pmean], W=[b_mean])
                    for g in range(4):
                        k.do(PE, lambda: T.matmul(pex2[:, 0:n], lhsT=onesb[:], rhs=cv2[:, g, 0:n], start=(g == 0),
                                                  stop=(g == 3)), R=[b_const, b_cv2], W=[b_pex2], inc=(g == 3))
                    k.do(DVE, lambda: V.tensor_tensor(out=var_sb[:, 0:n], in0=mean_sb[:, 0:n], in1=mean_sb[:, 0:n],
                                                      op=ALU.mult), R=[b_mean], W=[b_var])
                    k.do(DVE, lambda: V.tensor_tensor(out=var_sb[:, 0:n], in0=pex2[:, 0:n], in1=var_sb[:, 0:n],
                                                      op=ALU.subtract), R=[b_pex2, b_var], W=[b_var])
                    k.do(DVE, lambda: V.tensor_scalar(out=var_sb[:, 0:n], in0=var_sb[:, 0:n], scalar1=0.0,
                                                      scalar2=None, op0=ALU.max), R=[b_var], W=[b_var])
                    k.do(ACT, lambda: A.activation(out=var_sb[:, 0:n], in_=var_sb[:, 0:n], func=AF.Sqrt, bias=EPS),
                         R=[b_var], W=[b_var])
                    k.do(DVE, lambda: V.reciprocal(out=rstd_sb[:, 0:n], in_=var_sb[:, 0:n]), R=[b_var], W=[b_rstd])
                    for g in range(4):
                        ti_ = gi_ % 2
                        gi_ += 1
                        k.do(DVE, lambda: V.tensor_tensor(out=t1[ti_][:, 0:n], in0=cv[:, g, 0:n], in1=mean_sb[:, 0:n],
                                                          op=ALU.subtract), R=[b_cv, b_mean], W=[b_t1[ti_]])
                        k.do(DVE, lambda: V.tensor_tensor(out=t1[ti_][:, 0:n], in0=t1[ti_][:, 0:n],
                                                          in1=rstd_sb[:, 0:n], op=ALU.mult),
                             R=[b_t1[ti_], b_rstd], W=[b_t1[ti_]])
                        k.do(ACT, lambda: A.activation(out=mx[:, g, 0:n], in_=t1[ti_][:, 0:n], func=AF.Silu,
                                                       scale=lng[:, g:g + 1], bias=lnb[:, g:g + 1]),
                             R=[b_t1[ti_], b_const], W=[bm])
                    for g in range(4):
                        ai = g % 2
                        k.do(ACT, lambda: A.activation(out=acc[ai][:, 0:n], in_=u_sb[sl][:, g, 0:n], func=AF.Identity,
                                                       scale=ccw[:, g, 0:1]), R=[b_u[sl], b_const], W=[b_acc[ai]])
                        for kk in (1, 2):
                            k.do(ACT, lambda: A.activation(out=acc2[kk - 1][:, 0:n], in_=u_sb[sl][:, g, kk:kk + n],
                                                           func=AF.Identity, scale=ccw[:, g, kk:kk + 1]),
                                 R=[b_u[sl], b_const], W=[b_acc2[kk - 1]])
                        for kk in (1, 2):
                            k.do(POOL, lambda: G.tensor_tensor(out=acc[ai][:, 0:n], in0=acc[ai][:, 0:n],
                                                               in1=acc2[kk - 1][:, 0:n], op=ALU.add),
                                 R=[b_acc2[kk - 1], b_acc[ai]], W=[b_acc[ai]])
                        k.do(POOL, lambda: G.tensor_tensor(out=mx[:, 12 + g, 0:n], in0=acc[ai][:, 0:n],
                                                           in1=cb_sb[sl][:, g, 0:n], op=ALU.mult),
                             R=[b_acc[ai], b_cb[sl]], W=[bm])
                    ntl_ = len(tl)
                    for e in range(ti * 16 // ntl_, (ti + 1) * 16 // ntl_):
                        cast_moe(l, e)
                    if l == 0:
                        for i in range(ti * 12 // ntl_, (ti + 1) * 12 // ntl_):
                            if i < 8:
                                cast_in(1, i)
                            else:
                                cast_out(1, i - 8)
                    for b in range(nb):
                        gb = gb0 + b
                        left_edge = (gb == bs + 2)
                        right_edge = (gb == bs + nbk - 3)
                        for hp in range(4):
                            kv = hp // 2
                            pair = [(0, 2 * hp), (1, 2 * hp + 1)]
                            for i2, h in pair:
                                k.do(PE, lambda: T.matmul(s_ps[i2][:, 0:384], lhsT=q_sb[sl][:, h, b * 128:(b + 1) * 128],
                                                          rhs=k_sb[sl][:, kv, b * 128:b * 128 + 384], start=True, stop=True),
                                     R=[b_q[sl], b_k[sl]], W=[b_sps[i2]])
                            for i2, h in pair:
                                sp, bsp = s_ps[i2], b_sps[i2]
                                ssb, bss = s_sb[i2], b_s[i2]
                                stt, bst = st[i2], b_st[i2]
                                k.do(DVE, lambda: V.scalar_tensor_tensor(out=ssb[:], in0=sp[:, 0:384], scalar=SCALE,
                                                                         in1=bias8[:, h, :], op0=ALU.mult, op1=ALU.add),
                                     R=[bsp, b_const], W=[bss])
                                if left_edge:
                                    fi = 4 + (0 if seg == 0 else 2)
                                    k.do(DVE, lambda: V.tensor_scalar(out=ssb[:, 0:128], in0=ssb[:, 0:128],
                                                                      scalar1=flg[:, fi:fi + 1], scalar2=None, op0=ALU.add),
                                         R=[bss, b_const], W=[bss])
                                if right_edge:
                                    fi = 4 + (1 if seg == 0 else 3)
                                    k.do(DVE, lambda: V.tensor_scalar(out=ssb[:, 256:384], in0=ssb[:, 256:384],
                                                                      scalar1=flg[:, fi:fi + 1], scalar2=None, op0=ALU.add),
                                         R=[bss, b_const], W=[bss])
                                k.do(DVE, lambda: V.reduce_max(out=stt[:, 0:1], in_=ssb[:], axis=AX.X), R=[bss], W=[bst])
                                k.do(DVE, lambda: V.tensor_scalar(out=stt[:, 1:2], in0=stt[:, 0:1], scalar1=sinkb[:, h:h + 1],
                                                                  scalar2=-1.0, op0=ALU.max, op1=ALU.mult),
                                     R=[bst, b_const], W=[bst])
                                k.do(DVE, lambda: V.memset(stt[:, 2:3], 0.0), R=[bst], W=[bst])
                            for i2, h in pair:
                                ssb, bss = s_sb[i2], b_s[i2]
                                stt, bst = st[i2], b_st[i2]
                                k.do(ACT, lambda: A.activation(out=p_sb[i2][:], in_=ssb[:], func=AF.Exp, bias=stt[:, 1:2],
                                                               accum_out=stt[:, 2:3]), R=[bss, bst], W=[b_p[i2], bst])
                                k.do(ACT, lambda: A.activation(out=stt[:, 3:4], in_=sinkb[:, h:h + 1], func=AF.Exp,
                                                               bias=stt[:, 1:2]), R=[bst, b_const], W=[bst])
                            for i2, h in pair:
                                stt, bst = st[i2], b_st[i2]
                                k.do(DVE, lambda: V.tensor_tensor(out=stt[:, 4:5], in0=stt[:, 2:3], in1=stt[:, 3:4],
                                                                  op=ALU.add), R=[bst], W=[bst])
                                k.do(DVE, lambda: V.reciprocal(out=stt[:, 5:6], in_=stt[:, 4:5]), R=[bst], W=[bst])
                                k.do(DVE, lambda: V.tensor_scalar(out=pn_sb[i2][:], in0=p_sb[i2][:], scalar1=stt[:, 5:6],
                                                                  scalar2=None, op0=ALU.mult),
                                     R=[b_p[i2], bst], W=[b_pn[i2]])
                            for i2, h in pair:
                                for j in range(3):
                                    k.do(PE, lambda: T.transpose(out=pT_ps[i2][:, j * 128:(j + 1) * 128],
                                                                 in_=pn_sb[i2][:, j * 128:(j + 1) * 128], identity=ident[:]),
                                         R=[b_pn[i2], b_const], W=[b_pTp[i2]], inc=(j == 2))
                            for i2, h in pair:
                                k.do(ACT, lambda: A.activation(out=pT_sb[i2][:],
                                                               in_=pT_ps[i2][:, 0:384], func=AF.Identity),
                                     R=[b_pTp[i2]], W=[b_pTs[i2]])
                            for i2, h in pair:
                                for j in range(3):
                                    k.do(PE, lambda: T.matmul(o_ps[i2][:, 0:128],
                                                              lhsT=v_sb[sl][:, b + j, kv * 128:(kv + 1) * 128],
                                                              rhs=pT_sb[i2][:, j * 128:(j + 1) * 128], start=(j == 0),
                                                              stop=(j == 2)),
                                         R=[b_v[sl], b_pTs[i2]], W=[b_ops[i2]], inc=(j == 2))
                            for i2, h in pair:
                                k.do(DVE, lambda: V.tensor_copy(out=mx[:, 4 + h, b * 128:(b + 1) * 128],
                                                                in_=o_ps[i2][:, 0:128]),
                                     R=[b_ops[i2]], W=[bm])
                    k.dma("st_mix%d" % sl, mix_d[:, :, t0:t0 + n].rearrange("g p t -> p g t"), mx[:, :, 0:n], R=[bm],
                          q=POOL)
                k.barrier()

            with ExitStack() as es:
                E = es.enter_context
                b_const = B()
                wo = E(sbt("wo", [128, 16, D], BF16))
                SP.wait_tok(conv_tok["out%d" % l])
                k.dma("c_wo", wo[:], wb_out[l].rearrange("(c p) n -> p c n", p=128), W=[b_const])
                gm = E(sbt("gm", [128, 2, D], F32))
                for s in range(2):
                    k.dma("c_gm", gm[:, s, :], mod_d[l, s:s + 1, 2 * D:3 * D].partition_broadcast(128), W=[b_const])
                mixs = [E(sbt("mixs%d" % i, [128, 16, 512], BF16)) for i in range(2)]
                xs = [E(sbt("xs%d" % i, [128, D], F32)) for i in range(2)]
                tmp = [E(sbt("tmp%d" % i, [128, 512], F32)) for i in range(2)]
                xo = [E(sbt("xo%d" % i, [128, D], F32)) for i in range(2)]
                po = [E(pst("po%d" % i, [128, 512], F32)) for i in range(8)]
                b_mixs, b_xs, b_tmp, b_xo, b_po = Bs(2), Bs(2), Bs(2), Bs(2), Bs(8)
                bi_ = 0
                tmi = 0
                for ti, (seg, gb0, nb) in enumerate(tl):
                    sl = ti % 2
                    n = nb * 128
                    t0 = gb0 * 128
                    k.dma("mixs%d" % sl, mixs[sl][:, :, 0:n], mix_d[:, :, t0:t0 + n].rearrange("g p t -> p g t"),
                          W=[b_mixs[sl]])
                    for b in range(nb):
                        i2 = bi_ % 2
                        bi_ += 1
                        r0 = (gb0 + b) * 128
                        k.dma("xs%d" % i2, xs[i2][:], x_src1[r0:r0 + 128, :], W=[b_xs[i2]])
                        for c in range(16):
                            for ft in range(4):
                                p = i2 * 4 + ft
                                k.do(PE, lambda: T.matmul(po[p][:], lhsT=mixs[sl][:, c, b * 128:(b + 1) * 128],
                                                          rhs=wo[:, c, ft * 512:(ft + 1) * 512], start=(c == 0),
                                                          stop=(c == 15)),
                                     R=[b_mixs[sl], b_const], W=[b_po[p]], inc=(c == 15 and ft == 3))
                        for ft in range(4):
                            p = i2 * 4 + ft
                            tm = tmi % 2
                            tmi += 1
                            k.do(DVE, lambda: V.tensor_tensor(out=tmp[tm][:], in0=po[p][:],
                                                              in1=gm[:, seg, ft * 512:(ft + 1) * 512], op=ALU.mult),
                                 R=[b_po[p], b_const], W=[b_tmp[tm]])
                            k.do(POOL, lambda: G.tensor_tensor(out=xo[i2][:, ft * 512:(ft + 1) * 512], in0=tmp[tm][:],
                                                               in1=xs[i2][:, ft * 512:(ft + 1) * 512], op=ALU.add),
                                 R=[b_tmp[tm], b_xs[i2]], W=[b_xo[i2]])
                        k.dma("st_xo%d" % i2, x_mid[r0:r0 + 128, :], xo[i2][:], R=[b_xo[i2]], q=POOL)
                k.barrier()

            blocks = [(seg, gb0 + b) for (seg, gb0, nb) in tl for b in range(nb)]
            NB3 = len(blocks)
            NTL = NB3 + 16
            NSLOT = NTL * 256
            Hs_l = Hslots[0:NSLOT, :]
            Ys_l = Yslots[0:NSLOT, :]
            with ExitStack() as es3:
                E3 = es3.enter_context
                b_rout = B()
                selA = E3(sbt("selA", [128, NB3, 16], F32))
                selB = E3(sbt("selB", [128, NB3, 16], F32))
                Rg = E3(sbt("Rg", [128, NB3, 16], F32))
                cA = E3(sbt("cA", [128, NB3], F32))
                cB = E3(sbt("cB", [128, NB3], F32))
                posA_i = E3(sbt("posA_i", [128, NB3], I32))
                posB_i = E3(sbt("posB_i", [128, NB3], I32))
                idxw = E3(sbt("idxw", [128, NTL], I32))
                base = E3(sbt("base", [128, 16], F32))

                with ExitStack() as es:
                    E = es.enter_context
                    b_const = B()
                    ident = load_const(E, "ident", [128, 128], BF16, ident_d, b_const)
                    utri = load_const(E, "utri", [128, 128], BF16, utri_d, b_const)
                    ones1 = load_const(E, "ones1", [128, 128], BF16, ones1_d, b_const)
                    thr = load_const(E, "thr", [128, 80], F32, thr_d, b_const)
                    iotap = load_const(E, "iotap", [128, 1], F32, iotap_d, b_const)
                    abc = E(sbt("abc", [128, 2, D], F32))
                    bbc = E(sbt("bbc", [128, 2, D], F32))
                    for s in range(2):
                        k.dma("c_abc", abc[:, s, :], mod_d[l, s:s + 1, 4 * D:5 * D].partition_broadcast(128), W=[b_const])
                        k.dma("c_bbc", bbc[:, s, :], mod_d[l, s:s + 1, 3 * D:4 * D].partition_broadcast(128), W=[b_const])
                    wr32 = load_const(E, "wr32", [128, 16, 20], F32, wr_d[l], b_const)
                    wrb = E(sbt("wrb", [128, 16, 20], BF16))
                    k.do(DVE, lambda: V.tensor_copy(out=wrb[:], in_=wr32[:]), R=[b_const], W=[b_const])
                    brb = load_const(E, "brb", [128, 20], F32, br_d[l].partition_broadcast(128), b_const)
                    xs = [E(sbt("xs%d" % i, [128, D], F32)) for i in range(2)]
                    tmpf = [E(sbt("tmpf%d" % i, [128, D], F32)) for i in range(2)]
                    junk = E(sbt("junk", [128, D], BF16))
                    ss = E(sbt("ss", [128, 12], F32))
                    xh = E(sbt("xh", [128, 4, D], BF16))
                    hT = E(sbt("hT", [128, 16, 512], BF16))
                    rt = E(sbt("rt", [128, 64], F32))
                    s16 = E(sbt("s16", [128, 16], BF16))
                    cmp = E(sbt("cmp", [128, 80], F32))
                    q16 = E(sbt("q16", [128, 64], F32))
                    etf = E(sbt("etf", [128, NTL], F32))
                    posf = E(sbt("posf", [128, 2, NB3], F32))
                    pT = [E(pst("pT%d" % i, [128, 1024], BF16)) for i in range(2)]
                    prt = E(pst("prt", [128, 512], F32))
                    prk = E(pst("prk", [128, 512], F32))
                    ptt = E(pst("ptt", [128, 512], F32))
                    b_xs, b_tmpf, b_pT = Bs(2), Bs(2), Bs(2)
                    b_junk, b_ss, b_xh, b_hT, b_rt, b_s16, b_prt, b_prk, b_ptt = B(), B(), B(), B(), B(), B(), B(), B(), B()
                    k.do(DVE, lambda: V.memset(base[:], 0.0), W=[b_rout])
                    zt = E(sbt("zt", [128, 4, D], BF16))
                    b_zt = B()
                    k.do(DVE, lambda: V.memset(zt[:], 0.0), W=[b_zt])
                    for j0 in range(0, 2 * NTL, 4):
                        k.dma("zero_hs", Hs_l[j0 * 128:(j0 + 4) * 128, :].rearrange("(j p) f -> p j f", p=128), zt[:],
                              R=[b_zt], q=POOL)
                    xi = 0
                    bi = 0
                    for ti, (seg, gb0, nb) in enumerate(tl):
                        n = nb * 128
                        k.do(DVE, lambda: V.memset(ss[:], 0.0), W=[b_ss])
                        for b in range(nb):
                            si = xi % 2
                            xi += 1
                            r0 = (gb0 + b) * 128
                            k.dma("xs%d" % si, xs[si][:], x_mid[r0:r0 + 128, :], W=[b_xs[si]])
                            k.do(ACT, lambda: A.activation(out=junk[:], in_=xs[si][:], func=AF.Square,
                                                           accum_out=ss[:, b:b + 1]), R=[b_xs[si], b_ss], W=[b_junk, b_ss])
                            k.do(ACT, lambda: A.activation(out=ss[:, 4 + b:5 + b], in_=ss[:, b:b + 1], func=AF.Sqrt,
                                                           scale=1.0 / D, bias=EPS), R=[b_ss], W=[b_ss])
                            k.do(DVE, lambda: V.reciprocal(out=ss[:, 8 + b:9 + b], in_=ss[:, 4 + b:5 + b]), R=[b_ss], W=[b_ss])
                            k.do(DVE, lambda: V.scalar_tensor_tensor(out=tmpf[si][:], in0=xs[si][:], scalar=ss[:, 8 + b:9 + b],
                                                                     in1=abc[:, seg, :], op0=ALU.mult, op1=ALU.mult),
                                 R=[b_xs[si], b_ss, b_const], W=[b_tmpf[si]])
                            k.do(POOL, lambda: G.tensor_tensor(out=xh[:, b, :], in0=tmpf[si][:], in1=bbc[:, seg, :], op=ALU.add),
                                 R=[b_tmpf[si], b_const], W=[b_xh])
                            k.dma("st_hrow", Hrows[r0:r0 + 128, :], xh[:, b, :], R=[b_xh], q=POOL)
                        for c in range(16):
                            pi = c % 2
                            for b in range(nb):
                                k.do(PE, lambda: T.transpose(out=pT[pi][:, b * 128:(b + 1) * 128],
                                                             in_=xh[:, b, c * 128:(c + 1) * 128], identity=ident[:]),
                                     R=[b_xh, b_const], W=[b_pT[pi]], inc=(b == nb - 1))
                            k.do(ACT, lambda: A.activation(out=hT[:, c, 0:n], in_=pT[pi][:, 0:n], func=AF.Identity),
                                 R=[b_pT[pi]], W=[b_hT])
                        for b in range(nb):
                            for c in range(16):
                                k.do(PE, lambda: T.matmul(prt[:, 0:20], lhsT=hT[:, c, b * 128:(b + 1) * 128], rhs=wrb[:, c, :],
                                                          start=(c == 0), stop=(c == 15)),
                                     R=[b_hT, b_const], W=[b_prt], inc=(c == 15))

                            def dv(fn, extraR=(), extraW=()):
                                k.do(DVE, fn, R=[b_rt] + list(extraR), W=[b_rt] + list(extraW))
                            dv(lambda: V.tensor_tensor(out=rt[:, 0:20], in0=prt[:, 0:20], in1=brb[:], op=ALU.add),
                               extraR=[b_prt, b_const])
                            dv(lambda: V.reduce_max(out=rt[:, 20:21], in_=rt[:, 0:4], axis=AX.X))
                            dv(lambda: V.tensor_scalar(out=rt[:, 24:28], in0=rt[:, 0:4], scalar1=rt[:, 20:21], scalar2=None,
                                                       op0=ALU.is_equal))
                            dv(lambda: V.tensor_scalar(out=rt[:, 21:22], in0=rt[:, 20:21], scalar1=-1.0, scalar2=None,
                                                       op0=ALU.mult))
                            dv(lambda: V.memset(rt[:, 22:23], 0.0))
                            k.do(ACT, lambda: A.activation(out=rt[:, 28:32], in_=rt[:, 0:4], func=AF.Exp, bias=rt[:, 21:22],
                                                           accum_out=rt[:, 22:23]), R=[b_rt], W=[b_rt])
                            dv(lambda: V.reciprocal(out=rt[:, 23:24], in_=rt[:, 22:23]))
                            dv(lambda: V.tensor_scalar(out=rt[:, 32:36], in0=rt[:, 4:8], scalar1=rt[:, 24:25], scalar2=None,
                                                       op0=ALU.mult))
                            for g in range(1, 4):
                                dv(lambda: V.scalar_tensor_tensor(out=rt[:, 32:36], in0=rt[:, 4 + 4 * g:8 + 4 * g],
                                                                  scalar=rt[:, 24 + g:25 + g], in1=rt[:, 32:36],
                                                                  op0=ALU.mult, op1=ALU.add))
                            dv(lambda: V.reduce_max(out=rt[:, 36:37], in_=rt[:, 32:36], axis=AX.X))
                            dv(lambda: V.tensor_scalar(out=rt[:, 40:44], in0=rt[:, 32:36], scalar1=rt[:, 36:37], scalar2=None,
                                                       op0=ALU.is_equal))
                            dv(lambda: V.scalar_tensor_tensor(out=rt[:, 44:48], in0=rt[:, 40:44], scalar=NEG,
                                                              in1=rt[:, 32:36], op0=ALU.mult, op1=ALU.add))
                            dv(lambda: V.reduce_max(out=rt[:, 37:38], in_=rt[:, 44:48], axis=AX.X))
                            dv(lambda: V.tensor_scalar(out=rt[:, 48:52], in0=rt[:, 44:48], scalar1=rt[:, 37:38], scalar2=None,
                                                       op0=ALU.is_equal))
                            dv(lambda: V.tensor_scalar(out=rt[:, 38:39], in0=rt[:, 36:37], scalar1=-1.0, scalar2=None,
                                                       op0=ALU.mult))
                            k.do(ACT, lambda: A.activation(out=rt[:, 52:56], in_=rt[:, 32:36], func=AF.Exp, bias=rt[:, 38:39]),
                                 R=[b_rt], W=[b_rt])
                            dv(lambda: V.tensor_tensor(out=rt[:, 56:60], in0=rt[:, 52:56], in1=rt[:, 48:52], op=ALU.mult))
                            dv(lambda: V.reduce_sum(out=rt[:, 39:40], in_=rt[:, 56:60], axis=AX.X))
                            dv(lambda: V.tensor_scalar(out=rt[:, 60:61], in0=rt[:, 39:40], scalar1=1.0, scalar2=None,
                                                       op0=ALU.add))
                            dv(lambda: V.reciprocal(out=rt[:, 61:62], in_=rt[:, 60:61]))
                            dv(lambda: V.tensor_tensor(out=cA[:, bi:bi + 1], in0=rt[:, 61:62], in1=rt[:, 23:24], op=ALU.mult),
                               extraW=[b_rout])
                            dv(lambda: V.tensor_tensor(out=cB[:, bi:bi + 1], in0=cA[:, bi:bi + 1], in1=rt[:, 39:40], op=ALU.mult),
                               extraR=[b_rout], extraW=[b_rout])
                            for g in range(4):
                                dv(lambda: V.tensor_scalar(out=selA[:, bi, 4 * g:4 * g + 4], in0=rt[:, 40:44],
                                                           scalar1=rt[:, 24 + g:25 + g], scalar2=None, op0=ALU.mult),
                                   extraW=[b_rout])
                                dv(lambda: V.tensor_scalar(out=selB[:, bi, 4 * g:4 * g + 4], in0=rt[:, 48:52],
                                                           scalar1=rt[:, 24 + g:25 + g], scalar2=None, op0=ALU.mult),
                                   extraW=[b_rout])
                            k.do(DVE, lambda: V.tensor_tensor(out=s16[:], in0=selA[:, bi, :], in1=selB[:, bi, :], op=ALU.add),
                                 R=[b_rout], W=[b_s16])
                            k.do(PE, lambda: T.matmul(prk[:, 0:16], lhsT=utri[:], rhs=s16[:], start=True, stop=True),
                                 R=[b_const, b_s16], W=[b_prk])
                            k.do(PE, lambda: T.matmul(ptt[:, 0:16], lhsT=ones1[:], rhs=s16[:], start=True, stop=True),
                                 R=[b_const, b_s16], W=[b_ptt])
                            k.do(DVE, lambda: V.tensor_tensor(out=Rg[:, bi, :], in0=prk[:, 0:16], in1=base[:], op=ALU.add),
                                 R=[b_prk, b_rout], W=[b_rout])
                            k.do(DVE, lambda: V.tensor_tensor(out=base[:], in0=ptt[:, 0:16], in1=base[:], op=ALU.add),
                                 R=[b_ptt, b_rout], W=[b_rout])
                            bi += 1
                    def dr(fn):
                        k.do(DVE, fn, R=[b_rout, b_rt, b_const], W=[b_rout, b_rt])
                    for e in range(16):
                        dr(lambda: V.tensor_scalar(out=cmp[:, 0:80], in0=thr[:, 0:80], scalar1=base[:, e:e + 1], scalar2=None,
                                                   op0=ALU.is_lt))
                        dr(lambda: V.reduce_sum(out=q16[:, e:e + 1], in_=cmp[:, 0:80], axis=AX.X))
                    dr(lambda: V.tensor_scalar(out=q16[:, 0:16], in0=q16[:, 0:16], scalar1=256.0, scalar2=None, op0=ALU.mult))
                    dr(lambda: V.memset(q16[:, 16:17], 0.0))
                    for e in range(1, 16):
                        dr(lambda: V.tensor_tensor(out=q16[:, 16 + e:17 + e], in0=q16[:, 15 + e:16 + e], in1=q16[:, e - 1:e],
                                                   op=ALU.add))
                    dr(lambda: V.tensor_tensor(out=q16[:, 32:48], in0=q16[:, 16:32], in1=q16[:, 0:16], op=ALU.add))
                    dr(lambda: V.memset(etf[:], 0.0))
                    for e in range(16):
                        dr(lambda: V.tensor_scalar(out=cmp[:, 0:NTL], in0=thr[:, 0:NTL], scalar1=q16[:, 32 + e:33 + e],
                                                   scalar2=None, op0=ALU.is_ge))
                        dr(lambda: V.tensor_tensor(out=etf[:], in0=etf[:], in1=cmp[:, 0:NTL], op=ALU.add))
                    dr(lambda: V.tensor_scalar(out=etf[:], in0=etf[:], scalar1=15.0, scalar2=None, op0=ALU.min))
                    dr(lambda: V.tensor_scalar(out=etf[:], in0=etf[:], scalar1=128.0, scalar2=iotap[:, 0:1], op0=ALU.mult,
                                               op1=ALU.add))
                    dr(lambda: V.tensor_copy(out=idxw[:], in_=etf[:]))
                    for bi in range(NB3):
                        dr(lambda: V.tensor_tensor(out=rt[:, 0:16], in0=Rg[:, bi, :], in1=q16[:, 16:32], op=ALU.add))
                        dr(lambda: V.tensor_tensor(out=rt[:, 16:32], in0=rt[:, 0:16], in1=selA[:, bi, :], op=ALU.mult))
                        dr(lambda: V.reduce_sum(out=posf[:, 0, bi:bi + 1], in_=rt[:, 16:32], axis=AX.X))
                        dr(lambda: V.tensor_tensor(out=rt[:, 32:48], in0=rt[:, 0:16], in1=selB[:, bi, :], op=ALU.mult))
                        dr(lambda: V.reduce_sum(out=posf[:, 1, bi:bi + 1], in_=rt[:, 32:48], axis=AX.X))
                    dr(lambda: V.tensor_copy(out=posA_i[:], in_=posf[:, 0, :]))
                    dr(lambda: V.tensor_copy(out=posB_i[:], in_=posf[:, 1, :]))
                    k.barrier()

                with ExitStack() as es:
                    E = es.enter_context
                    hr = [E(sbt("hr%d" % i, [128, D], BF16)) for i in range(4)]
                    b_hr = Bs(4)
                    for bi, (seg, gb) in enumerate(blocks):
                        hi = bi % 4
                        k.dma("hr%d" % hi, hr[hi][:], Hrows[gb * 128:(gb + 1) * 128, :], W=[b_hr[hi]])
                        k.dma_ind("sc_a%d" % hi, Hs_l, bass.IndirectOffsetOnAxis(ap=posA_i[:, bi:bi + 1], axis=0), hr[hi][:], None,
                                  NSLOT, R=[b_hr[hi], b_rout])
                        k.dma_ind("sc_b%d" % hi, Hs_l, bass.IndirectOffsetOnAxis(ap=posB_i[:, bi:bi + 1], axis=0), hr[hi][:], None,
                                  NSLOT, R=[b_hr[hi], b_rout])
                    k.barrier()

                with ExitStack() as es:
                    E = es.enter_context
                    b_const = B()
                    ident = load_const(E, "ident", [128, 128], BF16, ident_d, b_const)
                    wq = [E(sbt("wq%d" % i, [128, 8192], BF16)) for i in range(6)]
                    hs = [E(sbt("hs%d" % i, [128, 2, D], BF16)) for i in range(2)]
                    hsT = [E(sbt("hsT%d" % i, [128, 16, 256], BF16)) for i in range(2)]
                    he = [E(sbt("he%d" % i, [128, 4, 256], BF16)) for i in range(2)]
                    sg = [E(sbt("sg%d" % i, [128, 256], F32)) for i in range(2)]
                    yo = [E(sbt("yo%d" % i, [128, D], F32)) for i in range(2)]
                    pT = [E(pst("pT%d" % i, [128, 1024], BF16)) for i in range(2)]
                    pg = [E(pst("pg%d" % i, [128, 512], F32)) for i in range(2)]
                    pu = [E(pst("pu%d" % i, [128, 512], F32)) for i in range(2)]
                    pd = [E(pst("pd%d" % i, [128, 512], F32)) for i in range(2)]
                    b_wq, b_hs, b_hsT, b_he, b_sg, b_yo = Bs(6), Bs(2), Bs(2), Bs(2), Bs(2), Bs(2)
                    b_pT, b_pg, b_pu, b_pd = Bs(2), Bs(2), Bs(2), Bs(2)
                    POOL.wait_tok(conv_tok["g%d" % l])
                    POOL.wait_tok(conv_tok["u%d" % l])
                    POOL.wait_tok(conv_tok["d%d" % l])
                    wsrc = [wb_g[l], wb_u[l], wb_d[l]]

                    def load_w(i):
                        for m in range(3):
                            ws = (3 * i + m) % 6
                            k.dma_ind("wq%d" % ws, wq[ws][:], None, wsrc[m],
                                      bass.IndirectOffsetOnAxis(ap=idxw[:, i:i + 1], axis=0), 2048, R=[b_rout], W=[b_wq[ws]])
                    load_w(0)
                    ji_ = 0
                    di_ = 0
                    ev_ = 0
                    for i in range(NTL):
                        sl = i % 2
                        if i + 1 < NTL:
                            load_w(i + 1)
                        if i == 0:
                            k.dma("hs0", hs[0][:], Hs_l[0:256, :].rearrange("(s p) f -> p s f", p=128), W=[b_hs[0]])
                        if i + 1 < NTL:
                            sn = (i + 1) % 2
                            k.dma("hs%d" % sn, hs[sn][:],
                                  Hs_l[(i + 1) * 256:(i + 2) * 256, :].rearrange("(s p) f -> p s f", p=128), W=[b_hs[sn]])
                        wgs, wus, wds = [(3 * i + m) % 6 for m in range(3)]
                        wgv = wq[wgs][:].rearrange("p (c n) -> p c n", c=16)
                        wuv = wq[wus][:].rearrange("p (c n) -> p c n", c=16)
                        wdv = wq[wds][:].rearrange("p (c n) -> p c n", c=4)
                        for c in range(16):
                            pi = c % 2
                            for sb_ in range(2):
                                k.do(PE, lambda: T.transpose(out=pT[pi][:, sb_ * 128:(sb_ + 1) * 128],
                                                             in_=hs[sl][:, sb_, c * 128:(c + 1) * 128], identity=ident[:]),
                                     R=[b_hs[sl], b_const], W=[b_pT[pi]], inc=(sb_ == 1))
                            if c % 2 == 0:
                                k.do(ACT, lambda: A.activation(out=hsT[sl][:, c, :], in_=pT[pi][:, 0:256], func=AF.Identity),
                                     R=[b_pT[pi]], W=[b_hsT[sl]])
                            else:
                                k.do(DVE, lambda: V.tensor_copy(out=hsT[sl][:, c, :], in_=pT[pi][:, 0:256]),
                                     R=[b_pT[pi]], W=[b_hsT[sl]])
                        for j in range(4):
                            pi = ji_ % 2
                            ji_ += 1
                            for c in range(16):
                                k.do(PE, lambda: T.matmul(pg[pi][:, 0:256], lhsT=wgv[:, c, j * 128:(j + 1) * 128],
                                                          rhs=hsT[sl][:, c, :], start=(c == 0), stop=(c == 15)),
                                     R=[b_wq[wgs], b_hsT[sl]], W=[b_pg[pi]], inc=(c == 15))
                            for c in range(16):
                                k.do(PE, lambda: T.matmul(pu[pi][:, 0:256], lhsT=wuv[:, c, j * 128:(j + 1) * 128],
                                                          rhs=hsT[sl][:, c, :], start=(c == 0), stop=(c == 15)),
                                     R=[b_wq[wus], b_hsT[sl]], W=[b_pu[pi]], inc=(c == 15))
                            k.do(ACT, lambda: A.activation(out=sg[pi][:], in_=pg[pi][:, 0:256], func=AF.Silu),
                                 R=[b_pg[pi]], W=[b_sg[pi]])
                            k.do(DVE, lambda: V.tensor_tensor(out=he[sl][:, j, :], in0=sg[pi][:], in1=pu[pi][:, 0:256],
                                                              op=ALU.mult), R=[b_sg[pi], b_pu[pi]], W=[b_he[sl]])
                        for sb_ in range(2):
                            yi = (2 * i + sb_) % 2
                            for ft in range(4):
                                pi = di_ % 2
                                di_ += 1
                                for j in range(4):
                                    k.do(PE, lambda: T.matmul(pd[pi][:], lhsT=he[sl][:, j, sb_ * 128:(sb_ + 1) * 128],
                                                              rhs=wdv[:, j, ft * 512:(ft + 1) * 512], start=(j == 0),
                                                              stop=(j == 3)),
                                         R=[b_he[sl], b_wq[wds]], W=[b_pd[pi]], inc=(j == 3))
                                ev_ += 1
                                if ev_ % 2 == 0:
                                    k.do(ACT, lambda: A.activation(out=yo[yi][:, ft * 512:(ft + 1) * 512], in_=pd[pi][:],
                                                                   func=AF.Identity), R=[b_pd[pi]], W=[b_yo[yi]])
                                else:
                                    k.do(DVE, lambda: V.tensor_copy(out=yo[yi][:, ft * 512:(ft + 1) * 512], in_=pd[pi][:]),
                                         R=[b_pd[pi]], W=[b_yo[yi]])
                            r0 = (2 * i + sb_) * 128
                            k.dma("st_yo%d" % yi, Ys_l[r0:r0 + 128, :], yo[yi][:], R=[b_yo[yi]])
                    k.barrier()

                with ExitStack() as es:
                    E = es.enter_context
                    b_const = B()
                    gf = E(sbt("gf", [128, 2, D], F32))
                    for s in range(2):
                        k.dma("c_gf", gf[:, s, :], mod_d[l, s:s + 1, 5 * D:6 * D].partition_broadcast(128), W=[b_const])
                    if l == 1:
                        fg = load_const(E, "fg", [128, D], F32, fng_d.partition_broadcast(128), b_const)
                        junk = E(sbt("junk", [128, D], BF16))
                        ss = E(sbt("ss", [128, 12], F32))
                        b_junk, b_ss = B(), B()
                    xs = [E(sbt("xs%d" % i, [128, D], F32)) for i in range(2)]
                    yA = [E(sbt("yA%d" % i, [128, D], F32)) for i in range(2)]
                    yB = [E(sbt("yB%d" % i, [128, D], F32)) for i in range(2)]
                    tt_ = [E(sbt("tt%d" % i, [128, D], F32)) for i in range(2)]
                    xo = [E(sbt("xo%d" % i, [128, D], F32)) for i in range(2)]
                    b_xs, b_yA, b_yB, b_tt, b_xo = Bs(2), Bs(2), Bs(2), Bs(2), Bs(2)
                    for bi, (seg, gb) in enumerate(blocks):
                        i2 = bi % 2
                        r0 = gb * 128
                        k.dma("xs%d" % i2, xs[i2][:], x_mid[r0:r0 + 128, :], W=[b_xs[i2]])
                        k.dma_ind("ga%d" % i2, yA[i2][:], None, Ys_l, bass.IndirectOffsetOnAxis(ap=posA_i[:, bi:bi + 1], axis=0),
                                  NSLOT, R=[b_rout], W=[b_yA[i2]])
                        k.dma_ind("gb%d" % i2, yB[i2][:], None, Ys_l, bass.IndirectOffsetOnAxis(ap=posB_i[:, bi:bi + 1], axis=0),
                                  NSLOT, R=[b_rout], W=[b_yB[i2]])
                        k.do(DVE, lambda: V.tensor_scalar(out=tt_[i2][:], in0=yA[i2][:], scalar1=cA[:, bi:bi + 1], scalar2=None,
                                                          op0=ALU.mult), R=[b_yA[i2], b_rout], W=[b_tt[i2]])
                        k.do(DVE, lambda: V.scalar_tensor_tensor(out=tt_[i2][:], in0=yB[i2][:], scalar=cB[:, bi:bi + 1],
                                                                 in1=tt_[i2][:], op0=ALU.mult, op1=ALU.add),
                             R=[b_yB[i2], b_rout, b_tt[i2]], W=[b_tt[i2]])
                        k.do(POOL, lambda: G.tensor_tensor(out=tt_[i2][:], in0=tt_[i2][:], in1=gf[:, seg, :], op=ALU.mult),
                             R=[b_tt[i2], b_const], W=[b_tt[i2]])
                        k.do(DVE, lambda: V.tensor_tensor(out=xo[i2][:], in0=tt_[i2][:], in1=xs[i2][:], op=ALU.add),
                             R=[b_tt[i2], b_xs[i2]], W=[b_xo[i2]])
                        if l == 0:
                            k.dma("st_xo%d" % i2, x_dst[r0:r0 + 128, :], xo[i2][:], R=[b_xo[i2]])
                        else:
                            k.do(DVE, lambda: V.memset(ss[:, 0:1], 0.0), W=[b_ss])
                            k.do(ACT, lambda: A.activation(out=junk[:], in_=xo[i2][:], func=AF.Square, accum_out=ss[:, 0:1]),
                                 R=[b_xo[i2], b_ss], W=[b_junk, b_ss])
                            k.do(ACT, lambda: A.activation(out=ss[:, 4:5], in_=ss[:, 0:1], func=AF.Sqrt, scale=1.0 / D,
                                                           bias=EPS), R=[b_ss], W=[b_ss])
                            k.do(DVE, lambda: V.reciprocal(out=ss[:, 8:9], in_=ss[:, 4:5]), R=[b_ss], W=[b_ss])
                            k.do(DVE, lambda: V.scalar_tensor_tensor(out=xo[i2][:], in0=xo[i2][:], scalar=ss[:, 8:9], in1=fg[:],
                                                                     op0=ALU.mult, op1=ALU.mult),
                                 R=[b_xo[i2], b_ss, b_const], W=[b_xo[i2]])
                            orow = (gb - 2) * 128 if seg == 0 else 2048 + (gb - 22) * 128
                            k.dma("st_y%d" % i2, y_d[orow:orow + 128, :], xo[i2][:], R=[b_xo[i2]])
                    k.barrier(final=(l == 1))
    return nc


_NC_CACHE = {}


def _alibi_bias():
    slopes = 2.0 ** (-8.0 * np.arange(1, 9, dtype=np.float64) / 8.0)
    q = np.arange(128)[:, None]
    s = np.arange(384)[None, :]
    dist = np.abs(s - 128 - q)
    out = np.empty((128, 8, 384), np.float32)
    for h in range(8):
        out[:, h, :] = np.where(dist <= 128, -slopes[h] * dist, NEG)
    return out


def kernel(x_prompt, x_sample, c_prompt, c_sample, norm_mix_g, norm_ffn_g, w_ada, b_ada, w_in, w_out,
           conv_a_w, conv_a_b, ln_a_g, ln_a_b, attn_sink, conv_c_w, w_router_group, b_router_group,
           w_router_expert, b_router_expert, w_gate, w_up, w_down, final_norm_g):
    f = lambda a: np.ascontiguousarray(np.asarray(a, dtype=np.float32))
    x_prompt, x_sample, c_prompt, c_sample = f(x_prompt), f(x_sample), f(c_prompt), f(c_sample)
    if "nc" not in _NC_CACHE:
        _NC_CACHE["nc"] = build_program()
    nc = _NC_CACHE["nc"]

    caw = f(np.transpose(f(conv_a_w).reshape(2, 31, 4, 128), (0, 3, 2, 1)))
    cab = f(np.transpose(f(conv_a_b).reshape(2, 4, 128), (0, 2, 1)))
    lng = f(np.transpose(f(ln_a_g).reshape(2, 4, 128), (0, 2, 1)))
    lnb = f(np.transpose(f(ln_a_b).reshape(2, 4, 128), (0, 2, 1)))
    ccw = f(np.transpose(f(conv_c_w).reshape(2, 3, 4, 128), (0, 3, 2, 1)))
    wr = np.concatenate([f(w_router_group), f(w_router_expert)], axis=-1)
    wr = f(np.transpose(wr.reshape(2, 16, 128, 20), (0, 2, 1, 3)))
    br = f(np.concatenate([f(b_router_group), f(b_router_expert)], axis=-1).reshape(2, 1, 20))
    shared = dict(
        ident=np.eye(128, dtype=np.float32).astype(ml_dtypes.bfloat16),
        ones=np.full((128, 128), 1.0 / 512, np.float32).astype(ml_dtypes.bfloat16),
        biasmat=_alibi_bias(),
        utri=np.triu(np.ones((128, 128), np.float32), 1).astype(ml_dtypes.bfloat16),
        ones1=np.ones((128, 128), np.float32).astype(ml_dtypes.bfloat16),
        thr=np.tile((256.0 * np.arange(80, dtype=np.float32))[None], (128, 1)),
        iotap=np.arange(128, dtype=np.float32).reshape(128, 1),
        norm_mix_g=f(norm_mix_g), norm_ffn_g=f(norm_ffn_g), w_ada=f(w_ada), b_ada=f(b_ada), w_in=f(w_in),
        w_out=f(w_out), caw=caw, cab=cab, lng=lng, lnb=lnb, sink=f(attn_sink).reshape(2, 1, 8), ccw=ccw, wr=wr, br=br,
        w_gate=f(w_gate), w_up=f(w_up), w_down=f(w_down), final_norm_g=f(final_norm_g).reshape(1, D),
    )
    in_maps = []
    for c in range(NCORES):
        sb, half = c // 2, c % 2
        xl = np.zeros((NT, D), np.float32)
        lo, hi = 2048 * c - 256, 2048 * c + 2048 + 256
        a, b = max(lo, 0), min(hi, 16384)
        xl[a - lo:b - lo] = x_prompt[0, a:b]
        lo, hi = 4096 * half - 256, 4096 * half + 4096 + 256
        a, b = max(lo, 0), min(hi, 8192)
        xl[2560 + a - lo:2560 + b - lo] = x_sample[sb, a:b]
        fl = np.array([c > 0, c < 7, half == 1, half == 0], np.float32)
        flags = np.zeros((128, 8), np.float32)
        flags[:, 0:4] = fl[None]
        flags[:, 4:8] = np.where(fl > 0, 0.0, NEG)[None]
        cc = np.stack([c_prompt[0], c_sample[sb]], axis=-1)
        cT = f(np.transpose(cc.reshape(16, 128, 2), (1, 0, 2)))
        in_maps.append(dict(shared, x_local=xl, flags=flags, cT=cT))

    res = run_bass_kernel_spmd(nc, in_maps, core_ids=list(range(NCORES)))
    y_prompt = np.empty((1, 16384, D), np.float32)
    y_sample = np.empty((4, 8192, D), np.float32)
    for c in range(NCORES):
        y = res.results[c]["y_local"]
        sb, half = c // 2, c % 2
        y_prompt[0, 2048 * c:2048 * c + 2048] = y[0:2048]
        y_sample[sb, 4096 * half:4096 * half + 4096] = y[2048:6144]
    return (y_prompt, y_sample)
```
